# Optimizing a Trainium2 kernel written in Bass

```python
import jax
import jax.numpy as jnp
from jax import lax
import numpy as np

D_MODEL = 1024
BATCH = 2
SEQ = 8192
DEPTH = 2
DEC_BATCH = 32
DEC_SEQ = 1
PAST_LEN = 16384
PAGE_SIZE = 128

N_MIXERS = 2
N_ATTN_LAYERS = (DEPTH + 1) // 2
N_CONV_LAYERS = DEPTH // 2
N_HEADS = 8
N_KV_HEADS = 2
GQA_GROUP = N_HEADS // N_KV_HEADS
HEAD_DIM = D_MODEL // N_HEADS
QKV_WIDTH = (N_HEADS + 2 * N_KV_HEADS) * HEAD_DIM
ROT_DIM = HEAD_DIM // 4
ROPE_THETA = 500000.0
ATTN_SCALE = HEAD_DIM ** -0.5
MOBA_BLOCK = 256
MOBA_TOPK = 3
Q_CHUNK = 32
CONV_WIDTH = 31
CONV_CHANNELS = D_MODEL
N_EXPERTS = 32
N_GROUPS = 4
EXPERTS_PER_GROUP = N_EXPERTS // N_GROUPS
TOP_K = 2
D_EXPERT = 512
MAX_ROWS = 512
MIN_ROWS = 8
DEEPNORM_ALPHA = (2 * DEPTH) ** 0.25
DEEPNORM_BETA = (8 * DEPTH) ** -0.25
LN_EPS = 1e-5

kernel_name = 'moba_conformer_moe_hybrid_step'


def layer_norm(x, g, b):
    xf = x.astype(jnp.float32)
    mu = jnp.mean(xf, axis=-1, keepdims=True)
    var = jnp.mean(jnp.square(xf - mu), axis=-1, keepdims=True)
    return ((xf - mu) * lax.rsqrt(var + LN_EPS)).astype(x.dtype) * g + b


def ada_modulation(c, w_ada, b_ada):
    m = jax.nn.silu(c) @ w_ada + b_ada
    return jnp.split(m[:, None, :], 6, axis=-1)


def modulate(x, shift, scale):
    return x * (1 + scale) + shift


def deepnorm_residual(x, out, gate, g, b):
    return layer_norm(DEEPNORM_ALPHA * x + gate * out, g, b)


def partial_rotary(x, pos):
    half = ROT_DIM // 2
    inv_freq = ROPE_THETA ** (-jnp.arange(half, dtype=jnp.float32) / half)
    ang = pos.astype(jnp.float32)[:, None] * inv_freq[None, :]
    cos = jnp.cos(ang)[:, None, :].astype(x.dtype)
    sin = jnp.sin(ang)[:, None, :].astype(x.dtype)
    x1, x2, x_pass = x[..., :half], x[..., half:ROT_DIM], x[..., ROT_DIM:]
    return jnp.concatenate([x1 * cos - x2 * sin, x2 * cos + x1 * sin, x_pass], axis=-1)


def qkv_project(h, w_qkv, pos):
    B, S, _ = h.shape
    qkv = h @ w_qkv
    nq, nk = N_HEADS * HEAD_DIM, N_KV_HEADS * HEAD_DIM
    q = qkv[..., :nq].reshape(B, S, N_HEADS, HEAD_DIM)
    k = qkv[..., nq:nq + nk].reshape(B, S, N_KV_HEADS, HEAD_DIM)
    v = qkv[..., nq + nk:].reshape(B, S, N_KV_HEADS, HEAD_DIM)
    return partial_rotary(q, pos), partial_rotary(k, pos), v


def softmax_over_parts(parts):
    p = jax.nn.softmax(jnp.concatenate(parts, axis=-1), axis=-1)
    cuts, acc = [], 0
    for t in parts[:-1]:
        acc += t.shape[-1]
        cuts.append(acc)
    return jnp.split(p, cuts, axis=-1)


def moba_prompt(q, k, v):
    B, S = q.shape[0], q.shape[1]
    n_blk = -(-S // MOBA_BLOCK)
    pad = ((0, 0), (0, n_blk * MOBA_BLOCK - S), (0, 0), (0, 0))
    kb = jnp.pad(k, pad).reshape(B, n_blk, MOBA_BLOCK, N_KV_HEADS, HEAD_DIM).transpose(0, 3, 1, 2, 4)
    vb = jnp.pad(v, pad).reshape(B, n_blk, MOBA_BLOCK, N_KV_HEADS, HEAD_DIM).transpose(0, 3, 1, 2, 4)
    kvh = jnp.arange(N_HEADS) // GQA_GROUP
    k_mean = jnp.mean(kb.astype(jnp.float32), axis=3)[:, kvh]
    topk = min(MOBA_TOPK, n_blk - 1)
    b_idx = jnp.arange(B)[:, None, None, None]
    h_kv = kvh[None, None, :, None]
    blk_ids = jnp.arange(n_blk)
    q_offs = jnp.arange(Q_CHUNK)
    blk_offs = jnp.arange(MOBA_BLOCK)

    def one_chunk(start):
        qc = lax.dynamic_slice_in_dim(q, start, Q_CHUNK, axis=1)
        qpos = start + q_offs
        own = start // MOBA_BLOCK
        k_own = lax.dynamic_index_in_dim(kb, own, axis=2, keepdims=False)
        v_own = lax.dynamic_index_in_dim(vb, own, axis=2, keepdims=False)
        kpos = own * MOBA_BLOCK + blk_offs
        qg = qc.reshape(B, Q_CHUNK, N_KV_HEADS, GQA_GROUP, HEAD_DIM)
        s_own = jnp.einsum('bqngd,bnkd->bqngk', qg, k_own).reshape(B, Q_CHUNK, N_HEADS, MOBA_BLOCK)
        causal = (kpos[None, :] <= qpos[:, None])[None, :, None, :]
        s_own = jnp.where(causal, s_own.astype(jnp.float32) * ATTN_SCALE, -jnp.inf)
        if topk == 0:
            p_own = jax.nn.softmax(s_own, axis=-1)
            out_sel = jnp.zeros(qc.shape, v.dtype)
        else:
            gate = jnp.einsum('bqhd,bhnd->bqhn', qc.astype(jnp.float32), k_mean)
            gate = jnp.where(blk_ids < own, gate, -jnp.inf)
            _, sel = lax.top_k(gate, topk)
            k_sel = kb[b_idx, h_kv, sel]
            v_sel = vb[b_idx, h_kv, sel]
            s_sel = jnp.einsum('bqhd,bqhjkd->bqhjk', qc, k_sel).astype(jnp.float32) * ATTN_SCALE
            s_sel = jnp.where((sel < own)[..., None], s_sel, -jnp.inf)
            s_sel = s_sel.reshape(B, Q_CHUNK, N_HEADS, topk * MOBA_BLOCK)
            p_own, p_sel = softmax_over_parts([s_own, s_sel])
            p_sel = p_sel.reshape(B, Q_CHUNK, N_HEADS, topk, MOBA_BLOCK).astype(v.dtype)
            out_sel = jnp.einsum('bqhjk,bqhjkd->bqhd', p_sel, v_sel)
        p_own = p_own.reshape(B, Q_CHUNK, N_KV_HEADS, GQA_GROUP, MOBA_BLOCK).astype(v.dtype)
        out_own = jnp.einsum('bqngk,bnkd->bqngd', p_own, v_own).reshape(B, Q_CHUNK, N_HEADS, HEAD_DIM)
        return out_own + out_sel

    starts = jnp.arange(S // Q_CHUNK) * Q_CHUNK
    outs = lax.map(one_chunk, starts)
    return outs.transpose(1, 0, 2, 3, 4).reshape(B, S, N_HEADS * HEAD_DIM)


def moba_sample(q, k_new, v_new, cache_k, cache_v, page_table, layer):
    DB, T = q.shape[0], q.shape[1]
    n_pages = PAST_LEN // PAGE_SIZE
    ppb = MOBA_BLOCK // PAGE_SIZE
    n_full = PAST_LEN // MOBA_BLOCK
    topk = min(MOBA_TOPK, n_full)
    kvh = jnp.arange(N_HEADS) // GQA_GROUP
    qg = q.reshape(DB, T, N_KV_HEADS, GQA_GROUP, HEAD_DIM)
    t_ids = jnp.arange(T)
    s_new = jnp.einsum('btngd,bsnd->btngs', qg, k_new).reshape(DB, T, N_HEADS, T)
    s_new = jnp.where((t_ids[None, :] <= t_ids[:, None])[None, :, None, :], s_new.astype(jnp.float32) * ATTN_SCALE, -jnp.inf)
    parts = [s_new]
    has_own_past = n_pages > n_full * ppb
    if has_own_past:
        own_pages = page_table[:, n_full * ppb:]
        k_op = cache_k[own_pages, layer].transpose(0, 2, 1, 3, 4).reshape(DB, N_KV_HEADS, -1, HEAD_DIM)
        v_op = cache_v[own_pages, layer].transpose(0, 2, 1, 3, 4).reshape(DB, N_KV_HEADS, -1, HEAD_DIM)
        s_op = jnp.einsum('btngd,bnkd->btngk', qg, k_op).reshape(DB, T, N_HEADS, -1)
        parts.append(s_op.astype(jnp.float32) * ATTN_SCALE)
    if topk > 0:
        full_pages = page_table[:, :n_full * ppb]
        k_full = cache_k[full_pages, layer].astype(jnp.float32)
        k_mean = jnp.mean(k_full.reshape(DB, n_full, ppb, N_KV_HEADS, PAGE_SIZE, HEAD_DIM), axis=(2, 4))[:, :, kvh]
        gate = jnp.einsum('bthd,bnhd->bthn', q.astype(jnp.float32), k_mean)
        _, sel = lax.top_k(gate, topk)
        sel_pages = sel[..., None] * ppb + jnp.arange(ppb)
        phys = page_table[jnp.arange(DB)[:, None, None, None, None], sel_pages]
        h_kv = kvh[None, None, :, None, None]
        k_sel = cache_k[phys, layer, h_kv].reshape(DB, T, N_HEADS, topk * MOBA_BLOCK, HEAD_DIM)
        v_sel = cache_v[phys, layer, h_kv].reshape(DB, T, N_HEADS, topk * MOBA_BLOCK, HEAD_DIM)
        s_sel = jnp.einsum('bthd,bthkd->bthk', q, k_sel)
        parts.append(s_sel.astype(jnp.float32) * ATTN_SCALE)
    probs = softmax_over_parts(parts)
    vd = v_new.dtype
    p_new = probs[0].reshape(DB, T, N_KV_HEADS, GQA_GROUP, T).astype(vd)
    out = jnp.einsum('btngs,bsnd->btngd', p_new, v_new).reshape(DB, T, N_HEADS, HEAD_DIM)
    idx = 1
    if has_own_past:
        p_op = probs[idx].reshape(DB, T, N_KV_HEADS, GQA_GROUP, -1).astype(vd)
        out = out + jnp.einsum('btngk,bnkd->btngd', p_op, v_op).reshape(DB, T, N_HEADS, HEAD_DIM)
        idx += 1
    if topk > 0:
        out = out + jnp.einsum('bthk,bthkd->bthd', probs[idx].astype(vd), v_sel)
    return out.reshape(DB, T, N_HEADS * HEAD_DIM)


def glu_in(h, w_in):
    a, g = jnp.split(h @ w_in, 2, axis=-1)
    return a * jax.nn.sigmoid(g)


def conv_module_tail(u_ext, w_dw, g, b, w_out):
    y = lax.conv_general_dilated(u_ext, w_dw[:, None, :].astype(u_ext.dtype), (1,), 'VALID',
                                 dimension_numbers=('NWC', 'WIO', 'NWC'), feature_group_count=CONV_CHANNELS)
    return jax.nn.silu(layer_norm(y, g, b)) @ w_out


def grouped_moe(h, w_router, b_router, w_gate, w_up, w_down):
    N, D = h.shape
    scores = jax.nn.sigmoid((h @ w_router).astype(jnp.float32))
    biased = (scores + b_router.astype(jnp.float32)).reshape(N, N_GROUPS, EXPERTS_PER_GROUP)
    group_score = jnp.sum(lax.top_k(biased, 2)[0], axis=-1)
    g_sel = jnp.argmax(group_score, axis=-1).astype(jnp.int32)
    in_group = jnp.take_along_axis(biased, g_sel[:, None, None], axis=1)[:, 0]
    _, local = lax.top_k(in_group, TOP_K)
    e_idx = g_sel[:, None] * EXPERTS_PER_GROUP + local
    wts = jnp.take_along_axis(scores, e_idx, axis=1)
    wts = wts / jnp.sum(wts, axis=-1, keepdims=True)
    n_assign = N * TOP_K
    per_exp = -(-n_assign // N_EXPERTS)
    rows = min(MAX_ROWS, max(MIN_ROWS, 1 << (per_exp - 1).bit_length()))
    n_blocks = -(-(n_assign + N_EXPERTS * (rows - 1)) // rows)
    flat_e = e_idx.reshape(-1)
    flat_tok = jnp.arange(n_assign, dtype=jnp.int32) // TOP_K
    order = jnp.argsort(flat_e)
    sorted_e = flat_e[order]
    counts = jnp.zeros((N_EXPERTS,), jnp.int32).at[flat_e].add(1)
    padded = (counts + rows - 1) // rows * rows
    start = jnp.cumsum(counts) - counts
    pend = jnp.cumsum(padded)
    pstart = pend - padded
    dest_sorted = pstart[sorted_e] + jnp.arange(n_assign, dtype=jnp.int32) - start[sorted_e]
    row_tok = jnp.zeros((n_blocks * rows,), jnp.int32).at[dest_sorted].set(flat_tok[order])
    blk_e = jnp.minimum(jnp.searchsorted(pend, jnp.arange(n_blocks, dtype=jnp.int32) * rows, side='right'), N_EXPERTS - 1)
    xs = h[row_tok].reshape(n_blocks, rows, D)

    def expert_rows(args):
        xb, e = args
        return (jax.nn.silu(xb @ w_gate[e]) * (xb @ w_up[e])) @ w_down[e]

    ys = lax.map(expert_rows, (xs, blk_e)).reshape(n_blocks * rows, D)
    dest = jnp.zeros((n_assign,), jnp.int32).at[order].set(dest_sorted)
    y = ys[dest].reshape(N, TOP_K, D)
    return jnp.einsum('nk,nkd->nd', wts.astype(y.dtype), y)


def setup_inputs(seed: int = 0) -> dict:
    key = jax.random.key(seed)
    ks = jax.random.split(key, 24)
    f32 = jnp.float32
    n_pages = PAST_LEN // PAGE_SIZE
    n_phys = (DEC_BATCH * n_pages * 5) // 4

    def nrm(k, shape, s):
        return jax.random.normal(k, shape, f32) * s

    x_prompt = nrm(ks[0], (BATCH, SEQ, D_MODEL), 1.0)
    x_sample = nrm(ks[1], (DEC_BATCH, DEC_SEQ, D_MODEL), 1.0)
    cache_k = nrm(ks[2], (n_phys, N_ATTN_LAYERS, N_KV_HEADS, PAGE_SIZE, HEAD_DIM), 1.0)
    cache_v = nrm(ks[3], (n_phys, N_ATTN_LAYERS, N_KV_HEADS, PAGE_SIZE, HEAD_DIM), 1.0)
    state_conv = nrm(ks[4], (N_CONV_LAYERS, DEC_BATCH, CONV_WIDTH - 1, CONV_CHANNELS), 0.5)
    page_table = jax.random.permutation(ks[5], n_phys)[:DEC_BATCH * n_pages].reshape(DEC_BATCH, n_pages).astype(jnp.int32)
    c_prompt = nrm(ks[6], (BATCH, D_MODEL), 1.0)
    c_sample = nrm(ks[7], (DEC_BATCH, D_MODEL), 1.0)
    w_ada = nrm(ks[8], (DEPTH, D_MODEL, 6 * D_MODEL), 0.5 * D_MODEL ** -0.5)
    b_ada = nrm(ks[9], (DEPTH, 6 * D_MODEL), 0.01)
    ln_g = 1.0 + nrm(ks[10], (DEPTH, 2, D_MODEL), 0.01)
    ln_b = nrm(ks[11], (DEPTH, 2, D_MODEL), 0.01)
    w_qkv = nrm(ks[12], (N_ATTN_LAYERS, D_MODEL, QKV_WIDTH), D_MODEL ** -0.5)
    w_o = nrm(ks[13], (N_ATTN_LAYERS, N_HEADS * HEAD_DIM, D_MODEL), (N_HEADS * HEAD_DIM) ** -0.5 * DEEPNORM_BETA)
    conv_w_in = nrm(ks[14], (N_CONV_LAYERS, D_MODEL, 2 * CONV_CHANNELS), D_MODEL ** -0.5)
    conv_w_dw = nrm(ks[15], (N_CONV_LAYERS, CONV_WIDTH, CONV_CHANNELS), CONV_WIDTH ** -0.5)
    conv_ln_g = 1.0 + nrm(ks[16], (N_CONV_LAYERS, CONV_CHANNELS), 0.01)
    conv_ln_b = nrm(ks[17], (N_CONV_LAYERS, CONV_CHANNELS), 0.01)
    conv_w_out = nrm(ks[18], (N_CONV_LAYERS, CONV_CHANNELS, D_MODEL), CONV_CHANNELS ** -0.5 * DEEPNORM_BETA)
    w_router = nrm(ks[19], (D_MODEL, N_EXPERTS), D_MODEL ** -0.5)
    b_router = nrm(ks[20], (N_EXPERTS,), 0.01)
    w_gate = nrm(ks[21], (DEPTH, N_EXPERTS, D_MODEL, D_EXPERT), D_MODEL ** -0.5)
    w_up = nrm(ks[22], (DEPTH, N_EXPERTS, D_MODEL, D_EXPERT), D_MODEL ** -0.5)
    w_down = nrm(ks[23], (DEPTH, N_EXPERTS, D_EXPERT, D_MODEL), D_EXPERT ** -0.5 * DEEPNORM_BETA)
    return {'x_prompt': x_prompt, 'x_sample': x_sample, 'cache_k': cache_k, 'cache_v': cache_v,
            'state_conv': state_conv, 'page_table': page_table, 'c_prompt': c_prompt, 'c_sample': c_sample,
            'w_ada': w_ada, 'b_ada': b_ada, 'ln_g': ln_g, 'ln_b': ln_b, 'w_qkv': w_qkv, 'w_o': w_o,
            'conv_w_in': conv_w_in, 'conv_w_dw': conv_w_dw, 'conv_ln_g': conv_ln_g, 'conv_ln_b': conv_ln_b,
            'conv_w_out': conv_w_out, 'w_router': w_router, 'b_router': b_router,
            'w_gate': w_gate, 'w_up': w_up, 'w_down': w_down}


def reference(x_prompt, x_sample, cache_k, cache_v, state_conv, page_table, c_prompt, c_sample,
              w_ada, b_ada, ln_g, ln_b, w_qkv, w_o, conv_w_in, conv_w_dw, conv_ln_g, conv_ln_b,
              conv_w_out, w_router, b_router, w_gate, w_up, w_down):
    pos_p = jnp.arange(SEQ)
    pos_s = PAST_LEN + jnp.arange(DEC_SEQ)
    xp, xs = x_prompt, x_sample
    kp_pages, vp_pages, ks_rows, vs_rows, conv_p, conv_s = [], [], [], [], [], []
    for i in range(DEPTH):
        mp = ada_modulation(c_prompt, w_ada[i], b_ada[i])
        ms = ada_modulation(c_sample, w_ada[i], b_ada[i])
        hp = modulate(xp, mp[0], mp[1])
        hs = modulate(xs, ms[0], ms[1])
        if i % N_MIXERS == 0:
            ia = i // N_MIXERS
            qp, kp, vp = qkv_project(hp, w_qkv[ia], pos_p)
            op = moba_prompt(qp, kp, vp) @ w_o[ia]
            qs, ks_, vs_ = qkv_project(hs, w_qkv[ia], pos_s)
            os_ = moba_sample(qs, ks_, vs_, cache_k, cache_v, page_table, ia) @ w_o[ia]
            npp = SEQ // PAGE_SIZE
            kp_pages.append(kp.reshape(BATCH, npp, PAGE_SIZE, N_KV_HEADS, HEAD_DIM).transpose(0, 1, 3, 2, 4))
            vp_pages.append(vp.reshape(BATCH, npp, PAGE_SIZE, N_KV_HEADS, HEAD_DIM).transpose(0, 1, 3, 2, 4))
            ks_rows.append(ks_.transpose(0, 2, 1, 3))
            vs_rows.append(vs_.transpose(0, 2, 1, 3))
        else:
            ic = i // N_MIXERS
            up_ext = jnp.pad(glu_in(hp, conv_w_in[ic]), ((0, 0), (CONV_WIDTH - 1, 0), (0, 0)))
            op = conv_module_tail(up_ext, conv_w_dw[ic], conv_ln_g[ic], conv_ln_b[ic], conv_w_out[ic])
            us = glu_in(hs, conv_w_in[ic])
            us_ext = jnp.concatenate([state_conv[ic].astype(us.dtype), us], axis=1)
            os_ = conv_module_tail(us_ext, conv_w_dw[ic], conv_ln_g[ic], conv_ln_b[ic], conv_w_out[ic])
            conv_p.append(up_ext[:, -(CONV_WIDTH - 1):])
            conv_s.append(us_ext[:, -(CONV_WIDTH - 1):])
        xp = deepnorm_residual(xp, op, mp[2], ln_g[i, 0], ln_b[i, 0])
        xs = deepnorm_residual(xs, os_, ms[2], ln_g[i, 0], ln_b[i, 0])
        hp = modulate(xp, mp[3], mp[4])
        hs = modulate(xs, ms[3], ms[4])
        fp = grouped_moe(hp.reshape(-1, D_MODEL), w_router, b_router, w_gate[i], w_up[i], w_down[i]).reshape(hp.shape)
        fs = grouped_moe(hs.reshape(-1, D_MODEL), w_router, b_router, w_gate[i], w_up[i], w_down[i]).reshape(hs.shape)
        xp = deepnorm_residual(xp, fp, mp[5], ln_g[i, 1], ln_b[i, 1])
        xs = deepnorm_residual(xs, fs, ms[5], ln_g[i, 1], ln_b[i, 1])
    k_prompt = jnp.stack(kp_pages, axis=2)
    v_prompt = jnp.stack(vp_pages, axis=2)
    conv_prompt = jnp.stack(conv_p, axis=0)
    k_sample = jnp.stack(ks_rows, axis=1)
    v_sample = jnp.stack(vs_rows, axis=1)
    conv_sample = jnp.stack(conv_s, axis=0)
    return (xp, xs, k_prompt, v_prompt, conv_prompt, k_sample, v_sample, conv_sample)
```

```python
import numpy as np
from contextlib import ExitStack
import concourse.bass as bass
import concourse.mybir as mybir
from concourse.bass_utils import run_bass_kernel_spmd

F32 = mybir.dt.float32
BF16 = mybir.dt.bfloat16
I32 = mybir.dt.int32
U32 = mybir.dt.uint32
AF = mybir.ActivationFunctionType
ALU = mybir.AluOpType
AX = mybir.AxisListType

EPOCH = 24000
D = 1024
H = 8
KVH = 2
HD = 128
ROPE_THETA = 500000.0
ATTN_SCALE = HD ** -0.5
CW = 31
DFF = 512
LN_EPS = 1e-5
BIG = 1.0e30


class Cfg:
    def __init__(self, B=2, SEQ=8192, NCB=4, DEC_BATCH=32, PAST_LEN=16384, NE=32, NG=4, DEPTH=2, MG=3,
                 stop=None):
        self.B, self.SEQ, self.NCB, self.DEC_BATCH, self.PAST_LEN = B, SEQ, NCB, DEC_BATCH, PAST_LEN
        self.NE, self.NG, self.EPG = NE, NG, NE // NG
        self.NC = B * NCB
        self.NSLOT = SEQ // 256
        self.LS = self.NSLOT // NCB
        self.NPT = 2 * self.LS + 1
        self.NT = self.NPT + 1
        self.NGT = 2 * self.NSLOT
        self.SPC = DEC_BATCH // self.NC
        self.NPG = PAST_LEN // 128
        self.NBLK = self.NPG // 2
        self.NPHYS = (DEC_BATCH * self.NPG * 5) // 4
        self.DEPTH = DEPTH
        self.MG = MG
        self.ALPHA = (2 * DEPTH) ** 0.25
        self.stop = stop
        assert self.SPC == 4 and self.EPG == 8 and self.NT % MG == 0


class Sync:
    def __init__(self, nc, es, same_engine_wait=True):
        self.nc = nc
        self.es = es
        self.engs = {'pe': nc.tensor, 'act': nc.scalar, 'dve': nc.vector,
                     'pool': nc.gpsimd, 'sp': nc.sync}
        self.sems = {}
        self.cnt = {k: 0 for k in self.engs}
        self.waited = {k: {} for k in self.engs}
        self.res = {}
        self.dsem = {}
        self.same = same_engine_wait
        self.n_ins = 0

    def _sem(self, key):
        if key not in self.sems:
            nm = "s%d" % len(self.sems)
            self.sems[key] = self.es.enter_context(self.nc.semaphore(nm))
        return self.sems[key]

    def _wait(self, eng, deps):
        best = {}
        for (sk, v) in deps:
            if best.get(sk, 0) < v:
                best[sk] = v
        for sk, v in best.items():
            if sk[0] == 'E' and sk[1] == eng:
                if not self.same or eng == 'pe':
                    continue
            if self.waited[eng].get(sk, 0) >= v:
                continue
            self.engs[eng].wait_ge(self._sem(sk), v)
            self.n_ins += 1
            self.waited[eng][sk] = v

    def _deps(self, reads, writes):
        deps = []
        for k in reads:
            r = self.res.get(k)
            if r and r['w']:
                deps.append(r['w'])
        for k in writes:
            r = self.res.get(k)
            if r:
                if r['w']:
                    deps.append(r['w'])
                deps += r['r']
        return deps

    def _record(self, ev, reads, writes):
        for k in reads:
            self.res.setdefault(k, {'w': None, 'r': []})['r'].append(ev)
        for k in writes:
            self.res[k] = {'w': ev, 'r': []}

    def op(self, eng, fn, reads=(), writes=()):
        self._wait(eng, self._deps(reads, writes))
        ins = fn(self.engs[eng])
        self.cnt[eng] += 1
        ep, v = divmod(self.cnt[eng] - 1, EPOCH)
        sk = ('E', eng, ep)
        ins.then_inc(self._sem(sk), 1)
        self.n_ins += 1
        self._record((sk, v + 1), reads, writes)
        return ins

    def dma(self, eng, out, in_, reads=(), writes=(), key=None, fn=None):
        self._wait(eng, self._deps(reads, writes))
        if key is None:
            key = ('D', writes[0] if writes else reads[0])
        if fn is None:
            ins = self.engs[eng].dma_start(out=out, in_=in_)
        else:
            ins = fn(self.engs[eng])
        c = self.dsem.get(key, 0) + 16
        self.dsem[key] = c
        ins.then_inc(self._sem(key), 16)
        self.n_ins += 1
        self._record((key, c), reads, writes)
        return ins

    def wait_keys(self, eng, keys):
        deps = []
        for k in keys:
            r = self.res.get(k)
            if r:
                if r['w']:
                    deps.append(r['w'])
                deps += r['r']
        self._wait(eng, deps)

    def barrier(self):
        deps = []
        for r in self.res.values():
            if r['w']:
                deps.append(r['w'])
            deps += r['r']
        for eng in self.engs:
            self._wait(eng, deps)
        self.res = {}


class Prog:
    def __init__(self, cfg):
        self.cfg = cfg
        self.nc = bass.Bass("TRN2", target_bir_lowering=False)
        self.es = ExitStack()
        self.din = {}
        self.dout = {}

    def inp(self, name, shape, dt=F32):
        self.din[name] = self.nc.dram_tensor(name, list(shape), dt, kind="ExternalInput").ap()
        return self.din[name]

    def outp(self, name, shape, dt=F32):
        self.dout[name] = self.nc.dram_tensor(name, list(shape), dt, kind="ExternalOutput").ap()
        return self.dout[name]

    def sb(self, name, shape, dt=F32):
        return self.es.enter_context(self.nc.sbuf_tensor("sb_" + name, list(shape), dt))

    def mm(self, out, lhsT, rhs, start, stop, reads, w):
        self.S.op('pe', lambda e: e.matmul(out, lhsT=lhsT, rhs=rhs, start=start, stop=stop), reads=reads, writes=[w])

    def tr(self, out, in_, ident, reads, w):
        self.S.op('pe', lambda e: e.transpose(out=out, in_=in_, identity=ident), reads=reads, writes=[w])

    def act(self, out, in_, func, reads, w, bias=None, scale=None, eng='act'):
        kw = {}
        if bias is not None:
            kw['bias'] = bias
        if scale is not None:
            kw['scale'] = scale
        self.S.op('act', lambda e: e.activation(out=out, in_=in_, func=func, **kw), reads=reads, writes=[w])

    def tt(self, out, a, b, op, reads, w, eng='dve'):
        self.S.op(eng, lambda e: e.tensor_tensor(out=out, in0=a, in1=b, op=op), reads=reads, writes=[w])

    def ts(self, out, a, s1, s2, op0, op1, reads, w, eng='dve'):
        if op1 is None:
            self.S.op(eng, lambda e: e.tensor_scalar(out=out, in0=a, scalar1=s1, scalar2=None, op0=op0), reads=reads, writes=[w])
        else:
            self.S.op(eng, lambda e: e.tensor_scalar(out=out, in0=a, scalar1=s1, scalar2=s2, op0=op0, op1=op1), reads=reads, writes=[w])

    def stt(self, out, a, s, b, op0, op1, reads, w):
        self.S.op('dve', lambda e: e.scalar_tensor_tensor(out=out, in0=a, scalar=s, in1=b, op0=op0, op1=op1), reads=reads, writes=[w])

    def cp(self, out, in_, reads, w, eng='dve'):
        if eng == 'act':
            self.S.op('act', lambda e: e.copy(out=out, in_=in_), reads=reads, writes=[w])
        else:
            self.S.op(eng, lambda e: e.tensor_copy(out=out, in_=in_), reads=reads, writes=[w])

    def memset(self, ap, val, w, eng='dve'):
        self.S.op(eng, lambda e: e.memset(ap, val), writes=[w])

    def ld(self, out, in_, w, reads=(), eng='sp'):
        self.S.dma(eng, out, in_, reads=list(reads), writes=[w])

    def st(self, out, in_, r, okey):
        self.S.dma('sp', out, in_, reads=[r], writes=[okey])
        self.outkeys.append(okey)

    def bcast_row(self, dst, row_ap, n, dkey, tmpname):
        rt = self.rowtmp
        for c0 in range(0, n, 512):
            cw = min(512, n - c0)
            self.ld(rt[0:1, 0:cw], row_ap[:, c0:c0 + cw], 'rowtmp')
            self.mm(self.ps[0][:, 0:cw], self.ones32[0:1, :], rt[0:1, 0:cw], True, True, ['rowtmp', 'ones32'], 'ps0')
            self.cp(dst[:, c0:c0 + cw], self.ps[0][:, 0:cw], ['ps0'], dkey, eng='act')

    def layernorm(self, z, out, gB, bB, zkey, okey, gkey):
        st_, mv, rstd = self.ln_st, self.ln_mv, self.ln_rstd
        for i in range(2):
            self.S.op('dve', lambda e: e.bn_stats(out=st_[:, i, :], in_=z[:, i * 512:(i + 1) * 512]), reads=[zkey], writes=['ln_st'])
        self.S.op('dve', lambda e: e.bn_aggr(out=mv[:], in_=st_[:].rearrange("p a b -> p (a b)")), reads=['ln_st'], writes=['ln_mv'])
        self.act(rstd[:], mv[:, 1:2], AF.Ln, ['ln_mv', 'epsT'], 'ln_rstd', bias=self.epsT[:], scale=1.0)
        self.act(rstd[:], rstd[:], AF.Exp, ['ln_rstd'], 'ln_rstd', scale=-0.5)
        self.ts(z, z, mv[:, 0:1], rstd[:, 0:1], ALU.subtract, ALU.mult, [zkey, 'ln_mv', 'ln_rstd'], zkey)
        self.tt(z, z, gB, ALU.mult, [zkey, gkey], zkey)
        self.tt(out, z, bB, ALU.add, [zkey, gkey], okey)

    def build(self):
        cfg = self.cfg
        nc, es = self.nc, self.es
        NT, NPT, NGT, NSLOT, LS, SPC, NE, NG, EPG = cfg.NT, cfg.NPT, cfg.NGT, cfg.NSLOT, cfg.LS, cfg.SPC, cfg.NE, cfg.NG, cfg.EPG
        NBLK, NPG, NPHYS = cfg.NBLK, cfg.NPG, cfg.NPHYS
        self.outkeys = []
        xg = self.inp("xg", [NGT * 128, D])
        rope_d = self.inp("rope", [128, (NGT + 1) * 32])
        xs_d = self.inp("xs", [128, D])
        cvec_d = self.inp("cvec", [8, D])
        nmask_d = self.inp("nmask", [128, NPT * NSLOT])
        v01_d = self.inp("v01", [128, NPT * NSLOT])
        flag_d = self.inp("flag", [128, 1])
        consts_d = self.inp("consts", [128, 256])
        consts2_d = self.inp("consts2", [128, 512])
        ptA_d = self.inp("ptA", [128, 4], I32)
        ptB_d = self.inp("ptB", [32, 2 * NBLK], I32)
        sttok_d = self.inp("st_tok", [SPC, 30, D])
        stfm_d = self.inp("st_fm", [128, 8 * SPC * 30])
        wada_d = self.inp("w_ada", [2, D, 6 * D])
        bada_d = self.inp("b_ada", [2, 6 * D])
        badaT_d = self.inp("b_adaT", [128, 96])
        lng_d = self.inp("ln_g", [4, D])
        lnb_d = self.inp("ln_b", [4, D])
        wqkv_d = self.inp("w_qkv", [D, 1536])
        wo_d = self.inp("w_o", [D, D])
        win_d = self.inp("conv_w_in", [D, 2 * D])
        wdwT_d = self.inp("wdwT", [128, 8 * CW])
        clng_d = self.inp("conv_ln_g", [1, D])
        clnb_d = self.inp("conv_ln_b", [1, D])
        wout_d = self.inp("conv_w_out", [D, D])
        wr_d = self.inp("w_router", [D, NE])
        br_d = self.inp("b_router", [1, NE])
        wg_d = self.inp("w_gate", [2, NE, D, DFF])
        wu_d = self.inp("w_up", [2, NE, D, DFF])
        wd_d = self.inp("w_down", [2, NE, DFF, D])
        ck_d = self.inp("cache_k", [NPHYS, 2 * 128 * 128])
        cv_d = self.inp("cache_v", [NPHYS, 2 * 128 * 128])

        y_p = self.outp("y_p", [2 * LS * 128, D])
        y_s = self.outp("y_s", [SPC, D])
        k_p = self.outp("k_p", [2 * LS, 2, 128, 128])
        v_p = self.outp("v_p", [2 * LS, 2, 128, 128])
        conv_p = self.outp("conv_p", [30, D])
        k_s = self.outp("k_s", [SPC, 256])
        v_s = self.outp("v_s", [SPC, 256])
        conv_s = self.outp("conv_s", [SPC, 30, D])
        dbg = self.outp("dbg", [NT * 128, D]) if cfg.stop else None

        self.S = S = Sync(nc, es)
        sb = self.sb
        self.ps = [es.enter_context(nc.psum_tensor("ps%d" % i, [128, 512], F32)) for i in range(8)]
        ps = self.ps
        PK = ['ps%d' % i for i in range(8)]
        xres = sb("xres", [128, NT, D])
        XK = ['xres%d' % t for t in range(NT)]
        consts = sb("consts", [128, 256])
        ident = consts[:, 0:128]
        identb = sb("identb", [128, 128], BF16)
        trib = sb("trib", [128, 128], BF16)
        self.ones32 = sb("ones32", [128, 128])
        onesb = sb("onesb", [128, 1], BF16)
        self.rowtmp = sb("rowtmp", [1, 512])
        self.ln_st = sb("ln_st", [128, 2, 6])
        self.ln_mv = sb("ln_mv", [128, 2])
        self.ln_rstd = sb("ln_rstd", [128, 1])
        flag = sb("flag", [128, 1])
        scT = sb("scT", [128, 8, 8])
        modT = sb("modT", [128, 2, 8, 8])
        badaT = sb("badaT", [128, 96])
        gP = sb("gP", [128, D])
        gS = sb("gS", [128, D])
        lnG = sb("lnG", [128, D])
        lnB = sb("lnB", [128, D])
        ks32 = sb("ks32", [128, 2 * NGT])
        kvS = sb("kvS", [128, 512])
        SCR_BYTES = 108 * 1024
        scr = sb("scr", [128, SCR_BYTES // 4])

        def carve(off_bytes, shape, dt):
            n = int(np.prod(shape))
            esz = 4 if dt in (F32, I32, U32) else 2
            assert off_bytes % 4 == 0
            a = scr[:, off_bytes // 4: off_bytes // 4 + (n * esz + 3) // 4]
            if dt != F32:
                a = a.bitcast(dt)
            a = a[:, 0:n]
            if len(shape) == 2:
                pat = "p (a b) -> p a b"
                return a.rearrange(pat, a=shape[0]), off_bytes + n * esz
            if len(shape) == 3:
                return a.rearrange("p (a b c) -> p a b c", a=shape[0], b=shape[1]), off_bytes + n * esz
            if len(shape) == 1:
                return a, off_bytes + n * esz
            raise ValueError

        self.ld(consts[:], consts_d, 'consts')
        self.ld(flag[:], flag_d, 'flag')
        self.ld(badaT[:], badaT_d, 'badaT')
        self.cp(identb[:], consts[:, 0:128], ['consts'], 'identb')
        self.cp(trib[:], consts[:, 128:256], ['consts'], 'trib')
        self.memset(self.ones32[:], 1.0, 'ones32')
        self.memset(onesb[:], 1.0, 'onesb')
        self.epsT = sb("epsT", [128, 1])
        self.memset(self.epsT[:], LN_EPS, 'epsT')

        ctmp_, o_ = carve(0, [1, D], F32)
        ctmp2_, o_ = carve(o_, [1, D], F32)
        ctmp = ctmp_[0:8, 0, :]
        ctmp2 = ctmp2_[0:8, 0, :]
        self.ld(ctmp, cvec_d, 'ctmp')
        self.act(ctmp2, ctmp, AF.Silu, ['ctmp'], 'ctmp2')
        for kc in range(8):
            self.tr(ps[0][:, kc * 8:(kc + 1) * 8], ctmp2[:, kc * 128:(kc + 1) * 128], ident[0:8, 0:8], ['ctmp2', 'consts'], 'ps0')
        self.cp(scT[:].rearrange("p a b -> p (a b)"), ps[0][:, 0:64], ['ps0'], 'scT')
        S.barrier()

        def ada(hl):
            layer, part = hl // 2, hl % 2
            base = part * 3 * D
            S.barrier()
            off = 0
            wa = []
            for i in range(2):
                a, off = carve(off, [8, 512], F32)
                wa.append(a)
            bPl, off = carve(off, [8, 128], F32)
            bSl, off = carve(off, [8, 128], F32)
            for kc in range(8):
                self.ts(bPl[:, kc, :], self.ones32[:], scT[:, kc, 0:1], None, ALU.mult, None, ['ones32', 'scT'], 'bPl')
            self.memset(bSl[:].rearrange("p a b -> p (a b)"), 0.0, 'bSl')
            for kc in range(8):
                self.cp(bSl[:, kc, 0:SPC], scT[:, kc, 1:1 + SPC], ['scT', 'bSl'], 'bSl')
            wsrc = wada_d[layer].rearrange("(kc p) n -> p kc n", p=128)
            for cgi in range(6):
                c0 = base + cgi * 512
                wt = wa[cgi % 2]
                wk = 'wa%d' % (cgi % 2)
                self.ld(wt[:], wsrc[:, :, c0:c0 + 512], wk)
                v = cgi // 2
                if v < 2:
                    for mi in range(4):
                        mc = (cgi % 2) * 4 + mi
                        for kc in range(8):
                            self.mm(ps[1][:, (v * 8 + mc) * 8:(v * 8 + mc) * 8 + 8], wt[:, kc, mi * 128:(mi + 1) * 128], scT[:, kc, :],
                                    kc == 0, kc == 7, [wk, 'scT'], 'ps1')
                else:
                    half = cgi % 2
                    for (lt, lk, pb) in ((bPl, 'bPl', 2), (bSl, 'bSl', 3)):
                        for kc in range(8):
                            self.mm(ps[pb][:, :], lt[:, kc, :], wt[:, kc, :], kc == 0, False, [wk, lk], PK[pb])
                        self.ld(self.rowtmp[0:1, 0:512], bada_d[layer:layer + 1, c0:c0 + 512], 'rowtmp')
                        self.mm(ps[pb][:, :], self.ones32[0:1, :], self.rowtmp[0:1, 0:512], False, True, ['rowtmp', 'ones32'], PK[pb])
                        dst = gP if pb == 2 else gS
                        self.cp(dst[:, half * 512:(half + 1) * 512], ps[pb][:, :], [PK[pb]], 'gP' if pb == 2 else 'gS', eng='act')
            bb = badaT[:, layer * 48 + part * 24: layer * 48 + part * 24 + 16]
            self.tt(modT[:].rearrange("p v c r -> p (v c) r"), ps[1][:, 0:128].rearrange("p (a r) -> p a r", r=8),
                    bb.unsqueeze(2).broadcast_to([128, 16, 8]), ALU.add, ['ps1', 'badaT'], 'modT')
            self.ts(modT[:, 1, :, :], modT[:, 1, :, :], 1.0, None, ALU.add, None, ['modT'], 'modT')
            self.bcast_row(lnG, lng_d[hl:hl + 1, :], D, 'lnG', 'lnG')
            self.bcast_row(lnB, lnb_d[hl:hl + 1, :], D, 'lnB', 'lnB')
            S.barrier()

        def tile_to_hT(src, skey, hdst, col0, sample=False, xT32=None):
            for hf in range(2):
                pb = 4 + hf
                for q in range(4):
                    kc = hf * 4 + q
                    self.tr(ps[pb][:, q * 128:(q + 1) * 128], src[:, kc * 128:(kc + 1) * 128], ident, [skey, 'consts'], PK[pb])
                for q in range(4):
                    kc = hf * 4 + q
                    if not sample:
                        self.act(hdst[:, kc, col0:col0 + 128], ps[pb][:, q * 128:(q + 1) * 128], AF.Identity,
                                 [PK[pb], 'modT'], 'hTg', bias=modT[:, 0, kc, 0:1], scale=modT[:, 1, kc, 0:1])
                        if xT32 is not None:
                            self.cp(xT32[:, kc, :], ps[pb][:, q * 128:(q + 1) * 128], [PK[pb]], 'xT32', eng='act')
                    else:
                        tmp = self.smod
                        self.tt(tmp[:, 0:SPC], ps[pb][:, q * 128:q * 128 + SPC], modT[:, 1, kc, 1:1 + SPC], ALU.mult, [PK[pb], 'modT'], 'smod')
                        self.tt(tmp[:, 0:SPC], tmp[:, 0:SPC], modT[:, 0, kc, 1:1 + SPC], ALU.add, ['smod', 'modT'], 'smod')
                        self.cp(hdst[:, kc, col0:col0 + 128], tmp[:], ['smod'], 'hTg')
                        if xT32 is not None:
                            self.cp(xT32[:, kc, :], tmp[:], ['smod'], 'xT32')

        self.smod = sb("smod", [128, 128])
        self.memset(self.smod[:], 0.0, 'smod')

        rt_ = sb("rot_tmp", [128, 4, 8, 16])

        def rotary(xv, nh, cs, xkey, rkey='rope'):
            cosb = cs[:, 0:16].unsqueeze(1).broadcast_to([128, nh, 16])
            sinb = cs[:, 16:32].unsqueeze(1).broadcast_to([128, nh, 16])
            x1 = xv[:, :, 0:16]
            x2 = xv[:, :, 16:32]
            self.tt(rt_[:, 0, 0:nh, :], x1, cosb, ALU.mult, [xkey, rkey], 'rt0')
            self.tt(rt_[:, 1, 0:nh, :], x2, sinb, ALU.mult, [xkey, rkey], 'rt1')
            self.tt(rt_[:, 2, 0:nh, :], x2, cosb, ALU.mult, [xkey, rkey], 'rt2')
            self.tt(rt_[:, 3, 0:nh, :], x1, sinb, ALU.mult, [xkey, rkey], 'rt3')
            self.tt(x1, rt_[:, 0, 0:nh, :], rt_[:, 1, 0:nh, :], ALU.subtract, ['rt0', 'rt1'], xkey)
            self.tt(x2, rt_[:, 2, 0:nh, :], rt_[:, 3, 0:nh, :], ALU.add, ['rt2', 'rt3'], xkey)

        ada(0)
        off = 0
        KT, off = carve(off, [2, NGT * 128], BF16)
        VS, off = carve(off, [NGT, 2, 130], BF16)
        kmT, off = carve(off, [2, NSLOT], BF16)
        off = (off + 3) // 4 * 4
        offC = off
        wkv, off = carve(off, [8, 512], BF16)
        ropes, off = carve(off, [NGT + 1, 32], F32)
        xg0, off = carve(off, [1, D], F32)
        xg1, off = carve(off, [1, D], F32)
        hT1, off = carve(off, [8, 128], BF16)
        kvf, off = carve(off, [1, 512], F32)
        kb16, off = carve(off, [1, 256], BF16)
        assert off <= SCR_BYTES, off
        xgt = [xg0[:, 0, :], xg1[:, 0, :]]
        kvf = kvf[:, 0, :]
        kb16 = kb16[:, 0, :]
        self.hT1 = hT1

        self.ld(ropes[:].rearrange("p t c -> p (t c)"), rope_d, 'rope')
        self.S.dma('pool', wkv[:], wqkv_d.rearrange("(kc p) n -> p kc n", p=128)[:, :, 1024:1536], writes=['wkv'])
        self.memset(VS[:, :, :, 128:130], 1.0, 'VS')

        def kv_tile(src, skey, rope_idx, g):
            tile_to_hT(src, skey, hT1, 0, sample=(g is None))
            for kc in range(8):
                self.mm(ps[0][:, :], hT1[:, kc, 0:128], wkv[:, kc, :], kc == 0, kc == 7, ['hTg', 'wkv'], 'ps0')
            dstf = kvf if g is not None else kvS
            dkey = 'kvf' if g is not None else 'kvS'
            self.cp(dstf[:], ps[0][:, :], ['ps0'], dkey)
            rotary(dstf[:, 0:256].rearrange("p (h d) -> p h d", h=2), 2, ropes[:, rope_idx, :], dkey)
            if g is not None:
                self.cp(VS[:, g, :, 0:128], kvf[:, 256:512].rearrange("p (h d) -> p h d", h=2), ['kvf'], 'VS', eng='act')
                self.cp(kb16[:], kvf[:, 0:256], ['kvf'], 'kb16', eng='act')
                pt = ps[1][:].bitcast(BF16)
                for kv in range(2):
                    self.tr(pt[:, kv * 128:(kv + 1) * 128], kb16[:, kv * 128:(kv + 1) * 128], identb[:], ['kb16', 'identb'], 'ps1')
                self.cp(KT[:, :, g * 128:(g + 1) * 128], pt[:, 0:256].rearrange("p (h t) -> p h t", h=2), ['ps1'], 'KT')
                for kv in range(2):
                    self.mm(ps[7][:, kv * NGT + g: kv * NGT + g + 1], kvf[:, kv * 128:(kv + 1) * 128], self.ones32[:, 0:1],
                            True, True, ['kvf', 'ones32'], 'ps7')
                if 2 <= g < 2 + 2 * LS:
                    pg = g - 2
                    self.st(k_p[pg].rearrange("h r d -> r h d"), kvf[:, 0:256].rearrange("p (h d) -> p h d", h=2), 'kvf', 'k_p')
                    self.st(v_p[pg].rearrange("h r d -> r h d"), kvf[:, 256:512].rearrange("p (h d) -> p h d", h=2), 'kvf', 'v_p')
            else:
                self.st(k_s[:, :], kvS[0:SPC, 0:256], 'kvS', 'k_s')
                self.st(v_s[:, :], kvS[0:SPC, 256:512], 'kvS', 'v_s')

        for g in range(NGT):
            src, skey = xgt[g % 2], 'xgt%d' % (g % 2)
            self.ld(src, xg[g * 128:(g + 1) * 128, :], skey)
            kv_tile(src, skey, g, g)
        tS = NT - 1
        self.ld(xres[:, tS, :], xs_d, XK[tS])
        kv_tile(xres[:, tS, :], XK[tS], NGT, None)

        self.cp(ks32[:], ps[7][:, 0:2 * NGT], ['ps7'], 'ks32')
        ksv = ks32[:].rearrange("p (k s two) -> p k s two", k=2, two=2)
        self.tt(ksv[:, :, :, 0], ksv[:, :, :, 0], ksv[:, :, :, 1], ALU.add, ['ks32'], 'ks32')
        self.ts(kmT[:], ksv[:, :, :, 0], 1.0 / 256.0, None, ALU.mult, None, ['ks32'], 'kmT')
        if cfg.stop == 'proj':
            self.finish(dbg, xres, XK)
            return

        S.barrier()
        off = offC
        wqb = []
        for i in range(2):
            a_, off = carve(off, [8, 512], BF16)
            wqb.append(a_)
        ropel, off = carve(off, [NT, 32], F32)
        v01, off = carve(off, [NPT, NSLOT], F32)
        QT1, off = carve(off, [8, 128], BF16)
        hT1, off = carve(off, [8, 128], BF16)
        acc, off = carve(off, [8, 130], F32)
        PT0, off = carve(off, [4, 128], BF16)
        PT1, off = carve(off, [4, 128], BF16)
        PT = [PT0, PT1]
        aT, off = carve(off, [8, 128], BF16)
        qb16, off = carve(off, [8, 128], BF16)
        zq, off = carve(off, [8, 128], F32)
        gsb, off = carve(off, [8, NSLOT], F32)
        sel, off = carve(off, [8, NSLOT], F32)
        t8, off = carve(off, [8, 8], F32)
        nm, off = carve(off, [1, NSLOT], F32)
        rden, off = carve(off, [1, 8], F32)
        assert off <= SCR_BYTES, off
        zq2 = zq[:].rearrange("p h d -> p (h d)")
        qb2 = qb16[:].rearrange("p h d -> p (h d)")

        self.ld(ropel[:, 0:NPT, :].rearrange("p t c -> p (t c)"), rope_d[:, 32:(NPT + 1) * 32], 'ropel')
        self.ld(ropel[:, NPT, :], rope_d[:, NGT * 32:(NGT + 1) * 32], 'ropel')
        self.ld(v01[:].rearrange("p a b -> p (a b)"), v01_d, 'v01')
        wq_src = wqkv_d.rearrange("(kc p) n -> p kc n", p=128)
        wo_src = wo_d.rearrange("(kc p) n -> p kc n", p=128)

        def q_proj(tau, sample):
            tile_to_hT(xres[:, tau, :], XK[tau], hT1, 0, sample=sample)
            for hf in range(2):
                self.S.dma('pool', wqb[hf][:], wq_src[:, :, hf * 512:(hf + 1) * 512], writes=['wqb%d' % hf])
                for kc in range(8):
                    self.mm(ps[2 + hf][:, :], hT1[:, kc, :], wqb[hf][:, kc, :], kc == 0, kc == 7, ['hTg', 'wqb%d' % hf], PK[2 + hf])
                self.cp(zq2[:, hf * 512:(hf + 1) * 512], ps[2 + hf][:, :], [PK[2 + hf]], 'zq', eng='act')
            rotary(zq[:], 8, ropel[:, tau, :], 'zq', 'ropel')

        def out_proj(tau, gate_tile, gkey):
            pt = ps[6][:].bitcast(BF16)
            for h in range(8):
                self.tr(pt[:, h * 128:(h + 1) * 128], qb16[:, h, :], identb[:], ['qb16', 'identb'], 'ps6')
            self.cp(aT[:].rearrange("p h t -> p (h t)"), pt[:, :], ['ps6'], 'aT')
            for hf in range(2):
                self.S.dma('pool', wqb[hf][:], wo_src[:, :, hf * 512:(hf + 1) * 512], writes=['wqb%d' % hf])
                for h in range(8):
                    self.mm(ps[6 + hf][:, :], aT[:, h, :], wqb[hf][:, h, :], h == 0, h == 7, ['aT', 'wqb%d' % hf], PK[6 + hf])
                self.tt(zq2[:, hf * 512:(hf + 1) * 512], ps[6 + hf][:, :], gate_tile[:, hf * 512:(hf + 1) * 512], ALU.mult, [PK[6 + hf], gkey], 'zq')
            self.stt(zq2, xres[:, tau, :], cfg.ALPHA, zq2, ALU.mult, ALU.add, [XK[tau], 'zq'], 'zq')
            self.layernorm(zq2, xres[:, tau, :], lnG[:], lnB[:], 'zq', XK[tau], 'lnG')

        for tau in range(NPT):
            self.ld(xres[:, tau, :], xg[(tau + 1) * 128:(tau + 2) * 128, :], XK[tau])
            q_proj(tau, False)
            self.cp(qb2, zq2, ['zq'], 'qb16', eng='act')
            pt = ps[6][:].bitcast(BF16)
            for h in range(8):
                self.tr(pt[:, h * 128:(h + 1) * 128], qb16[:, h, :], identb[:], ['qb16', 'identb'], 'ps6')
            self.cp(QT1[:].rearrange("p h t -> p (h t)"), pt[:, :], ['ps6'], 'QT1')
            for h in range(8):
                self.mm(ps[4][:, h * NSLOT:(h + 1) * NSLOT], QT1[:, h, :], kmT[:, h // 4, :], True, True, ['QT1', 'kmT'], 'ps4')
            self.ts(nm[:, 0, :], v01[:, tau, :], 1.0, BIG, ALU.subtract, ALU.mult, ['v01'], 'nm')
            v01b = v01[:, tau, :].unsqueeze(1).broadcast_to([128, 8, NSLOT])
            self.tt(gsb[:], ps[4][:, 0:8 * NSLOT].rearrange("p (h s) -> p h s", h=8), v01b, ALU.mult, ['ps4', 'v01'], 'gsb')
            self.tt(gsb[:], gsb[:], nm[:, 0, :].unsqueeze(1).broadcast_to([128, 8, NSLOT]), ALU.add, ['gsb', 'nm'], 'gsb')
            for h in range(8):
                self.S.op('dve', lambda e: e.max(out=t8[:, h, :], in_=gsb[:, h, :]), reads=['gsb'], writes=['t8'])
            self.tt(sel[:], gsb[:], t8[:, :, 2:3].broadcast_to([128, 8, NSLOT]), ALU.is_ge, ['gsb', 't8'], 'sel')
            self.tt(sel[:], sel[:], v01b, ALU.mult, ['sel', 'v01'], 'sel')
            self.memset(acc[:].rearrange("p h d -> p (h d)"), 0.0, 'acc')
            m_own = (tau + 1) // 2
            second = (tau + 1) % 2
            it = 0
            for kv in range(2):
                qrhs = QT1[:, kv * 4:(kv + 1) * 4, :].rearrange("p h q -> p (h q)")
                for s_ in range(NSLOT):
                    own = (s_ == m_own)
                    kts = [0, 1]
                    if own and not second:
                        kts = [0]
                    for kt in kts:
                        g = 2 * s_ + kt
                        self.mm(ps[kt][:, :], KT[:, kv, g * 128:(g + 1) * 128], qrhs, True, True, ['KT', 'QT1'], PK[kt])
                        self.act(PT[kt][:].rearrange("p h q -> p (h q)"), ps[kt][:, :], AF.Exp, [PK[kt]], 'PT%d' % kt, scale=ATTN_SCALE)
                        if own and kt == (1 if second else 0):
                            self.tt(PT[kt][:], PT[kt][:], trib[:].unsqueeze(1).broadcast_to([128, 4, 128]), ALU.mult, ['PT%d' % kt, 'trib'], 'PT%d' % kt)
                    ob = 2 + 2 * (it % 2)
                    for hh in range(4):
                        bank = ps[ob + hh // 2]
                        col = (hh % 2) * 256
                        for i, kt in enumerate(kts):
                            self.mm(bank[:, col:col + 129], PT[kt][:, hh, :], VS[:, 2 * s_ + kt, kv, 0:129], i == 0, i == len(kts) - 1,
                                    ['PT%d' % kt, 'VS'], PK[ob + hh // 2])
                    for hh in range(4):
                        h = kv * 4 + hh
                        bank = ps[ob + hh // 2]
                        col = (hh % 2) * 256
                        if own:
                            self.tt(acc[:, h, 0:129], acc[:, h, 0:129], bank[:, col:col + 129], ALU.add, ['acc', PK[ob + hh // 2]], 'acc')
                        else:
                            self.stt(acc[:, h, 0:129], bank[:, col:col + 129], sel[:, h, s_:s_ + 1], acc[:, h, 0:129], ALU.mult, ALU.add,
                                     ['acc', 'sel', PK[ob + hh // 2]], 'acc')
                    it += 1
            self.S.op('dve', lambda e: e.reciprocal(out=rden[:, 0, :], in_=acc[:, :, 128]), reads=['acc'], writes=['rden'])
            self.tt(qb16[:], acc[:, :, 0:128], rden[:, 0, :].unsqueeze(2).broadcast_to([128, 8, 128]), ALU.mult, ['acc', 'rden'], 'qb16')
            out_proj(tau, gP, 'gP')

        tS = NT - 1
        q_proj(tS, True)
        for h in range(8):
            self.tr(ps[0][:, h * 4:(h + 1) * 4], zq[0:SPC, h, :], ident[0:SPC, 0:SPC], ['zq', 'consts'], 'ps0')
        S.barrier()
        off = 0
        c2, off = carve(off, [1, 512], F32)
        QTa, off = carve(off, [1, 32], F32)
        QTm, off = carve(off, [8, 32], F32)
        qsel, off = carve(off, [1, 128], F32)
        ptAi, off = carve(off, [1, 4], I32)
        ptBi, off = carve(off, [1, 2 * NBLK], I32)
        ptAf, off = carve(off, [1, 4], F32)
        idxAf, off = carve(off, [4, 8], F32)
        idxA, off = carve(off, [4, 8], I32)
        idxSf, off = carve(off, [6, 4], F32)
        idxS, off = carve(off, [6, 4], I32)
        ptBf, off = carve(off, [1, 2 * NBLK], F32)
        ksum0, off = carve(off, [2, 128], F32)
        ksum1, off = carve(off, [2, 128], F32)
        ksr, off = carve(off, [1, 128], F32)
        kmTs, off = carve(off, [4, 128], F32)
        gate_s, off = carve(off, [1, NBLK], F32)
        t8s, off = carve(off, [1, 8], F32)
        oh, off = carve(off, [1, NBLK], F32)
        ohp, off = carve(off, [1, NBLK], F32)
        phys, off = carve(off, [1, 8], F32)
        idxf, off = carve(off, [1, 8], F32)
        idxi, off = carve(off, [1, 8], I32)
        ksel, off = carve(off, [1, 128], F32)
        vsel, off = carve(off, [1, 128], F32)
        sc, off = carve(off, [1, 6 * 128 + 8], F32)
        Pm, off = carve(off, [1, 6 * 128 + 8], F32)
        den, off = carve(off, [1, 1], F32)
        pv, off = carve(off, [24, 128], F32)
        osum, off = carve(off, [1, 128], F32)
        qb16, off = carve(off, [8, 128], BF16)
        aT, off = carve(off, [8, 128], BF16)
        zq, off = carve(off, [8, 128], F32)
        wqb = []
        for i in range(2):
            a_, off = carve(off, [8, 512], BF16)
            wqb.append(a_)
        G0, off = carve(off, [1, 32 * 128], F32)
        G1, off = carve(off, [1, 32 * 128], F32)
        assert off <= SCR_BYTES, off
        GB = [G0[:, 0, :], G1[:, 0, :]]
        zq2 = zq[:].rearrange("p h d -> p (h d)")
        c2 = c2[:, 0, :]
        ksum = [ksum0, ksum1]
        self.cp(QTa[:, 0, :], ps[0][:, 0:32], ['ps0'], 'QTa')
        self.ld(c2, consts2_d, 'c2')
        self.ld(ptAi[:, 0, :], ptA_d, 'ptAi')
        self.ld(ptBi[0:32, 0, :], ptB_d, 'ptBi')
        self.cp(ptBf[0:32, 0, :], ptBi[0:32, 0, :], ['ptBi'], 'ptBf')
        self.cp(ptAf[:, 0, :], ptAi[:, 0, :], ['ptAi'], 'ptAf')
        self.ts(idxAf[:], ptAf[:, 0, :].unsqueeze(2).broadcast_to([128, 4, 8]), 8.0, None, ALU.mult, None, ['ptAf'], 'idxAf')
        self.tt(idxAf[:], idxAf[:], c2[:, 353:361].unsqueeze(1).broadcast_to([128, 4, 8]), ALU.add, ['idxAf', 'c2'], 'idxAf')
        self.cp(idxA[:], idxAf[:], ['idxAf'], 'idxA')
        ck8 = ck_d.rearrange("n (c x) -> (n c) x", x=4096)
        cv8 = cv_d.rearrange("n (c x) -> (n c) x", x=4096)
        self.tt(QTm[:], QTa[:, 0, :].unsqueeze(1).broadcast_to([128, 8, 32]), c2[:, 0:256].rearrange("p (a b) -> p a b", a=8), ALU.mult, ['QTa', 'c2'], 'QTm')
        self.tr(ps[1][0:32, 0:128], QTa[:, 0, :], ident, ['QTa', 'consts'], 'ps1')
        self.cp(qsel[0:32, 0, :], ps[1][0:32, 0:128], ['ps1'], 'qsel')
        RC = 32
        gi = 0
        for r in range(2):
            for kv in range(2):
                first = True
                for eo in range(2):
                    for rc in range(128 // RC):
                        gb = GB[gi % 2][:, 0:RC * 128]
                        gk = 'G%d' % (gi % 2)
                        self.S.dma('pool', None, None, reads=['idxA'], writes=[gk],
                                   fn=lambda e: e.indirect_dma_start(out=gb, out_offset=None, in_=ck8,
                                                                     in_offset=bass.IndirectOffsetOnAxis(ap=idxA[:, r * 2 + eo, kv * 4 + rc:kv * 4 + rc + 1], axis=0)))
                        dst = ksum[r][:, kv, :] if first else ksr[:, 0, :]
                        dk = 'ksum%d' % r if first else 'ksr'
                        self.S.op('dve', lambda e: e.tensor_reduce(out=dst, in_=gb.rearrange("p (r d) -> p d r", d=128), axis=AX.X, op=ALU.add),
                                  reads=[gk], writes=[dk])
                        if not first:
                            self.tt(ksum[r][:, kv, :], ksum[r][:, kv, :], ksr[:, 0, :], ALU.add, ['ksum%d' % r, 'ksr'], 'ksum%d' % r)
                        first = False
                        gi += 1
        for r in range(2):
            for kv in range(2):
                self.tr(ps[2][:, (kv * 2 + r) * 128:(kv * 2 + r + 1) * 128], ksum[r][:, kv, :], ident, ['ksum%d' % r, 'consts'], 'ps2')
        self.ts(kmTs[:].rearrange("p a b -> p (a b)"), ps[2][:, :], 1.0 / 256.0, None, ALU.mult, None, ['ps2'], 'kmTs')
        n = 0
        for s_ in range(SPC):
            for kv in range(2):
                self.mm(ps[3][0:32, 0:NBLK], QTm[:, s_ * 2 + kv, :], kmTs[:, kv * 2 + s_ // 2, (s_ % 2) * NBLK:(s_ % 2 + 1) * NBLK],
                        n == 0, n == 2 * SPC - 1, ['QTm', 'kmTs'], 'ps3')
                n += 1
        self.cp(gate_s[0:32, 0, :], ps[3][0:32, 0:NBLK], ['ps3'], 'gate_s')
        self.S.op('dve', lambda e: e.max(out=t8s[0:32, 0, :], in_=gate_s[0:32, 0, :]), reads=['gate_s'], writes=['t8s'])
        for j in range(3):
            self.ts(oh[0:32, 0, :], gate_s[0:32, 0, :], t8s[0:32, 0, j:j + 1], None, ALU.is_equal, None, ['gate_s', 't8s'], 'oh')
            for eo in range(2):
                self.tt(ohp[0:32, 0, :], oh[0:32, 0, :], ptBf[0:32, 0, eo * NBLK:(eo + 1) * NBLK], ALU.mult, ['oh', 'ptBf'], 'ohp')
                self.S.op('dve', lambda e: e.tensor_reduce(out=phys[0:32, 0, j * 2 + eo:j * 2 + eo + 1], in_=ohp[0:32, 0, :], axis=AX.X, op=ALU.add),
                          reads=['ohp'], writes=['phys'])
        self.ts(idxf[0:32, 0, 0:6], phys[0:32, 0, 0:6], 2.0, c2[0:32, 256:257], ALU.mult, ALU.add, ['phys', 'c2'], 'idxf')
        self.ts(idxSf[0:32], idxf[0:32, 0, 0:6].unsqueeze(2).broadcast_to([32, 6, 4]), 4.0, None, ALU.mult, None, ['idxf'], 'idxSf')
        self.tt(idxSf[0:32], idxSf[0:32], c2[0:32, 353:357].unsqueeze(1).broadcast_to([32, 6, 4]), ALU.add, ['idxSf', 'c2'], 'idxSf')
        self.cp(idxS[0:32], idxSf[0:32], ['idxSf'], 'idxi')
        for (dst, dk, c0) in ((ksel, 'ksel', 0), (vsel, 'vsel', 256)):
            self.mm(ps[4][0:32, 0:128], c2[0:SPC, 257:289], kvS[0:SPC, c0:c0 + 128], True, False, ['c2', 'kvS'], 'ps4')
            self.mm(ps[4][0:32, 0:128], c2[0:SPC, 289:321], kvS[0:SPC, c0 + 128:c0 + 256], False, True, ['c2', 'kvS'], 'ps4')
            self.cp(dst[0:32, 0, :], ps[4][0:32, 0:128], ['ps4'], dk)
        ck2 = ck_d.rearrange("n (h x) -> (n h) x", h=2)
        cv2 = cv_d.rearrange("n (h x) -> (n h) x", h=2)
        qb_ = qsel[0:32, 0, :].unsqueeze(1).broadcast_to([32, 32, 128])
        for c in range(6):
            for rh in range(4):
                gb = GB[gi % 2][0:32, :]
                gk = 'G%d' % (gi % 2)
                self.S.dma('pool', None, None, reads=['idxi'], writes=[gk],
                           fn=lambda e: e.indirect_dma_start(out=gb, out_offset=None, in_=ck8,
                                                             in_offset=bass.IndirectOffsetOnAxis(ap=idxS[0:32, c, rh:rh + 1], axis=0)))
                g3 = gb.rearrange("p (r d) -> p r d", d=128)
                self.tt(g3, g3, qb_, ALU.mult, [gk, 'qsel'], gk)
                self.S.op('dve', lambda e: e.tensor_reduce(out=sc[0:32, 0, c * 128 + rh * 32:c * 128 + rh * 32 + 32], in_=g3, axis=AX.X, op=ALU.add),
                          reads=[gk], writes=['sc'])
                gi += 1
        self.tt(osum[0:32, 0, :], qsel[0:32, 0, :], ksel[0:32, 0, :], ALU.mult, ['qsel', 'ksel'], 'osum')
        self.S.op('dve', lambda e: e.tensor_reduce(out=sc[0:32, 0, 768:769], in_=osum[0:32, 0, :], axis=AX.X, op=ALU.add), reads=['osum'], writes=['sc'])
        self.S.op('act', lambda e: e.activation(out=Pm[0:32, 0, 0:769], in_=sc[0:32, 0, 0:769], func=AF.Exp, scale=ATTN_SCALE),
                  reads=['sc'], writes=['Pm'])
        self.S.op('dve', lambda e: e.tensor_reduce(out=den[0:32, 0, :], in_=Pm[0:32, 0, 0:769], axis=AX.X, op=ALU.add), reads=['Pm'], writes=['den'])
        for c in range(6):
            for rh in range(4):
                gb = GB[gi % 2][0:32, :]
                gk = 'G%d' % (gi % 2)
                self.S.dma('pool', None, None, reads=['idxi'], writes=[gk],
                           fn=lambda e: e.indirect_dma_start(out=gb, out_offset=None, in_=cv8,
                                                             in_offset=bass.IndirectOffsetOnAxis(ap=idxS[0:32, c, rh:rh + 1], axis=0)))
                g3 = gb.rearrange("p (r d) -> p r d", d=128)
                pb_ = Pm[0:32, 0, c * 128 + rh * 32:c * 128 + rh * 32 + 32].unsqueeze(2).broadcast_to([32, 32, 128])
                self.tt(g3, g3, pb_, ALU.mult, [gk, 'Pm'], gk)
                self.S.op('dve', lambda e: e.tensor_reduce(out=pv[0:32, c * 4 + rh, :], in_=gb.rearrange("p (r d) -> p d r", d=128), axis=AX.X, op=ALU.add),
                          reads=[gk], writes=['pv'])
                gi += 1
        self.S.op('dve', lambda e: e.tensor_reduce(out=osum[0:32, 0, :], in_=pv[0:32, :, :].rearrange("p i d -> p d i"), axis=AX.X, op=ALU.add),
                  reads=['pv'], writes=['osum'])
        self.stt(osum[0:32, 0, :], vsel[0:32, 0, :], Pm[0:32, 0, 768:769], osum[0:32, 0, :], ALU.mult, ALU.add, ['vsel', 'Pm', 'osum'], 'osum')
        self.S.op('dve', lambda e: e.reciprocal(out=den[0:32, 0, :], in_=den[0:32, 0, :]), reads=['den'], writes=['den'])
        self.ts(osum[0:32, 0, :], osum[0:32, 0, :], den[0:32, 0, 0:1], None, ALU.mult, None, ['osum', 'den'], 'osum')
        for h in range(8):
            self.mm(ps[2 + h // 4][0:SPC, (h % 4) * 128:(h % 4 + 1) * 128], c2[0:32, 321 + h * 4:321 + h * 4 + 4], osum[0:32, 0, :], True, True,
                    ['c2', 'osum'], PK[2 + h // 4])
        self.memset(qb16[:].rearrange("p h d -> p (h d)"), 0.0, 'qb16')
        for hf in range(2):
            self.cp(qb16[0:SPC, hf * 4:(hf + 1) * 4, :].rearrange("p h d -> p (h d)"), ps[2 + hf][0:SPC, :], [PK[2 + hf], 'qb16'], 'qb16')
        out_proj(tS, gS, 'gS')

        if cfg.stop == 'attn':
            self.finish(dbg, xres, XK)
            return

        TG = NT // cfg.MG

        def moe(layer):
            ada(2 * layer + 1)
            if cfg.stop == 'ada1':
                return
            off = 0
            wr32, off = carve(off, [8, NE], F32)
            wrm, off = carve(off, [8, NE], F32)
            rb, off = carve(off, [1, NE], F32)
            brB, off = carve(off, [1, NE], F32)
            Wt, off = carve(off, [NT, NE], F32)
            hTm, off = carve(off, [8, TG * 128], BF16)
            xT32, off = carve(off, [8, 128], F32)
            accm, off = carve(off, [TG, D], F32)
            wg2, wu2, wd2, AT, sg = [], [], [], [], []
            for i in range(2):
                a_, off = carve(off, [8, 512], BF16); wg2.append(a_)
                a_, off = carve(off, [8, 512], BF16); wu2.append(a_)
                a_, off = carve(off, [4, D], BF16); wd2.append(a_)
                a_, off = carve(off, [4, 384], BF16); AT.append(a_)
                a_, off = carve(off, [1, 384], F32); sg.append(a_)
            sc_, off = carve(off, [1, NE], F32)
            bi, off = carve(off, [NG, 8], F32)
            msk, off = carve(off, [NG, 8], F32)
            t8g, off = carve(off, [NG, 8], F32)
            gs, off = carve(off, [1, NG], F32)
            gmax, off = carve(off, [1, 1], F32)
            ohg, off = carve(off, [1, NG], F32)
            t8e, off = carve(off, [1, 8], F32)
            sel2, off = carve(off, [1, NE], F32)
            wun, off = carve(off, [1, NE], F32)
            dsum, off = carve(off, [1, 1], F32)
            zq, off = carve(off, [1, D], F32)
            assert off <= SCR_BYTES, off
            zq2 = zq[:, 0, :]
            bi2 = bi[:].rearrange("p g e -> p (g e)")
            msk2 = msk[:].rearrange("p g e -> p (g e)")

            self.ld(wr32[:], wr_d.rearrange("(kc p) e -> p kc e", p=128), 'wr32')
            for kc in range(8):
                self.ts(wrm[:, kc, :], wr32[:, kc, :], modT[:, 1, kc, 0:1], None, ALU.mult, None, ['wr32', 'modT'], 'wrm')
                self.mm(ps[0][0:1, 0:NE], modT[:, 0, kc, 0:1], wr32[:, kc, :], kc == 0, kc == 7, ['modT', 'wr32'], 'ps0')
            self.cp(rb[0:1, 0, :], ps[0][0:1, 0:NE], ['ps0'], 'rb')
            self.bcast_row(brB[:, 0, :], br_d, NE, 'brB', 'brB')
            if cfg.stop == 'rsetup':
                return

            for G in range(cfg.MG):
                tiles = list(range(G * TG, (G + 1) * TG))
                for ti, tau in enumerate(tiles):
                    smp = (tau == NT - 1)
                    tile_to_hT(xres[:, tau, :], XK[tau], hTm, ti * 128, sample=smp, xT32=xT32)
                    if cfg.stop == 'r_0':
                        return
                    wsel, wk = (wr32, 'wr32') if smp else (wrm, 'wrm')
                    for kc in range(8):
                        self.mm(ps[6][:, 0:NE], xT32[:, kc, :], wsel[:, kc, :], kc == 0, smp and kc == 7, ['xT32', wk], 'ps6')
                    if not smp:
                        self.mm(ps[6][:, 0:NE], self.ones32[0:1, :], rb[0:1, 0, :], False, True, ['ones32', 'rb'], 'ps6')
                    if cfg.stop == 'r_a':
                        return
                    self.act(sc_[:, 0, :], ps[6][:, 0:NE], AF.Sigmoid, ['ps6'], 'sc_')
                    self.tt(bi2, sc_[:, 0, :], brB[:, 0, :], ALU.add, ['sc_', 'brB'], 'bi')
                    for g in range(NG):
                        self.S.op('dve', lambda e: e.max(out=t8g[:, g, :], in_=bi[:, g, :]), reads=['bi'], writes=['t8g'])
                    if cfg.stop == 'r_b':
                        return
                    self.tt(gs[:, 0, :], t8g[:, :, 0], t8g[:, :, 1], ALU.add, ['t8g'], 'gs')
                    self.tt(gmax[:, 0, :], gs[:, 0, 0:1], gs[:, 0, 1:2], ALU.max, ['gs'], 'gmax')
                    for g in range(2, NG):
                        self.tt(gmax[:, 0, :], gmax[:, 0, :], gs[:, 0, g:g + 1], ALU.max, ['gs', 'gmax'], 'gmax')
                    self.ts(ohg[:, 0, :], gs[:, 0, :], gmax[:, 0, 0:1], None, ALU.is_equal, None, ['gs', 'gmax'], 'ohg')
                    self.ts(ohg[:, 0, :], ohg[:, 0, :], BIG, -BIG, ALU.mult, ALU.add, ['ohg'], 'ohg')
                    self.tt(msk[:], bi[:], ohg[:, 0, :].unsqueeze(2).broadcast_to([128, NG, 8]), ALU.add, ['bi', 'ohg'], 'msk')
                    self.S.op('dve', lambda e: e.max(out=t8e[:, 0, :], in_=msk2), reads=['msk'], writes=['t8e'])
                    self.ts(sel2[:, 0, :], msk2, t8e[:, 0, 1:2], None, ALU.is_ge, None, ['msk', 't8e'], 'sel2')
                    self.tt(wun[:, 0, :], sc_[:, 0, :], sel2[:, 0, :], ALU.mult, ['sc_', 'sel2'], 'wun')
                    self.S.op('dve', lambda e: e.tensor_reduce(out=dsum[:, 0, :], in_=wun[:, 0, :], axis=AX.X, op=ALU.add), reads=['wun'], writes=['dsum'])
                    self.S.op('dve', lambda e: e.reciprocal(out=dsum[:, 0, :], in_=dsum[:, 0, :]), reads=['dsum'], writes=['dsum'])
                    self.ts(Wt[:, tau, :], wun[:, 0, :], dsum[:, 0, 0:1], None, ALU.mult, None, ['wun', 'dsum'], 'Wt')
                if cfg.stop == 'route':
                    return
                chunks = [(t0, min(t0 + 3, TG)) for t0 in range(0, TG, 3)]
                ci = 0
                for e_ in range(NE):
                    sl = e_ % 2
                    self.S.dma('pool', wg2[sl][:], wg_d[layer, e_].rearrange("(kc p) n -> p kc n", p=128), writes=['wg%d' % sl])
                    self.S.dma('pool', wu2[sl][:], wu_d[layer, e_].rearrange("(kc p) n -> p kc n", p=128), writes=['wu%d' % sl])
                    self.S.dma('pool', wd2[sl][:], wd_d[layer, e_].rearrange("(kc p) n -> p kc n", p=128), writes=['wd%d' % sl])
                    for (t0, t1) in chunks:
                        ncol = (t1 - t0) * 128
                        c0 = t0 * 128
                        ab = ci % 2
                        ci += 1
                        for fc in range(4):
                            for kc in range(8):
                                self.mm(ps[fc % 2][:, 0:ncol], wg2[sl][:, kc, fc * 128:(fc + 1) * 128], hTm[:, kc, c0:c0 + ncol], kc == 0, kc == 7,
                                        ['wg%d' % sl, 'hTg'], PK[fc % 2])
                            for kc in range(8):
                                self.mm(ps[2 + fc % 2][:, 0:ncol], wu2[sl][:, kc, fc * 128:(fc + 1) * 128], hTm[:, kc, c0:c0 + ncol], kc == 0, kc == 7,
                                        ['wu%d' % sl, 'hTg'], PK[2 + fc % 2])
                            self.act(sg[ab][:, 0, 0:ncol], ps[fc % 2][:, 0:ncol], AF.Silu, [PK[fc % 2]], 'sg%d' % ab)
                            self.tt(AT[ab][:, fc, 0:ncol], sg[ab][:, 0, 0:ncol], ps[2 + fc % 2][:, 0:ncol], ALU.mult, ['sg%d' % ab, PK[2 + fc % 2]], 'AT%d' % ab)
                        for ti in range(t0, t1):
                            tau = tiles[ti]
                            for hf in range(2):
                                pb = 4 + 2 * (ti % 2) + hf
                                for fc in range(4):
                                    self.mm(ps[pb][:, :], AT[ab][:, fc, (ti - t0) * 128:(ti - t0 + 1) * 128], wd2[sl][:, fc, hf * 512:(hf + 1) * 512],
                                            fc == 0, fc == 3, ['AT%d' % ab, 'wd%d' % sl], PK[pb])
                                dst = accm[:, ti, hf * 512:(hf + 1) * 512]
                                if e_ == 0:
                                    self.ts(dst, ps[pb][:, :], Wt[:, tau, e_:e_ + 1], None, ALU.mult, None, [PK[pb], 'Wt'], 'accm%d' % ti)
                                else:
                                    self.stt(dst, ps[pb][:, :], Wt[:, tau, e_:e_ + 1], dst, ALU.mult, ALU.add, [PK[pb], 'Wt', 'accm%d' % ti], 'accm%d' % ti)
                for ti, tau in enumerate(tiles):
                    smp = (tau == NT - 1)
                    gt, gk = (gS, 'gS') if smp else (gP, 'gP')
                    self.tt(zq2, accm[:, ti, :], gt[:], ALU.mult, ['accm%d' % ti, gk], 'zq')
                    self.stt(zq2, xres[:, tau, :], cfg.ALPHA, zq2, ALU.mult, ALU.add, [XK[tau], 'zq'], 'zq')
                    self.layernorm(zq2, xres[:, tau, :], lnG[:], lnB[:], 'zq', XK[tau], 'lnG')

        moe(0)
        if cfg.stop in ('moe0', 'route', 'ada1', 'rsetup', 'r_a', 'r_b', 'r_0'):
            self.finish(dbg, xres, XK)
            return

        ada(2)
        off = 0
        uT, off = carve(off, [8, 30 + 512], F32)
        hTc, off = carve(off, [8, 512], BF16)
        winc = []
        for i in range(2):
            a_, off = carve(off, [8, 256], BF16); winc.append(a_)
        yg, off = carve(off, [8, 512], F32)
        ytok, off = carve(off, [1, D], F32)
        zb, off = carve(off, [8, 128], BF16)
        zT, off = carve(off, [8, 128], BF16)
        wout, off = carve(off, [8, D], BF16)
        wdw, off = carve(off, [8, CW], F32)
        cg_, off = carve(off, [1, D], F32)
        cb_, off = carve(off, [1, D], F32)
        sgm, off = carve(off, [1, 512], F32)
        zq, off = carve(off, [1, D], F32)
        uS, off = carve(off, [8, SPC], F32)
        stT, off = carve(off, [8 * SPC, CW], F32)
        prodS, off = carve(off, [8 * SPC, CW], F32)
        yS, off = carve(off, [8, SPC], F32)
        utok, off = carve(off, [1, D], F32)
        assert off <= SCR_BYTES, off
        zq2 = zq[:, 0, :]
        ytok2 = ytok[:, 0, :]
        utok2 = utok[:, 0, :]
        self.memset(uT[:].rearrange("p a b -> p (a b)"), 0.0, 'uT')
        self.ld(wdw[:].rearrange("p a b -> p (a b)"), wdwT_d, 'wdw')
        self.S.dma('pool', wout[:], wout_d.rearrange("(kc p) n -> p kc n", p=128), writes=['wout'])
        self.bcast_row(cg_[:, 0, :], clng_d, D, 'cgb', 'cg')
        self.bcast_row(cb_[:, 0, :], clnb_d, D, 'cgb', 'cb')
        win_src = win_d.rearrange("(kc p) n -> p kc n", p=128)
        wi = [0]

        def glu_cols(ncol, dst_fn):
            for cc in range(8):
                wb = winc[wi[0] % 2]
                wk = 'winc%d' % (wi[0] % 2)
                wi[0] += 1
                self.S.dma('pool', wb[:, :, 0:128], win_src[:, :, cc * 128:(cc + 1) * 128], writes=[wk])
                self.S.dma('pool', wb[:, :, 128:256], win_src[:, :, D + cc * 128:D + (cc + 1) * 128], writes=[wk])
                for kc in range(8):
                    self.mm(ps[0][:, 0:ncol], wb[:, kc, 0:128], hTc[:, kc, 0:ncol], kc == 0, kc == 7, [wk, 'hTg'], 'ps0')
                for kc in range(8):
                    self.mm(ps[1][:, 0:ncol], wb[:, kc, 128:256], hTc[:, kc, 0:ncol], kc == 0, kc == 7, [wk, 'hTg'], 'ps1')
                self.act(sgm[:, 0, 0:ncol], ps[1][:, 0:ncol], AF.Sigmoid, ['ps1'], 'sgm')
                dst, dk = dst_fn(cc)
                self.tt(dst, ps[0][:, 0:ncol], sgm[:, 0, 0:ncol], ALU.mult, ['ps0', 'sgm'], dk)

        def conv_tail(tau, col0, gate_tile, gkey):
            for hf in range(2):
                for q in range(4):
                    cc = hf * 4 + q
                    self.tr(ps[2 + hf][:, q * 128:(q + 1) * 128], yg[:, cc, col0:col0 + 128], ident, ['yg', 'consts'], PK[2 + hf])
                self.cp(ytok2[:, hf * 512:(hf + 1) * 512], ps[2 + hf][:, :], [PK[2 + hf]], 'ytok', eng='act')
            self.layernorm(ytok2, ytok2, cg_[:, 0, :], cb_[:, 0, :], 'ytok', 'ytok', 'cgb')
            self.act(zb[:].rearrange("p a b -> p (a b)"), ytok2, AF.Silu, ['ytok'], 'zb')
            pt = ps[4][:].bitcast(BF16)
            for cc in range(8):
                self.tr(pt[:, cc * 128:(cc + 1) * 128], zb[:, cc, :], identb[:], ['zb', 'identb'], 'ps4')
            self.cp(zT[:].rearrange("p a b -> p (a b)"), pt[:, :], ['ps4'], 'zT')
            for hf in range(2):
                for cc in range(8):
                    self.mm(ps[6 + hf][:, :], zT[:, cc, :], wout[:, cc, hf * 512:(hf + 1) * 512], cc == 0, cc == 7, ['zT', 'wout'], PK[6 + hf])
                self.tt(zq2[:, hf * 512:(hf + 1) * 512], ps[6 + hf][:, :], gate_tile[:, hf * 512:(hf + 1) * 512], ALU.mult, [PK[6 + hf], gkey], 'zq')
            self.stt(zq2, xres[:, tau, :], cfg.ALPHA, zq2, ALU.mult, ALU.add, [XK[tau], 'zq'], 'zq')
            self.layernorm(zq2, xres[:, tau, :], lnG[:], lnB[:], 'zq', XK[tau], 'lnG')

        for g0 in range(0, NPT, 4):
            tiles = list(range(g0, min(g0 + 4, NPT)))
            ncol = len(tiles) * 128
            for ti, tau in enumerate(tiles):
                tile_to_hT(xres[:, tau, :], XK[tau], hTc, ti * 128)
            glu_cols(ncol, lambda cc: (uT[:, cc, 30:30 + ncol], 'uT'))
            if g0 == 0:
                self.ts(uT[:, :, 30:158], uT[:, :, 30:158], flag[:, 0:1], None, ALU.mult, None, ['uT', 'flag'], 'uT')
            for cc in range(8):
                for k in range(CW):
                    if k == 0:
                        self.ts(yg[:, cc, 0:ncol], uT[:, cc, 0:ncol], wdw[:, cc, 0:1], None, ALU.mult, None, ['uT', 'wdw'], 'yg')
                    else:
                        self.stt(yg[:, cc, 0:ncol], uT[:, cc, k:k + ncol], wdw[:, cc, k:k + 1], yg[:, cc, 0:ncol], ALU.mult, ALU.add, ['uT', 'wdw', 'yg'], 'yg')
            if tiles[-1] == NPT - 1:
                cl = 30 + (len(tiles) - 1) * 128 + 98
                for hf in range(2):
                    for q in range(4):
                        cc = hf * 4 + q
                        self.tr(ps[2 + hf][0:30, q * 128:(q + 1) * 128], uT[:, cc, cl:cl + 30], ident, ['uT', 'consts'], PK[2 + hf])
                    self.cp(utok2[0:30, hf * 512:(hf + 1) * 512], ps[2 + hf][0:30, :], [PK[2 + hf]], 'utok', eng='act')
                self.st(conv_p[:, :], utok2[0:30, :], 'utok', 'conv_p')
            else:
                self.cp(uT[:, :, 0:30], uT[:, :, ncol:ncol + 30], ['uT'], 'uT')
            if cfg.stop == 'c_conv':
                self.finish(dbg, xres, XK)
                return
            for ti, tau in enumerate(tiles):
                if tau >= 1:
                    conv_tail(tau, ti * 128, gP, 'gP')
            if cfg.stop == 'c_tail':
                self.finish(dbg, xres, XK)
                return
        tile_to_hT(xres[:, tS, :], XK[tS], hTc, 0, sample=True)
        glu_cols(SPC, lambda cc: (uS[:, cc, :], 'uS'))
        st4 = stT[:].rearrange("p (c s) k -> p c s k", c=8)
        pflat = prodS[:].rearrange("p a k -> p (a k)")[:, 0:8 * SPC * 30]
        self.ld(pflat, stfm_d, 'prodS')
        self.cp(st4[:, :, :, 0:30], pflat.rearrange("p (c s k) -> p c s k", c=8, s=SPC), ['prodS'], 'stT')
        self.cp(st4[:, :, :, 30], uS[:], ['uS', 'stT'], 'stT')
        self.tt(prodS[:].rearrange("p (c s) k -> p c s k", c=8), st4, wdw[:].unsqueeze(2).broadcast_to([128, 8, SPC, CW]), ALU.mult, ['stT', 'wdw'], 'prodS')
        self.S.op('dve', lambda e: e.tensor_reduce(out=yS[:].rearrange("p c s -> p (c s)"), in_=prodS[:], axis=AX.X, op=ALU.add), reads=['prodS'], writes=['yS'])
        self.memset(yg[:, :, 0:128], 0.0, 'yg')
        self.cp(yg[:, :, 0:SPC], yS[:], ['yS', 'yg'], 'yg')
        conv_tail(tS, 0, gS, 'gS')
        for s_ in range(SPC):
            self.ld(ytok2[s_ * 29:(s_ + 1) * 29, :], sttok_d[s_, 1:30, :], 'ytok')
        for s_ in range(SPC):
            self.st(conv_s[s_, 0:29, :], ytok2[s_ * 29:(s_ + 1) * 29, :], 'ytok', 'conv_s')
        for hf in range(2):
            for q in range(4):
                cc = hf * 4 + q
                self.tr(ps[2 + hf][0:SPC, q * 128:(q + 1) * 128], uS[:, cc, :], ident, ['uS', 'consts'], PK[2 + hf])
            self.cp(utok2[0:SPC, hf * 512:(hf + 1) * 512], ps[2 + hf][0:SPC, :], [PK[2 + hf]], 'utok', eng='act')
        self.st(conv_s[:, 29, :], utok2[0:SPC, :], 'utok', 'conv_s')
        if cfg.stop == 'conv':
            self.finish(dbg, xres, XK)
            return

        moe(1)
        for tau in range(1, NPT):
            self.st(y_p[(tau - 1) * 128:tau * 128, :], xres[:, tau, :], XK[tau], 'y_p')
        self.st(y_s[:, :], xres[0:SPC, tS, :], XK[tS], 'y_s')
        self.finish(dbg, xres, XK)

    def finish(self, dbg, xres, XK):
        cfg = self.cfg
        if dbg is not None:
            for t in range(cfg.NT):
                self.st(dbg[t * 128:(t + 1) * 128, :], xres[:, t, :], XK[t], 'dbg')
        self.S.wait_keys('sp', list(set(self.outkeys)))
        self.es.close()


def prep_inputs(cfg, I):
    f32 = np.float32
    NC, NCB, LS, NSLOT, NPT, NGT, SPC, NBLK = cfg.NC, cfg.NCB, cfg.LS, cfg.NSLOT, cfg.NPT, cfg.NGT, cfg.SPC, cfg.NBLK
    half = 8
    inv_freq = (f32(ROPE_THETA) ** (-np.arange(half, dtype=f32) / f32(half))).astype(f32) if False else None
    half = 16
    inv_freq = np.power(f32(ROPE_THETA), -(np.arange(half, dtype=f32) / f32(half))).astype(f32)
    ident = np.eye(128, dtype=f32)
    tri = (np.arange(128)[:, None] <= np.arange(128)[None, :]).astype(f32)
    consts = np.concatenate([ident, tri], axis=1)
    c2 = np.zeros((128, 512), f32)
    cm = np.zeros((8, 32), f32)
    for s_ in range(4):
        for kv in range(2):
            for h in range(kv * 4, kv * 4 + 4):
                cm[s_ * 2 + kv, h * 4 + s_] = 1.0
    c2[:, 0:256] = cm.reshape(1, 256)
    c2[0:32, 256] = (np.arange(32) // 4) // 4
    for s_ in range(4):
        for h in range(8):
            c2[s_, (257 if h < 4 else 289) + h * 4 + s_] = 1.0
    c2[0:32, 321:353] = np.eye(32, dtype=f32)
    c2[:, 353:361] = np.arange(8, dtype=f32)[None, :]
    shared = {
        "consts": consts, "consts2": c2,
        "w_ada": np.ascontiguousarray(I["w_ada"]), "b_ada": np.ascontiguousarray(I["b_ada"]),
        "b_adaT": np.ascontiguousarray(I["b_ada"].reshape(2, 48, 128).transpose(2, 0, 1).reshape(128, 96)),
        "ln_g": np.ascontiguousarray(I["ln_g"].reshape(4, D)), "ln_b": np.ascontiguousarray(I["ln_b"].reshape(4, D)),
        "w_qkv": np.ascontiguousarray(I["w_qkv"][0]), "w_o": np.ascontiguousarray(I["w_o"][0]),
        "conv_w_in": np.ascontiguousarray(I["conv_w_in"][0]),
        "wdwT": np.ascontiguousarray(I["conv_w_dw"][0].T.reshape(8, 128, CW).transpose(1, 0, 2).reshape(128, 8 * CW)),
        "conv_ln_g": np.ascontiguousarray(I["conv_ln_g"].reshape(1, D)), "conv_ln_b": np.ascontiguousarray(I["conv_ln_b"].reshape(1, D)),
        "conv_w_out": np.ascontiguousarray(I["conv_w_out"][0]),
        "w_router": np.ascontiguousarray(I["w_router"]), "b_router": np.ascontiguousarray(I["b_router"].reshape(1, -1)),
        "w_gate": np.ascontiguousarray(I["w_gate"]), "w_up": np.ascontiguousarray(I["w_up"]), "w_down": np.ascontiguousarray(I["w_down"]),
        "cache_k": np.ascontiguousarray(I["cache_k"].reshape(cfg.NPHYS, -1)),
        "cache_v": np.ascontiguousarray(I["cache_v"].reshape(cfg.NPHYS, -1)),
    }
    maps = []
    pt = np.asarray(I["page_table"]).astype(np.int32)
    for c in range(NC):
        b, j = c // NCB, c % NCB
        rot = LS * j - 1
        m = dict(shared)
        m["xg"] = np.ascontiguousarray(np.roll(I["x_prompt"][b], -rot * 256, axis=0))
        gslot = (rot + np.arange(NSLOT)) % NSLOT
        pos = (gslot[:, None] * 256 + np.arange(256)[None, :]).reshape(-1)
        pos = np.concatenate([pos, np.full(128, cfg.PAST_LEN)]).astype(f32)
        ang = pos[:, None] * inv_freq[None, :]
        tab = np.concatenate([np.cos(ang), np.sin(ang)], axis=1).astype(f32)
        m["rope"] = np.ascontiguousarray(tab.reshape(NGT + 1, 128, 32).transpose(1, 0, 2).reshape(128, (NGT + 1) * 32))
        sid = np.arange(c * SPC, (c + 1) * SPC)
        xs = np.zeros((128, D), f32)
        xs[:SPC] = I["x_sample"][sid, 0]
        m["xs"] = xs
        cv = np.zeros((8, D), f32)
        cv[0] = I["c_prompt"][b]
        cv[1:1 + SPC] = I["c_sample"][sid]
        m["cvec"] = cv
        valid = np.zeros((NPT, NSLOT), bool)
        for tau in range(NPT):
            own = rot + (tau + 1) // 2
            valid[tau] = gslot < own
        m["nmask"] = np.ascontiguousarray(np.broadcast_to(np.where(valid, 0.0, -BIG).astype(f32).reshape(1, -1), (128, NPT * NSLOT)))
        m["v01"] = np.ascontiguousarray(np.broadcast_to(valid.astype(f32).reshape(1, -1), (128, NPT * NSLOT)))
        m["flag"] = np.full((128, 1), 1.0 if j > 0 else 0.0, f32)
        ptA = np.zeros((128, 4), np.int32)
        for r in range(2):
            for eo in range(2):
                for s2 in range(2):
                    ptA[s2 * NBLK:(s2 + 1) * NBLK, r * 2 + eo] = pt[sid[2 * r + s2], eo::2]
        m["ptA"] = ptA
        ptB = np.zeros((32, 2 * NBLK), np.int32)
        for h in range(8):
            for s in range(SPC):
                ptB[h * 4 + s, :NBLK] = pt[sid[s], 0::2]
                ptB[h * 4 + s, NBLK:] = pt[sid[s], 1::2]
        m["ptB"] = ptB
        st = I["state_conv"][0][sid]
        m["st_tok"] = np.ascontiguousarray(st)
        m["st_fm"] = np.ascontiguousarray(st.reshape(SPC, 30, 8, 128).transpose(3, 2, 0, 1).reshape(128, 8 * SPC * 30))
        maps.append(m)
    return maps


_PROG_CACHE = {}


def run_cfg(cfg, inputs):
    key = (cfg.B, cfg.SEQ, cfg.NCB, cfg.DEC_BATCH, cfg.PAST_LEN, cfg.NE, cfg.NG, cfg.MG, cfg.stop)
    if key not in _PROG_CACHE:
        p = Prog(cfg)
        p.build()
        _PROG_CACHE[key] = p
    p = _PROG_CACHE[key]
    maps = prep_inputs(cfg, inputs)
    maps = [{k: v for k, v in m.items() if k in p.din} for m in maps]
    res = run_bass_kernel_spmd(p.nc, maps, core_ids=list(range(cfg.NC)))
    return res.results


def assemble(cfg, R):
    f32 = np.float32
    B, NCB, LS, SPC = cfg.B, cfg.NCB, cfg.LS, cfg.SPC
    y_p = np.zeros((B, cfg.SEQ, D), f32)
    k_p = np.zeros((B, cfg.SEQ // 128, 1, 2, 128, 128), f32)
    v_p = np.zeros_like(k_p)
    conv_p = np.zeros((1, B, 30, D), f32)
    y_s = np.zeros((cfg.DEC_BATCH, 1, D), f32)
    k_s = np.zeros((cfg.DEC_BATCH, 1, 2, 1, 128), f32)
    v_s = np.zeros_like(k_s)
    conv_s = np.zeros((1, cfg.DEC_BATCH, 30, D), f32)
    for c, r in enumerate(R):
        b, j = c // NCB, c % NCB
        t0 = j * LS * 256
        y_p[b, t0:t0 + LS * 256] = r["y_p"]
        k_p[b, j * 2 * LS:(j + 1) * 2 * LS, 0] = r["k_p"]
        v_p[b, j * 2 * LS:(j + 1) * 2 * LS, 0] = r["v_p"]
        if j == NCB - 1:
            conv_p[0, b] = r["conv_p"]
        sl = slice(c * SPC, (c + 1) * SPC)
        y_s[sl, 0] = r["y_s"]
        k_s[sl, 0, :, 0, :] = r["k_s"].reshape(SPC, 2, 128)
        v_s[sl, 0, :, 0, :] = r["v_s"].reshape(SPC, 2, 128)
        conv_s[0, sl] = r["conv_s"]
    return (y_p, y_s, k_p, v_p, conv_p, k_s, v_s, conv_s)


def kernel(**inputs):
    cfg = Cfg()
    inputs = {k: np.asarray(v) for k, v in inputs.items()}
    R = run_cfg(cfg, inputs)
    return assemble(cfg, R)
```

```python
import numpy as np
from contextlib import ExitStack
import concourse.bass as bass
import concourse.mybir as mybir
from concourse.bass_utils import run_bass_kernel_spmd

F32 = mybir.dt.float32
BF16 = mybir.dt.bfloat16
I32 = mybir.dt.int32
U32 = mybir.dt.uint32
AF = mybir.ActivationFunctionType
ALU = mybir.AluOpType
AX = mybir.AxisListType

EPOCH = 24000
D = 1024
H = 8
KVH = 2
HD = 128
ROPE_THETA = 500000.0
ATTN_SCALE = HD ** -0.5
CW = 31
DFF = 512
LN_EPS = 1e-5
BIG = 1.0e30


class Cfg:
    def __init__(self, B=2, SEQ=8192, NCB=4, DEC_BATCH=32, PAST_LEN=16384, NE=32, NG=4, DEPTH=2, MG=3,
                 stop=None):
        self.B, self.SEQ, self.NCB, self.DEC_BATCH, self.PAST_LEN = B, SEQ, NCB, DEC_BATCH, PAST_LEN
        self.NE, self.NG, self.EPG = NE, NG, NE // NG
        self.NC = B * NCB
        self.NSLOT = SEQ // 256
        self.LS = self.NSLOT // NCB
        self.NPT = 2 * self.LS + 1
        self.NT = self.NPT + 1
        self.NGT = 2 * self.NSLOT
        self.SPC = DEC_BATCH // self.NC
        self.NPG = PAST_LEN // 128
        self.NBLK = self.NPG // 2
        self.NPHYS = (DEC_BATCH * self.NPG * 5) // 4
        self.DEPTH = DEPTH
        self.MG = MG
        self.ALPHA = (2 * DEPTH) ** 0.25
        self.stop = stop
        assert self.SPC == 4 and self.EPG == 8 and self.NT % MG == 0


class Sync:
    def __init__(self, nc, es, same_engine_wait=True):
        self.nc = nc
        self.es = es
        self.engs = {'pe': nc.tensor, 'act': nc.scalar, 'dve': nc.vector,
                     'pool': nc.gpsimd, 'sp': nc.sync}
        self.sems = {}
        self.cnt = {k: 0 for k in self.engs}
        self.waited = {k: {} for k in self.engs}
        self.res = {}
        self.dsem = {}
        self.same = same_engine_wait
        self.n_ins = 0

    def _sem(self, key):
        if key not in self.sems:
            nm = "s%d" % len(self.sems)
            self.sems[key] = self.es.enter_context(self.nc.semaphore(nm))
        return self.sems[key]

    def _wait(self, eng, deps):
        best = {}
        for (sk, v) in deps:
            if best.get(sk, 0) < v:
                best[sk] = v
        for sk, v in best.items():
            if sk[0] == 'E' and sk[1] == eng:
                if not self.same or eng == 'pe':
                    continue
            if self.waited[eng].get(sk, 0) >= v:
                continue
            self.engs[eng].wait_ge(self._sem(sk), v)
            self.n_ins += 1
            self.waited[eng][sk] = v

    def _deps(self, reads, writes):
        deps = []
        for k in reads:
            r = self.res.get(k)
            if r and r['w']:
                deps.append(r['w'])
        for k in writes:
            r = self.res.get(k)
            if r:
                if r['w']:
                    deps.append(r['w'])
                deps += r['r']
        return deps

    def _record(self, ev, reads, writes):
        for k in reads:
            self.res.setdefault(k, {'w': None, 'r': []})['r'].append(ev)
        for k in writes:
            self.res[k] = {'w': ev, 'r': []}

    def op(self, eng, fn, reads=(), writes=()):
        self._wait(eng, self._deps(reads, writes))
        ins = fn(self.engs[eng])
        self.cnt[eng] += 1
        ep, v = divmod(self.cnt[eng] - 1, EPOCH)
        sk = ('E', eng, ep)
        ins.then_inc(self._sem(sk), 1)
        self.n_ins += 1
        self._record((sk, v + 1), reads, writes)
        return ins

    def dma(self, eng, out, in_, reads=(), writes=(), key=None, fn=None):
        self._wait(eng, self._deps(reads, writes))
        if key is None:
            key = ('D', writes[0] if writes else reads[0])
        if fn is None:
            ins = self.engs[eng].dma_start(out=out, in_=in_)
        else:
            ins = fn(self.engs[eng])
        c = self.dsem.get(key, 0) + 16
        self.dsem[key] = c
        ins.then_inc(self._sem(key), 16)
        self.n_ins += 1
        self._record((key, c), reads, writes)
        return ins

    def wait_keys(self, eng, keys):
        deps = []
        for k in keys:
            r = self.res.get(k)
            if r:
                if r['w']:
                    deps.append(r['w'])
                deps += r['r']
        self._wait(eng, deps)

    def barrier(self):
        deps = []
        for r in self.res.values():
            if r['w']:
                deps.append(r['w'])
            deps += r['r']
        for eng in self.engs:
            self._wait(eng, deps)
        self.res = {}


class Prog:
    def __init__(self, cfg):
        self.cfg = cfg
        self.nc = bass.Bass("TRN2", target_bir_lowering=False)
        self.es = ExitStack()
        self.din = {}
        self.dout = {}

    def inp(self, name, shape, dt=F32):
        self.din[name] = self.nc.dram_tensor(name, list(shape), dt, kind="ExternalInput").ap()
        return self.din[name]

    def outp(self, name, shape, dt=F32):
        self.dout[name] = self.nc.dram_tensor(name, list(shape), dt, kind="ExternalOutput").ap()
        return self.dout[name]

    def sb(self, name, shape, dt=F32):
        return self.es.enter_context(self.nc.sbuf_tensor("sb_" + name, list(shape), dt))

    def mm(self, out, lhsT, rhs, start, stop, reads, w):
        self.S.op('pe', lambda e: e.matmul(out, lhsT=lhsT, rhs=rhs, start=start, stop=stop), reads=reads, writes=[w])

    def tr(self, out, in_, ident, reads, w):
        self.S.op('pe', lambda e: e.transpose(out=out, in_=in_, identity=ident), reads=reads, writes=[w])

    def act(self, out, in_, func, reads, w, bias=None, scale=None, eng='act'):
        kw = {}
        if bias is not None:
            kw['bias'] = bias
        if scale is not None:
            kw['scale'] = scale
        self.S.op('act', lambda e: e.activation(out=out, in_=in_, func=func, **kw), reads=reads, writes=[w])

    def tt(self, out, a, b, op, reads, w, eng='dve'):
        self.S.op(eng, lambda e: e.tensor_tensor(out=out, in0=a, in1=b, op=op), reads=reads, writes=[w])

    def ts(self, out, a, s1, s2, op0, op1, reads, w, eng='dve'):
        if op1 is None:
            self.S.op(eng, lambda e: e.tensor_scalar(out=out, in0=a, scalar1=s1, scalar2=None, op0=op0), reads=reads, writes=[w])
        else:
            self.S.op(eng, lambda e: e.tensor_scalar(out=out, in0=a, scalar1=s1, scalar2=s2, op0=op0, op1=op1), reads=reads, writes=[w])

    def stt(self, out, a, s, b, op0, op1, reads, w):
        self.S.op('dve', lambda e: e.scalar_tensor_tensor(out=out, in0=a, scalar=s, in1=b, op0=op0, op1=op1), reads=reads, writes=[w])

    def cp(self, out, in_, reads, w, eng='dve'):
        if eng == 'act':
            self.S.op('act', lambda e: e.copy(out=out, in_=in_), reads=reads, writes=[w])
        else:
            self.S.op(eng, lambda e: e.tensor_copy(out=out, in_=in_), reads=reads, writes=[w])

    def memset(self, ap, val, w, eng='dve'):
        self.S.op(eng, lambda e: e.memset(ap, val), writes=[w])

    def ld(self, out, in_, w, reads=(), eng='sp'):
        self.S.dma(eng, out, in_, reads=list(reads), writes=[w])

    def st(self, out, in_, r, okey):
        self.S.dma('sp', out, in_, reads=[r], writes=[okey])
        self.outkeys.append(okey)

    def bcast_row(self, dst, row_ap, n, dkey, tmpname):
        rt = self.rowtmp
        for c0 in range(0, n, 512):
            cw = min(512, n - c0)
            self.ld(rt[0:1, 0:cw], row_ap[:, c0:c0 + cw], 'rowtmp')
            self.mm(self.ps[0][:, 0:cw], self.ones32[0:1, :], rt[0:1, 0:cw], True, True, ['rowtmp', 'ones32'], 'ps0')
            self.cp(dst[:, c0:c0 + cw], self.ps[0][:, 0:cw], ['ps0'], dkey, eng='act')

    def layernorm(self, z, out, gB, bB, zkey, okey, gkey):
        st_, mv, rstd = self.ln_st, self.ln_mv, self.ln_rstd
        for i in range(2):
            self.S.op('dve', lambda e: e.bn_stats(out=st_[:, i, :], in_=z[:, i * 512:(i + 1) * 512]), reads=[zkey], writes=['ln_st'])
        self.S.op('dve', lambda e: e.bn_aggr(out=mv[:], in_=st_[:].rearrange("p a b -> p (a b)")), reads=['ln_st'], writes=['ln_mv'])
        self.act(rstd[:], mv[:, 1:2], AF.Ln, ['ln_mv', 'epsT'], 'ln_rstd', bias=self.epsT[:], scale=1.0)
        self.act(rstd[:], rstd[:], AF.Exp, ['ln_rstd'], 'ln_rstd', scale=-0.5)
        self.ts(z, z, mv[:, 0:1], rstd[:, 0:1], ALU.subtract, ALU.mult, [zkey, 'ln_mv', 'ln_rstd'], zkey)
        self.tt(z, z, gB, ALU.mult, [zkey, gkey], zkey)
        self.tt(out, z, bB, ALU.add, [zkey, gkey], okey)

    def build(self):
        cfg = self.cfg
        nc, es = self.nc, self.es
        NT, NPT, NGT, NSLOT, LS, SPC, NE, NG, EPG = cfg.NT, cfg.NPT, cfg.NGT, cfg.NSLOT, cfg.LS, cfg.SPC, cfg.NE, cfg.NG, cfg.EPG
        NBLK, NPG, NPHYS = cfg.NBLK, cfg.NPG, cfg.NPHYS
        self.outkeys = []
        xg = self.inp("xg", [NGT * 128, D])
        rope_d = self.inp("rope", [128, (NGT + 1) * 32])
        xs_d = self.inp("xs", [128, D])
        cvec_d = self.inp("cvec", [8, D])
        nmask_d = self.inp("nmask", [128, NPT * NSLOT])
        v01_d = self.inp("v01", [128, NPT * NSLOT])
        flag_d = self.inp("flag", [128, 1])
        consts_d = self.inp("consts", [128, 256])
        consts2_d = self.inp("consts2", [128, 512])
        ptA_d = self.inp("ptA", [128, 4], I32)
        ptB_d = self.inp("ptB", [32, 2 * NBLK], I32)
        sttok_d = self.inp("st_tok", [SPC, 30, D])
        stfm_d = self.inp("st_fm", [128, 8 * SPC * 30])
        wada_d = self.inp("w_ada", [2, D, 6 * D])
        bada_d = self.inp("b_ada", [2, 6 * D])
        badaT_d = self.inp("b_adaT", [128, 96])
        lng_d = self.inp("ln_g", [4, D])
        lnb_d = self.inp("ln_b", [4, D])
        wqkv_d = self.inp("w_qkv", [D, 1536])
        wo_d = self.inp("w_o", [D, D])
        win_d = self.inp("conv_w_in", [D, 2 * D])
        wdwT_d = self.inp("wdwT", [128, 8 * CW])
        clng_d = self.inp("conv_ln_g", [1, D])
        clnb_d = self.inp("conv_ln_b", [1, D])
        wout_d = self.inp("conv_w_out", [D, D])
        wr_d = self.inp("w_router", [D, NE])
        br_d = self.inp("b_router", [1, NE])
        wg_d = self.inp("w_gate", [2, NE, D, DFF])
        wu_d = self.inp("w_up", [2, NE, D, DFF])
        wd_d = self.inp("w_down", [2, NE, DFF, D])
        ck_d = self.inp("cache_k", [NPHYS, 2 * 128 * 128])
        cv_d = self.inp("cache_v", [NPHYS, 2 * 128 * 128])

        y_p = self.outp("y_p", [2 * LS * 128, D])
        y_s = self.outp("y_s", [SPC, D])
        k_p = self.outp("k_p", [2 * LS, 2, 128, 128])
        v_p = self.outp("v_p", [2 * LS, 2, 128, 128])
        conv_p = self.outp("conv_p", [30, D])
        k_s = self.outp("k_s", [SPC, 256])
        v_s = self.outp("v_s", [SPC, 256])
        conv_s = self.outp("conv_s", [SPC, 30, D])
        dbg = self.outp("dbg", [NT * 128, D]) if cfg.stop else None

        self.S = S = Sync(nc, es)
        sb = self.sb
        self.ps = [es.enter_context(nc.psum_tensor("ps%d" % i, [128, 512], F32)) for i in range(8)]
        ps = self.ps
        PK = ['ps%d' % i for i in range(8)]
        xres = sb("xres", [128, NT, D])
        XK = ['xres%d' % t for t in range(NT)]
        consts = sb("consts", [128, 256])
        ident = consts[:, 0:128]
        identb = sb("identb", [128, 128], BF16)
        trib = sb("trib", [128, 128], BF16)
        self.ones32 = sb("ones32", [128, 128])
        onesb = sb("onesb", [128, 1], BF16)
        self.rowtmp = sb("rowtmp", [1, 512])
        self.ln_st = sb("ln_st", [128, 2, 6])
        self.ln_mv = sb("ln_mv", [128, 2])
        self.ln_rstd = sb("ln_rstd", [128, 1])
        flag = sb("flag", [128, 1])
        scT = sb("scT", [128, 8, 8])
        modT = sb("modT", [128, 2, 8, 8])
        badaT = sb("badaT", [128, 96])
        gP = sb("gP", [128, D])
        gS = sb("gS", [128, D])
        lnG = sb("lnG", [128, D])
        lnB = sb("lnB", [128, D])
        ks32 = sb("ks32", [128, 2 * NGT])
        kvS = sb("kvS", [128, 512])
        SCR_BYTES = 108 * 1024
        scr = sb("scr", [128, SCR_BYTES // 4])

        def carve(off_bytes, shape, dt):
            n = int(np.prod(shape))
            esz = 4 if dt in (F32, I32, U32) else 2
            assert off_bytes % 4 == 0
            a = scr[:, off_bytes // 4: off_bytes // 4 + (n * esz + 3) // 4]
            if dt != F32:
                a = a.bitcast(dt)
            a = a[:, 0:n]
            if len(shape) == 2:
                pat = "p (a b) -> p a b"
                return a.rearrange(pat, a=shape[0]), off_bytes + n * esz
            if len(shape) == 3:
                return a.rearrange("p (a b c) -> p a b c", a=shape[0], b=shape[1]), off_bytes + n * esz
            if len(shape) == 1:
                return a, off_bytes + n * esz
            raise ValueError

        self.ld(consts[:], consts_d, 'consts')
        self.ld(flag[:], flag_d, 'flag')
        self.ld(badaT[:], badaT_d, 'badaT')
        self.cp(identb[:], consts[:, 0:128], ['consts'], 'identb')
        self.cp(trib[:], consts[:, 128:256], ['consts'], 'trib')
        self.memset(self.ones32[:], 1.0, 'ones32')
        self.memset(onesb[:], 1.0, 'onesb')
        self.epsT = sb("epsT", [128, 1])
        self.memset(self.epsT[:], LN_EPS, 'epsT')

        ctmp_, o_ = carve(0, [1, D], F32)
        ctmp2_, o_ = carve(o_, [1, D], F32)
        ctmp = ctmp_[0:8, 0, :]
        ctmp2 = ctmp2_[0:8, 0, :]
        self.ld(ctmp, cvec_d, 'ctmp')
        self.act(ctmp2, ctmp, AF.Silu, ['ctmp'], 'ctmp2')
        for kc in range(8):
            self.tr(ps[0][:, kc * 8:(kc + 1) * 8], ctmp2[:, kc * 128:(kc + 1) * 128], ident[0:8, 0:8], ['ctmp2', 'consts'], 'ps0')
        self.cp(scT[:].rearrange("p a b -> p (a b)"), ps[0][:, 0:64], ['ps0'], 'scT')
        S.barrier()

        def ada(hl):
            layer, part = hl // 2, hl % 2
            base = part * 3 * D
            S.barrier()
            off = 0
            wa = []
            for i in range(2):
                a, off = carve(off, [8, 512], F32)
                wa.append(a)
            bPl, off = carve(off, [8, 128], F32)
            bSl, off = carve(off, [8, 128], F32)
            for kc in range(8):
                self.ts(bPl[:, kc, :], self.ones32[:], scT[:, kc, 0:1], None, ALU.mult, None, ['ones32', 'scT'], 'bPl')
            self.memset(bSl[:].rearrange("p a b -> p (a b)"), 0.0, 'bSl')
            for kc in range(8):
                self.cp(bSl[:, kc, 0:SPC], scT[:, kc, 1:1 + SPC], ['scT', 'bSl'], 'bSl')
            wsrc = wada_d[layer].rearrange("(kc p) n -> p kc n", p=128)
            for cgi in range(6):
                c0 = base + cgi * 512
                wt = wa[cgi % 2]
                wk = 'wa%d' % (cgi % 2)
                self.ld(wt[:], wsrc[:, :, c0:c0 + 512], wk)
                v = cgi // 2
                if v < 2:
                    for mi in range(4):
                        mc = (cgi % 2) * 4 + mi
                        for kc in range(8):
                            self.mm(ps[1][:, (v * 8 + mc) * 8:(v * 8 + mc) * 8 + 8], wt[:, kc, mi * 128:(mi + 1) * 128], scT[:, kc, :],
                                    kc == 0, kc == 7, [wk, 'scT'], 'ps1')
                else:
                    half = cgi % 2
                    for (lt, lk, pb) in ((bPl, 'bPl', 2), (bSl, 'bSl', 3)):
                        for kc in range(8):
                            self.mm(ps[pb][:, :], lt[:, kc, :], wt[:, kc, :], kc == 0, False, [wk, lk], PK[pb])
                        self.ld(self.rowtmp[0:1, 0:512], bada_d[layer:layer + 1, c0:c0 + 512], 'rowtmp')
                        self.mm(ps[pb][:, :], self.ones32[0:1, :], self.rowtmp[0:1, 0:512], False, True, ['rowtmp', 'ones32'], PK[pb])
                        dst = gP if pb == 2 else gS
                        self.cp(dst[:, half * 512:(half + 1) * 512], ps[pb][:, :], [PK[pb]], 'gP' if pb == 2 else 'gS', eng='act')
            bb = badaT[:, layer * 48 + part * 24: layer * 48 + part * 24 + 16]
            self.tt(modT[:].rearrange("p v c r -> p (v c) r"), ps[1][:, 0:128].rearrange("p (a r) -> p a r", r=8),
                    bb.unsqueeze(2).broadcast_to([128, 16, 8]), ALU.add, ['ps1', 'badaT'], 'modT')
            self.ts(modT[:, 1, :, :], modT[:, 1, :, :], 1.0, None, ALU.add, None, ['modT'], 'modT')
            self.bcast_row(lnG, lng_d[hl:hl + 1, :], D, 'lnG', 'lnG')
            self.bcast_row(lnB, lnb_d[hl:hl + 1, :], D, 'lnB', 'lnB')
            S.barrier()

        def tile_to_hT(src, skey, hdst, col0, sample=False, xT32=None):
            for hf in range(2):
                pb = 4 + hf
                for q in range(4):
                    kc = hf * 4 + q
                    self.tr(ps[pb][:, q * 128:(q + 1) * 128], src[:, kc * 128:(kc + 1) * 128], ident, [skey, 'consts'], PK[pb])
                for q in range(4):
                    kc = hf * 4 + q
                    if not sample:
                        self.act(hdst[:, kc, col0:col0 + 128], ps[pb][:, q * 128:(q + 1) * 128], AF.Identity,
                                 [PK[pb], 'modT'], 'hTg', bias=modT[:, 0, kc, 0:1], scale=modT[:, 1, kc, 0:1])
                        if xT32 is not None:
                            self.cp(xT32[:, kc, :], ps[pb][:, q * 128:(q + 1) * 128], [PK[pb]], 'xT32', eng='act')
                    else:
                        tmp = self.smod
                        self.tt(tmp[:, 0:SPC], ps[pb][:, q * 128:q * 128 + SPC], modT[:, 1, kc, 1:1 + SPC], ALU.mult, [PK[pb], 'modT'], 'smod')
                        self.tt(tmp[:, 0:SPC], tmp[:, 0:SPC], modT[:, 0, kc, 1:1 + SPC], ALU.add, ['smod', 'modT'], 'smod')
                        self.cp(hdst[:, kc, col0:col0 + 128], tmp[:], ['smod'], 'hTg')
                        if xT32 is not None:
                            self.cp(xT32[:, kc, :], tmp[:], ['smod'], 'xT32')

        self.smod = sb("smod", [128, 128])
        self.memset(self.smod[:], 0.0, 'smod')

        rt_ = sb("rot_tmp", [128, 4, 8, 16])

        def rotary(xv, nh, cs, xkey, rkey='rope'):
            cosb = cs[:, 0:16].unsqueeze(1).broadcast_to([128, nh, 16])
            sinb = cs[:, 16:32].unsqueeze(1).broadcast_to([128, nh, 16])
            x1 = xv[:, :, 0:16]
            x2 = xv[:, :, 16:32]
            self.tt(rt_[:, 0, 0:nh, :], x1, cosb, ALU.mult, [xkey, rkey], 'rt0')
            self.tt(rt_[:, 1, 0:nh, :], x2, sinb, ALU.mult, [xkey, rkey], 'rt1')
            self.tt(rt_[:, 2, 0:nh, :], x2, cosb, ALU.mult, [xkey, rkey], 'rt2')
            self.tt(rt_[:, 3, 0:nh, :], x1, sinb, ALU.mult, [xkey, rkey], 'rt3')
            self.tt(x1, rt_[:, 0, 0:nh, :], rt_[:, 1, 0:nh, :], ALU.subtract, ['rt0', 'rt1'], xkey)
            self.tt(x2, rt_[:, 2, 0:nh, :], rt_[:, 3, 0:nh, :], ALU.add, ['rt2', 'rt3'], xkey)

        ada(0)
        off = 0
        KT, off = carve(off, [2, NGT * 128], BF16)
        VS, off = carve(off, [NGT, 2, 130], BF16)
        kmT, off = carve(off, [2, NSLOT], BF16)
        off = (off + 3) // 4 * 4
        offC = off
        wkv, off = carve(off, [8, 512], BF16)
        ropes, off = carve(off, [NGT + 1, 32], F32)
        xg0, off = carve(off, [1, D], F32)
        xg1, off = carve(off, [1, D], F32)
        hT1, off = carve(off, [8, 128], BF16)
        kvf, off = carve(off, [1, 512], F32)
        kb16, off = carve(off, [1, 256], BF16)
        assert off <= SCR_BYTES, off
        xgt = [xg0[:, 0, :], xg1[:, 0, :]]
        kvf = kvf[:, 0, :]
        kb16 = kb16[:, 0, :]
        self.hT1 = hT1

        self.ld(ropes[:].rearrange("p t c -> p (t c)"), rope_d, 'rope')
        self.S.dma('pool', wkv[:], wqkv_d.rearrange("(kc p) n -> p kc n", p=128)[:, :, 1024:1536], writes=['wkv'])
        self.memset(VS[:, :, :, 128:130], 1.0, 'VS')

        def kv_tile(src, skey, rope_idx, g):
            tile_to_hT(src, skey, hT1, 0, sample=(g is None))
            for kc in range(8):
                self.mm(ps[0][:, :], hT1[:, kc, 0:128], wkv[:, kc, :], kc == 0, kc == 7, ['hTg', 'wkv'], 'ps0')
            dstf = kvf if g is not None else kvS
            dkey = 'kvf' if g is not None else 'kvS'
            self.cp(dstf[:], ps[0][:, :], ['ps0'], dkey)
            rotary(dstf[:, 0:256].rearrange("p (h d) -> p h d", h=2), 2, ropes[:, rope_idx, :], dkey)
            if g is not None:
                self.cp(VS[:, g, :, 0:128], kvf[:, 256:512].rearrange("p (h d) -> p h d", h=2), ['kvf'], 'VS', eng='act')
                self.cp(kb16[:], kvf[:, 0:256], ['kvf'], 'kb16', eng='act')
                pt = ps[1][:].bitcast(BF16)
                for kv in range(2):
                    self.tr(pt[:, kv * 128:(kv + 1) * 128], kb16[:, kv * 128:(kv + 1) * 128], identb[:], ['kb16', 'identb'], 'ps1')
                self.cp(KT[:, :, g * 128:(g + 1) * 128], pt[:, 0:256].rearrange("p (h t) -> p h t", h=2), ['ps1'], 'KT')
                for kv in range(2):
                    self.mm(ps[7][:, kv * NGT + g: kv * NGT + g + 1], kvf[:, kv * 128:(kv + 1) * 128], self.ones32[:, 0:1],
                            True, True, ['kvf', 'ones32'], 'ps7')
                if 2 <= g < 2 + 2 * LS:
                    pg = g - 2
                    self.st(k_p[pg].rearrange("h r d -> r h d"), kvf[:, 0:256].rearrange("p (h d) -> p h d", h=2), 'kvf', 'k_p')
                    self.st(v_p[pg].rearrange("h r d -> r h d"), kvf[:, 256:512].rearrange("p (h d) -> p h d", h=2), 'kvf', 'v_p')
            else:
                self.st(k_s[:, :], kvS[0:SPC, 0:256], 'kvS', 'k_s')
                self.st(v_s[:, :], kvS[0:SPC, 256:512], 'kvS', 'v_s')

        for g in range(NGT):
            src, skey = xgt[g % 2], 'xgt%d' % (g % 2)
            self.ld(src, xg[g * 128:(g + 1) * 128, :], skey)
            kv_tile(src, skey, g, g)
        tS = NT - 1
        self.ld(xres[:, tS, :], xs_d, XK[tS])
        kv_tile(xres[:, tS, :], XK[tS], NGT, None)

        self.cp(ks32[:], ps[7][:, 0:2 * NGT], ['ps7'], 'ks32')
        ksv = ks32[:].rearrange("p (k s two) -> p k s two", k=2, two=2)
        self.tt(ksv[:, :, :, 0], ksv[:, :, :, 0], ksv[:, :, :, 1], ALU.add, ['ks32'], 'ks32')
        self.ts(kmT[:], ksv[:, :, :, 0], 1.0 / 256.0, None, ALU.mult, None, ['ks32'], 'kmT')
        if cfg.stop == 'proj':
            self.finish(dbg, xres, XK)
            return

        S.barrier()
        off = offC
        wqb = []
        for i in range(2):
            a_, off = carve(off, [8, 512], BF16)
            wqb.append(a_)
        ropel, off = carve(off, [NT, 32], F32)
        v01, off = carve(off, [NPT, NSLOT], F32)
        QT1, off = carve(off, [8, 128], BF16)
        hT1, off = carve(off, [8, 128], BF16)
        acc, off = carve(off, [8, 130], F32)
        PT0, off = carve(off, [4, 128], BF16)
        PT1, off = carve(off, [4, 128], BF16)
        PT = [PT0, PT1]
        aT, off = carve(off, [8, 128], BF16)
        qb16, off = carve(off, [8, 128], BF16)
        zq, off = carve(off, [8, 128], F32)
        gsb, off = carve(off, [8, NSLOT], F32)
        sel, off = carve(off, [8, NSLOT], F32)
        t8, off = carve(off, [8, 8], F32)
        nm, off = carve(off, [1, NSLOT], F32)
        rden, off = carve(off, [1, 8], F32)
        assert off <= SCR_BYTES, off
        zq2 = zq[:].rearrange("p h d -> p (h d)")
        qb2 = qb16[:].rearrange("p h d -> p (h d)")

        self.ld(ropel[:, 0:NPT, :].rearrange("p t c -> p (t c)"), rope_d[:, 32:(NPT + 1) * 32], 'ropel')
        self.ld(ropel[:, NPT, :], rope_d[:, NGT * 32:(NGT + 1) * 32], 'ropel')
        self.ld(v01[:].rearrange("p a b -> p (a b)"), v01_d, 'v01')
        wq_src = wqkv_d.rearrange("(kc p) n -> p kc n", p=128)
        wo_src = wo_d.rearrange("(kc p) n -> p kc n", p=128)

        def q_proj(tau, sample):
            tile_to_hT(xres[:, tau, :], XK[tau], hT1, 0, sample=sample)
            for hf in range(2):
                self.S.dma('pool', wqb[hf][:], wq_src[:, :, hf * 512:(hf + 1) * 512], writes=['wqb%d' % hf])
                for kc in range(8):
                    self.mm(ps[2 + hf][:, :], hT1[:, kc, :], wqb[hf][:, kc, :], kc == 0, kc == 7, ['hTg', 'wqb%d' % hf], PK[2 + hf])
                self.cp(zq2[:, hf * 512:(hf + 1) * 512], ps[2 + hf][:, :], [PK[2 + hf]], 'zq', eng='act')
            rotary(zq[:], 8, ropel[:, tau, :], 'zq', 'ropel')

        def out_proj(tau, gate_tile, gkey):
            pt = ps[6][:].bitcast(BF16)
            for h in range(8):
                self.tr(pt[:, h * 128:(h + 1) * 128], qb16[:, h, :], identb[:], ['qb16', 'identb'], 'ps6')
            self.cp(aT[:].rearrange("p h t -> p (h t)"), pt[:, :], ['ps6'], 'aT')
            for hf in range(2):
                self.S.dma('pool', wqb[hf][:], wo_src[:, :, hf * 512:(hf + 1) * 512], writes=['wqb%d' % hf])
                for h in range(8):
                    self.mm(ps[6 + hf][:, :], aT[:, h, :], wqb[hf][:, h, :], h == 0, h == 7, ['aT', 'wqb%d' % hf], PK[6 + hf])
                self.tt(zq2[:, hf * 512:(hf + 1) * 512], ps[6 + hf][:, :], gate_tile[:, hf * 512:(hf + 1) * 512], ALU.mult, [PK[6 + hf], gkey], 'zq')
            self.stt(zq2, xres[:, tau, :], cfg.ALPHA, zq2, ALU.mult, ALU.add, [XK[tau], 'zq'], 'zq')
            self.layernorm(zq2, xres[:, tau, :], lnG[:], lnB[:], 'zq', XK[tau], 'lnG')

        for tau in range(NPT):
            self.ld(xres[:, tau, :], xg[(tau + 1) * 128:(tau + 2) * 128, :], XK[tau])
            q_proj(tau, False)
            self.cp(qb2, zq2, ['zq'], 'qb16', eng='act')
            pt = ps[6][:].bitcast(BF16)
            for h in range(8):
                self.tr(pt[:, h * 128:(h + 1) * 128], qb16[:, h, :], identb[:], ['qb16', 'identb'], 'ps6')
            self.cp(QT1[:].rearrange("p h t -> p (h t)"), pt[:, :], ['ps6'], 'QT1')
            for h in range(8):
                self.mm(ps[4][:, h * NSLOT:(h + 1) * NSLOT], QT1[:, h, :], kmT[:, h // 4, :], True, True, ['QT1', 'kmT'], 'ps4')
            self.ts(nm[:, 0, :], v01[:, tau, :], 1.0, BIG, ALU.subtract, ALU.mult, ['v01'], 'nm')
            v01b = v01[:, tau, :].unsqueeze(1).broadcast_to([128, 8, NSLOT])
            self.tt(gsb[:], ps[4][:, 0:8 * NSLOT].rearrange("p (h s) -> p h s", h=8), v01b, ALU.mult, ['ps4', 'v01'], 'gsb')
            self.tt(gsb[:], gsb[:], nm[:, 0, :].unsqueeze(1).broadcast_to([128, 8, NSLOT]), ALU.add, ['gsb', 'nm'], 'gsb')
            for h in range(8):
                self.S.op('dve', lambda e: e.max(out=t8[:, h, :], in_=gsb[:, h, :]), reads=['gsb'], writes=['t8'])
            self.tt(sel[:], gsb[:], t8[:, :, 2:3].broadcast_to([128, 8, NSLOT]), ALU.is_ge, ['gsb', 't8'], 'sel')
            self.tt(sel[:], sel[:], v01b, ALU.mult, ['sel', 'v01'], 'sel')
            for h in range(8):
                self.memset(acc[:, h, :], 0.0, 'acc%d' % h)
            m_own = (tau + 1) // 2
            second = (tau + 1) % 2
            it = 0
            for kv in range(2):
                qrhs = QT1[:, kv * 4:(kv + 1) * 4, :].rearrange("p h q -> p (h q)")
                for s_ in range(NSLOT):
                    own = (s_ == m_own)
                    kts = [0, 1]
                    if own and not second:
                        kts = [0]
                    for kt in kts:
                        g = 2 * s_ + kt
                        self.mm(ps[kt][:, :], KT[:, kv, g * 128:(g + 1) * 128], qrhs, True, True, ['KT', 'QT1'], PK[kt])
                        self.act(PT[kt][:].rearrange("p h q -> p (h q)"), ps[kt][:, :], AF.Exp, [PK[kt]], 'PT%d' % kt, scale=ATTN_SCALE)
                        if own and kt == (1 if second else 0):
                            self.tt(PT[kt][:], PT[kt][:], trib[:].unsqueeze(1).broadcast_to([128, 4, 128]), ALU.mult, ['PT%d' % kt, 'trib'], 'PT%d' % kt)
                    ob = 2 + 2 * (it % 2)
                    for hh in range(4):
                        bank = ps[ob + hh // 2]
                        col = (hh % 2) * 256
                        for i, kt in enumerate(kts):
                            self.mm(bank[:, col:col + 129], PT[kt][:, hh, :], VS[:, 2 * s_ + kt, kv, 0:129], i == 0, i == len(kts) - 1,
                                    ['PT%d' % kt, 'VS'], PK[ob + hh // 2])
                    for hh in range(4):
                        h = kv * 4 + hh
                        bank = ps[ob + hh // 2]
                        col = (hh % 2) * 256
                        if own:
                            self.tt(acc[:, h, 0:129], acc[:, h, 0:129], bank[:, col:col + 129], ALU.add, ['acc%d' % h, PK[ob + hh // 2]], 'acc%d' % h)
                        else:
                            self.stt(acc[:, h, 0:129], bank[:, col:col + 129], sel[:, h, s_:s_ + 1], acc[:, h, 0:129], ALU.mult, ALU.add,
                                     ['acc%d' % h, 'sel', PK[ob + hh // 2]], 'acc%d' % h)
                    it += 1
            AK = ['acc%d' % h for h in range(8)]
            self.S.op('dve', lambda e: e.reciprocal(out=rden[:, 0, :], in_=acc[:, :, 128]), reads=AK, writes=['rden'])
            self.tt(qb16[:], acc[:, :, 0:128], rden[:, 0, :].unsqueeze(2).broadcast_to([128, 8, 128]), ALU.mult, AK + ['rden'], 'qb16')
            out_proj(tau, gP, 'gP')

        tS = NT - 1
        q_proj(tS, True)
        for h in range(8):
            self.tr(ps[0][:, h * 4:(h + 1) * 4], zq[0:SPC, h, :], ident[0:SPC, 0:SPC], ['zq', 'consts'], 'ps0')
        S.barrier()
        off = 0
        c2, off = carve(off, [1, 512], F32)
        QTa, off = carve(off, [1, 32], F32)
        QTm, off = carve(off, [8, 32], F32)
        qsel, off = carve(off, [1, 128], F32)
        ptAi, off = carve(off, [1, 4], I32)
        ptBi, off = carve(off, [1, 2 * NBLK], I32)
        ptAf, off = carve(off, [1, 4], F32)
        idxAf, off = carve(off, [4, 8], F32)
        idxA, off = carve(off, [4, 8], I32)
        idxSf, off = carve(off, [6, 4], F32)
        idxS, off = carve(off, [6, 4], I32)
        ptBf, off = carve(off, [1, 2 * NBLK], F32)
        ksum0, off = carve(off, [2, 128], F32)
        ksum1, off = carve(off, [2, 128], F32)
        ksr, off = carve(off, [1, 128], F32)
        kmTs, off = carve(off, [4, 128], F32)
        gate_s, off = carve(off, [1, NBLK], F32)
        t8s, off = carve(off, [1, 8], F32)
        oh, off = carve(off, [1, NBLK], F32)
        ohp, off = carve(off, [1, NBLK], F32)
        phys, off = carve(off, [1, 8], F32)
        idxf, off = carve(off, [1, 8], F32)
        idxi, off = carve(off, [1, 8], I32)
        ksel, off = carve(off, [1, 128], F32)
        vsel, off = carve(off, [1, 128], F32)
        sc, off = carve(off, [1, 6 * 128 + 8], F32)
        Pm, off = carve(off, [1, 6 * 128 + 8], F32)
        den, off = carve(off, [1, 1], F32)
        pv, off = carve(off, [24, 128], F32)
        osum, off = carve(off, [1, 128], F32)
        qb16, off = carve(off, [8, 128], BF16)
        aT, off = carve(off, [8, 128], BF16)
        zq, off = carve(off, [8, 128], F32)
        wqb = []
        for i in range(2):
            a_, off = carve(off, [8, 512], BF16)
            wqb.append(a_)
        G0, off = carve(off, [1, 32 * 128], F32)
        G1, off = carve(off, [1, 32 * 128], F32)
        assert off <= SCR_BYTES, off
        GB = [G0[:, 0, :], G1[:, 0, :]]
        zq2 = zq[:].rearrange("p h d -> p (h d)")
        c2 = c2[:, 0, :]
        ksum = [ksum0, ksum1]
        self.cp(QTa[:, 0, :], ps[0][:, 0:32], ['ps0'], 'QTa')
        self.ld(c2, consts2_d, 'c2')
        self.ld(ptAi[:, 0, :], ptA_d, 'ptAi')
        self.ld(ptBi[0:32, 0, :], ptB_d, 'ptBi')
        self.cp(ptBf[0:32, 0, :], ptBi[0:32, 0, :], ['ptBi'], 'ptBf')
        self.cp(ptAf[:, 0, :], ptAi[:, 0, :], ['ptAi'], 'ptAf')
        self.ts(idxAf[:], ptAf[:, 0, :].unsqueeze(2).broadcast_to([128, 4, 8]), 8.0, None, ALU.mult, None, ['ptAf'], 'idxAf')
        self.tt(idxAf[:], idxAf[:], c2[:, 353:361].unsqueeze(1).broadcast_to([128, 4, 8]), ALU.add, ['idxAf', 'c2'], 'idxAf')
        self.cp(idxA[:], idxAf[:], ['idxAf'], 'idxA')
        ck8 = ck_d.rearrange("n (c x) -> (n c) x", x=4096)
        cv8 = cv_d.rearrange("n (c x) -> (n c) x", x=4096)
        self.tt(QTm[:], QTa[:, 0, :].unsqueeze(1).broadcast_to([128, 8, 32]), c2[:, 0:256].rearrange("p (a b) -> p a b", a=8), ALU.mult, ['QTa', 'c2'], 'QTm')
        self.tr(ps[1][0:32, 0:128], QTa[:, 0, :], ident, ['QTa', 'consts'], 'ps1')
        self.cp(qsel[0:32, 0, :], ps[1][0:32, 0:128], ['ps1'], 'qsel')
        RC = 32
        gi = 0
        for r in range(2):
            for kv in range(2):
                first = True
                for eo in range(2):
                    for rc in range(128 // RC):
                        gb = GB[gi % 2][:, 0:RC * 128]
                        gk = 'G%d' % (gi % 2)
                        self.S.dma('pool', None, None, reads=['idxA'], writes=[gk],
                                   fn=lambda e: e.indirect_dma_start(out=gb, out_offset=None, in_=ck8,
                                                                     in_offset=bass.IndirectOffsetOnAxis(ap=idxA[:, r * 2 + eo, kv * 4 + rc:kv * 4 + rc + 1], axis=0)))
                        dst = ksum[r][:, kv, :] if first else ksr[:, 0, :]
                        dk = 'ksum%d' % r if first else 'ksr'
                        self.S.op('dve', lambda e: e.tensor_reduce(out=dst, in_=gb.rearrange("p (r d) -> p d r", d=128), axis=AX.X, op=ALU.add),
                                  reads=[gk], writes=[dk])
                        if not first:
                            self.tt(ksum[r][:, kv, :], ksum[r][:, kv, :], ksr[:, 0, :], ALU.add, ['ksum%d' % r, 'ksr'], 'ksum%d' % r)
                        first = False
                        gi += 1
        for r in range(2):
            for kv in range(2):
                self.tr(ps[2][:, (kv * 2 + r) * 128:(kv * 2 + r + 1) * 128], ksum[r][:, kv, :], ident, ['ksum%d' % r, 'consts'], 'ps2')
        self.ts(kmTs[:].rearrange("p a b -> p (a b)"), ps[2][:, :], 1.0 / 256.0, None, ALU.mult, None, ['ps2'], 'kmTs')
        n = 0
        for s_ in range(SPC):
            for kv in range(2):
                self.mm(ps[3][0:32, 0:NBLK], QTm[:, s_ * 2 + kv, :], kmTs[:, kv * 2 + s_ // 2, (s_ % 2) * NBLK:(s_ % 2 + 1) * NBLK],
                        n == 0, n == 2 * SPC - 1, ['QTm', 'kmTs'], 'ps3')
                n += 1
        self.cp(gate_s[0:32, 0, :], ps[3][0:32, 0:NBLK], ['ps3'], 'gate_s')
        self.S.op('dve', lambda e: e.max(out=t8s[0:32, 0, :], in_=gate_s[0:32, 0, :]), reads=['gate_s'], writes=['t8s'])
        for j in range(3):
            self.ts(oh[0:32, 0, :], gate_s[0:32, 0, :], t8s[0:32, 0, j:j + 1], None, ALU.is_equal, None, ['gate_s', 't8s'], 'oh')
            for eo in range(2):
                self.tt(ohp[0:32, 0, :], oh[0:32, 0, :], ptBf[0:32, 0, eo * NBLK:(eo + 1) * NBLK], ALU.mult, ['oh', 'ptBf'], 'ohp')
                self.S.op('dve', lambda e: e.tensor_reduce(out=phys[0:32, 0, j * 2 + eo:j * 2 + eo + 1], in_=ohp[0:32, 0, :], axis=AX.X, op=ALU.add),
                          reads=['ohp'], writes=['phys'])
        self.ts(idxf[0:32, 0, 0:6], phys[0:32, 0, 0:6], 2.0, c2[0:32, 256:257], ALU.mult, ALU.add, ['phys', 'c2'], 'idxf')
        self.ts(idxSf[0:32], idxf[0:32, 0, 0:6].unsqueeze(2).broadcast_to([32, 6, 4]), 4.0, None, ALU.mult, None, ['idxf'], 'idxSf')
        self.tt(idxSf[0:32], idxSf[0:32], c2[0:32, 353:357].unsqueeze(1).broadcast_to([32, 6, 4]), ALU.add, ['idxSf', 'c2'], 'idxSf')
        self.cp(idxS[0:32], idxSf[0:32], ['idxSf'], 'idxi')
        for (dst, dk, c0) in ((ksel, 'ksel', 0), (vsel, 'vsel', 256)):
            self.mm(ps[4][0:32, 0:128], c2[0:SPC, 257:289], kvS[0:SPC, c0:c0 + 128], True, False, ['c2', 'kvS'], 'ps4')
            self.mm(ps[4][0:32, 0:128], c2[0:SPC, 289:321], kvS[0:SPC, c0 + 128:c0 + 256], False, True, ['c2', 'kvS'], 'ps4')
            self.cp(dst[0:32, 0, :], ps[4][0:32, 0:128], ['ps4'], dk)
        ck2 = ck_d.rearrange("n (h x) -> (n h) x", h=2)
        cv2 = cv_d.rearrange("n (h x) -> (n h) x", h=2)
        qb_ = qsel[0:32, 0, :].unsqueeze(1).broadcast_to([32, 32, 128])
        for c in range(6):
            for rh in range(4):
                gb = GB[gi % 2][0:32, :]
                gk = 'G%d' % (gi % 2)
                self.S.dma('pool', None, None, reads=['idxi'], writes=[gk],
                           fn=lambda e: e.indirect_dma_start(out=gb, out_offset=None, in_=ck8,
                                                             in_offset=bass.IndirectOffsetOnAxis(ap=idxS[0:32, c, rh:rh + 1], axis=0)))
                g3 = gb.rearrange("p (r d) -> p r d", d=128)
                self.tt(g3, g3, qb_, ALU.mult, [gk, 'qsel'], gk)
                self.S.op('dve', lambda e: e.tensor_reduce(out=sc[0:32, 0, c * 128 + rh * 32:c * 128 + rh * 32 + 32], in_=g3, axis=AX.X, op=ALU.add),
                          reads=[gk], writes=['sc'])
                gi += 1
        self.tt(osum[0:32, 0, :], qsel[0:32, 0, :], ksel[0:32, 0, :], ALU.mult, ['qsel', 'ksel'], 'osum')
        self.S.op('dve', lambda e: e.tensor_reduce(out=sc[0:32, 0, 768:769], in_=osum[0:32, 0, :], axis=AX.X, op=ALU.add), reads=['osum'], writes=['sc'])
        self.S.op('act', lambda e: e.activation(out=Pm[0:32, 0, 0:769], in_=sc[0:32, 0, 0:769], func=AF.Exp, scale=ATTN_SCALE),
                  reads=['sc'], writes=['Pm'])
        self.S.op('dve', lambda e: e.tensor_reduce(out=den[0:32, 0, :], in_=Pm[0:32, 0, 0:769], axis=AX.X, op=ALU.add), reads=['Pm'], writes=['den'])
        for c in range(6):
            for rh in range(4):
                gb = GB[gi % 2][0:32, :]
                gk = 'G%d' % (gi % 2)
                self.S.dma('pool', None, None, reads=['idxi'], writes=[gk],
                           fn=lambda e: e.indirect_dma_start(out=gb, out_offset=None, in_=cv8,
                                                             in_offset=bass.IndirectOffsetOnAxis(ap=idxS[0:32, c, rh:rh + 1], axis=0)))
                g3 = gb.rearrange("p (r d) -> p r d", d=128)
                pb_ = Pm[0:32, 0, c * 128 + rh * 32:c * 128 + rh * 32 + 32].unsqueeze(2).broadcast_to([32, 32, 128])
                self.tt(g3, g3, pb_, ALU.mult, [gk, 'Pm'], gk)
                self.S.op('dve', lambda e: e.tensor_reduce(out=pv[0:32, c * 4 + rh, :], in_=gb.rearrange("p (r d) -> p d r", d=128), axis=AX.X, op=ALU.add),
                          reads=[gk], writes=['pv'])
                gi += 1
        self.S.op('dve', lambda e: e.tensor_reduce(out=osum[0:32, 0, :], in_=pv[0:32, :, :].rearrange("p i d -> p d i"), axis=AX.X, op=ALU.add),
                  reads=['pv'], writes=['osum'])
        self.stt(osum[0:32, 0, :], vsel[0:32, 0, :], Pm[0:32, 0, 768:769], osum[0:32, 0, :], ALU.mult, ALU.add, ['vsel', 'Pm', 'osum'], 'osum')
        self.S.op('dve', lambda e: e.reciprocal(out=den[0:32, 0, :], in_=den[0:32, 0, :]), reads=['den'], writes=['den'])
        self.ts(osum[0:32, 0, :], osum[0:32, 0, :], den[0:32, 0, 0:1], None, ALU.mult, None, ['osum', 'den'], 'osum')
        for h in range(8):
            self.mm(ps[2 + h // 4][0:SPC, (h % 4) * 128:(h % 4 + 1) * 128], c2[0:32, 321 + h * 4:321 + h * 4 + 4], osum[0:32, 0, :], True, True,
                    ['c2', 'osum'], PK[2 + h // 4])
        self.memset(qb16[:].rearrange("p h d -> p (h d)"), 0.0, 'qb16')
        for hf in range(2):
            self.cp(qb16[0:SPC, hf * 4:(hf + 1) * 4, :].rearrange("p h d -> p (h d)"), ps[2 + hf][0:SPC, :], [PK[2 + hf], 'qb16'], 'qb16')
        out_proj(tS, gS, 'gS')

        if cfg.stop == 'attn':
            self.finish(dbg, xres, XK)
            return

        TG = NT // cfg.MG

        def moe(layer):
            ada(2 * layer + 1)
            if cfg.stop == 'ada1':
                return
            off = 0
            wr32, off = carve(off, [8, NE], F32)
            wrm, off = carve(off, [8, NE], F32)
            rb, off = carve(off, [1, NE], F32)
            brB, off = carve(off, [1, NE], F32)
            Wt, off = carve(off, [NT, NE], F32)
            hTm, off = carve(off, [8, TG * 128], BF16)
            xT32, off = carve(off, [8, 128], F32)
            accm, off = carve(off, [TG, D], F32)
            wg2, wu2, wd2, AT, sg, Ab = [], [], [], [], [], []
            for i in range(2):
                a_, off = carve(off, [8, 512], BF16); wg2.append(a_)
                a_, off = carve(off, [8, 512], BF16); wu2.append(a_)
                a_, off = carve(off, [4, D], BF16); wd2.append(a_)
                a_, off = carve(off, [4, 128], BF16); AT.append(a_)
                a_, off = carve(off, [1, 512], F32); sg.append(a_)
                a_, off = carve(off, [1, 512], BF16); Ab.append(a_)
            sc_, off = carve(off, [1, NE], F32)
            bi, off = carve(off, [NG, 8], F32)
            msk, off = carve(off, [NG, 8], F32)
            t8g, off = carve(off, [NG, 8], F32)
            gs, off = carve(off, [1, NG], F32)
            gmax, off = carve(off, [1, 1], F32)
            ohg, off = carve(off, [1, NG], F32)
            t8e, off = carve(off, [1, 8], F32)
            sel2, off = carve(off, [1, NE], F32)
            wun, off = carve(off, [1, NE], F32)
            dsum, off = carve(off, [1, 1], F32)
            zq, off = carve(off, [1, D], F32)
            assert off <= SCR_BYTES, off
            zq2 = zq[:, 0, :]
            bi2 = bi[:].rearrange("p g e -> p (g e)")
            msk2 = msk[:].rearrange("p g e -> p (g e)")

            self.ld(wr32[:], wr_d.rearrange("(kc p) e -> p kc e", p=128), 'wr32')
            for kc in range(8):
                self.ts(wrm[:, kc, :], wr32[:, kc, :], modT[:, 1, kc, 0:1], None, ALU.mult, None, ['wr32', 'modT'], 'wrm')
                self.mm(ps[0][0:1, 0:NE], modT[:, 0, kc, 0:1], wr32[:, kc, :], kc == 0, kc == 7, ['modT', 'wr32'], 'ps0')
            self.cp(rb[0:1, 0, :], ps[0][0:1, 0:NE], ['ps0'], 'rb')
            self.bcast_row(brB[:, 0, :], br_d, NE, 'brB', 'brB')
            if cfg.stop == 'rsetup':
                return

            for G in range(cfg.MG):
                tiles = list(range(G * TG, (G + 1) * TG))
                for ti, tau in enumerate(tiles):
                    smp = (tau == NT - 1)
                    tile_to_hT(xres[:, tau, :], XK[tau], hTm, ti * 128, sample=smp, xT32=xT32)
                    if cfg.stop == 'r_0':
                        return
                    wsel, wk = (wr32, 'wr32') if smp else (wrm, 'wrm')
                    for kc in range(8):
                        self.mm(ps[6][:, 0:NE], xT32[:, kc, :], wsel[:, kc, :], kc == 0, smp and kc == 7, ['xT32', wk], 'ps6')
                    if not smp:
                        self.mm(ps[6][:, 0:NE], self.ones32[0:1, :], rb[0:1, 0, :], False, True, ['ones32', 'rb'], 'ps6')
                    if cfg.stop == 'r_a':
                        return
                    self.act(sc_[:, 0, :], ps[6][:, 0:NE], AF.Sigmoid, ['ps6'], 'sc_')
                    self.tt(bi2, sc_[:, 0, :], brB[:, 0, :], ALU.add, ['sc_', 'brB'], 'bi')
                    for g in range(NG):
                        self.S.op('dve', lambda e: e.max(out=t8g[:, g, :], in_=bi[:, g, :]), reads=['bi'], writes=['t8g'])
                    if cfg.stop == 'r_b':
                        return
                    self.tt(gs[:, 0, :], t8g[:, :, 0], t8g[:, :, 1], ALU.add, ['t8g'], 'gs')
                    self.tt(gmax[:, 0, :], gs[:, 0, 0:1], gs[:, 0, 1:2], ALU.max, ['gs'], 'gmax')
                    for g in range(2, NG):
                        self.tt(gmax[:, 0, :], gmax[:, 0, :], gs[:, 0, g:g + 1], ALU.max, ['gs', 'gmax'], 'gmax')
                    self.ts(ohg[:, 0, :], gs[:, 0, :], gmax[:, 0, 0:1], None, ALU.is_equal, None, ['gs', 'gmax'], 'ohg')
                    self.ts(ohg[:, 0, :], ohg[:, 0, :], BIG, -BIG, ALU.mult, ALU.add, ['ohg'], 'ohg')
                    self.tt(msk[:], bi[:], ohg[:, 0, :].unsqueeze(2).broadcast_to([128, NG, 8]), ALU.add, ['bi', 'ohg'], 'msk')
                    self.S.op('dve', lambda e: e.max(out=t8e[:, 0, :], in_=msk2), reads=['msk'], writes=['t8e'])
                    self.ts(sel2[:, 0, :], msk2, t8e[:, 0, 1:2], None, ALU.is_ge, None, ['msk', 't8e'], 'sel2')
                    self.tt(wun[:, 0, :], sc_[:, 0, :], sel2[:, 0, :], ALU.mult, ['sc_', 'sel2'], 'wun')
                    self.S.op('dve', lambda e: e.tensor_reduce(out=dsum[:, 0, :], in_=wun[:, 0, :], axis=AX.X, op=ALU.add), reads=['wun'], writes=['dsum'])
                    self.S.op('dve', lambda e: e.reciprocal(out=dsum[:, 0, :], in_=dsum[:, 0, :]), reads=['dsum'], writes=['dsum'])
                    self.ts(Wt[:, tau, :], wun[:, 0, :], dsum[:, 0, 0:1], None, ALU.mult, None, ['wun', 'dsum'], 'Wt')
                if cfg.stop == 'route':
                    return
                items = [(e_, ti) for e_ in range(NE) for ti in range(TG)]

                def stage_a(n):
                    e_, ti = items[n]
                    sl = e_ % 2
                    ab = n % 2
                    if ti == 0:
                        self.S.dma('pool', wg2[sl][:], wg_d[layer, e_].rearrange("(kc p) n -> p kc n", p=128), writes=['wg%d' % sl])
                        self.S.dma('pool', wu2[sl][:], wu_d[layer, e_].rearrange("(kc p) n -> p kc n", p=128), writes=['wu%d' % sl])
                        self.S.dma('pool', wd2[sl][:], wd_d[layer, e_].rearrange("(kc p) n -> p kc n", p=128), writes=['wd%d' % sl])
                    for kc in range(8):
                        self.mm(ps[ab][:, :], hTm[:, kc, ti * 128:(ti + 1) * 128], wg2[sl][:, kc, :], kc == 0, kc == 7, ['hTg', 'wg%d' % sl], PK[ab])
                    for kc in range(8):
                        self.mm(ps[2 + ab][:, :], hTm[:, kc, ti * 128:(ti + 1) * 128], wu2[sl][:, kc, :], kc == 0, kc == 7, ['hTg', 'wu%d' % sl], PK[2 + ab])
                    self.act(sg[ab][:, 0, :], ps[ab][:, :], AF.Silu, [PK[ab]], 'sg%d' % ab)
                    self.tt(Ab[ab][:, 0, :], sg[ab][:, 0, :], ps[2 + ab][:, :], ALU.mult, ['sg%d' % ab, PK[2 + ab]], 'Ab%d' % ab)

                def stage_b(n):
                    ab = n % 2
                    pt = ps[4 + ab][:].bitcast(BF16)
                    for fc in range(4):
                        self.tr(pt[:, fc * 128:(fc + 1) * 128], Ab[ab][:, 0, fc * 128:(fc + 1) * 128], identb[:], ['Ab%d' % ab, 'identb'], PK[4 + ab])
                    self.cp(AT[ab][:].rearrange("p f t -> p (f t)"), pt[:, 0:512], [PK[4 + ab]], 'AT%d' % ab, eng='act')

                def stage_c(n):
                    e_, ti = items[n]
                    sl = e_ % 2
                    ab = n % 2
                    tau = tiles[ti]
                    for hf in range(2):
                        pb = 6 + hf
                        for fc in range(4):
                            self.mm(ps[pb][:, :], AT[ab][:, fc, :], wd2[sl][:, fc, hf * 512:(hf + 1) * 512], fc == 0, fc == 3, ['AT%d' % ab, 'wd%d' % sl], PK[pb])
                        dst = accm[:, ti, hf * 512:(hf + 1) * 512]
                        if e_ == 0:
                            self.ts(dst, ps[pb][:, :], Wt[:, tau, e_:e_ + 1], None, ALU.mult, None, [PK[pb], 'Wt'], 'accm%d' % ti)
                        else:
                            self.stt(dst, ps[pb][:, :], Wt[:, tau, e_:e_ + 1], dst, ALU.mult, ALU.add, [PK[pb], 'Wt', 'accm%d' % ti], 'accm%d' % ti)

                nit = len(items)
                for step in range(nit + 2):
                    if step < nit:
                        stage_a(step)
                    if 1 <= step <= nit:
                        stage_b(step - 1)
                    if step >= 2:
                        stage_c(step - 2)
                for ti, tau in enumerate(tiles):
                    smp = (tau == NT - 1)
                    gt, gk = (gS, 'gS') if smp else (gP, 'gP')
                    self.tt(zq2, accm[:, ti, :], gt[:], ALU.mult, ['accm%d' % ti, gk], 'zq')
                    self.stt(zq2, xres[:, tau, :], cfg.ALPHA, zq2, ALU.mult, ALU.add, [XK[tau], 'zq'], 'zq')
                    self.layernorm(zq2, xres[:, tau, :], lnG[:], lnB[:], 'zq', XK[tau], 'lnG')

        moe(0)
        if cfg.stop in ('moe0', 'route', 'ada1', 'rsetup', 'r_a', 'r_b', 'r_0'):
            self.finish(dbg, xres, XK)
            return

        ada(2)
        off = 0
        uT, off = carve(off, [8, 30 + 512], F32)
        hTc, off = carve(off, [8, 512], BF16)
        winc = []
        for i in range(2):
            a_, off = carve(off, [8, 256], BF16); winc.append(a_)
        yg, off = carve(off, [8, 512], F32)
        ytok, off = carve(off, [1, D], F32)
        zb, off = carve(off, [8, 128], BF16)
        zT, off = carve(off, [8, 128], BF16)
        wout, off = carve(off, [8, D], BF16)
        wdw, off = carve(off, [8, CW], F32)
        cg_, off = carve(off, [1, D], F32)
        cb_, off = carve(off, [1, D], F32)
        sgm, off = carve(off, [1, 512], F32)
        zq, off = carve(off, [1, D], F32)
        uS, off = carve(off, [8, SPC], F32)
        stT, off = carve(off, [8 * SPC, CW], F32)
        prodS, off = carve(off, [8 * SPC, CW], F32)
        yS, off = carve(off, [8, SPC], F32)
        utok, off = carve(off, [1, D], F32)
        assert off <= SCR_BYTES, off
        zq2 = zq[:, 0, :]
        ytok2 = ytok[:, 0, :]
        utok2 = utok[:, 0, :]
        self.memset(uT[:].rearrange("p a b -> p (a b)"), 0.0, 'uT')
        self.ld(wdw[:].rearrange("p a b -> p (a b)"), wdwT_d, 'wdw')
        self.S.dma('pool', wout[:], wout_d.rearrange("(kc p) n -> p kc n", p=128), writes=['wout'])
        self.bcast_row(cg_[:, 0, :], clng_d, D, 'cgb', 'cg')
        self.bcast_row(cb_[:, 0, :], clnb_d, D, 'cgb', 'cb')
        win_src = win_d.rearrange("(kc p) n -> p kc n", p=128)
        wi = [0]

        def glu_cols(ncol, dst_fn):
            for cc in range(8):
                wb = winc[wi[0] % 2]
                wk = 'winc%d' % (wi[0] % 2)
                wi[0] += 1
                self.S.dma('pool', wb[:, :, 0:128], win_src[:, :, cc * 128:(cc + 1) * 128], writes=[wk])
                self.S.dma('pool', wb[:, :, 128:256], win_src[:, :, D + cc * 128:D + (cc + 1) * 128], writes=[wk])
                for kc in range(8):
                    self.mm(ps[0][:, 0:ncol], wb[:, kc, 0:128], hTc[:, kc, 0:ncol], kc == 0, kc == 7, [wk, 'hTg'], 'ps0')
                for kc in range(8):
                    self.mm(ps[1][:, 0:ncol], wb[:, kc, 128:256], hTc[:, kc, 0:ncol], kc == 0, kc == 7, [wk, 'hTg'], 'ps1')
                self.act(sgm[:, 0, 0:ncol], ps[1][:, 0:ncol], AF.Sigmoid, ['ps1'], 'sgm')
                dst, dk = dst_fn(cc)
                self.tt(dst, ps[0][:, 0:ncol], sgm[:, 0, 0:ncol], ALU.mult, ['ps0', 'sgm'], dk)

        def conv_tail(tau, col0, gate_tile, gkey):
            for hf in range(2):
                for q in range(4):
                    cc = hf * 4 + q
                    self.tr(ps[2 + hf][:, q * 128:(q + 1) * 128], yg[:, cc, col0:col0 + 128], ident, ['yg', 'consts'], PK[2 + hf])
                self.cp(ytok2[:, hf * 512:(hf + 1) * 512], ps[2 + hf][:, :], [PK[2 + hf]], 'ytok', eng='act')
            self.layernorm(ytok2, ytok2, cg_[:, 0, :], cb_[:, 0, :], 'ytok', 'ytok', 'cgb')
            self.act(zb[:].rearrange("p a b -> p (a b)"), ytok2, AF.Silu, ['ytok'], 'zb')
            pt = ps[4][:].bitcast(BF16)
            for cc in range(8):
                self.tr(pt[:, cc * 128:(cc + 1) * 128], zb[:, cc, :], identb[:], ['zb', 'identb'], 'ps4')
            self.cp(zT[:].rearrange("p a b -> p (a b)"), pt[:, :], ['ps4'], 'zT')
            for hf in range(2):
                for cc in range(8):
                    self.mm(ps[6 + hf][:, :], zT[:, cc, :], wout[:, cc, hf * 512:(hf + 1) * 512], cc == 0, cc == 7, ['zT', 'wout'], PK[6 + hf])
                self.tt(zq2[:, hf * 512:(hf + 1) * 512], ps[6 + hf][:, :], gate_tile[:, hf * 512:(hf + 1) * 512], ALU.mult, [PK[6 + hf], gkey], 'zq')
            self.stt(zq2, xres[:, tau, :], cfg.ALPHA, zq2, ALU.mult, ALU.add, [XK[tau], 'zq'], 'zq')
            self.layernorm(zq2, xres[:, tau, :], lnG[:], lnB[:], 'zq', XK[tau], 'lnG')

        for g0 in range(0, NPT, 4):
            tiles = list(range(g0, min(g0 + 4, NPT)))
            ncol = len(tiles) * 128
            for ti, tau in enumerate(tiles):
                tile_to_hT(xres[:, tau, :], XK[tau], hTc, ti * 128)
            glu_cols(ncol, lambda cc: (uT[:, cc, 30:30 + ncol], 'uT'))
            if g0 == 0:
                self.ts(uT[:, :, 30:158], uT[:, :, 30:158], flag[:, 0:1], None, ALU.mult, None, ['uT', 'flag'], 'uT')
            for cc in range(8):
                for k in range(CW):
                    if k == 0:
                        self.ts(yg[:, cc, 0:ncol], uT[:, cc, 0:ncol], wdw[:, cc, 0:1], None, ALU.mult, None, ['uT', 'wdw'], 'yg')
                    else:
                        self.stt(yg[:, cc, 0:ncol], uT[:, cc, k:k + ncol], wdw[:, cc, k:k + 1], yg[:, cc, 0:ncol], ALU.mult, ALU.add, ['uT', 'wdw', 'yg'], 'yg')
            if tiles[-1] == NPT - 1:
                cl = 30 + (len(tiles) - 1) * 128 + 98
                for hf in range(2):
                    for q in range(4):
                        cc = hf * 4 + q
                        self.tr(ps[2 + hf][0:30, q * 128:(q + 1) * 128], uT[:, cc, cl:cl + 30], ident, ['uT', 'consts'], PK[2 + hf])
                    self.cp(utok2[0:30, hf * 512:(hf + 1) * 512], ps[2 + hf][0:30, :], [PK[2 + hf]], 'utok', eng='act')
                self.st(conv_p[:, :], utok2[0:30, :], 'utok', 'conv_p')
            else:
                self.cp(uT[:, :, 0:30], uT[:, :, ncol:ncol + 30], ['uT'], 'uT')
            if cfg.stop == 'c_conv':
                self.finish(dbg, xres, XK)
                return
            for ti, tau in enumerate(tiles):
                if tau >= 1:
                    conv_tail(tau, ti * 128, gP, 'gP')
            if cfg.stop == 'c_tail':
                self.finish(dbg, xres, XK)
                return
        tile_to_hT(xres[:, tS, :], XK[tS], hTc, 0, sample=True)
        glu_cols(SPC, lambda cc: (uS[:, cc, :], 'uS'))
        st4 = stT[:].rearrange("p (c s) k -> p c s k", c=8)
        pflat = prodS[:].rearrange("p a k -> p (a k)")[:, 0:8 * SPC * 30]
        self.ld(pflat, stfm_d, 'prodS')
        self.cp(st4[:, :, :, 0:30], pflat.rearrange("p (c s k) -> p c s k", c=8, s=SPC), ['prodS'], 'stT')
        self.cp(st4[:, :, :, 30], uS[:], ['uS', 'stT'], 'stT')
        self.tt(prodS[:].rearrange("p (c s) k -> p c s k", c=8), st4, wdw[:].unsqueeze(2).broadcast_to([128, 8, SPC, CW]), ALU.mult, ['stT', 'wdw'], 'prodS')
        self.S.op('dve', lambda e: e.tensor_reduce(out=yS[:].rearrange("p c s -> p (c s)"), in_=prodS[:], axis=AX.X, op=ALU.add), reads=['prodS'], writes=['yS'])
        self.memset(yg[:, :, 0:128], 0.0, 'yg')
        self.cp(yg[:, :, 0:SPC], yS[:], ['yS', 'yg'], 'yg')
        conv_tail(tS, 0, gS, 'gS')
        for s_ in range(SPC):
            self.ld(ytok2[s_ * 29:(s_ + 1) * 29, :], sttok_d[s_, 1:30, :], 'ytok')
        for s_ in range(SPC):
            self.st(conv_s[s_, 0:29, :], ytok2[s_ * 29:(s_ + 1) * 29, :], 'ytok', 'conv_s')
        for hf in range(2):
            for q in range(4):
                cc = hf * 4 + q
                self.tr(ps[2 + hf][0:SPC, q * 128:(q + 1) * 128], uS[:, cc, :], ident, ['uS', 'consts'], PK[2 + hf])
            self.cp(utok2[0:SPC, hf * 512:(hf + 1) * 512], ps[2 + hf][0:SPC, :], [PK[2 + hf]], 'utok', eng='act')
        self.st(conv_s[:, 29, :], utok2[0:SPC, :], 'utok', 'conv_s')
        if cfg.stop == 'conv':
            self.finish(dbg, xres, XK)
            return

        moe(1)
        for tau in range(1, NPT):
            self.st(y_p[(tau - 1) * 128:tau * 128, :], xres[:, tau, :], XK[tau], 'y_p')
        self.st(y_s[:, :], xres[0:SPC, tS, :], XK[tS], 'y_s')
        self.finish(dbg, xres, XK)

    def finish(self, dbg, xres, XK):
        cfg = self.cfg
        if dbg is not None:
            for t in range(cfg.NT):
                self.st(dbg[t * 128:(t + 1) * 128, :], xres[:, t, :], XK[t], 'dbg')
        self.S.wait_keys('sp', list(set(self.outkeys)))
        self.es.close()


def prep_inputs(cfg, I):
    f32 = np.float32
    NC, NCB, LS, NSLOT, NPT, NGT, SPC, NBLK = cfg.NC, cfg.NCB, cfg.LS, cfg.NSLOT, cfg.NPT, cfg.NGT, cfg.SPC, cfg.NBLK
    half = 8
    inv_freq = (f32(ROPE_THETA) ** (-np.arange(half, dtype=f32) / f32(half))).astype(f32) if False else None
    half = 16
    inv_freq = np.power(f32(ROPE_THETA), -(np.arange(half, dtype=f32) / f32(half))).astype(f32)
    ident = np.eye(128, dtype=f32)
    tri = (np.arange(128)[:, None] <= np.arange(128)[None, :]).astype(f32)
    consts = np.concatenate([ident, tri], axis=1)
    c2 = np.zeros((128, 512), f32)
    cm = np.zeros((8, 32), f32)
    for s_ in range(4):
        for kv in range(2):
            for h in range(kv * 4, kv * 4 + 4):
                cm[s_ * 2 + kv, h * 4 + s_] = 1.0
    c2[:, 0:256] = cm.reshape(1, 256)
    c2[0:32, 256] = (np.arange(32) // 4) // 4
    for s_ in range(4):
        for h in range(8):
            c2[s_, (257 if h < 4 else 289) + h * 4 + s_] = 1.0
    c2[0:32, 321:353] = np.eye(32, dtype=f32)
    c2[:, 353:361] = np.arange(8, dtype=f32)[None, :]
    shared = {
        "consts": consts, "consts2": c2,
        "w_ada": np.ascontiguousarray(I["w_ada"]), "b_ada": np.ascontiguousarray(I["b_ada"]),
        "b_adaT": np.ascontiguousarray(I["b_ada"].reshape(2, 48, 128).transpose(2, 0, 1).reshape(128, 96)),
        "ln_g": np.ascontiguousarray(I["ln_g"].reshape(4, D)), "ln_b": np.ascontiguousarray(I["ln_b"].reshape(4, D)),
        "w_qkv": np.ascontiguousarray(I["w_qkv"][0]), "w_o": np.ascontiguousarray(I["w_o"][0]),
        "conv_w_in": np.ascontiguousarray(I["conv_w_in"][0]),
        "wdwT": np.ascontiguousarray(I["conv_w_dw"][0].T.reshape(8, 128, CW).transpose(1, 0, 2).reshape(128, 8 * CW)),
        "conv_ln_g": np.ascontiguousarray(I["conv_ln_g"].reshape(1, D)), "conv_ln_b": np.ascontiguousarray(I["conv_ln_b"].reshape(1, D)),
        "conv_w_out": np.ascontiguousarray(I["conv_w_out"][0]),
        "w_router": np.ascontiguousarray(I["w_router"]), "b_router": np.ascontiguousarray(I["b_router"].reshape(1, -1)),
        "w_gate": np.ascontiguousarray(I["w_gate"]), "w_up": np.ascontiguousarray(I["w_up"]), "w_down": np.ascontiguousarray(I["w_down"]),
        "cache_k": np.ascontiguousarray(I["cache_k"].reshape(cfg.NPHYS, -1)),
        "cache_v": np.ascontiguousarray(I["cache_v"].reshape(cfg.NPHYS, -1)),
    }
    maps = []
    pt = np.asarray(I["page_table"]).astype(np.int32)
    for c in range(NC):
        b, j = c // NCB, c % NCB
        rot = LS * j - 1
        m = dict(shared)
        m["xg"] = np.ascontiguousarray(np.roll(I["x_prompt"][b], -rot * 256, axis=0))
        gslot = (rot + np.arange(NSLOT)) % NSLOT
        pos = (gslot[:, None] * 256 + np.arange(256)[None, :]).reshape(-1)
        pos = np.concatenate([pos, np.full(128, cfg.PAST_LEN)]).astype(f32)
        ang = pos[:, None] * inv_freq[None, :]
        tab = np.concatenate([np.cos(ang), np.sin(ang)], axis=1).astype(f32)
        m["rope"] = np.ascontiguousarray(tab.reshape(NGT + 1, 128, 32).transpose(1, 0, 2).reshape(128, (NGT + 1) * 32))
        sid = np.arange(c * SPC, (c + 1) * SPC)
        xs = np.zeros((128, D), f32)
        xs[:SPC] = I["x_sample"][sid, 0]
        m["xs"] = xs
        cv = np.zeros((8, D), f32)
        cv[0] = I["c_prompt"][b]
        cv[1:1 + SPC] = I["c_sample"][sid]
        m["cvec"] = cv
        valid = np.zeros((NPT, NSLOT), bool)
        for tau in range(NPT):
            own = rot + (tau + 1) // 2
            valid[tau] = gslot < own
        m["nmask"] = np.ascontiguousarray(np.broadcast_to(np.where(valid, 0.0, -BIG).astype(f32).reshape(1, -1), (128, NPT * NSLOT)))
        m["v01"] = np.ascontiguousarray(np.broadcast_to(valid.astype(f32).reshape(1, -1), (128, NPT * NSLOT)))
        m["flag"] = np.full((128, 1), 1.0 if j > 0 else 0.0, f32)
        ptA = np.zeros((128, 4), np.int32)
        for r in range(2):
            for eo in range(2):
                for s2 in range(2):
                    ptA[s2 * NBLK:(s2 + 1) * NBLK, r * 2 + eo] = pt[sid[2 * r + s2], eo::2]
        m["ptA"] = ptA
        ptB = np.zeros((32, 2 * NBLK), np.int32)
        for h in range(8):
            for s in range(SPC):
                ptB[h * 4 + s, :NBLK] = pt[sid[s], 0::2]
                ptB[h * 4 + s, NBLK:] = pt[sid[s], 1::2]
        m["ptB"] = ptB
        st = I["state_conv"][0][sid]
        m["st_tok"] = np.ascontiguousarray(st)
        m["st_fm"] = np.ascontiguousarray(st.reshape(SPC, 30, 8, 128).transpose(3, 2, 0, 1).reshape(128, 8 * SPC * 30))
        maps.append(m)
    return maps


_PROG_CACHE = {}


def run_cfg(cfg, inputs):
    key = (cfg.B, cfg.SEQ, cfg.NCB, cfg.DEC_BATCH, cfg.PAST_LEN, cfg.NE, cfg.NG, cfg.MG, cfg.stop)
    if key not in _PROG_CACHE:
        p = Prog(cfg)
        p.build()
        _PROG_CACHE[key] = p
    p = _PROG_CACHE[key]
    maps = prep_inputs(cfg, inputs)
    maps = [{k: v for k, v in m.items() if k in p.din} for m in maps]
    res = run_bass_kernel_spmd(p.nc, maps, core_ids=list(range(cfg.NC)))
    return res.results


def assemble(cfg, R):
    f32 = np.float32
    B, NCB, LS, SPC = cfg.B, cfg.NCB, cfg.LS, cfg.SPC
    y_p = np.zeros((B, cfg.SEQ, D), f32)
    k_p = np.zeros((B, cfg.SEQ // 128, 1, 2, 128, 128), f32)
    v_p = np.zeros_like(k_p)
    conv_p = np.zeros((1, B, 30, D), f32)
    y_s = np.zeros((cfg.DEC_BATCH, 1, D), f32)
    k_s = np.zeros((cfg.DEC_BATCH, 1, 2, 1, 128), f32)
    v_s = np.zeros_like(k_s)
    conv_s = np.zeros((1, cfg.DEC_BATCH, 30, D), f32)
    for c, r in enumerate(R):
        b, j = c // NCB, c % NCB
        t0 = j * LS * 256
        y_p[b, t0:t0 + LS * 256] = r["y_p"]
        k_p[b, j * 2 * LS:(j + 1) * 2 * LS, 0] = r["k_p"]
        v_p[b, j * 2 * LS:(j + 1) * 2 * LS, 0] = r["v_p"]
        if j == NCB - 1:
            conv_p[0, b] = r["conv_p"]
        sl = slice(c * SPC, (c + 1) * SPC)
        y_s[sl, 0] = r["y_s"]
        k_s[sl, 0, :, 0, :] = r["k_s"].reshape(SPC, 2, 128)
        v_s[sl, 0, :, 0, :] = r["v_s"].reshape(SPC, 2, 128)
        conv_s[0, sl] = r["conv_s"]
    return (y_p, y_s, k_p, v_p, conv_p, k_s, v_s, conv_s)


def kernel(**inputs):
    cfg = Cfg()
    inputs = {k: np.asarray(v) for k, v in inputs.items()}
    R = run_cfg(cfg, inputs)
    return assemble(cfg, R)
```

```python
import numpy as np
from contextlib import ExitStack
import concourse.bass as bass
import concourse.mybir as mybir
from concourse.bass_utils import run_bass_kernel_spmd

F32 = mybir.dt.float32
BF16 = mybir.dt.bfloat16
I32 = mybir.dt.int32
U32 = mybir.dt.uint32
AF = mybir.ActivationFunctionType
ALU = mybir.AluOpType
AX = mybir.AxisListType

EPOCH = 24000
D = 1024
H = 8
KVH = 2
HD = 128
ROPE_THETA = 500000.0
ATTN_SCALE = HD ** -0.5
CW = 31
DFF = 512
LN_EPS = 1e-5
BIG = 1.0e30


class Cfg:
    def __init__(self, B=2, SEQ=8192, NCB=4, DEC_BATCH=32, PAST_LEN=16384, NE=32, NG=4, DEPTH=2, MG=3,
                 stop=None):
        self.B, self.SEQ, self.NCB, self.DEC_BATCH, self.PAST_LEN = B, SEQ, NCB, DEC_BATCH, PAST_LEN
        self.NE, self.NG, self.EPG = NE, NG, NE // NG
        self.NC = B * NCB
        self.NSLOT = SEQ // 256
        self.LS = self.NSLOT // NCB
        self.NPT = 2 * self.LS + 1
        self.NT = self.NPT + 1
        self.NGT = 2 * self.NSLOT
        self.SPC = DEC_BATCH // self.NC
        self.NPG = PAST_LEN // 128
        self.NBLK = self.NPG // 2
        self.NPHYS = (DEC_BATCH * self.NPG * 5) // 4
        self.DEPTH = DEPTH
        self.MG = MG
        self.ALPHA = (2 * DEPTH) ** 0.25
        self.stop = stop
        assert self.SPC == 4 and self.EPG == 8 and self.NT % MG == 0


class Sync:
    def __init__(self, nc, es, same_engine_wait=True):
        self.nc = nc
        self.es = es
        self.engs = {'pe': nc.tensor, 'act': nc.scalar, 'dve': nc.vector,
                     'pool': nc.gpsimd, 'sp': nc.sync}
        self.sems = {}
        self.cnt = {k: 0 for k in self.engs}
        self.waited = {k: {} for k in self.engs}
        self.res = {}
        self.dsem = {}
        self.same = same_engine_wait
        self.n_ins = 0

    def _sem(self, key):
        if key not in self.sems:
            nm = "s%d" % len(self.sems)
            self.sems[key] = self.es.enter_context(self.nc.semaphore(nm))
        return self.sems[key]

    def _wait(self, eng, deps):
        best = {}
        for (sk, v) in deps:
            if best.get(sk, 0) < v:
                best[sk] = v
        for sk, v in best.items():
            if sk[0] == 'E' and sk[1] == eng:
                if not self.same or eng == 'pe':
                    continue
            if self.waited[eng].get(sk, 0) >= v:
                continue
            self.engs[eng].wait_ge(self._sem(sk), v)
            self.n_ins += 1
            self.waited[eng][sk] = v

    def _deps(self, reads, writes):
        deps = []
        for k in reads:
            r = self.res.get(k)
            if r and r['w']:
                deps.append(r['w'])
        for k in writes:
            r = self.res.get(k)
            if r:
                if r['w']:
                    deps.append(r['w'])
                deps += r['r']
        return deps

    def _record(self, ev, reads, writes):
        for k in reads:
            self.res.setdefault(k, {'w': None, 'r': []})['r'].append(ev)
        for k in writes:
            self.res[k] = {'w': ev, 'r': []}

    def op(self, eng, fn, reads=(), writes=()):
        self._wait(eng, self._deps(reads, writes))
        ins = fn(self.engs[eng])
        self.cnt[eng] += 1
        ep, v = divmod(self.cnt[eng] - 1, EPOCH)
        sk = ('E', eng, ep)
        ins.then_inc(self._sem(sk), 1)
        self.n_ins += 1
        self._record((sk, v + 1), reads, writes)
        return ins

    def dma(self, eng, out, in_, reads=(), writes=(), key=None, fn=None):
        self._wait(eng, self._deps(reads, writes))
        if key is None:
            key = ('D', writes[0] if writes else reads[0])
        if fn is None:
            ins = self.engs[eng].dma_start(out=out, in_=in_)
        else:
            ins = fn(self.engs[eng])
        c = self.dsem.get(key, 0) + 16
        self.dsem[key] = c
        ins.then_inc(self._sem(key), 16)
        self.n_ins += 1
        self._record((key, c), reads, writes)
        return ins

    def wait_keys(self, eng, keys):
        deps = []
        for k in keys:
            r = self.res.get(k)
            if r:
                if r['w']:
                    deps.append(r['w'])
                deps += r['r']
        self._wait(eng, deps)

    def barrier(self):
        deps = []
        for r in self.res.values():
            if r['w']:
                deps.append(r['w'])
            deps += r['r']
        for eng in self.engs:
            self._wait(eng, deps)
        self.res = {}


class Prog:
    def __init__(self, cfg):
        self.cfg = cfg
        self.nc = bass.Bass("TRN2", target_bir_lowering=False)
        self.es = ExitStack()
        self.din = {}
        self.dout = {}

    def inp(self, name, shape, dt=F32):
        self.din[name] = self.nc.dram_tensor(name, list(shape), dt, kind="ExternalInput").ap()
        return self.din[name]

    def outp(self, name, shape, dt=F32):
        self.dout[name] = self.nc.dram_tensor(name, list(shape), dt, kind="ExternalOutput").ap()
        return self.dout[name]

    def sb(self, name, shape, dt=F32):
        return self.es.enter_context(self.nc.sbuf_tensor("sb_" + name, list(shape), dt))

    def mm(self, out, lhsT, rhs, start, stop, reads, w):
        self.S.op('pe', lambda e: e.matmul(out, lhsT=lhsT, rhs=rhs, start=start, stop=stop), reads=reads, writes=[w])

    def tr(self, out, in_, ident, reads, w):
        self.S.op('pe', lambda e: e.transpose(out=out, in_=in_, identity=ident), reads=reads, writes=[w])

    def act(self, out, in_, func, reads, w, bias=None, scale=None, eng='act'):
        kw = {}
        if bias is not None:
            kw['bias'] = bias
        if scale is not None:
            kw['scale'] = scale
        self.S.op('act', lambda e: e.activation(out=out, in_=in_, func=func, **kw), reads=reads, writes=[w])

    def tt(self, out, a, b, op, reads, w, eng='dve'):
        self.S.op(eng, lambda e: e.tensor_tensor(out=out, in0=a, in1=b, op=op), reads=reads, writes=[w])

    def ts(self, out, a, s1, s2, op0, op1, reads, w, eng='dve'):
        if op1 is None:
            self.S.op(eng, lambda e: e.tensor_scalar(out=out, in0=a, scalar1=s1, scalar2=None, op0=op0), reads=reads, writes=[w])
        else:
            self.S.op(eng, lambda e: e.tensor_scalar(out=out, in0=a, scalar1=s1, scalar2=s2, op0=op0, op1=op1), reads=reads, writes=[w])

    def stt(self, out, a, s, b, op0, op1, reads, w):
        self.S.op('dve', lambda e: e.scalar_tensor_tensor(out=out, in0=a, scalar=s, in1=b, op0=op0, op1=op1), reads=reads, writes=[w])

    def cp(self, out, in_, reads, w, eng='dve'):
        if eng == 'act':
            self.S.op('act', lambda e: e.copy(out=out, in_=in_), reads=reads, writes=[w])
        else:
            self.S.op(eng, lambda e: e.tensor_copy(out=out, in_=in_), reads=reads, writes=[w])

    def memset(self, ap, val, w, eng='dve'):
        self.S.op(eng, lambda e: e.memset(ap, val), writes=[w])

    def ld(self, out, in_, w, reads=(), eng='sp'):
        self.S.dma(eng, out, in_, reads=list(reads), writes=[w])

    def st(self, out, in_, r, okey):
        self.S.dma('sp', out, in_, reads=[r], writes=[okey])
        self.outkeys.append(okey)

    def bcast_row(self, dst, row_ap, n, dkey, tmpname):
        rt = self.rowtmp
        for c0 in range(0, n, 512):
            cw = min(512, n - c0)
            self.ld(rt[0:1, 0:cw], row_ap[:, c0:c0 + cw], 'rowtmp')
            self.mm(self.ps[0][:, 0:cw], self.ones32[0:1, :], rt[0:1, 0:cw], True, True, ['rowtmp', 'ones32'], 'ps0')
            self.cp(dst[:, c0:c0 + cw], self.ps[0][:, 0:cw], ['ps0'], dkey, eng='act')

    def layernorm(self, z, out, gB, bB, zkey, okey, gkey):
        st_, mv, rstd = self.ln_st, self.ln_mv, self.ln_rstd
        for i in range(2):
            self.S.op('dve', lambda e: e.bn_stats(out=st_[:, i, :], in_=z[:, i * 512:(i + 1) * 512]), reads=[zkey], writes=['ln_st'])
        self.S.op('dve', lambda e: e.bn_aggr(out=mv[:], in_=st_[:].rearrange("p a b -> p (a b)")), reads=['ln_st'], writes=['ln_mv'])
        self.act(rstd[:], mv[:, 1:2], AF.Ln, ['ln_mv', 'epsT'], 'ln_rstd', bias=self.epsT[:], scale=1.0)
        self.act(rstd[:], rstd[:], AF.Exp, ['ln_rstd'], 'ln_rstd', scale=-0.5)
        self.ts(z, z, mv[:, 0:1], rstd[:, 0:1], ALU.subtract, ALU.mult, [zkey, 'ln_mv', 'ln_rstd'], zkey)
        self.tt(z, z, gB, ALU.mult, [zkey, gkey], zkey)
        self.tt(out, z, bB, ALU.add, [zkey, gkey], okey)

    def build(self):
        cfg = self.cfg
        nc, es = self.nc, self.es
        NT, NPT, NGT, NSLOT, LS, SPC, NE, NG, EPG = cfg.NT, cfg.NPT, cfg.NGT, cfg.NSLOT, cfg.LS, cfg.SPC, cfg.NE, cfg.NG, cfg.EPG
        NBLK, NPG, NPHYS = cfg.NBLK, cfg.NPG, cfg.NPHYS
        self.outkeys = []
        xg = self.inp("xg", [NGT * 128, D])
        rope_d = self.inp("rope", [128, (NGT + 1) * 32])
        xs_d = self.inp("xs", [128, D])
        cvec_d = self.inp("cvec", [8, D])
        nmask_d = self.inp("nmask", [128, NPT * NSLOT])
        v01_d = self.inp("v01", [128, NPT * NSLOT])
        flag_d = self.inp("flag", [128, 1])
        consts_d = self.inp("consts", [128, 256])
        consts2_d = self.inp("consts2", [128, 512])
        ptA_d = self.inp("ptA", [128, 4], I32)
        ptB_d = self.inp("ptB", [32, 2 * NBLK], I32)
        sttok_d = self.inp("st_tok", [SPC, 30, D])
        stfm_d = self.inp("st_fm", [128, 8 * SPC * 30])
        wada_d = self.inp("w_ada", [2, D, 6 * D])
        bada_d = self.inp("b_ada", [2, 6 * D])
        badaT_d = self.inp("b_adaT", [128, 96])
        lng_d = self.inp("ln_g", [4, D])
        lnb_d = self.inp("ln_b", [4, D])
        wqkv_d = self.inp("w_qkv", [D, 1536])
        wo_d = self.inp("w_o", [D, D])
        win_d = self.inp("conv_w_in", [D, 2 * D])
        wdwT_d = self.inp("wdwT", [128, 8 * CW])
        clng_d = self.inp("conv_ln_g", [1, D])
        clnb_d = self.inp("conv_ln_b", [1, D])
        wout_d = self.inp("conv_w_out", [D, D])
        wr_d = self.inp("w_router", [D, NE])
        br_d = self.inp("b_router", [1, NE])
        wg_d = self.inp("w_gate", [2, NE, D, DFF])
        wu_d = self.inp("w_up", [2, NE, D, DFF])
        wd_d = self.inp("w_down", [2, NE, DFF, D])
        ck_d = self.inp("cache_k", [NPHYS, 2 * 128 * 128])
        cv_d = self.inp("cache_v", [NPHYS, 2 * 128 * 128])

        y_p = self.outp("y_p", [2 * LS * 128, D])
        y_s = self.outp("y_s", [SPC, D])
        k_p = self.outp("k_p", [2 * LS, 2, 128, 128])
        v_p = self.outp("v_p", [2 * LS, 2, 128, 128])
        conv_p = self.outp("conv_p", [30, D])
        k_s = self.outp("k_s", [SPC, 256])
        v_s = self.outp("v_s", [SPC, 256])
        conv_s = self.outp("conv_s", [SPC, 30, D])
        dbg = self.outp("dbg", [NT * 128, D]) if cfg.stop else None

        self.S = S = Sync(nc, es)
        sb = self.sb
        self.ps = [es.enter_context(nc.psum_tensor("ps%d" % i, [128, 512], F32)) for i in range(8)]
        ps = self.ps
        PK = ['ps%d' % i for i in range(8)]
        xres = sb("xres", [128, NT, D])
        XK = ['xres%d' % t for t in range(NT)]
        consts = sb("consts", [128, 256])
        ident = consts[:, 0:128]
        identb = sb("identb", [128, 128], BF16)
        trib = sb("trib", [128, 128], BF16)
        self.ones32 = sb("ones32", [128, 128])
        onesb = sb("onesb", [128, 1], BF16)
        self.rowtmp = sb("rowtmp", [1, 512])
        self.ln_st = sb("ln_st", [128, 2, 6])
        self.ln_mv = sb("ln_mv", [128, 2])
        self.ln_rstd = sb("ln_rstd", [128, 1])
        flag = sb("flag", [128, 1])
        scT = sb("scT", [128, 8, 8])
        modT = sb("modT", [128, 2, 8, 8])
        badaT = sb("badaT", [128, 96])
        gP = sb("gP", [128, D])
        gS = sb("gS", [128, D])
        lnG = sb("lnG", [128, D])
        lnB = sb("lnB", [128, D])
        ks32 = sb("ks32", [128, 2 * NGT])
        kvS = sb("kvS", [128, 512])
        SCR_BYTES = 108 * 1024
        scr = sb("scr", [128, SCR_BYTES // 4])

        def carve(off_bytes, shape, dt):
            n = int(np.prod(shape))
            esz = 4 if dt in (F32, I32, U32) else 2
            assert off_bytes % 4 == 0
            a = scr[:, off_bytes // 4: off_bytes // 4 + (n * esz + 3) // 4]
            if dt != F32:
                a = a.bitcast(dt)
            a = a[:, 0:n]
            if len(shape) == 2:
                pat = "p (a b) -> p a b"
                return a.rearrange(pat, a=shape[0]), off_bytes + n * esz
            if len(shape) == 3:
                return a.rearrange("p (a b c) -> p a b c", a=shape[0], b=shape[1]), off_bytes + n * esz
            if len(shape) == 1:
                return a, off_bytes + n * esz
            raise ValueError

        self.ld(consts[:], consts_d, 'consts')
        self.ld(flag[:], flag_d, 'flag')
        self.ld(badaT[:], badaT_d, 'badaT')
        self.cp(identb[:], consts[:, 0:128], ['consts'], 'identb')
        self.cp(trib[:], consts[:, 128:256], ['consts'], 'trib')
        self.memset(self.ones32[:], 1.0, 'ones32')
        self.memset(onesb[:], 1.0, 'onesb')
        self.epsT = sb("epsT", [128, 1])
        self.memset(self.epsT[:], LN_EPS, 'epsT')

        ctmp_, o_ = carve(0, [1, D], F32)
        ctmp2_, o_ = carve(o_, [1, D], F32)
        ctmp = ctmp_[0:8, 0, :]
        ctmp2 = ctmp2_[0:8, 0, :]
        self.ld(ctmp, cvec_d, 'ctmp')
        self.act(ctmp2, ctmp, AF.Silu, ['ctmp'], 'ctmp2')
        for kc in range(8):
            self.tr(ps[0][:, kc * 8:(kc + 1) * 8], ctmp2[:, kc * 128:(kc + 1) * 128], ident[0:8, 0:8], ['ctmp2', 'consts'], 'ps0')
        self.cp(scT[:].rearrange("p a b -> p (a b)"), ps[0][:, 0:64], ['ps0'], 'scT')
        S.barrier()

        def ada(hl):
            layer, part = hl // 2, hl % 2
            base = part * 3 * D
            S.barrier()
            off = 0
            wa = []
            for i in range(2):
                a, off = carve(off, [8, 512], F32)
                wa.append(a)
            bPl, off = carve(off, [8, 128], F32)
            bSl, off = carve(off, [8, 128], F32)
            for kc in range(8):
                self.ts(bPl[:, kc, :], self.ones32[:], scT[:, kc, 0:1], None, ALU.mult, None, ['ones32', 'scT'], 'bPl')
            self.memset(bSl[:].rearrange("p a b -> p (a b)"), 0.0, 'bSl')
            for kc in range(8):
                self.cp(bSl[:, kc, 0:SPC], scT[:, kc, 1:1 + SPC], ['scT', 'bSl'], 'bSl')
            wsrc = wada_d[layer].rearrange("(kc p) n -> p kc n", p=128)
            for cgi in range(6):
                c0 = base + cgi * 512
                wt = wa[cgi % 2]
                wk = 'wa%d' % (cgi % 2)
                self.ld(wt[:], wsrc[:, :, c0:c0 + 512], wk)
                v = cgi // 2
                if v < 2:
                    for mi in range(4):
                        mc = (cgi % 2) * 4 + mi
                        for kc in range(8):
                            self.mm(ps[1][:, (v * 8 + mc) * 8:(v * 8 + mc) * 8 + 8], wt[:, kc, mi * 128:(mi + 1) * 128], scT[:, kc, :],
                                    kc == 0, kc == 7, [wk, 'scT'], 'ps1')
                else:
                    half = cgi % 2
                    for (lt, lk, pb) in ((bPl, 'bPl', 2), (bSl, 'bSl', 3)):
                        for kc in range(8):
                            self.mm(ps[pb][:, :], lt[:, kc, :], wt[:, kc, :], kc == 0, False, [wk, lk], PK[pb])
                        self.ld(self.rowtmp[0:1, 0:512], bada_d[layer:layer + 1, c0:c0 + 512], 'rowtmp')
                        self.mm(ps[pb][:, :], self.ones32[0:1, :], self.rowtmp[0:1, 0:512], False, True, ['rowtmp', 'ones32'], PK[pb])
                        dst = gP if pb == 2 else gS
                        self.cp(dst[:, half * 512:(half + 1) * 512], ps[pb][:, :], [PK[pb]], 'gP' if pb == 2 else 'gS', eng='act')
            bb = badaT[:, layer * 48 + part * 24: layer * 48 + part * 24 + 16]
            self.tt(modT[:].rearrange("p v c r -> p (v c) r"), ps[1][:, 0:128].rearrange("p (a r) -> p a r", r=8),
                    bb.unsqueeze(2).broadcast_to([128, 16, 8]), ALU.add, ['ps1', 'badaT'], 'modT')
            self.ts(modT[:, 1, :, :], modT[:, 1, :, :], 1.0, None, ALU.add, None, ['modT'], 'modT')
            self.bcast_row(lnG, lng_d[hl:hl + 1, :], D, 'lnG', 'lnG')
            self.bcast_row(lnB, lnb_d[hl:hl + 1, :], D, 'lnB', 'lnB')
            S.barrier()

        def tile_to_hT(src, skey, hdst, col0, sample=False, xT32=None):
            for hf in range(2):
                pb = 4 + hf
                for q in range(4):
                    kc = hf * 4 + q
                    self.tr(ps[pb][:, q * 128:(q + 1) * 128], src[:, kc * 128:(kc + 1) * 128], ident, [skey, 'consts'], PK[pb])
                for q in range(4):
                    kc = hf * 4 + q
                    if not sample:
                        self.act(hdst[:, kc, col0:col0 + 128], ps[pb][:, q * 128:(q + 1) * 128], AF.Identity,
                                 [PK[pb], 'modT'], 'hTg', bias=modT[:, 0, kc, 0:1], scale=modT[:, 1, kc, 0:1])
                        if xT32 is not None:
                            self.cp(xT32[:, kc, :], ps[pb][:, q * 128:(q + 1) * 128], [PK[pb]], 'xT32', eng='act')
                    else:
                        tmp = self.smod
                        self.tt(tmp[:, 0:SPC], ps[pb][:, q * 128:q * 128 + SPC], modT[:, 1, kc, 1:1 + SPC], ALU.mult, [PK[pb], 'modT'], 'smod')
                        self.tt(tmp[:, 0:SPC], tmp[:, 0:SPC], modT[:, 0, kc, 1:1 + SPC], ALU.add, ['smod', 'modT'], 'smod')
                        self.cp(hdst[:, kc, col0:col0 + 128], tmp[:], ['smod'], 'hTg')
                        if xT32 is not None:
                            self.cp(xT32[:, kc, :], tmp[:], ['smod'], 'xT32')

        self.smod = sb("smod", [128, 128])
        self.memset(self.smod[:], 0.0, 'smod')

        rt_ = sb("rot_tmp", [128, 4, 8, 16])

        def rotary(xv, nh, cs, xkey, rkey='rope'):
            cosb = cs[:, 0:16].unsqueeze(1).broadcast_to([128, nh, 16])
            sinb = cs[:, 16:32].unsqueeze(1).broadcast_to([128, nh, 16])
            x1 = xv[:, :, 0:16]
            x2 = xv[:, :, 16:32]
            self.tt(rt_[:, 0, 0:nh, :], x1, cosb, ALU.mult, [xkey, rkey], 'rt0')
            self.tt(rt_[:, 1, 0:nh, :], x2, sinb, ALU.mult, [xkey, rkey], 'rt1')
            self.tt(rt_[:, 2, 0:nh, :], x2, cosb, ALU.mult, [xkey, rkey], 'rt2')
            self.tt(rt_[:, 3, 0:nh, :], x1, sinb, ALU.mult, [xkey, rkey], 'rt3')
            self.tt(x1, rt_[:, 0, 0:nh, :], rt_[:, 1, 0:nh, :], ALU.subtract, ['rt0', 'rt1'], xkey)
            self.tt(x2, rt_[:, 2, 0:nh, :], rt_[:, 3, 0:nh, :], ALU.add, ['rt2', 'rt3'], xkey)

        ada(0)
        off = 0
        KT, off = carve(off, [2, NGT * 128], BF16)
        VS, off = carve(off, [NGT, 2, 130], BF16)
        kmT, off = carve(off, [2, NSLOT], BF16)
        off = (off + 3) // 4 * 4
        offC = off
        wkv, off = carve(off, [8, 512], BF16)
        ropes, off = carve(off, [NGT + 1, 32], F32)
        xg0, off = carve(off, [1, D], F32)
        xg1, off = carve(off, [1, D], F32)
        hT1, off = carve(off, [8, 128], BF16)
        kvf, off = carve(off, [1, 512], F32)
        kb16, off = carve(off, [1, 256], BF16)
        assert off <= SCR_BYTES, off
        xgt = [xg0[:, 0, :], xg1[:, 0, :]]
        kvf = kvf[:, 0, :]
        kb16 = kb16[:, 0, :]
        self.hT1 = hT1

        self.ld(ropes[:].rearrange("p t c -> p (t c)"), rope_d, 'rope')
        self.S.dma('pool', wkv[:], wqkv_d.rearrange("(kc p) n -> p kc n", p=128)[:, :, 1024:1536], writes=['wkv'])
        self.memset(VS[:, :, :, 128:130], 1.0, 'VS')

        def kv_tile(src, skey, rope_idx, g):
            tile_to_hT(src, skey, hT1, 0, sample=(g is None))
            for kc in range(8):
                self.mm(ps[0][:, :], hT1[:, kc, 0:128], wkv[:, kc, :], kc == 0, kc == 7, ['hTg', 'wkv'], 'ps0')
            dstf = kvf if g is not None else kvS
            dkey = 'kvf' if g is not None else 'kvS'
            self.cp(dstf[:], ps[0][:, :], ['ps0'], dkey)
            rotary(dstf[:, 0:256].rearrange("p (h d) -> p h d", h=2), 2, ropes[:, rope_idx, :], dkey)
            if g is not None:
                self.cp(VS[:, g, :, 0:128], kvf[:, 256:512].rearrange("p (h d) -> p h d", h=2), ['kvf'], 'VS', eng='act')
                self.cp(kb16[:], kvf[:, 0:256], ['kvf'], 'kb16', eng='act')
                pt = ps[1][:].bitcast(BF16)
                for kv in range(2):
                    self.tr(pt[:, kv * 128:(kv + 1) * 128], kb16[:, kv * 128:(kv + 1) * 128], identb[:], ['kb16', 'identb'], 'ps1')
                self.cp(KT[:, :, g * 128:(g + 1) * 128], pt[:, 0:256].rearrange("p (h t) -> p h t", h=2), ['ps1'], 'KT')
                for kv in range(2):
                    self.mm(ps[7][:, kv * NGT + g: kv * NGT + g + 1], kvf[:, kv * 128:(kv + 1) * 128], self.ones32[:, 0:1],
                            True, True, ['kvf', 'ones32'], 'ps7')
                if 2 <= g < 2 + 2 * LS:
                    pg = g - 2
                    self.st(k_p[pg].rearrange("h r d -> r h d"), kvf[:, 0:256].rearrange("p (h d) -> p h d", h=2), 'kvf', 'k_p')
                    self.st(v_p[pg].rearrange("h r d -> r h d"), kvf[:, 256:512].rearrange("p (h d) -> p h d", h=2), 'kvf', 'v_p')
            else:
                self.st(k_s[:, :], kvS[0:SPC, 0:256], 'kvS', 'k_s')
                self.st(v_s[:, :], kvS[0:SPC, 256:512], 'kvS', 'v_s')

        for g in range(NGT):
            src, skey = xgt[g % 2], 'xgt%d' % (g % 2)
            self.ld(src, xg[g * 128:(g + 1) * 128, :], skey)
            kv_tile(src, skey, g, g)
        tS = NT - 1
        self.ld(xres[:, tS, :], xs_d, XK[tS])
        kv_tile(xres[:, tS, :], XK[tS], NGT, None)

        self.cp(ks32[:], ps[7][:, 0:2 * NGT], ['ps7'], 'ks32')
        ksv = ks32[:].rearrange("p (k s two) -> p k s two", k=2, two=2)
        self.tt(ksv[:, :, :, 0], ksv[:, :, :, 0], ksv[:, :, :, 1], ALU.add, ['ks32'], 'ks32')
        self.ts(kmT[:], ksv[:, :, :, 0], 1.0 / 256.0, None, ALU.mult, None, ['ks32'], 'kmT')
        if cfg.stop == 'proj':
            self.finish(dbg, xres, XK)
            return

        S.barrier()
        off = offC
        wqb = []
        for i in range(2):
            a_, off = carve(off, [8, 512], BF16)
            wqb.append(a_)
        ropel, off = carve(off, [NT, 32], F32)
        v01, off = carve(off, [NPT, NSLOT], F32)
        QT1, off = carve(off, [8, 128], BF16)
        hT1, off = carve(off, [8, 128], BF16)
        acc, off = carve(off, [8, 130], F32)
        PT = []
        for i in range(4):
            a_, off = carve(off, [4, 128], BF16)
            PT.append(a_)
        aT = hT1
        qb16, off = carve(off, [8, 128], BF16)
        zq, off = carve(off, [8, 128], F32)
        gsb, off = carve(off, [8, NSLOT], F32)
        sel, off = carve(off, [8, NSLOT], F32)
        t8, off = carve(off, [8, 8], F32)
        nm, off = carve(off, [1, NSLOT], F32)
        rden, off = carve(off, [1, 8], F32)
        assert off <= SCR_BYTES, off
        zq2 = zq[:].rearrange("p h d -> p (h d)")
        qb2 = qb16[:].rearrange("p h d -> p (h d)")

        self.ld(ropel[:, 0:NPT, :].rearrange("p t c -> p (t c)"), rope_d[:, 32:(NPT + 1) * 32], 'ropel')
        self.ld(ropel[:, NPT, :], rope_d[:, NGT * 32:(NGT + 1) * 32], 'ropel')
        self.ld(v01[:].rearrange("p a b -> p (a b)"), v01_d, 'v01')
        wq_src = wqkv_d.rearrange("(kc p) n -> p kc n", p=128)
        wo_src = wo_d.rearrange("(kc p) n -> p kc n", p=128)

        def q_proj(tau, sample):
            tile_to_hT(xres[:, tau, :], XK[tau], hT1, 0, sample=sample)
            for hf in range(2):
                self.S.dma('pool', wqb[hf][:], wq_src[:, :, hf * 512:(hf + 1) * 512], writes=['wqb%d' % hf])
                for kc in range(8):
                    self.mm(ps[2 + hf][:, :], hT1[:, kc, :], wqb[hf][:, kc, :], kc == 0, kc == 7, ['hTg', 'wqb%d' % hf], PK[2 + hf])
                self.cp(zq2[:, hf * 512:(hf + 1) * 512], ps[2 + hf][:, :], [PK[2 + hf]], 'zq', eng='act')
            rotary(zq[:], 8, ropel[:, tau, :], 'zq', 'ropel')

        def out_proj(tau, gate_tile, gkey):
            pt = ps[6][:].bitcast(BF16)
            for h in range(8):
                self.tr(pt[:, h * 128:(h + 1) * 128], qb16[:, h, :], identb[:], ['qb16', 'identb'], 'ps6')
            self.cp(aT[:].rearrange("p h t -> p (h t)"), pt[:, :], ['ps6'], 'hTg')
            for hf in range(2):
                self.S.dma('pool', wqb[hf][:], wo_src[:, :, hf * 512:(hf + 1) * 512], writes=['wqb%d' % hf])
                for h in range(8):
                    self.mm(ps[6 + hf][:, :], aT[:, h, :], wqb[hf][:, h, :], h == 0, h == 7, ['hTg', 'wqb%d' % hf], PK[6 + hf])
                self.tt(zq2[:, hf * 512:(hf + 1) * 512], ps[6 + hf][:, :], gate_tile[:, hf * 512:(hf + 1) * 512], ALU.mult, [PK[6 + hf], gkey], 'zq')
            self.stt(zq2, xres[:, tau, :], cfg.ALPHA, zq2, ALU.mult, ALU.add, [XK[tau], 'zq'], 'zq')
            self.layernorm(zq2, xres[:, tau, :], lnG[:], lnB[:], 'zq', XK[tau], 'lnG')

        for tau in range(NPT):
            self.ld(xres[:, tau, :], xg[(tau + 1) * 128:(tau + 2) * 128, :], XK[tau])
            q_proj(tau, False)
            self.cp(qb2, zq2, ['zq'], 'qb16', eng='act')
            pt = ps[6][:].bitcast(BF16)
            for h in range(8):
                self.tr(pt[:, h * 128:(h + 1) * 128], qb16[:, h, :], identb[:], ['qb16', 'identb'], 'ps6')
            self.cp(QT1[:].rearrange("p h t -> p (h t)"), pt[:, :], ['ps6'], 'QT1')
            for h in range(8):
                self.mm(ps[4][:, h * NSLOT:(h + 1) * NSLOT], QT1[:, h, :], kmT[:, h // 4, :], True, True, ['QT1', 'kmT'], 'ps4')
            self.ts(nm[:, 0, :], v01[:, tau, :], 1.0, BIG, ALU.subtract, ALU.mult, ['v01'], 'nm')
            v01b = v01[:, tau, :].unsqueeze(1).broadcast_to([128, 8, NSLOT])
            self.tt(gsb[:], ps[4][:, 0:8 * NSLOT].rearrange("p (h s) -> p h s", h=8), v01b, ALU.mult, ['ps4', 'v01'], 'gsb')
            self.tt(gsb[:], gsb[:], nm[:, 0, :].unsqueeze(1).broadcast_to([128, 8, NSLOT]), ALU.add, ['gsb', 'nm'], 'gsb')
            for h in range(8):
                self.S.op('dve', lambda e: e.max(out=t8[:, h, :], in_=gsb[:, h, :]), reads=['gsb'], writes=['t8'])
            self.tt(sel[:], gsb[:], t8[:, :, 2:3].broadcast_to([128, 8, NSLOT]), ALU.is_ge, ['gsb', 't8'], 'sel')
            self.tt(sel[:], sel[:], v01b, ALU.mult, ['sel', 'v01'], 'sel')
            for h in range(8):
                self.memset(acc[:, h, :], 0.0, 'acc%d' % h)
            m_own = (tau + 1) // 2
            second = (tau + 1) % 2
            its = [(kv, s_) for kv in range(2) for s_ in range(NSLOT)]

            def kts_of(s_):
                if s_ == m_own and not second:
                    return [0]
                return [0, 1]

            def qk_exp(n):
                kv, s_ = its[n]
                own = (s_ == m_own)
                qrhs = QT1[:, kv * 4:(kv + 1) * 4, :].rearrange("p h q -> p (h q)")
                for kt in kts_of(s_):
                    g = 2 * s_ + kt
                    pi = (n % 2) * 2 + kt
                    self.mm(ps[kt][:, :], KT[:, kv, g * 128:(g + 1) * 128], qrhs, True, True, ['KT', 'QT1'], PK[kt])
                    self.act(PT[pi][:].rearrange("p h q -> p (h q)"), ps[kt][:, :], AF.Exp, [PK[kt]], 'PT%d' % pi, scale=ATTN_SCALE)
                    if own and kt == (1 if second else 0):
                        self.tt(PT[pi][:], PT[pi][:], trib[:].unsqueeze(1).broadcast_to([128, 4, 128]), ALU.mult, ['PT%d' % pi, 'trib'], 'PT%d' % pi)

            def pv_acc(n):
                kv, s_ = its[n]
                own = (s_ == m_own)
                kts = kts_of(s_)
                ob = 2 + 2 * (n % 2)
                for hh in range(4):
                    bank = ps[ob + hh // 2]
                    col = (hh % 2) * 256
                    for i, kt in enumerate(kts):
                        pi = (n % 2) * 2 + kt
                        self.mm(bank[:, col:col + 129], PT[pi][:, hh, :], VS[:, 2 * s_ + kt, kv, 0:129], i == 0, i == len(kts) - 1,
                                ['PT%d' % pi, 'VS'], PK[ob + hh // 2])
                for hh in range(4):
                    h = kv * 4 + hh
                    bank = ps[ob + hh // 2]
                    col = (hh % 2) * 256
                    if own:
                        self.tt(acc[:, h, 0:129], acc[:, h, 0:129], bank[:, col:col + 129], ALU.add, ['acc%d' % h, PK[ob + hh // 2]], 'acc%d' % h)
                    else:
                        self.stt(acc[:, h, 0:129], bank[:, col:col + 129], sel[:, h, s_:s_ + 1], acc[:, h, 0:129], ALU.mult, ALU.add,
                                 ['acc%d' % h, 'sel', PK[ob + hh // 2]], 'acc%d' % h)

            for n in range(len(its) + 1):
                if n < len(its):
                    qk_exp(n)
                if n >= 1:
                    pv_acc(n - 1)
            AK = ['acc%d' % h for h in range(8)]
            self.S.op('dve', lambda e: e.reciprocal(out=rden[:, 0, :], in_=acc[:, :, 128]), reads=AK, writes=['rden'])
            self.tt(qb16[:], acc[:, :, 0:128], rden[:, 0, :].unsqueeze(2).broadcast_to([128, 8, 128]), ALU.mult, AK + ['rden'], 'qb16')
            out_proj(tau, gP, 'gP')

        tS = NT - 1
        q_proj(tS, True)
        for h in range(8):
            self.tr(ps[0][:, h * 4:(h + 1) * 4], zq[0:SPC, h, :], ident[0:SPC, 0:SPC], ['zq', 'consts'], 'ps0')
        S.barrier()
        off = 0
        c2, off = carve(off, [1, 512], F32)
        QTa, off = carve(off, [1, 32], F32)
        QTm, off = carve(off, [8, 32], F32)
        qsel, off = carve(off, [1, 128], F32)
        ptAi, off = carve(off, [1, 4], I32)
        ptBi, off = carve(off, [1, 2 * NBLK], I32)
        ptAf, off = carve(off, [1, 4], F32)
        idxAf, off = carve(off, [4, 8], F32)
        idxA, off = carve(off, [4, 8], I32)
        idxSf, off = carve(off, [6, 4], F32)
        idxS, off = carve(off, [6, 4], I32)
        ptBf, off = carve(off, [1, 2 * NBLK], F32)
        ksum0, off = carve(off, [2, 128], F32)
        ksum1, off = carve(off, [2, 128], F32)
        ksr, off = carve(off, [1, 128], F32)
        kmTs, off = carve(off, [4, 128], F32)
        gate_s, off = carve(off, [1, NBLK], F32)
        t8s, off = carve(off, [1, 8], F32)
        oh, off = carve(off, [1, NBLK], F32)
        ohp, off = carve(off, [1, NBLK], F32)
        phys, off = carve(off, [1, 8], F32)
        idxf, off = carve(off, [1, 8], F32)
        idxi, off = carve(off, [1, 8], I32)
        ksel, off = carve(off, [1, 128], F32)
        vsel, off = carve(off, [1, 128], F32)
        sc, off = carve(off, [1, 6 * 128 + 8], F32)
        Pm, off = carve(off, [1, 6 * 128 + 8], F32)
        den, off = carve(off, [1, 1], F32)
        pv, off = carve(off, [24, 128], F32)
        osum, off = carve(off, [1, 128], F32)
        qb16, off = carve(off, [8, 128], BF16)
        aT, off = carve(off, [8, 128], BF16)
        zq, off = carve(off, [8, 128], F32)
        wqb = []
        for i in range(2):
            a_, off = carve(off, [8, 512], BF16)
            wqb.append(a_)
        G0, off = carve(off, [1, 32 * 128], F32)
        G1, off = carve(off, [1, 32 * 128], F32)
        assert off <= SCR_BYTES, off
        GB = [G0[:, 0, :], G1[:, 0, :]]
        zq2 = zq[:].rearrange("p h d -> p (h d)")
        c2 = c2[:, 0, :]
        ksum = [ksum0, ksum1]
        self.cp(QTa[:, 0, :], ps[0][:, 0:32], ['ps0'], 'QTa')
        self.ld(c2, consts2_d, 'c2')
        self.ld(ptAi[:, 0, :], ptA_d, 'ptAi')
        self.ld(ptBi[0:32, 0, :], ptB_d, 'ptBi')
        self.cp(ptBf[0:32, 0, :], ptBi[0:32, 0, :], ['ptBi'], 'ptBf')
        self.cp(ptAf[:, 0, :], ptAi[:, 0, :], ['ptAi'], 'ptAf')
        self.ts(idxAf[:], ptAf[:, 0, :].unsqueeze(2).broadcast_to([128, 4, 8]), 8.0, None, ALU.mult, None, ['ptAf'], 'idxAf')
        self.tt(idxAf[:], idxAf[:], c2[:, 353:361].unsqueeze(1).broadcast_to([128, 4, 8]), ALU.add, ['idxAf', 'c2'], 'idxAf')
        self.cp(idxA[:], idxAf[:], ['idxAf'], 'idxA')
        ck8 = ck_d.rearrange("n (c x) -> (n c) x", x=4096)
        cv8 = cv_d.rearrange("n (c x) -> (n c) x", x=4096)
        self.tt(QTm[:], QTa[:, 0, :].unsqueeze(1).broadcast_to([128, 8, 32]), c2[:, 0:256].rearrange("p (a b) -> p a b", a=8), ALU.mult, ['QTa', 'c2'], 'QTm')
        self.tr(ps[1][0:32, 0:128], QTa[:, 0, :], ident, ['QTa', 'consts'], 'ps1')
        self.cp(qsel[0:32, 0, :], ps[1][0:32, 0:128], ['ps1'], 'qsel')
        RC = 32
        gi = 0
        for r in range(2):
            for kv in range(2):
                first = True
                for eo in range(2):
                    for rc in range(128 // RC):
                        gb = GB[gi % 2][:, 0:RC * 128]
                        gk = 'G%d' % (gi % 2)
                        self.S.dma('pool', None, None, reads=['idxA'], writes=[gk],
                                   fn=lambda e: e.indirect_dma_start(out=gb, out_offset=None, in_=ck8,
                                                                     in_offset=bass.IndirectOffsetOnAxis(ap=idxA[:, r * 2 + eo, kv * 4 + rc:kv * 4 + rc + 1], axis=0)))
                        dst = ksum[r][:, kv, :] if first else ksr[:, 0, :]
                        dk = 'ksum%d' % r if first else 'ksr'
                        self.S.op('dve', lambda e: e.tensor_reduce(out=dst, in_=gb.rearrange("p (r d) -> p d r", d=128), axis=AX.X, op=ALU.add),
                                  reads=[gk], writes=[dk])
                        if not first:
                            self.tt(ksum[r][:, kv, :], ksum[r][:, kv, :], ksr[:, 0, :], ALU.add, ['ksum%d' % r, 'ksr'], 'ksum%d' % r)
                        first = False
                        gi += 1
        for r in range(2):
            for kv in range(2):
                self.tr(ps[2][:, (kv * 2 + r) * 128:(kv * 2 + r + 1) * 128], ksum[r][:, kv, :], ident, ['ksum%d' % r, 'consts'], 'ps2')
        self.ts(kmTs[:].rearrange("p a b -> p (a b)"), ps[2][:, :], 1.0 / 256.0, None, ALU.mult, None, ['ps2'], 'kmTs')
        n = 0
        for s_ in range(SPC):
            for kv in range(2):
                self.mm(ps[3][0:32, 0:NBLK], QTm[:, s_ * 2 + kv, :], kmTs[:, kv * 2 + s_ // 2, (s_ % 2) * NBLK:(s_ % 2 + 1) * NBLK],
                        n == 0, n == 2 * SPC - 1, ['QTm', 'kmTs'], 'ps3')
                n += 1
        self.cp(gate_s[0:32, 0, :], ps[3][0:32, 0:NBLK], ['ps3'], 'gate_s')
        self.S.op('dve', lambda e: e.max(out=t8s[0:32, 0, :], in_=gate_s[0:32, 0, :]), reads=['gate_s'], writes=['t8s'])
        for j in range(3):
            self.ts(oh[0:32, 0, :], gate_s[0:32, 0, :], t8s[0:32, 0, j:j + 1], None, ALU.is_equal, None, ['gate_s', 't8s'], 'oh')
            for eo in range(2):
                self.tt(ohp[0:32, 0, :], oh[0:32, 0, :], ptBf[0:32, 0, eo * NBLK:(eo + 1) * NBLK], ALU.mult, ['oh', 'ptBf'], 'ohp')
                self.S.op('dve', lambda e: e.tensor_reduce(out=phys[0:32, 0, j * 2 + eo:j * 2 + eo + 1], in_=ohp[0:32, 0, :], axis=AX.X, op=ALU.add),
                          reads=['ohp'], writes=['phys'])
        self.ts(idxf[0:32, 0, 0:6], phys[0:32, 0, 0:6], 2.0, c2[0:32, 256:257], ALU.mult, ALU.add, ['phys', 'c2'], 'idxf')
        self.ts(idxSf[0:32], idxf[0:32, 0, 0:6].unsqueeze(2).broadcast_to([32, 6, 4]), 4.0, None, ALU.mult, None, ['idxf'], 'idxSf')
        self.tt(idxSf[0:32], idxSf[0:32], c2[0:32, 353:357].unsqueeze(1).broadcast_to([32, 6, 4]), ALU.add, ['idxSf', 'c2'], 'idxSf')
        self.cp(idxS[0:32], idxSf[0:32], ['idxSf'], 'idxi')
        for (dst, dk, c0) in ((ksel, 'ksel', 0), (vsel, 'vsel', 256)):
            self.mm(ps[4][0:32, 0:128], c2[0:SPC, 257:289], kvS[0:SPC, c0:c0 + 128], True, False, ['c2', 'kvS'], 'ps4')
            self.mm(ps[4][0:32, 0:128], c2[0:SPC, 289:321], kvS[0:SPC, c0 + 128:c0 + 256], False, True, ['c2', 'kvS'], 'ps4')
            self.cp(dst[0:32, 0, :], ps[4][0:32, 0:128], ['ps4'], dk)
        ck2 = ck_d.rearrange("n (h x) -> (n h) x", h=2)
        cv2 = cv_d.rearrange("n (h x) -> (n h) x", h=2)
        qb_ = qsel[0:32, 0, :].unsqueeze(1).broadcast_to([32, 32, 128])
        for c in range(6):
            for rh in range(4):
                gb = GB[gi % 2][0:32, :]
                gk = 'G%d' % (gi % 2)
                self.S.dma('pool', None, None, reads=['idxi'], writes=[gk],
                           fn=lambda e: e.indirect_dma_start(out=gb, out_offset=None, in_=ck8,
                                                             in_offset=bass.IndirectOffsetOnAxis(ap=idxS[0:32, c, rh:rh + 1], axis=0)))
                g3 = gb.rearrange("p (r d) -> p r d", d=128)
                self.tt(g3, g3, qb_, ALU.mult, [gk, 'qsel'], gk)
                self.S.op('dve', lambda e: e.tensor_reduce(out=sc[0:32, 0, c * 128 + rh * 32:c * 128 + rh * 32 + 32], in_=g3, axis=AX.X, op=ALU.add),
                          reads=[gk], writes=['sc'])
                gi += 1
        self.tt(osum[0:32, 0, :], qsel[0:32, 0, :], ksel[0:32, 0, :], ALU.mult, ['qsel', 'ksel'], 'osum')
        self.S.op('dve', lambda e: e.tensor_reduce(out=sc[0:32, 0, 768:769], in_=osum[0:32, 0, :], axis=AX.X, op=ALU.add), reads=['osum'], writes=['sc'])
        self.S.op('act', lambda e: e.activation(out=Pm[0:32, 0, 0:769], in_=sc[0:32, 0, 0:769], func=AF.Exp, scale=ATTN_SCALE),
                  reads=['sc'], writes=['Pm'])
        self.S.op('dve', lambda e: e.tensor_reduce(out=den[0:32, 0, :], in_=Pm[0:32, 0, 0:769], axis=AX.X, op=ALU.add), reads=['Pm'], writes=['den'])
        for c in range(6):
            for rh in range(4):
                gb = GB[gi % 2][0:32, :]
                gk = 'G%d' % (gi % 2)
                self.S.dma('pool', None, None, reads=['idxi'], writes=[gk],
                           fn=lambda e: e.indirect_dma_start(out=gb, out_offset=None, in_=cv8,
                                                             in_offset=bass.IndirectOffsetOnAxis(ap=idxS[0:32, c, rh:rh + 1], axis=0)))
                g3 = gb.rearrange("p (r d) -> p r d", d=128)
                pb_ = Pm[0:32, 0, c * 128 + rh * 32:c * 128 + rh * 32 + 32].unsqueeze(2).broadcast_to([32, 32, 128])
                self.tt(g3, g3, pb_, ALU.mult, [gk, 'Pm'], gk)
                self.S.op('dve', lambda e: e.tensor_reduce(out=pv[0:32, c * 4 + rh, :], in_=gb.rearrange("p (r d) -> p d r", d=128), axis=AX.X, op=ALU.add),
                          reads=[gk], writes=['pv'])
                gi += 1
        self.S.op('dve', lambda e: e.tensor_reduce(out=osum[0:32, 0, :], in_=pv[0:32, :, :].rearrange("p i d -> p d i"), axis=AX.X, op=ALU.add),
                  reads=['pv'], writes=['osum'])
        self.stt(osum[0:32, 0, :], vsel[0:32, 0, :], Pm[0:32, 0, 768:769], osum[0:32, 0, :], ALU.mult, ALU.add, ['vsel', 'Pm', 'osum'], 'osum')
        self.S.op('dve', lambda e: e.reciprocal(out=den[0:32, 0, :], in_=den[0:32, 0, :]), reads=['den'], writes=['den'])
        self.ts(osum[0:32, 0, :], osum[0:32, 0, :], den[0:32, 0, 0:1], None, ALU.mult, None, ['osum', 'den'], 'osum')
        for h in range(8):
            self.mm(ps[2 + h // 4][0:SPC, (h % 4) * 128:(h % 4 + 1) * 128], c2[0:32, 321 + h * 4:321 + h * 4 + 4], osum[0:32, 0, :], True, True,
                    ['c2', 'osum'], PK[2 + h // 4])
        self.memset(qb16[:].rearrange("p h d -> p (h d)"), 0.0, 'qb16')
        for hf in range(2):
            self.cp(qb16[0:SPC, hf * 4:(hf + 1) * 4, :].rearrange("p h d -> p (h d)"), ps[2 + hf][0:SPC, :], [PK[2 + hf], 'qb16'], 'qb16')
        out_proj(tS, gS, 'gS')

        if cfg.stop == 'attn':
            self.finish(dbg, xres, XK)
            return

        TG = NT // cfg.MG

        def moe(layer):
            ada(2 * layer + 1)
            if cfg.stop == 'ada1':
                return
            off = 0
            wr32, off = carve(off, [8, NE], F32)
            wrm, off = carve(off, [8, NE], F32)
            rb, off = carve(off, [1, NE], F32)
            brB, off = carve(off, [1, NE], F32)
            Wt, off = carve(off, [NT, NE], F32)
            hTm, off = carve(off, [8, TG * 128], BF16)
            xT32, off = carve(off, [8, 128], F32)
            accm, off = carve(off, [TG, D], F32)
            wg2, wu2, wd2, AT, sg, Ab = [], [], [], [], [], []
            for i in range(2):
                a_, off = carve(off, [8, 512], BF16); wg2.append(a_)
                a_, off = carve(off, [8, 512], BF16); wu2.append(a_)
                a_, off = carve(off, [4, D], BF16); wd2.append(a_)
                a_, off = carve(off, [4, 128], BF16); AT.append(a_)
                a_, off = carve(off, [1, 512], F32); sg.append(a_)
                a_, off = carve(off, [1, 512], BF16); Ab.append(a_)
            sc_, off = carve(off, [1, NE], F32)
            bi, off = carve(off, [NG, 8], F32)
            msk, off = carve(off, [NG, 8], F32)
            t8g, off = carve(off, [NG, 8], F32)
            gs, off = carve(off, [1, NG], F32)
            gmax, off = carve(off, [1, 1], F32)
            ohg, off = carve(off, [1, NG], F32)
            t8e, off = carve(off, [1, 8], F32)
            sel2, off = carve(off, [1, NE], F32)
            wun, off = carve(off, [1, NE], F32)
            dsum, off = carve(off, [1, 1], F32)
            zq, off = carve(off, [1, D], F32)
            assert off <= SCR_BYTES, off
            zq2 = zq[:, 0, :]
            bi2 = bi[:].rearrange("p g e -> p (g e)")
            msk2 = msk[:].rearrange("p g e -> p (g e)")

            self.ld(wr32[:], wr_d.rearrange("(kc p) e -> p kc e", p=128), 'wr32')
            for kc in range(8):
                self.ts(wrm[:, kc, :], wr32[:, kc, :], modT[:, 1, kc, 0:1], None, ALU.mult, None, ['wr32', 'modT'], 'wrm')
                self.mm(ps[0][0:1, 0:NE], modT[:, 0, kc, 0:1], wr32[:, kc, :], kc == 0, kc == 7, ['modT', 'wr32'], 'ps0')
            self.cp(rb[0:1, 0, :], ps[0][0:1, 0:NE], ['ps0'], 'rb')
            self.bcast_row(brB[:, 0, :], br_d, NE, 'brB', 'brB')
            if cfg.stop == 'rsetup':
                return

            for G in range(cfg.MG):
                tiles = list(range(G * TG, (G + 1) * TG))
                for ti, tau in enumerate(tiles):
                    smp = (tau == NT - 1)
                    tile_to_hT(xres[:, tau, :], XK[tau], hTm, ti * 128, sample=smp, xT32=xT32)
                    if cfg.stop == 'r_0':
                        return
                    wsel, wk = (wr32, 'wr32') if smp else (wrm, 'wrm')
                    for kc in range(8):
                        self.mm(ps[6][:, 0:NE], xT32[:, kc, :], wsel[:, kc, :], kc == 0, smp and kc == 7, ['xT32', wk], 'ps6')
                    if not smp:
                        self.mm(ps[6][:, 0:NE], self.ones32[0:1, :], rb[0:1, 0, :], False, True, ['ones32', 'rb'], 'ps6')
                    if cfg.stop == 'r_a':
                        return
                    self.act(sc_[:, 0, :], ps[6][:, 0:NE], AF.Sigmoid, ['ps6'], 'sc_')
                    self.tt(bi2, sc_[:, 0, :], brB[:, 0, :], ALU.add, ['sc_', 'brB'], 'bi')
                    for g in range(NG):
                        self.S.op('dve', lambda e: e.max(out=t8g[:, g, :], in_=bi[:, g, :]), reads=['bi'], writes=['t8g'])
                    if cfg.stop == 'r_b':
                        return
                    self.tt(gs[:, 0, :], t8g[:, :, 0], t8g[:, :, 1], ALU.add, ['t8g'], 'gs')
                    self.tt(gmax[:, 0, :], gs[:, 0, 0:1], gs[:, 0, 1:2], ALU.max, ['gs'], 'gmax')
                    for g in range(2, NG):
                        self.tt(gmax[:, 0, :], gmax[:, 0, :], gs[:, 0, g:g + 1], ALU.max, ['gs', 'gmax'], 'gmax')
                    self.ts(ohg[:, 0, :], gs[:, 0, :], gmax[:, 0, 0:1], None, ALU.is_equal, None, ['gs', 'gmax'], 'ohg')
                    self.ts(ohg[:, 0, :], ohg[:, 0, :], BIG, -BIG, ALU.mult, ALU.add, ['ohg'], 'ohg')
                    self.tt(msk[:], bi[:], ohg[:, 0, :].unsqueeze(2).broadcast_to([128, NG, 8]), ALU.add, ['bi', 'ohg'], 'msk')
                    self.S.op('dve', lambda e: e.max(out=t8e[:, 0, :], in_=msk2), reads=['msk'], writes=['t8e'])
                    self.ts(sel2[:, 0, :], msk2, t8e[:, 0, 1:2], None, ALU.is_ge, None, ['msk', 't8e'], 'sel2')
                    self.tt(wun[:, 0, :], sc_[:, 0, :], sel2[:, 0, :], ALU.mult, ['sc_', 'sel2'], 'wun')
                    self.S.op('dve', lambda e: e.tensor_reduce(out=dsum[:, 0, :], in_=wun[:, 0, :], axis=AX.X, op=ALU.add), reads=['wun'], writes=['dsum'])
                    self.S.op('dve', lambda e: e.reciprocal(out=dsum[:, 0, :], in_=dsum[:, 0, :]), reads=['dsum'], writes=['dsum'])
                    self.ts(Wt[:, tau, :], wun[:, 0, :], dsum[:, 0, 0:1], None, ALU.mult, None, ['wun', 'dsum'], 'Wt')
                if cfg.stop == 'route':
                    return
                items = [(e_, ti) for e_ in range(NE) for ti in range(TG)]

                def stage_a(n):
                    e_, ti = items[n]
                    sl = e_ % 2
                    ab = n % 2
                    if ti == 0:
                        self.S.dma('pool', wg2[sl][:], wg_d[layer, e_].rearrange("(kc p) n -> p kc n", p=128), writes=['wg%d' % sl])
                        self.S.dma('pool', wu2[sl][:], wu_d[layer, e_].rearrange("(kc p) n -> p kc n", p=128), writes=['wu%d' % sl])
                        self.S.dma('pool', wd2[sl][:], wd_d[layer, e_].rearrange("(kc p) n -> p kc n", p=128), writes=['wd%d' % sl])
                    for kc in range(8):
                        self.mm(ps[ab][:, :], hTm[:, kc, ti * 128:(ti + 1) * 128], wg2[sl][:, kc, :], kc == 0, kc == 7, ['hTg', 'wg%d' % sl], PK[ab])
                    for kc in range(8):
                        self.mm(ps[2 + ab][:, :], hTm[:, kc, ti * 128:(ti + 1) * 128], wu2[sl][:, kc, :], kc == 0, kc == 7, ['hTg', 'wu%d' % sl], PK[2 + ab])
                    self.act(sg[ab][:, 0, :], ps[ab][:, :], AF.Silu, [PK[ab]], 'sg%d' % ab)
                    self.tt(Ab[ab][:, 0, :], sg[ab][:, 0, :], ps[2 + ab][:, :], ALU.mult, ['sg%d' % ab, PK[2 + ab]], 'Ab%d' % ab)

                def stage_b(n):
                    ab = n % 2
                    pt = ps[4 + ab][:].bitcast(BF16)
                    for fc in range(4):
                        self.tr(pt[:, fc * 128:(fc + 1) * 128], Ab[ab][:, 0, fc * 128:(fc + 1) * 128], identb[:], ['Ab%d' % ab, 'identb'], PK[4 + ab])
                    self.cp(AT[ab][:].rearrange("p f t -> p (f t)"), pt[:, 0:512], [PK[4 + ab]], 'AT%d' % ab, eng='act')

                def stage_c(n):
                    e_, ti = items[n]
                    sl = e_ % 2
                    ab = n % 2
                    tau = tiles[ti]
                    for hf in range(2):
                        pb = 6 + hf
                        for fc in range(4):
                            self.mm(ps[pb][:, :], AT[ab][:, fc, :], wd2[sl][:, fc, hf * 512:(hf + 1) * 512], fc == 0, fc == 3, ['AT%d' % ab, 'wd%d' % sl], PK[pb])
                        dst = accm[:, ti, hf * 512:(hf + 1) * 512]
                        if e_ == 0:
                            self.ts(dst, ps[pb][:, :], Wt[:, tau, e_:e_ + 1], None, ALU.mult, None, [PK[pb], 'Wt'], 'accm%d' % ti)
                        else:
                            self.stt(dst, ps[pb][:, :], Wt[:, tau, e_:e_ + 1], dst, ALU.mult, ALU.add, [PK[pb], 'Wt', 'accm%d' % ti], 'accm%d' % ti)

                nit = len(items)
                for step in range(nit + 2):
                    if step < nit:
                        stage_a(step)
                    if 1 <= step <= nit:
                        stage_b(step - 1)
                    if step >= 2:
                        stage_c(step - 2)
                for ti, tau in enumerate(tiles):
                    smp = (tau == NT - 1)
                    gt, gk = (gS, 'gS') if smp else (gP, 'gP')
                    self.tt(zq2, accm[:, ti, :], gt[:], ALU.mult, ['accm%d' % ti, gk], 'zq')
                    self.stt(zq2, xres[:, tau, :], cfg.ALPHA, zq2, ALU.mult, ALU.add, [XK[tau], 'zq'], 'zq')
                    self.layernorm(zq2, xres[:, tau, :], lnG[:], lnB[:], 'zq', XK[tau], 'lnG')

        moe(0)
        if cfg.stop in ('moe0', 'route', 'ada1', 'rsetup', 'r_a', 'r_b', 'r_0'):
            self.finish(dbg, xres, XK)
            return

        ada(2)
        off = 0
        uT, off = carve(off, [8, 30 + 512], F32)
        hTc, off = carve(off, [8, 512], BF16)
        winc = []
        for i in range(2):
            a_, off = carve(off, [8, 256], BF16); winc.append(a_)
        yg, off = carve(off, [8, 512], F32)
        ytok, off = carve(off, [1, D], F32)
        zb, off = carve(off, [8, 128], BF16)
        zT, off = carve(off, [8, 128], BF16)
        wout, off = carve(off, [8, D], BF16)
        wdw, off = carve(off, [8, CW], F32)
        cg_, off = carve(off, [1, D], F32)
        cb_, off = carve(off, [1, D], F32)
        sgm, off = carve(off, [1, 512], F32)
        zq, off = carve(off, [1, D], F32)
        uS, off = carve(off, [8, SPC], F32)
        stT, off = carve(off, [8 * SPC, CW], F32)
        prodS, off = carve(off, [8 * SPC, CW], F32)
        yS, off = carve(off, [8, SPC], F32)
        utok, off = carve(off, [1, D], F32)
        assert off <= SCR_BYTES, off
        zq2 = zq[:, 0, :]
        ytok2 = ytok[:, 0, :]
        utok2 = utok[:, 0, :]
        self.memset(uT[:].rearrange("p a b -> p (a b)"), 0.0, 'uT')
        self.ld(wdw[:].rearrange("p a b -> p (a b)"), wdwT_d, 'wdw')
        self.S.dma('pool', wout[:], wout_d.rearrange("(kc p) n -> p kc n", p=128), writes=['wout'])
        self.bcast_row(cg_[:, 0, :], clng_d, D, 'cgb', 'cg')
        self.bcast_row(cb_[:, 0, :], clnb_d, D, 'cgb', 'cb')
        win_src = win_d.rearrange("(kc p) n -> p kc n", p=128)
        wi = [0]

        def glu_cols(ncol, dst_fn):
            for cc in range(8):
                wb = winc[wi[0] % 2]
                wk = 'winc%d' % (wi[0] % 2)
                wi[0] += 1
                self.S.dma('pool', wb[:, :, 0:128], win_src[:, :, cc * 128:(cc + 1) * 128], writes=[wk])
                self.S.dma('pool', wb[:, :, 128:256], win_src[:, :, D + cc * 128:D + (cc + 1) * 128], writes=[wk])
                for kc in range(8):
                    self.mm(ps[0][:, 0:ncol], wb[:, kc, 0:128], hTc[:, kc, 0:ncol], kc == 0, kc == 7, [wk, 'hTg'], 'ps0')
                for kc in range(8):
                    self.mm(ps[1][:, 0:ncol], wb[:, kc, 128:256], hTc[:, kc, 0:ncol], kc == 0, kc == 7, [wk, 'hTg'], 'ps1')
                self.act(sgm[:, 0, 0:ncol], ps[1][:, 0:ncol], AF.Sigmoid, ['ps1'], 'sgm')
                dst, dk = dst_fn(cc)
                self.tt(dst, ps[0][:, 0:ncol], sgm[:, 0, 0:ncol], ALU.mult, ['ps0', 'sgm'], dk)

        def conv_tail(tau, col0, gate_tile, gkey):
            for hf in range(2):
                for q in range(4):
                    cc = hf * 4 + q
                    self.tr(ps[2 + hf][:, q * 128:(q + 1) * 128], yg[:, cc, col0:col0 + 128], ident, ['yg', 'consts'], PK[2 + hf])
                self.cp(ytok2[:, hf * 512:(hf + 1) * 512], ps[2 + hf][:, :], [PK[2 + hf]], 'ytok', eng='act')
            self.layernorm(ytok2, ytok2, cg_[:, 0, :], cb_[:, 0, :], 'ytok', 'ytok', 'cgb')
            self.act(zb[:].rearrange("p a b -> p (a b)"), ytok2, AF.Silu, ['ytok'], 'zb')
            pt = ps[4][:].bitcast(BF16)
            for cc in range(8):
                self.tr(pt[:, cc * 128:(cc + 1) * 128], zb[:, cc, :], identb[:], ['zb', 'identb'], 'ps4')
            self.cp(zT[:].rearrange("p a b -> p (a b)"), pt[:, :], ['ps4'], 'zT')
            for hf in range(2):
                for cc in range(8):
                    self.mm(ps[6 + hf][:, :], zT[:, cc, :], wout[:, cc, hf * 512:(hf + 1) * 512], cc == 0, cc == 7, ['zT', 'wout'], PK[6 + hf])
                self.tt(zq2[:, hf * 512:(hf + 1) * 512], ps[6 + hf][:, :], gate_tile[:, hf * 512:(hf + 1) * 512], ALU.mult, [PK[6 + hf], gkey], 'zq')
            self.stt(zq2, xres[:, tau, :], cfg.ALPHA, zq2, ALU.mult, ALU.add, [XK[tau], 'zq'], 'zq')
            self.layernorm(zq2, xres[:, tau, :], lnG[:], lnB[:], 'zq', XK[tau], 'lnG')

        for g0 in range(0, NPT, 4):
            tiles = list(range(g0, min(g0 + 4, NPT)))
            ncol = len(tiles) * 128
            for ti, tau in enumerate(tiles):
                tile_to_hT(xres[:, tau, :], XK[tau], hTc, ti * 128)
            glu_cols(ncol, lambda cc: (uT[:, cc, 30:30 + ncol], 'uT'))
            if g0 == 0:
                self.ts(uT[:, :, 30:158], uT[:, :, 30:158], flag[:, 0:1], None, ALU.mult, None, ['uT', 'flag'], 'uT')
            for cc in range(8):
                for k in range(CW):
                    if k == 0:
                        self.ts(yg[:, cc, 0:ncol], uT[:, cc, 0:ncol], wdw[:, cc, 0:1], None, ALU.mult, None, ['uT', 'wdw'], 'yg')
                    else:
                        self.stt(yg[:, cc, 0:ncol], uT[:, cc, k:k + ncol], wdw[:, cc, k:k + 1], yg[:, cc, 0:ncol], ALU.mult, ALU.add, ['uT', 'wdw', 'yg'], 'yg')
            if tiles[-1] == NPT - 1:
                cl = 30 + (len(tiles) - 1) * 128 + 98
                for hf in range(2):
                    for q in range(4):
                        cc = hf * 4 + q
                        self.tr(ps[2 + hf][0:30, q * 128:(q + 1) * 128], uT[:, cc, cl:cl + 30], ident, ['uT', 'consts'], PK[2 + hf])
                    self.cp(utok2[0:30, hf * 512:(hf + 1) * 512], ps[2 + hf][0:30, :], [PK[2 + hf]], 'utok', eng='act')
                self.st(conv_p[:, :], utok2[0:30, :], 'utok', 'conv_p')
            else:
                self.cp(uT[:, :, 0:30], uT[:, :, ncol:ncol + 30], ['uT'], 'uT')
            if cfg.stop == 'c_conv':
                self.finish(dbg, xres, XK)
                return
            for ti, tau in enumerate(tiles):
                if tau >= 1:
                    conv_tail(tau, ti * 128, gP, 'gP')
            if cfg.stop == 'c_tail':
                self.finish(dbg, xres, XK)
                return
        tile_to_hT(xres[:, tS, :], XK[tS], hTc, 0, sample=True)
        glu_cols(SPC, lambda cc: (uS[:, cc, :], 'uS'))
        st4 = stT[:].rearrange("p (c s) k -> p c s k", c=8)
        pflat = prodS[:].rearrange("p a k -> p (a k)")[:, 0:8 * SPC * 30]
        self.ld(pflat, stfm_d, 'prodS')
        self.cp(st4[:, :, :, 0:30], pflat.rearrange("p (c s k) -> p c s k", c=8, s=SPC), ['prodS'], 'stT')
        self.cp(st4[:, :, :, 30], uS[:], ['uS', 'stT'], 'stT')
        self.tt(prodS[:].rearrange("p (c s) k -> p c s k", c=8), st4, wdw[:].unsqueeze(2).broadcast_to([128, 8, SPC, CW]), ALU.mult, ['stT', 'wdw'], 'prodS')
        self.S.op('dve', lambda e: e.tensor_reduce(out=yS[:].rearrange("p c s -> p (c s)"), in_=prodS[:], axis=AX.X, op=ALU.add), reads=['prodS'], writes=['yS'])
        self.memset(yg[:, :, 0:128], 0.0, 'yg')
        self.cp(yg[:, :, 0:SPC], yS[:], ['yS', 'yg'], 'yg')
        conv_tail(tS, 0, gS, 'gS')
        for s_ in range(SPC):
            self.ld(ytok2[s_ * 29:(s_ + 1) * 29, :], sttok_d[s_, 1:30, :], 'ytok')
        for s_ in range(SPC):
            self.st(conv_s[s_, 0:29, :], ytok2[s_ * 29:(s_ + 1) * 29, :], 'ytok', 'conv_s')
        for hf in range(2):
            for q in range(4):
                cc = hf * 4 + q
                self.tr(ps[2 + hf][0:SPC, q * 128:(q + 1) * 128], uS[:, cc, :], ident, ['uS', 'consts'], PK[2 + hf])
            self.cp(utok2[0:SPC, hf * 512:(hf + 1) * 512], ps[2 + hf][0:SPC, :], [PK[2 + hf]], 'utok', eng='act')
        self.st(conv_s[:, 29, :], utok2[0:SPC, :], 'utok', 'conv_s')
        if cfg.stop == 'conv':
            self.finish(dbg, xres, XK)
            return

        moe(1)
        for tau in range(1, NPT):
            self.st(y_p[(tau - 1) * 128:tau * 128, :], xres[:, tau, :], XK[tau], 'y_p')
        self.st(y_s[:, :], xres[0:SPC, tS, :], XK[tS], 'y_s')
        self.finish(dbg, xres, XK)

    def finish(self, dbg, xres, XK):
        cfg = self.cfg
        if dbg is not None:
            for t in range(cfg.NT):
                self.st(dbg[t * 128:(t + 1) * 128, :], xres[:, t, :], XK[t], 'dbg')
        self.S.wait_keys('sp', list(set(self.outkeys)))
        self.es.close()


def prep_inputs(cfg, I):
    f32 = np.float32
    NC, NCB, LS, NSLOT, NPT, NGT, SPC, NBLK = cfg.NC, cfg.NCB, cfg.LS, cfg.NSLOT, cfg.NPT, cfg.NGT, cfg.SPC, cfg.NBLK
    half = 8
    inv_freq = (f32(ROPE_THETA) ** (-np.arange(half, dtype=f32) / f32(half))).astype(f32) if False else None
    half = 16
    inv_freq = np.power(f32(ROPE_THETA), -(np.arange(half, dtype=f32) / f32(half))).astype(f32)
    ident = np.eye(128, dtype=f32)
    tri = (np.arange(128)[:, None] <= np.arange(128)[None, :]).astype(f32)
    consts = np.concatenate([ident, tri], axis=1)
    c2 = np.zeros((128, 512), f32)
    cm = np.zeros((8, 32), f32)
    for s_ in range(4):
        for kv in range(2):
            for h in range(kv * 4, kv * 4 + 4):
                cm[s_ * 2 + kv, h * 4 + s_] = 1.0
    c2[:, 0:256] = cm.reshape(1, 256)
    c2[0:32, 256] = (np.arange(32) // 4) // 4
    for s_ in range(4):
        for h in range(8):
            c2[s_, (257 if h < 4 else 289) + h * 4 + s_] = 1.0
    c2[0:32, 321:353] = np.eye(32, dtype=f32)
    c2[:, 353:361] = np.arange(8, dtype=f32)[None, :]
    shared = {
        "consts": consts, "consts2": c2,
        "w_ada": np.ascontiguousarray(I["w_ada"]), "b_ada": np.ascontiguousarray(I["b_ada"]),
        "b_adaT": np.ascontiguousarray(I["b_ada"].reshape(2, 48, 128).transpose(2, 0, 1).reshape(128, 96)),
        "ln_g": np.ascontiguousarray(I["ln_g"].reshape(4, D)), "ln_b": np.ascontiguousarray(I["ln_b"].reshape(4, D)),
        "w_qkv": np.ascontiguousarray(I["w_qkv"][0]), "w_o": np.ascontiguousarray(I["w_o"][0]),
        "conv_w_in": np.ascontiguousarray(I["conv_w_in"][0]),
        "wdwT": np.ascontiguousarray(I["conv_w_dw"][0].T.reshape(8, 128, CW).transpose(1, 0, 2).reshape(128, 8 * CW)),
        "conv_ln_g": np.ascontiguousarray(I["conv_ln_g"].reshape(1, D)), "conv_ln_b": np.ascontiguousarray(I["conv_ln_b"].reshape(1, D)),
        "conv_w_out": np.ascontiguousarray(I["conv_w_out"][0]),
        "w_router": np.ascontiguousarray(I["w_router"]), "b_router": np.ascontiguousarray(I["b_router"].reshape(1, -1)),
        "w_gate": np.ascontiguousarray(I["w_gate"]), "w_up": np.ascontiguousarray(I["w_up"]), "w_down": np.ascontiguousarray(I["w_down"]),
        "cache_k": np.ascontiguousarray(I["cache_k"].reshape(cfg.NPHYS, -1)),
        "cache_v": np.ascontiguousarray(I["cache_v"].reshape(cfg.NPHYS, -1)),
    }
    maps = []
    pt = np.asarray(I["page_table"]).astype(np.int32)
    for c in range(NC):
        b, j = c // NCB, c % NCB
        rot = LS * j - 1
        m = dict(shared)
        m["xg"] = np.ascontiguousarray(np.roll(I["x_prompt"][b], -rot * 256, axis=0))
        gslot = (rot + np.arange(NSLOT)) % NSLOT
        pos = (gslot[:, None] * 256 + np.arange(256)[None, :]).reshape(-1)
        pos = np.concatenate([pos, np.full(128, cfg.PAST_LEN)]).astype(f32)
        ang = pos[:, None] * inv_freq[None, :]
        tab = np.concatenate([np.cos(ang), np.sin(ang)], axis=1).astype(f32)
        m["rope"] = np.ascontiguousarray(tab.reshape(NGT + 1, 128, 32).transpose(1, 0, 2).reshape(128, (NGT + 1) * 32))
        sid = np.arange(c * SPC, (c + 1) * SPC)
        xs = np.zeros((128, D), f32)
        xs[:SPC] = I["x_sample"][sid, 0]
        m["xs"] = xs
        cv = np.zeros((8, D), f32)
        cv[0] = I["c_prompt"][b]
        cv[1:1 + SPC] = I["c_sample"][sid]
        m["cvec"] = cv
        valid = np.zeros((NPT, NSLOT), bool)
        for tau in range(NPT):
            own = rot + (tau + 1) // 2
            valid[tau] = gslot < own
        m["nmask"] = np.ascontiguousarray(np.broadcast_to(np.where(valid, 0.0, -BIG).astype(f32).reshape(1, -1), (128, NPT * NSLOT)))
        m["v01"] = np.ascontiguousarray(np.broadcast_to(valid.astype(f32).reshape(1, -1), (128, NPT * NSLOT)))
        m["flag"] = np.full((128, 1), 1.0 if j > 0 else 0.0, f32)
        ptA = np.zeros((128, 4), np.int32)
        for r in range(2):
            for eo in range(2):
                for s2 in range(2):
                    ptA[s2 * NBLK:(s2 + 1) * NBLK, r * 2 + eo] = pt[sid[2 * r + s2], eo::2]
        m["ptA"] = ptA
        ptB = np.zeros((32, 2 * NBLK), np.int32)
        for h in range(8):
            for s in range(SPC):
                ptB[h * 4 + s, :NBLK] = pt[sid[s], 0::2]
                ptB[h * 4 + s, NBLK:] = pt[sid[s], 1::2]
        m["ptB"] = ptB
        st = I["state_conv"][0][sid]
        m["st_tok"] = np.ascontiguousarray(st)
        m["st_fm"] = np.ascontiguousarray(st.reshape(SPC, 30, 8, 128).transpose(3, 2, 0, 1).reshape(128, 8 * SPC * 30))
        maps.append(m)
    return maps


_PROG_CACHE = {}


def run_cfg(cfg, inputs):
    key = (cfg.B, cfg.SEQ, cfg.NCB, cfg.DEC_BATCH, cfg.PAST_LEN, cfg.NE, cfg.NG, cfg.MG, cfg.stop)
    if key not in _PROG_CACHE:
        p = Prog(cfg)
        p.build()
        _PROG_CACHE[key] = p
    p = _PROG_CACHE[key]
    maps = prep_inputs(cfg, inputs)
    maps = [{k: v for k, v in m.items() if k in p.din} for m in maps]
    res = run_bass_kernel_spmd(p.nc, maps, core_ids=list(range(cfg.NC)))
    return res.results


def assemble(cfg, R):
    f32 = np.float32
    B, NCB, LS, SPC = cfg.B, cfg.NCB, cfg.LS, cfg.SPC
    y_p = np.zeros((B, cfg.SEQ, D), f32)
    k_p = np.zeros((B, cfg.SEQ // 128, 1, 2, 128, 128), f32)
    v_p = np.zeros_like(k_p)
    conv_p = np.zeros((1, B, 30, D), f32)
    y_s = np.zeros((cfg.DEC_BATCH, 1, D), f32)
    k_s = np.zeros((cfg.DEC_BATCH, 1, 2, 1, 128), f32)
    v_s = np.zeros_like(k_s)
    conv_s = np.zeros((1, cfg.DEC_BATCH, 30, D), f32)
    for c, r in enumerate(R):
        b, j = c // NCB, c % NCB
        t0 = j * LS * 256
        y_p[b, t0:t0 + LS * 256] = r["y_p"]
        k_p[b, j * 2 * LS:(j + 1) * 2 * LS, 0] = r["k_p"]
        v_p[b, j * 2 * LS:(j + 1) * 2 * LS, 0] = r["v_p"]
        if j == NCB - 1:
            conv_p[0, b] = r["conv_p"]
        sl = slice(c * SPC, (c + 1) * SPC)
        y_s[sl, 0] = r["y_s"]
        k_s[sl, 0, :, 0, :] = r["k_s"].reshape(SPC, 2, 128)
        v_s[sl, 0, :, 0, :] = r["v_s"].reshape(SPC, 2, 128)
        conv_s[0, sl] = r["conv_s"]
    return (y_p, y_s, k_p, v_p, conv_p, k_s, v_s, conv_s)


def kernel(**inputs):
    cfg = Cfg()
    inputs = {k: np.asarray(v) for k, v in inputs.items()}
    R = run_cfg(cfg, inputs)
    return assemble(cfg, R)
```

```python
import numpy as np
from contextlib import ExitStack
import concourse.bass as bass
import concourse.mybir as mybir
from concourse.bass_utils import run_bass_kernel_spmd

F32 = mybir.dt.float32
BF16 = mybir.dt.bfloat16
I32 = mybir.dt.int32
U32 = mybir.dt.uint32
AF = mybir.ActivationFunctionType
ALU = mybir.AluOpType
AX = mybir.AxisListType

EPOCH = 24000
D = 1024
H = 8
KVH = 2
HD = 128
ROPE_THETA = 500000.0
ATTN_SCALE = HD ** -0.5
CW = 31
DFF = 512
LN_EPS = 1e-5
BIG = 1.0e30


class Cfg:
    def __init__(self, B=2, SEQ=8192, NCB=4, DEC_BATCH=32, PAST_LEN=16384, NE=32, NG=4, DEPTH=2, MG=3,
                 stop=None):
        self.B, self.SEQ, self.NCB, self.DEC_BATCH, self.PAST_LEN = B, SEQ, NCB, DEC_BATCH, PAST_LEN
        self.NE, self.NG, self.EPG = NE, NG, NE // NG
        self.NC = B * NCB
        self.NSLOT = SEQ // 256
        self.LS = self.NSLOT // NCB
        self.NPT = 2 * self.LS + 1
        self.NT = self.NPT + 1
        self.NGT = 2 * self.NSLOT
        self.SPC = DEC_BATCH // self.NC
        self.NPG = PAST_LEN // 128
        self.NBLK = self.NPG // 2
        self.NPHYS = (DEC_BATCH * self.NPG * 5) // 4
        self.DEPTH = DEPTH
        self.MG = MG
        self.ALPHA = (2 * DEPTH) ** 0.25
        self.stop = stop
        assert self.SPC == 4 and self.EPG == 8 and self.NT % MG == 0


class Sync:
    def __init__(self, nc, es, same_engine_wait=True):
        self.nc = nc
        self.es = es
        self.engs = {'pe': nc.tensor, 'act': nc.scalar, 'dve': nc.vector,
                     'pool': nc.gpsimd, 'sp': nc.sync}
        self.sems = {}
        self.cnt = {k: 0 for k in self.engs}
        self.waited = {k: {} for k in self.engs}
        self.res = {}
        self.dsem = {}
        self.same = same_engine_wait
        self.n_ins = 0

    def _sem(self, key):
        if key not in self.sems:
            nm = "s%d" % len(self.sems)
            self.sems[key] = self.es.enter_context(self.nc.semaphore(nm))
        return self.sems[key]

    def _wait(self, eng, deps):
        best = {}
        for (sk, v) in deps:
            if best.get(sk, 0) < v:
                best[sk] = v
        for sk, v in best.items():
            if sk[0] == 'E' and sk[1] == eng:
                if not self.same or eng == 'pe':
                    continue
            if self.waited[eng].get(sk, 0) >= v:
                continue
            self.engs[eng].wait_ge(self._sem(sk), v)
            self.n_ins += 1
            self.waited[eng][sk] = v

    def _deps(self, reads, writes):
        deps = []
        for k in reads:
            r = self.res.get(k)
            if r and r['w']:
                deps.append(r['w'])
        for k in writes:
            r = self.res.get(k)
            if r:
                if r['w']:
                    deps.append(r['w'])
                deps += r['r']
        return deps

    def _record(self, ev, reads, writes):
        for k in reads:
            self.res.setdefault(k, {'w': None, 'r': []})['r'].append(ev)
        for k in writes:
            self.res[k] = {'w': ev, 'r': []}

    def op(self, eng, fn, reads=(), writes=()):
        self._wait(eng, self._deps(reads, writes))
        ins = fn(self.engs[eng])
        self.cnt[eng] += 1
        ep, v = divmod(self.cnt[eng] - 1, EPOCH)
        sk = ('E', eng, ep)
        ins.then_inc(self._sem(sk), 1)
        self.n_ins += 1
        self._record((sk, v + 1), reads, writes)
        return ins

    def dma(self, eng, out, in_, reads=(), writes=(), key=None, fn=None):
        self._wait(eng, self._deps(reads, writes))
        if key is None:
            key = ('D', writes[0] if writes else reads[0])
        if fn is None:
            ins = self.engs[eng].dma_start(out=out, in_=in_)
        else:
            ins = fn(self.engs[eng])
        c = self.dsem.get(key, 0) + 16
        self.dsem[key] = c
        ins.then_inc(self._sem(key), 16)
        self.n_ins += 1
        self._record((key, c), reads, writes)
        return ins

    def wait_keys(self, eng, keys):
        deps = []
        for k in keys:
            r = self.res.get(k)
            if r:
                if r['w']:
                    deps.append(r['w'])
                deps += r['r']
        self._wait(eng, deps)

    def barrier(self):
        deps = []
        for r in self.res.values():
            if r['w']:
                deps.append(r['w'])
            deps += r['r']
        for eng in self.engs:
            self._wait(eng, deps)
        self.res = {}


class Prog:
    def __init__(self, cfg):
        self.cfg = cfg
        self.nc = bass.Bass("TRN2", target_bir_lowering=False)
        self.es = ExitStack()
        self.din = {}
        self.dout = {}

    def inp(self, name, shape, dt=F32):
        self.din[name] = self.nc.dram_tensor(name, list(shape), dt, kind="ExternalInput").ap()
        return self.din[name]

    def outp(self, name, shape, dt=F32):
        self.dout[name] = self.nc.dram_tensor(name, list(shape), dt, kind="ExternalOutput").ap()
        return self.dout[name]

    def sb(self, name, shape, dt=F32):
        return self.es.enter_context(self.nc.sbuf_tensor("sb_" + name, list(shape), dt))

    def mm(self, out, lhsT, rhs, start, stop, reads, w):
        self.S.op('pe', lambda e: e.matmul(out, lhsT=lhsT, rhs=rhs, start=start, stop=stop), reads=reads, writes=[w])

    def tr(self, out, in_, ident, reads, w):
        self.S.op('pe', lambda e: e.transpose(out=out, in_=in_, identity=ident), reads=reads, writes=[w])

    def act(self, out, in_, func, reads, w, bias=None, scale=None, eng='act'):
        kw = {}
        if bias is not None:
            kw['bias'] = bias
        if scale is not None:
            kw['scale'] = scale
        self.S.op('act', lambda e: e.activation(out=out, in_=in_, func=func, **kw), reads=reads, writes=[w])

    def tt(self, out, a, b, op, reads, w, eng='dve'):
        self.S.op(eng, lambda e: e.tensor_tensor(out=out, in0=a, in1=b, op=op), reads=reads, writes=[w])

    def ts(self, out, a, s1, s2, op0, op1, reads, w, eng='dve'):
        if op1 is None:
            self.S.op(eng, lambda e: e.tensor_scalar(out=out, in0=a, scalar1=s1, scalar2=None, op0=op0), reads=reads, writes=[w])
        else:
            self.S.op(eng, lambda e: e.tensor_scalar(out=out, in0=a, scalar1=s1, scalar2=s2, op0=op0, op1=op1), reads=reads, writes=[w])

    def stt(self, out, a, s, b, op0, op1, reads, w):
        self.S.op('dve', lambda e: e.scalar_tensor_tensor(out=out, in0=a, scalar=s, in1=b, op0=op0, op1=op1), reads=reads, writes=[w])

    def cp(self, out, in_, reads, w, eng='dve'):
        if eng == 'act':
            self.S.op('act', lambda e: e.copy(out=out, in_=in_), reads=reads, writes=[w])
        else:
            self.S.op(eng, lambda e: e.tensor_copy(out=out, in_=in_), reads=reads, writes=[w])

    def memset(self, ap, val, w, eng='dve'):
        self.S.op(eng, lambda e: e.memset(ap, val), writes=[w])

    def ld(self, out, in_, w, reads=(), eng='sp'):
        self.S.dma(eng, out, in_, reads=list(reads), writes=[w])

    def st(self, out, in_, r, okey):
        self.S.dma('sp', out, in_, reads=[r], writes=[okey])
        self.outkeys.append(okey)

    def bcast_row(self, dst, row_ap, n, dkey, tmpname):
        rt = self.rowtmp
        for c0 in range(0, n, 512):
            cw = min(512, n - c0)
            self.ld(rt[0:1, 0:cw], row_ap[:, c0:c0 + cw], 'rowtmp')
            self.mm(self.ps[0][:, 0:cw], self.ones32[0:1, :], rt[0:1, 0:cw], True, True, ['rowtmp', 'ones32'], 'ps0')
            self.cp(dst[:, c0:c0 + cw], self.ps[0][:, 0:cw], ['ps0'], dkey, eng='act')

    def layernorm(self, z, out, gB, bB, zkey, okey, gkey):
        st_, mv, rstd = self.ln_st, self.ln_mv, self.ln_rstd
        for i in range(2):
            self.S.op('dve', lambda e: e.bn_stats(out=st_[:, i, :], in_=z[:, i * 512:(i + 1) * 512]), reads=[zkey], writes=['ln_st'])
        self.S.op('dve', lambda e: e.bn_aggr(out=mv[:], in_=st_[:].rearrange("p a b -> p (a b)")), reads=['ln_st'], writes=['ln_mv'])
        self.act(rstd[:], mv[:, 1:2], AF.Ln, ['ln_mv', 'epsT'], 'ln_rstd', bias=self.epsT[:], scale=1.0)
        self.act(rstd[:], rstd[:], AF.Exp, ['ln_rstd'], 'ln_rstd', scale=-0.5)
        self.ts(z, z, mv[:, 0:1], rstd[:, 0:1], ALU.subtract, ALU.mult, [zkey, 'ln_mv', 'ln_rstd'], zkey)
        self.tt(z, z, gB, ALU.mult, [zkey, gkey], zkey)
        self.tt(out, z, bB, ALU.add, [zkey, gkey], okey)

    def build(self):
        cfg = self.cfg
        nc, es = self.nc, self.es
        NT, NPT, NGT, NSLOT, LS, SPC, NE, NG, EPG = cfg.NT, cfg.NPT, cfg.NGT, cfg.NSLOT, cfg.LS, cfg.SPC, cfg.NE, cfg.NG, cfg.EPG
        NBLK, NPG, NPHYS = cfg.NBLK, cfg.NPG, cfg.NPHYS
        self.outkeys = []
        xg = self.inp("xg", [NGT * 128, D])
        rope_d = self.inp("rope", [128, (NGT + 1) * 32])
        xs_d = self.inp("xs", [128, D])
        cvec_d = self.inp("cvec", [8, D])
        nmask_d = self.inp("nmask", [128, NPT * NSLOT])
        v01_d = self.inp("v01", [128, NPT * NSLOT])
        flag_d = self.inp("flag", [128, 1])
        consts_d = self.inp("consts", [128, 256])
        consts2_d = self.inp("consts2", [128, 512])
        ptA_d = self.inp("ptA", [128, 4], I32)
        ptB_d = self.inp("ptB", [32, 2 * NBLK], I32)
        sttok_d = self.inp("st_tok", [SPC, 30, D])
        stfm_d = self.inp("st_fm", [128, 8 * SPC * 30])
        wada_d = self.inp("w_ada", [2, D, 6 * D])
        bada_d = self.inp("b_ada", [2, 6 * D])
        badaT_d = self.inp("b_adaT", [128, 96])
        lng_d = self.inp("ln_g", [4, D])
        lnb_d = self.inp("ln_b", [4, D])
        wqkv_d = self.inp("w_qkv", [D, 1536])
        wo_d = self.inp("w_o", [D, D])
        win_d = self.inp("conv_w_in", [D, 2 * D])
        wdwT_d = self.inp("wdwT", [128, 8 * CW])
        clng_d = self.inp("conv_ln_g", [1, D])
        clnb_d = self.inp("conv_ln_b", [1, D])
        wout_d = self.inp("conv_w_out", [D, D])
        wr_d = self.inp("w_router", [D, NE])
        br_d = self.inp("b_router", [1, NE])
        wg_d = self.inp("w_gate", [2, NE, D, DFF])
        wu_d = self.inp("w_up", [2, NE, D, DFF])
        wd_d = self.inp("w_down", [2, NE, DFF, D])
        ck_d = self.inp("cache_k", [NPHYS, 2 * 128 * 128])
        cv_d = self.inp("cache_v", [NPHYS, 2 * 128 * 128])

        y_p = self.outp("y_p", [2 * LS * 128, D])
        y_s = self.outp("y_s", [SPC, D])
        k_p = self.outp("k_p", [2 * LS, 2, 128, 128])
        v_p = self.outp("v_p", [2 * LS, 2, 128, 128])
        conv_p = self.outp("conv_p", [30, D])
        k_s = self.outp("k_s", [SPC, 256])
        v_s = self.outp("v_s", [SPC, 256])
        conv_s = self.outp("conv_s", [SPC, 30, D])
        dbg = self.outp("dbg", [NT * 128, D]) if cfg.stop else None

        self.S = S = Sync(nc, es)
        sb = self.sb
        self.ps = [es.enter_context(nc.psum_tensor("ps%d" % i, [128, 512], F32)) for i in range(8)]
        ps = self.ps
        PK = ['ps%d' % i for i in range(8)]
        xres = sb("xres", [128, NT, D])
        XK = ['xres%d' % t for t in range(NT)]
        consts = sb("consts", [128, 256])
        ident = consts[:, 0:128]
        identb = sb("identb", [128, 128], BF16)
        trib = sb("trib", [128, 128], BF16)
        self.ones32 = sb("ones32", [128, 128])
        onesb = sb("onesb", [128, 1], BF16)
        self.rowtmp = sb("rowtmp", [1, 512])
        self.ln_st = sb("ln_st", [128, 2, 6])
        self.ln_mv = sb("ln_mv", [128, 2])
        self.ln_rstd = sb("ln_rstd", [128, 1])
        flag = sb("flag", [128, 1])
        scT = sb("scT", [128, 8, 8])
        modT = sb("modT", [128, 2, 8, 8])
        badaT = sb("badaT", [128, 96])
        gP = sb("gP", [128, D])
        gS = sb("gS", [128, D])
        lnG = sb("lnG", [128, D])
        lnB = sb("lnB", [128, D])
        ks32 = sb("ks32", [128, 2 * NGT])
        kvS = sb("kvS", [128, 512])
        SCR_BYTES = 108 * 1024
        scr = sb("scr", [128, SCR_BYTES // 4])

        def carve(off_bytes, shape, dt):
            n = int(np.prod(shape))
            esz = 4 if dt in (F32, I32, U32) else 2
            assert off_bytes % 4 == 0
            a = scr[:, off_bytes // 4: off_bytes // 4 + (n * esz + 3) // 4]
            if dt != F32:
                a = a.bitcast(dt)
            a = a[:, 0:n]
            if len(shape) == 2:
                pat = "p (a b) -> p a b"
                return a.rearrange(pat, a=shape[0]), off_bytes + n * esz
            if len(shape) == 3:
                return a.rearrange("p (a b c) -> p a b c", a=shape[0], b=shape[1]), off_bytes + n * esz
            if len(shape) == 1:
                return a, off_bytes + n * esz
            raise ValueError

        self.ld(consts[:], consts_d, 'consts')
        self.ld(flag[:], flag_d, 'flag')
        self.ld(badaT[:], badaT_d, 'badaT')
        self.cp(identb[:], consts[:, 0:128], ['consts'], 'identb')
        self.cp(trib[:], consts[:, 128:256], ['consts'], 'trib')
        self.memset(self.ones32[:], 1.0, 'ones32')
        self.memset(onesb[:], 1.0, 'onesb')
        self.epsT = sb("epsT", [128, 1])
        self.memset(self.epsT[:], LN_EPS, 'epsT')

        ctmp_, o_ = carve(0, [1, D], F32)
        ctmp2_, o_ = carve(o_, [1, D], F32)
        ctmp = ctmp_[0:8, 0, :]
        ctmp2 = ctmp2_[0:8, 0, :]
        self.ld(ctmp, cvec_d, 'ctmp')
        self.act(ctmp2, ctmp, AF.Silu, ['ctmp'], 'ctmp2')
        for kc in range(8):
            self.tr(ps[0][:, kc * 8:(kc + 1) * 8], ctmp2[:, kc * 128:(kc + 1) * 128], ident[0:8, 0:8], ['ctmp2', 'consts'], 'ps0')
        self.cp(scT[:].rearrange("p a b -> p (a b)"), ps[0][:, 0:64], ['ps0'], 'scT')
        S.barrier()

        def ada(hl):
            layer, part = hl // 2, hl % 2
            base = part * 3 * D
            S.barrier()
            off = 0
            wa = []
            for i in range(2):
                a, off = carve(off, [8, 512], F32)
                wa.append(a)
            bPl, off = carve(off, [8, 128], F32)
            bSl, off = carve(off, [8, 128], F32)
            for kc in range(8):
                self.ts(bPl[:, kc, :], self.ones32[:], scT[:, kc, 0:1], None, ALU.mult, None, ['ones32', 'scT'], 'bPl')
            self.memset(bSl[:].rearrange("p a b -> p (a b)"), 0.0, 'bSl')
            for kc in range(8):
                self.cp(bSl[:, kc, 0:SPC], scT[:, kc, 1:1 + SPC], ['scT', 'bSl'], 'bSl')
            wsrc = wada_d[layer].rearrange("(kc p) n -> p kc n", p=128)
            for cgi in range(6):
                c0 = base + cgi * 512
                wt = wa[cgi % 2]
                wk = 'wa%d' % (cgi % 2)
                self.ld(wt[:], wsrc[:, :, c0:c0 + 512], wk)
                v = cgi // 2
                if v < 2:
                    for mi in range(4):
                        mc = (cgi % 2) * 4 + mi
                        for kc in range(8):
                            self.mm(ps[1][:, (v * 8 + mc) * 8:(v * 8 + mc) * 8 + 8], wt[:, kc, mi * 128:(mi + 1) * 128], scT[:, kc, :],
                                    kc == 0, kc == 7, [wk, 'scT'], 'ps1')
                else:
                    half = cgi % 2
                    for (lt, lk, pb) in ((bPl, 'bPl', 2), (bSl, 'bSl', 3)):
                        for kc in range(8):
                            self.mm(ps[pb][:, :], lt[:, kc, :], wt[:, kc, :], kc == 0, False, [wk, lk], PK[pb])
                        self.ld(self.rowtmp[0:1, 0:512], bada_d[layer:layer + 1, c0:c0 + 512], 'rowtmp')
                        self.mm(ps[pb][:, :], self.ones32[0:1, :], self.rowtmp[0:1, 0:512], False, True, ['rowtmp', 'ones32'], PK[pb])
                        dst = gP if pb == 2 else gS
                        self.cp(dst[:, half * 512:(half + 1) * 512], ps[pb][:, :], [PK[pb]], 'gP' if pb == 2 else 'gS', eng='act')
            bb = badaT[:, layer * 48 + part * 24: layer * 48 + part * 24 + 16]
            self.tt(modT[:].rearrange("p v c r -> p (v c) r"), ps[1][:, 0:128].rearrange("p (a r) -> p a r", r=8),
                    bb.unsqueeze(2).broadcast_to([128, 16, 8]), ALU.add, ['ps1', 'badaT'], 'modT')
            self.ts(modT[:, 1, :, :], modT[:, 1, :, :], 1.0, None, ALU.add, None, ['modT'], 'modT')
            self.bcast_row(lnG, lng_d[hl:hl + 1, :], D, 'lnG', 'lnG')
            self.bcast_row(lnB, lnb_d[hl:hl + 1, :], D, 'lnB', 'lnB')
            S.barrier()

        def tile_to_hT(src, skey, hdst, col0, sample=False, xT32=None):
            for hf in range(2):
                pb = 4 + hf
                for q in range(4):
                    kc = hf * 4 + q
                    self.tr(ps[pb][:, q * 128:(q + 1) * 128], src[:, kc * 128:(kc + 1) * 128], ident, [skey, 'consts'], PK[pb])
                for q in range(4):
                    kc = hf * 4 + q
                    if not sample:
                        self.act(hdst[:, kc, col0:col0 + 128], ps[pb][:, q * 128:(q + 1) * 128], AF.Identity,
                                 [PK[pb], 'modT'], 'hTg', bias=modT[:, 0, kc, 0:1], scale=modT[:, 1, kc, 0:1])
                        if xT32 is not None:
                            self.cp(xT32[:, kc, :], ps[pb][:, q * 128:(q + 1) * 128], [PK[pb]], 'xT32', eng='act')
                    else:
                        tmp = self.smod
                        self.tt(tmp[:, 0:SPC], ps[pb][:, q * 128:q * 128 + SPC], modT[:, 1, kc, 1:1 + SPC], ALU.mult, [PK[pb], 'modT'], 'smod')
                        self.tt(tmp[:, 0:SPC], tmp[:, 0:SPC], modT[:, 0, kc, 1:1 + SPC], ALU.add, ['smod', 'modT'], 'smod')
                        self.cp(hdst[:, kc, col0:col0 + 128], tmp[:], ['smod'], 'hTg')
                        if xT32 is not None:
                            self.cp(xT32[:, kc, :], tmp[:], ['smod'], 'xT32')

        self.smod = sb("smod", [128, 128])
        self.memset(self.smod[:], 0.0, 'smod')

        rt_ = sb("rot_tmp", [128, 4, 8, 16])

        def rotary(xv, nh, cs, xkey, rkey='rope'):
            cosb = cs[:, 0:16].unsqueeze(1).broadcast_to([128, nh, 16])
            sinb = cs[:, 16:32].unsqueeze(1).broadcast_to([128, nh, 16])
            x1 = xv[:, :, 0:16]
            x2 = xv[:, :, 16:32]
            self.tt(rt_[:, 0, 0:nh, :], x1, cosb, ALU.mult, [xkey, rkey], 'rt0')
            self.tt(rt_[:, 1, 0:nh, :], x2, sinb, ALU.mult, [xkey, rkey], 'rt1')
            self.tt(rt_[:, 2, 0:nh, :], x2, cosb, ALU.mult, [xkey, rkey], 'rt2')
            self.tt(rt_[:, 3, 0:nh, :], x1, sinb, ALU.mult, [xkey, rkey], 'rt3')
            self.tt(x1, rt_[:, 0, 0:nh, :], rt_[:, 1, 0:nh, :], ALU.subtract, ['rt0', 'rt1'], xkey)
            self.tt(x2, rt_[:, 2, 0:nh, :], rt_[:, 3, 0:nh, :], ALU.add, ['rt2', 'rt3'], xkey)

        ada(0)
        off = 0
        KT, off = carve(off, [2, NGT * 128], BF16)
        VS, off = carve(off, [NGT, 2, 130], BF16)
        kmT, off = carve(off, [2, NSLOT], BF16)
        off = (off + 3) // 4 * 4
        offC = off
        wkv, off = carve(off, [8, 512], BF16)
        ropes, off = carve(off, [NGT + 1, 32], F32)
        xg0, off = carve(off, [1, D], F32)
        xg1, off = carve(off, [1, D], F32)
        hT1, off = carve(off, [8, 128], BF16)
        kvf, off = carve(off, [1, 512], F32)
        kb16, off = carve(off, [1, 256], BF16)
        kvf1, off = carve(off, [1, 512], F32)
        kb161, off = carve(off, [1, 256], BF16)
        assert off <= SCR_BYTES, off
        xgt = [xg0[:, 0, :], xg1[:, 0, :]]
        kvf = kvf[:, 0, :]
        kb16 = kb16[:, 0, :]
        kvf1 = kvf1[:, 0, :]
        kb161 = kb161[:, 0, :]
        self.hT1 = hT1

        self.ld(ropes[:].rearrange("p t c -> p (t c)"), rope_d, 'rope')
        self.S.dma('pool', wkv[:], wqkv_d.rearrange("(kc p) n -> p kc n", p=128)[:, :, 1024:1536], writes=['wkv'])
        self.memset(VS[:, :, :, 128:130], 1.0, 'VS')

        kvf_b = [kvf, kvf1]
        kb16_b = [kb16, kb161]

        def kv_part1(src, skey, rope_idx, g):
            par = 0 if g is None else g % 2
            pso = ps[0] if par == 0 else ps[2]
            pk = 'ps0' if par == 0 else 'ps2'
            tile_to_hT(src, skey, hT1, 0, sample=(g is None))
            for kc in range(8):
                self.mm(pso[:, :], hT1[:, kc, 0:128], wkv[:, kc, :], kc == 0, kc == 7, ['hTg', 'wkv'], pk)
            dstf = kvf_b[par] if g is not None else kvS
            dkey = ('kvf%d' % par) if g is not None else 'kvS'
            self.cp(dstf[:], pso[:, :], [pk], dkey)
            rotary(dstf[:, 0:256].rearrange("p (h d) -> p h d", h=2), 2, ropes[:, rope_idx, :], dkey)
            if g is not None:
                self.cp(VS[:, g, :, 0:128], dstf[:, 256:512].rearrange("p (h d) -> p h d", h=2), [dkey], 'VS', eng='act')
                self.cp(kb16_b[par][:], dstf[:, 0:256], [dkey], 'kb16%d' % par, eng='act')
            else:
                self.st(k_s[:, :], kvS[0:SPC, 0:256], 'kvS', 'k_s')
                self.st(v_s[:, :], kvS[0:SPC, 256:512], 'kvS', 'v_s')

        def kv_part2(g):
            par = g % 2
            kf_, kb_ = kvf_b[par], kb16_b[par]
            fk, bk = 'kvf%d' % par, 'kb16%d' % par
            pt = ps[1][:].bitcast(BF16)
            for kv in range(2):
                self.tr(pt[:, kv * 128:(kv + 1) * 128], kb_[:, kv * 128:(kv + 1) * 128], identb[:], [bk, 'identb'], 'ps1')
            self.cp(KT[:, :, g * 128:(g + 1) * 128], pt[:, 0:256].rearrange("p (h t) -> p h t", h=2), ['ps1'], 'KT')
            for kv in range(2):
                self.mm(ps[7][:, kv * NGT + g: kv * NGT + g + 1], kf_[:, kv * 128:(kv + 1) * 128], self.ones32[:, 0:1],
                        True, True, [fk, 'ones32'], 'ps7')
            if 2 <= g < 2 + 2 * LS:
                pg = g - 2
                self.st(k_p[pg].rearrange("h r d -> r h d"), kf_[:, 0:256].rearrange("p (h d) -> p h d", h=2), fk, 'k_p')
                self.st(v_p[pg].rearrange("h r d -> r h d"), kf_[:, 256:512].rearrange("p (h d) -> p h d", h=2), fk, 'v_p')

        for g in range(NGT + 1):
            if g < NGT:
                src, skey = xgt[g % 2], 'xgt%d' % (g % 2)
                self.ld(src, xg[g * 128:(g + 1) * 128, :], skey)
                kv_part1(src, skey, g, g)
            if g >= 1:
                kv_part2(g - 1)
        tS = NT - 1
        self.ld(xres[:, tS, :], xs_d, XK[tS])
        kv_part1(xres[:, tS, :], XK[tS], NGT, None)

        self.cp(ks32[:], ps[7][:, 0:2 * NGT], ['ps7'], 'ks32')
        ksv = ks32[:].rearrange("p (k s two) -> p k s two", k=2, two=2)
        self.tt(ksv[:, :, :, 0], ksv[:, :, :, 0], ksv[:, :, :, 1], ALU.add, ['ks32'], 'ks32')
        self.ts(kmT[:], ksv[:, :, :, 0], 1.0 / 256.0, None, ALU.mult, None, ['ks32'], 'kmT')
        if cfg.stop == 'proj':
            self.finish(dbg, xres, XK)
            return

        S.barrier()
        off = offC
        wqb = []
        for i in range(2):
            a_, off = carve(off, [8, 512], BF16)
            wqb.append(a_)
        ropel, off = carve(off, [NT, 32], F32)
        v01, off = carve(off, [NPT, NSLOT], F32)
        QT1, off = carve(off, [8, 128], BF16)
        hT1, off = carve(off, [8, 128], BF16)
        acc, off = carve(off, [8, 130], F32)
        PT = []
        for i in range(4):
            a_, off = carve(off, [4, 128], BF16)
            PT.append(a_)
        aT = hT1
        qb16, off = carve(off, [8, 128], BF16)
        zq, off = carve(off, [8, 128], F32)
        gsb, off = carve(off, [8, NSLOT], F32)
        sel, off = carve(off, [8, NSLOT], F32)
        t8, off = carve(off, [8, 8], F32)
        nm, off = carve(off, [1, NSLOT], F32)
        rden, off = carve(off, [1, 8], F32)
        assert off <= SCR_BYTES, off
        zq2 = zq[:].rearrange("p h d -> p (h d)")
        qb2 = qb16[:].rearrange("p h d -> p (h d)")

        self.ld(ropel[:, 0:NPT, :].rearrange("p t c -> p (t c)"), rope_d[:, 32:(NPT + 1) * 32], 'ropel')
        self.ld(ropel[:, NPT, :], rope_d[:, NGT * 32:(NGT + 1) * 32], 'ropel')
        self.ld(v01[:].rearrange("p a b -> p (a b)"), v01_d, 'v01')
        wq_src = wqkv_d.rearrange("(kc p) n -> p kc n", p=128)
        wo_src = wo_d.rearrange("(kc p) n -> p kc n", p=128)

        def q_proj(tau, sample):
            tile_to_hT(xres[:, tau, :], XK[tau], hT1, 0, sample=sample)
            for hf in range(2):
                self.S.dma('pool', wqb[hf][:], wq_src[:, :, hf * 512:(hf + 1) * 512], writes=['wqb%d' % hf])
                for kc in range(8):
                    self.mm(ps[2 + hf][:, :], hT1[:, kc, :], wqb[hf][:, kc, :], kc == 0, kc == 7, ['hTg', 'wqb%d' % hf], PK[2 + hf])
                self.cp(zq2[:, hf * 512:(hf + 1) * 512], ps[2 + hf][:, :], [PK[2 + hf]], 'zq', eng='act')
            rotary(zq[:], 8, ropel[:, tau, :], 'zq', 'ropel')

        def out_proj(tau, gate_tile, gkey):
            pt = ps[6][:].bitcast(BF16)
            for h in range(8):
                self.tr(pt[:, h * 128:(h + 1) * 128], qb16[:, h, :], identb[:], ['qb16', 'identb'], 'ps6')
            self.cp(aT[:].rearrange("p h t -> p (h t)"), pt[:, :], ['ps6'], 'hTg')
            for hf in range(2):
                self.S.dma('pool', wqb[hf][:], wo_src[:, :, hf * 512:(hf + 1) * 512], writes=['wqb%d' % hf])
                for h in range(8):
                    self.mm(ps[6 + hf][:, :], aT[:, h, :], wqb[hf][:, h, :], h == 0, h == 7, ['hTg', 'wqb%d' % hf], PK[6 + hf])
                self.tt(zq2[:, hf * 512:(hf + 1) * 512], ps[6 + hf][:, :], gate_tile[:, hf * 512:(hf + 1) * 512], ALU.mult, [PK[6 + hf], gkey], 'zq')
            self.stt(zq2, xres[:, tau, :], cfg.ALPHA, zq2, ALU.mult, ALU.add, [XK[tau], 'zq'], 'zq')
            self.layernorm(zq2, xres[:, tau, :], lnG[:], lnB[:], 'zq', XK[tau], 'lnG')

        for tau in range(NPT):
            self.ld(xres[:, tau, :], xg[(tau + 1) * 128:(tau + 2) * 128, :], XK[tau])
            q_proj(tau, False)
            self.cp(qb2, zq2, ['zq'], 'qb16', eng='act')
            pt = ps[6][:].bitcast(BF16)
            for h in range(8):
                self.tr(pt[:, h * 128:(h + 1) * 128], qb16[:, h, :], identb[:], ['qb16', 'identb'], 'ps6')
            self.cp(QT1[:].rearrange("p h t -> p (h t)"), pt[:, :], ['ps6'], 'QT1')
            for h in range(8):
                self.mm(ps[4][:, h * NSLOT:(h + 1) * NSLOT], QT1[:, h, :], kmT[:, h // 4, :], True, True, ['QT1', 'kmT'], 'ps4')
            self.ts(nm[:, 0, :], v01[:, tau, :], 1.0, BIG, ALU.subtract, ALU.mult, ['v01'], 'nm')
            v01b = v01[:, tau, :].unsqueeze(1).broadcast_to([128, 8, NSLOT])
            self.tt(gsb[:], ps[4][:, 0:8 * NSLOT].rearrange("p (h s) -> p h s", h=8), v01b, ALU.mult, ['ps4', 'v01'], 'gsb')
            self.tt(gsb[:], gsb[:], nm[:, 0, :].unsqueeze(1).broadcast_to([128, 8, NSLOT]), ALU.add, ['gsb', 'nm'], 'gsb')
            for h in range(8):
                self.S.op('dve', lambda e: e.max(out=t8[:, h, :], in_=gsb[:, h, :]), reads=['gsb'], writes=['t8'])
            self.tt(sel[:], gsb[:], t8[:, :, 2:3].broadcast_to([128, 8, NSLOT]), ALU.is_ge, ['gsb', 't8'], 'sel')
            self.tt(sel[:], sel[:], v01b, ALU.mult, ['sel', 'v01'], 'sel')
            for h in range(8):
                self.memset(acc[:, h, :], 0.0, 'acc%d' % h)
            m_own = (tau + 1) // 2
            second = (tau + 1) % 2
            its = [(kv, s_) for kv in range(2) for s_ in range(NSLOT)]

            def kts_of(s_):
                if s_ == m_own and not second:
                    return [0]
                return [0, 1]

            def qk_exp(n):
                kv, s_ = its[n]
                own = (s_ == m_own)
                qrhs = QT1[:, kv * 4:(kv + 1) * 4, :].rearrange("p h q -> p (h q)")
                for kt in kts_of(s_):
                    g = 2 * s_ + kt
                    pi = (n % 2) * 2 + kt
                    self.mm(ps[kt][:, :], KT[:, kv, g * 128:(g + 1) * 128], qrhs, True, True, ['KT', 'QT1'], PK[kt])
                    self.act(PT[pi][:].rearrange("p h q -> p (h q)"), ps[kt][:, :], AF.Exp, [PK[kt]], 'PT%d' % pi, scale=ATTN_SCALE)
                    if own and kt == (1 if second else 0):
                        self.tt(PT[pi][:], PT[pi][:], trib[:].unsqueeze(1).broadcast_to([128, 4, 128]), ALU.mult, ['PT%d' % pi, 'trib'], 'PT%d' % pi)

            def pv_acc(n):
                kv, s_ = its[n]
                own = (s_ == m_own)
                kts = kts_of(s_)
                ob = 2 + 2 * (n % 2)
                for hh in range(4):
                    bank = ps[ob + hh // 2]
                    col = (hh % 2) * 256
                    for i, kt in enumerate(kts):
                        pi = (n % 2) * 2 + kt
                        self.mm(bank[:, col:col + 129], PT[pi][:, hh, :], VS[:, 2 * s_ + kt, kv, 0:129], i == 0, i == len(kts) - 1,
                                ['PT%d' % pi, 'VS'], PK[ob + hh // 2])
                for hh in range(4):
                    h = kv * 4 + hh
                    bank = ps[ob + hh // 2]
                    col = (hh % 2) * 256
                    if own:
                        self.tt(acc[:, h, 0:129], acc[:, h, 0:129], bank[:, col:col + 129], ALU.add, ['acc%d' % h, PK[ob + hh // 2]], 'acc%d' % h)
                    else:
                        self.stt(acc[:, h, 0:129], bank[:, col:col + 129], sel[:, h, s_:s_ + 1], acc[:, h, 0:129], ALU.mult, ALU.add,
                                 ['acc%d' % h, 'sel', PK[ob + hh // 2]], 'acc%d' % h)

            for n in range(len(its) + 1):
                if n < len(its):
                    qk_exp(n)
                if n >= 1:
                    pv_acc(n - 1)
            AK = ['acc%d' % h for h in range(8)]
            self.S.op('dve', lambda e: e.reciprocal(out=rden[:, 0, :], in_=acc[:, :, 128]), reads=AK, writes=['rden'])
            self.tt(qb16[:], acc[:, :, 0:128], rden[:, 0, :].unsqueeze(2).broadcast_to([128, 8, 128]), ALU.mult, AK + ['rden'], 'qb16')
            out_proj(tau, gP, 'gP')

        tS = NT - 1
        q_proj(tS, True)
        for h in range(8):
            self.tr(ps[0][:, h * 4:(h + 1) * 4], zq[0:SPC, h, :], ident[0:SPC, 0:SPC], ['zq', 'consts'], 'ps0')
        S.barrier()
        off = 0
        c2, off = carve(off, [1, 512], F32)
        QTa, off = carve(off, [1, 32], F32)
        QTm, off = carve(off, [8, 32], F32)
        qsel, off = carve(off, [1, 128], F32)
        ptAi, off = carve(off, [1, 4], I32)
        ptBi, off = carve(off, [1, 2 * NBLK], I32)
        ptAf, off = carve(off, [1, 4], F32)
        idxAf, off = carve(off, [4, 8], F32)
        idxA, off = carve(off, [4, 8], I32)
        idxSf, off = carve(off, [6, 4], F32)
        idxS, off = carve(off, [6, 4], I32)
        ptBf, off = carve(off, [1, 2 * NBLK], F32)
        ksum0, off = carve(off, [2, 128], F32)
        ksum1, off = carve(off, [2, 128], F32)
        ksr, off = carve(off, [1, 128], F32)
        kmTs, off = carve(off, [4, 128], F32)
        gate_s, off = carve(off, [1, NBLK], F32)
        t8s, off = carve(off, [1, 8], F32)
        oh, off = carve(off, [1, NBLK], F32)
        ohp, off = carve(off, [1, NBLK], F32)
        phys, off = carve(off, [1, 8], F32)
        idxf, off = carve(off, [1, 8], F32)
        idxi, off = carve(off, [1, 8], I32)
        ksel, off = carve(off, [1, 128], F32)
        vsel, off = carve(off, [1, 128], F32)
        sc, off = carve(off, [1, 6 * 128 + 8], F32)
        Pm, off = carve(off, [1, 6 * 128 + 8], F32)
        den, off = carve(off, [1, 1], F32)
        pv, off = carve(off, [24, 128], F32)
        osum, off = carve(off, [1, 128], F32)
        qb16, off = carve(off, [8, 128], BF16)
        aT, off = carve(off, [8, 128], BF16)
        zq, off = carve(off, [8, 128], F32)
        wqb = []
        for i in range(2):
            a_, off = carve(off, [8, 512], BF16)
            wqb.append(a_)
        G0, off = carve(off, [1, 32 * 128], F32)
        G1, off = carve(off, [1, 32 * 128], F32)
        assert off <= SCR_BYTES, off
        GB = [G0[:, 0, :], G1[:, 0, :]]
        zq2 = zq[:].rearrange("p h d -> p (h d)")
        c2 = c2[:, 0, :]
        ksum = [ksum0, ksum1]
        self.cp(QTa[:, 0, :], ps[0][:, 0:32], ['ps0'], 'QTa')
        self.ld(c2, consts2_d, 'c2')
        self.ld(ptAi[:, 0, :], ptA_d, 'ptAi')
        self.ld(ptBi[0:32, 0, :], ptB_d, 'ptBi')
        self.cp(ptBf[0:32, 0, :], ptBi[0:32, 0, :], ['ptBi'], 'ptBf')
        self.cp(ptAf[:, 0, :], ptAi[:, 0, :], ['ptAi'], 'ptAf')
        self.ts(idxAf[:], ptAf[:, 0, :].unsqueeze(2).broadcast_to([128, 4, 8]), 8.0, None, ALU.mult, None, ['ptAf'], 'idxAf')
        self.tt(idxAf[:], idxAf[:], c2[:, 353:361].unsqueeze(1).broadcast_to([128, 4, 8]), ALU.add, ['idxAf', 'c2'], 'idxAf')
        self.cp(idxA[:], idxAf[:], ['idxAf'], 'idxA')
        ck8 = ck_d.rearrange("n (c x) -> (n c) x", x=4096)
        cv8 = cv_d.rearrange("n (c x) -> (n c) x", x=4096)
        self.tt(QTm[:], QTa[:, 0, :].unsqueeze(1).broadcast_to([128, 8, 32]), c2[:, 0:256].rearrange("p (a b) -> p a b", a=8), ALU.mult, ['QTa', 'c2'], 'QTm')
        self.tr(ps[1][0:32, 0:128], QTa[:, 0, :], ident, ['QTa', 'consts'], 'ps1')
        self.cp(qsel[0:32, 0, :], ps[1][0:32, 0:128], ['ps1'], 'qsel')
        RC = 32
        gi = 0
        for r in range(2):
            for kv in range(2):
                first = True
                for eo in range(2):
                    for rc in range(128 // RC):
                        gb = GB[gi % 2][:, 0:RC * 128]
                        gk = 'G%d' % (gi % 2)
                        self.S.dma('pool', None, None, reads=['idxA'], writes=[gk],
                                   fn=lambda e: e.indirect_dma_start(out=gb, out_offset=None, in_=ck8,
                                                                     in_offset=bass.IndirectOffsetOnAxis(ap=idxA[:, r * 2 + eo, kv * 4 + rc:kv * 4 + rc + 1], axis=0)))
                        dst = ksum[r][:, kv, :] if first else ksr[:, 0, :]
                        dk = 'ksum%d' % r if first else 'ksr'
                        self.S.op('dve', lambda e: e.tensor_reduce(out=dst, in_=gb.rearrange("p (r d) -> p d r", d=128), axis=AX.X, op=ALU.add),
                                  reads=[gk], writes=[dk])
                        if not first:
                            self.tt(ksum[r][:, kv, :], ksum[r][:, kv, :], ksr[:, 0, :], ALU.add, ['ksum%d' % r, 'ksr'], 'ksum%d' % r)
                        first = False
                        gi += 1
        for r in range(2):
            for kv in range(2):
                self.tr(ps[2][:, (kv * 2 + r) * 128:(kv * 2 + r + 1) * 128], ksum[r][:, kv, :], ident, ['ksum%d' % r, 'consts'], 'ps2')
        self.ts(kmTs[:].rearrange("p a b -> p (a b)"), ps[2][:, :], 1.0 / 256.0, None, ALU.mult, None, ['ps2'], 'kmTs')
        n = 0
        for s_ in range(SPC):
            for kv in range(2):
                self.mm(ps[3][0:32, 0:NBLK], QTm[:, s_ * 2 + kv, :], kmTs[:, kv * 2 + s_ // 2, (s_ % 2) * NBLK:(s_ % 2 + 1) * NBLK],
                        n == 0, n == 2 * SPC - 1, ['QTm', 'kmTs'], 'ps3')
                n += 1
        self.cp(gate_s[0:32, 0, :], ps[3][0:32, 0:NBLK], ['ps3'], 'gate_s')
        self.S.op('dve', lambda e: e.max(out=t8s[0:32, 0, :], in_=gate_s[0:32, 0, :]), reads=['gate_s'], writes=['t8s'])
        for j in range(3):
            self.ts(oh[0:32, 0, :], gate_s[0:32, 0, :], t8s[0:32, 0, j:j + 1], None, ALU.is_equal, None, ['gate_s', 't8s'], 'oh')
            for eo in range(2):
                self.tt(ohp[0:32, 0, :], oh[0:32, 0, :], ptBf[0:32, 0, eo * NBLK:(eo + 1) * NBLK], ALU.mult, ['oh', 'ptBf'], 'ohp')
                self.S.op('dve', lambda e: e.tensor_reduce(out=phys[0:32, 0, j * 2 + eo:j * 2 + eo + 1], in_=ohp[0:32, 0, :], axis=AX.X, op=ALU.add),
                          reads=['ohp'], writes=['phys'])
        self.ts(idxf[0:32, 0, 0:6], phys[0:32, 0, 0:6], 2.0, c2[0:32, 256:257], ALU.mult, ALU.add, ['phys', 'c2'], 'idxf')
        self.ts(idxSf[0:32], idxf[0:32, 0, 0:6].unsqueeze(2).broadcast_to([32, 6, 4]), 4.0, None, ALU.mult, None, ['idxf'], 'idxSf')
        self.tt(idxSf[0:32], idxSf[0:32], c2[0:32, 353:357].unsqueeze(1).broadcast_to([32, 6, 4]), ALU.add, ['idxSf', 'c2'], 'idxSf')
        self.cp(idxS[0:32], idxSf[0:32], ['idxSf'], 'idxi')
        for (dst, dk, c0) in ((ksel, 'ksel', 0), (vsel, 'vsel', 256)):
            self.mm(ps[4][0:32, 0:128], c2[0:SPC, 257:289], kvS[0:SPC, c0:c0 + 128], True, False, ['c2', 'kvS'], 'ps4')
            self.mm(ps[4][0:32, 0:128], c2[0:SPC, 289:321], kvS[0:SPC, c0 + 128:c0 + 256], False, True, ['c2', 'kvS'], 'ps4')
            self.cp(dst[0:32, 0, :], ps[4][0:32, 0:128], ['ps4'], dk)
        ck2 = ck_d.rearrange("n (h x) -> (n h) x", h=2)
        cv2 = cv_d.rearrange("n (h x) -> (n h) x", h=2)
        qb_ = qsel[0:32, 0, :].unsqueeze(1).broadcast_to([32, 32, 128])
        for c in range(6):
            for rh in range(4):
                gb = GB[gi % 2][0:32, :]
                gk = 'G%d' % (gi % 2)
                self.S.dma('pool', None, None, reads=['idxi'], writes=[gk],
                           fn=lambda e: e.indirect_dma_start(out=gb, out_offset=None, in_=ck8,
                                                             in_offset=bass.IndirectOffsetOnAxis(ap=idxS[0:32, c, rh:rh + 1], axis=0)))
                g3 = gb.rearrange("p (r d) -> p r d", d=128)
                self.tt(g3, g3, qb_, ALU.mult, [gk, 'qsel'], gk)
                self.S.op('dve', lambda e: e.tensor_reduce(out=sc[0:32, 0, c * 128 + rh * 32:c * 128 + rh * 32 + 32], in_=g3, axis=AX.X, op=ALU.add),
                          reads=[gk], writes=['sc'])
                gi += 1
        self.tt(osum[0:32, 0, :], qsel[0:32, 0, :], ksel[0:32, 0, :], ALU.mult, ['qsel', 'ksel'], 'osum')
        self.S.op('dve', lambda e: e.tensor_reduce(out=sc[0:32, 0, 768:769], in_=osum[0:32, 0, :], axis=AX.X, op=ALU.add), reads=['osum'], writes=['sc'])
        self.S.op('act', lambda e: e.activation(out=Pm[0:32, 0, 0:769], in_=sc[0:32, 0, 0:769], func=AF.Exp, scale=ATTN_SCALE),
                  reads=['sc'], writes=['Pm'])
        self.S.op('dve', lambda e: e.tensor_reduce(out=den[0:32, 0, :], in_=Pm[0:32, 0, 0:769], axis=AX.X, op=ALU.add), reads=['Pm'], writes=['den'])
        for c in range(6):
            for rh in range(4):
                gb = GB[gi % 2][0:32, :]
                gk = 'G%d' % (gi % 2)
                self.S.dma('pool', None, None, reads=['idxi'], writes=[gk],
                           fn=lambda e: e.indirect_dma_start(out=gb, out_offset=None, in_=cv8,
                                                             in_offset=bass.IndirectOffsetOnAxis(ap=idxS[0:32, c, rh:rh + 1], axis=0)))
                g3 = gb.rearrange("p (r d) -> p r d", d=128)
                pb_ = Pm[0:32, 0, c * 128 + rh * 32:c * 128 + rh * 32 + 32].unsqueeze(2).broadcast_to([32, 32, 128])
                self.tt(g3, g3, pb_, ALU.mult, [gk, 'Pm'], gk)
                self.S.op('dve', lambda e: e.tensor_reduce(out=pv[0:32, c * 4 + rh, :], in_=gb.rearrange("p (r d) -> p d r", d=128), axis=AX.X, op=ALU.add),
                          reads=[gk], writes=['pv'])
                gi += 1
        self.S.op('dve', lambda e: e.tensor_reduce(out=osum[0:32, 0, :], in_=pv[0:32, :, :].rearrange("p i d -> p d i"), axis=AX.X, op=ALU.add),
                  reads=['pv'], writes=['osum'])
        self.stt(osum[0:32, 0, :], vsel[0:32, 0, :], Pm[0:32, 0, 768:769], osum[0:32, 0, :], ALU.mult, ALU.add, ['vsel', 'Pm', 'osum'], 'osum')
        self.S.op('dve', lambda e: e.reciprocal(out=den[0:32, 0, :], in_=den[0:32, 0, :]), reads=['den'], writes=['den'])
        self.ts(osum[0:32, 0, :], osum[0:32, 0, :], den[0:32, 0, 0:1], None, ALU.mult, None, ['osum', 'den'], 'osum')
        for h in range(8):
            self.mm(ps[2 + h // 4][0:SPC, (h % 4) * 128:(h % 4 + 1) * 128], c2[0:32, 321 + h * 4:321 + h * 4 + 4], osum[0:32, 0, :], True, True,
                    ['c2', 'osum'], PK[2 + h // 4])
        self.memset(qb16[:].rearrange("p h d -> p (h d)"), 0.0, 'qb16')
        for hf in range(2):
            self.cp(qb16[0:SPC, hf * 4:(hf + 1) * 4, :].rearrange("p h d -> p (h d)"), ps[2 + hf][0:SPC, :], [PK[2 + hf], 'qb16'], 'qb16')
        out_proj(tS, gS, 'gS')

        if cfg.stop == 'attn':
            self.finish(dbg, xres, XK)
            return

        TG = NT // cfg.MG

        def moe(layer):
            ada(2 * layer + 1)
            if cfg.stop == 'ada1':
                return
            off = 0
            wr32, off = carve(off, [8, NE], F32)
            wrm, off = carve(off, [8, NE], F32)
            rb, off = carve(off, [1, NE], F32)
            brB, off = carve(off, [1, NE], F32)
            Wt, off = carve(off, [NT, NE], F32)
            hTm, off = carve(off, [8, TG * 128], BF16)
            xT32, off = carve(off, [8, 128], F32)
            accm, off = carve(off, [TG, D], F32)
            wg2, wu2, wd2, AT, sg, Ab = [], [], [], [], [], []
            for i in range(2):
                a_, off = carve(off, [8, 512], BF16); wg2.append(a_)
                a_, off = carve(off, [8, 512], BF16); wu2.append(a_)
                a_, off = carve(off, [4, D], BF16); wd2.append(a_)
                a_, off = carve(off, [4, 128], BF16); AT.append(a_)
                a_, off = carve(off, [1, 512], F32); sg.append(a_)
                a_, off = carve(off, [1, 512], BF16); Ab.append(a_)
            sc_, off = carve(off, [1, NE], F32)
            bi, off = carve(off, [NG, 8], F32)
            msk, off = carve(off, [NG, 8], F32)
            t8g, off = carve(off, [NG, 8], F32)
            gs, off = carve(off, [1, NG], F32)
            gmax, off = carve(off, [1, 1], F32)
            ohg, off = carve(off, [1, NG], F32)
            t8e, off = carve(off, [1, 8], F32)
            sel2, off = carve(off, [1, NE], F32)
            wun, off = carve(off, [1, NE], F32)
            dsum, off = carve(off, [1, 1], F32)
            zq, off = carve(off, [1, D], F32)
            assert off <= SCR_BYTES, off
            zq2 = zq[:, 0, :]
            bi2 = bi[:].rearrange("p g e -> p (g e)")
            msk2 = msk[:].rearrange("p g e -> p (g e)")

            self.ld(wr32[:], wr_d.rearrange("(kc p) e -> p kc e", p=128), 'wr32')
            for kc in range(8):
                self.ts(wrm[:, kc, :], wr32[:, kc, :], modT[:, 1, kc, 0:1], None, ALU.mult, None, ['wr32', 'modT'], 'wrm')
                self.mm(ps[0][0:1, 0:NE], modT[:, 0, kc, 0:1], wr32[:, kc, :], kc == 0, kc == 7, ['modT', 'wr32'], 'ps0')
            self.cp(rb[0:1, 0, :], ps[0][0:1, 0:NE], ['ps0'], 'rb')
            self.bcast_row(brB[:, 0, :], br_d, NE, 'brB', 'brB')
            if cfg.stop == 'rsetup':
                return

            for G in range(cfg.MG):
                tiles = list(range(G * TG, (G + 1) * TG))
                if layer == 1 and G == 0:
                    tiles = tiles[1:]
                for ti, tau in enumerate(tiles):
                    smp = (tau == NT - 1)
                    tile_to_hT(xres[:, tau, :], XK[tau], hTm, ti * 128, sample=smp, xT32=xT32)
                    if cfg.stop == 'r_0':
                        return
                    wsel, wk = (wr32, 'wr32') if smp else (wrm, 'wrm')
                    for kc in range(8):
                        self.mm(ps[6][:, 0:NE], xT32[:, kc, :], wsel[:, kc, :], kc == 0, smp and kc == 7, ['xT32', wk], 'ps6')
                    if not smp:
                        self.mm(ps[6][:, 0:NE], self.ones32[0:1, :], rb[0:1, 0, :], False, True, ['ones32', 'rb'], 'ps6')
                    if cfg.stop == 'r_a':
                        return
                    self.act(sc_[:, 0, :], ps[6][:, 0:NE], AF.Sigmoid, ['ps6'], 'sc_')
                    self.tt(bi2, sc_[:, 0, :], brB[:, 0, :], ALU.add, ['sc_', 'brB'], 'bi')
                    for g in range(NG):
                        self.S.op('dve', lambda e: e.max(out=t8g[:, g, :], in_=bi[:, g, :]), reads=['bi'], writes=['t8g'])
                    if cfg.stop == 'r_b':
                        return
                    self.tt(gs[:, 0, :], t8g[:, :, 0], t8g[:, :, 1], ALU.add, ['t8g'], 'gs')
                    self.tt(gmax[:, 0, :], gs[:, 0, 0:1], gs[:, 0, 1:2], ALU.max, ['gs'], 'gmax')
                    for g in range(2, NG):
                        self.tt(gmax[:, 0, :], gmax[:, 0, :], gs[:, 0, g:g + 1], ALU.max, ['gs', 'gmax'], 'gmax')
                    self.ts(ohg[:, 0, :], gs[:, 0, :], gmax[:, 0, 0:1], None, ALU.is_equal, None, ['gs', 'gmax'], 'ohg')
                    self.ts(ohg[:, 0, :], ohg[:, 0, :], BIG, -BIG, ALU.mult, ALU.add, ['ohg'], 'ohg')
                    self.tt(msk[:], bi[:], ohg[:, 0, :].unsqueeze(2).broadcast_to([128, NG, 8]), ALU.add, ['bi', 'ohg'], 'msk')
                    self.S.op('dve', lambda e: e.max(out=t8e[:, 0, :], in_=msk2), reads=['msk'], writes=['t8e'])
                    self.ts(sel2[:, 0, :], msk2, t8e[:, 0, 1:2], None, ALU.is_ge, None, ['msk', 't8e'], 'sel2')
                    self.tt(wun[:, 0, :], sc_[:, 0, :], sel2[:, 0, :], ALU.mult, ['sc_', 'sel2'], 'wun')
                    self.S.op('dve', lambda e: e.tensor_reduce(out=dsum[:, 0, :], in_=wun[:, 0, :], axis=AX.X, op=ALU.add), reads=['wun'], writes=['dsum'])
                    self.S.op('dve', lambda e: e.reciprocal(out=dsum[:, 0, :], in_=dsum[:, 0, :]), reads=['dsum'], writes=['dsum'])
                    self.ts(Wt[:, tau, :], wun[:, 0, :], dsum[:, 0, 0:1], None, ALU.mult, None, ['wun', 'dsum'], 'Wt')
                if cfg.stop == 'route':
                    return
                items = [(e_, ti) for e_ in range(NE) for ti in range(len(tiles))]

                def stage_a(n):
                    e_, ti = items[n]
                    sl = e_ % 2
                    ab = n % 2
                    if ti == 0:
                        self.S.dma('pool', wg2[sl][:], wg_d[layer, e_].rearrange("(kc p) n -> p kc n", p=128), writes=['wg%d' % sl])
                        self.S.dma('pool', wu2[sl][:], wu_d[layer, e_].rearrange("(kc p) n -> p kc n", p=128), writes=['wu%d' % sl])
                        self.S.dma('pool', wd2[sl][:], wd_d[layer, e_].rearrange("(kc p) n -> p kc n", p=128), writes=['wd%d' % sl])
                    for kc in range(8):
                        self.mm(ps[ab][:, :], hTm[:, kc, ti * 128:(ti + 1) * 128], wg2[sl][:, kc, :], kc == 0, kc == 7, ['hTg', 'wg%d' % sl], PK[ab])
                    for kc in range(8):
                        self.mm(ps[2 + ab][:, :], hTm[:, kc, ti * 128:(ti + 1) * 128], wu2[sl][:, kc, :], kc == 0, kc == 7, ['hTg', 'wu%d' % sl], PK[2 + ab])
                    self.act(sg[ab][:, 0, :], ps[ab][:, :], AF.Silu, [PK[ab]], 'sg%d' % ab)
                    self.tt(Ab[ab][:, 0, :], sg[ab][:, 0, :], ps[2 + ab][:, :], ALU.mult, ['sg%d' % ab, PK[2 + ab]], 'Ab%d' % ab)

                def stage_b(n):
                    ab = n % 2
                    pt = ps[4 + ab][:].bitcast(BF16)
                    for fc in range(4):
                        self.tr(pt[:, fc * 128:(fc + 1) * 128], Ab[ab][:, 0, fc * 128:(fc + 1) * 128], identb[:], ['Ab%d' % ab, 'identb'], PK[4 + ab])
                    self.cp(AT[ab][:].rearrange("p f t -> p (f t)"), pt[:, 0:512], [PK[4 + ab]], 'AT%d' % ab, eng='act')

                def stage_c(n):
                    e_, ti = items[n]
                    sl = e_ % 2
                    ab = n % 2
                    tau = tiles[ti]
                    for hf in range(2):
                        pb = 6 + hf
                        for fc in range(4):
                            self.mm(ps[pb][:, :], AT[ab][:, fc, :], wd2[sl][:, fc, hf * 512:(hf + 1) * 512], fc == 0, fc == 3, ['AT%d' % ab, 'wd%d' % sl], PK[pb])
                        dst = accm[:, ti, hf * 512:(hf + 1) * 512]
                        if e_ == 0:
                            self.ts(dst, ps[pb][:, :], Wt[:, tau, e_:e_ + 1], None, ALU.mult, None, [PK[pb], 'Wt'], 'accm%d' % ti)
                        else:
                            self.stt(dst, ps[pb][:, :], Wt[:, tau, e_:e_ + 1], dst, ALU.mult, ALU.add, [PK[pb], 'Wt', 'accm%d' % ti], 'accm%d' % ti)

                nit = len(items)
                for step in range(nit + 2):
                    if step < nit:
                        stage_a(step)
                    if 1 <= step <= nit:
                        stage_b(step - 1)
                    if step >= 2:
                        stage_c(step - 2)
                for ti, tau in enumerate(tiles):
                    smp = (tau == NT - 1)
                    gt, gk = (gS, 'gS') if smp else (gP, 'gP')
                    self.tt(zq2, accm[:, ti, :], gt[:], ALU.mult, ['accm%d' % ti, gk], 'zq')
                    self.stt(zq2, xres[:, tau, :], cfg.ALPHA, zq2, ALU.mult, ALU.add, [XK[tau], 'zq'], 'zq')
                    self.layernorm(zq2, xres[:, tau, :], lnG[:], lnB[:], 'zq', XK[tau], 'lnG')

        moe(0)
        if cfg.stop in ('moe0', 'route', 'ada1', 'rsetup', 'r_a', 'r_b', 'r_0'):
            self.finish(dbg, xres, XK)
            return

        ada(2)
        off = 0
        uT, off = carve(off, [8, 30 + 512], F32)
        hTc, off = carve(off, [8, 512], BF16)
        winc = []
        for i in range(2):
            a_, off = carve(off, [8, 256], BF16); winc.append(a_)
        yg, off = carve(off, [8, 512], F32)
        ytok, off = carve(off, [1, D], F32)
        zb, off = carve(off, [8, 128], BF16)
        zT, off = carve(off, [8, 128], BF16)
        wout, off = carve(off, [8, D], BF16)
        wdw, off = carve(off, [8, CW], F32)
        cg_, off = carve(off, [1, D], F32)
        cb_, off = carve(off, [1, D], F32)
        sgm, off = carve(off, [1, 512], F32)
        zq, off = carve(off, [1, D], F32)
        uS, off = carve(off, [8, SPC], F32)
        stT, off = carve(off, [8 * SPC, CW], F32)
        prodS, off = carve(off, [8 * SPC, CW], F32)
        yS, off = carve(off, [8, SPC], F32)
        utok, off = carve(off, [1, D], F32)
        assert off <= SCR_BYTES, off
        zq2 = zq[:, 0, :]
        ytok2 = ytok[:, 0, :]
        utok2 = utok[:, 0, :]
        self.memset(uT[:].rearrange("p a b -> p (a b)"), 0.0, 'uT')
        self.ld(wdw[:].rearrange("p a b -> p (a b)"), wdwT_d, 'wdw')
        self.S.dma('pool', wout[:], wout_d.rearrange("(kc p) n -> p kc n", p=128), writes=['wout'])
        self.bcast_row(cg_[:, 0, :], clng_d, D, 'cgb', 'cg')
        self.bcast_row(cb_[:, 0, :], clnb_d, D, 'cgb', 'cb')
        win_src = win_d.rearrange("(kc p) n -> p kc n", p=128)
        wi = [0]

        def glu_cols(ncol, dst_fn):
            for cc in range(8):
                wb = winc[wi[0] % 2]
                wk = 'winc%d' % (wi[0] % 2)
                wi[0] += 1
                self.S.dma('pool', wb[:, :, 0:128], win_src[:, :, cc * 128:(cc + 1) * 128], writes=[wk])
                self.S.dma('pool', wb[:, :, 128:256], win_src[:, :, D + cc * 128:D + (cc + 1) * 128], writes=[wk])
                for kc in range(8):
                    self.mm(ps[0][:, 0:ncol], wb[:, kc, 0:128], hTc[:, kc, 0:ncol], kc == 0, kc == 7, [wk, 'hTg'], 'ps0')
                for kc in range(8):
                    self.mm(ps[1][:, 0:ncol], wb[:, kc, 128:256], hTc[:, kc, 0:ncol], kc == 0, kc == 7, [wk, 'hTg'], 'ps1')
                self.act(sgm[:, 0, 0:ncol], ps[1][:, 0:ncol], AF.Sigmoid, ['ps1'], 'sgm')
                dst, dk = dst_fn(cc)
                self.tt(dst, ps[0][:, 0:ncol], sgm[:, 0, 0:ncol], ALU.mult, ['ps0', 'sgm'], dk)

        def conv_tail(tau, col0, gate_tile, gkey):
            for hf in range(2):
                for q in range(4):
                    cc = hf * 4 + q
                    self.tr(ps[2 + hf][:, q * 128:(q + 1) * 128], yg[:, cc, col0:col0 + 128], ident, ['yg', 'consts'], PK[2 + hf])
                self.cp(ytok2[:, hf * 512:(hf + 1) * 512], ps[2 + hf][:, :], [PK[2 + hf]], 'ytok', eng='act')
            self.layernorm(ytok2, ytok2, cg_[:, 0, :], cb_[:, 0, :], 'ytok', 'ytok', 'cgb')
            self.act(zb[:].rearrange("p a b -> p (a b)"), ytok2, AF.Silu, ['ytok'], 'zb')
            pt = ps[4][:].bitcast(BF16)
            for cc in range(8):
                self.tr(pt[:, cc * 128:(cc + 1) * 128], zb[:, cc, :], identb[:], ['zb', 'identb'], 'ps4')
            self.cp(zT[:].rearrange("p a b -> p (a b)"), pt[:, :], ['ps4'], 'zT')
            for hf in range(2):
                for cc in range(8):
                    self.mm(ps[6 + hf][:, :], zT[:, cc, :], wout[:, cc, hf * 512:(hf + 1) * 512], cc == 0, cc == 7, ['zT', 'wout'], PK[6 + hf])
                self.tt(zq2[:, hf * 512:(hf + 1) * 512], ps[6 + hf][:, :], gate_tile[:, hf * 512:(hf + 1) * 512], ALU.mult, [PK[6 + hf], gkey], 'zq')
            self.stt(zq2, xres[:, tau, :], cfg.ALPHA, zq2, ALU.mult, ALU.add, [XK[tau], 'zq'], 'zq')
            self.layernorm(zq2, xres[:, tau, :], lnG[:], lnB[:], 'zq', XK[tau], 'lnG')

        for g0 in range(0, NPT, 4):
            tiles = list(range(g0, min(g0 + 4, NPT)))
            ncol = len(tiles) * 128
            for ti, tau in enumerate(tiles):
                tile_to_hT(xres[:, tau, :], XK[tau], hTc, ti * 128)
            glu_cols(ncol, lambda cc: (uT[:, cc, 30:30 + ncol], 'uT'))
            if g0 == 0:
                self.ts(uT[:, :, 30:158], uT[:, :, 30:158], flag[:, 0:1], None, ALU.mult, None, ['uT', 'flag'], 'uT')
            for cc in range(8):
                for k in range(CW):
                    if k == 0:
                        self.ts(yg[:, cc, 0:ncol], uT[:, cc, 0:ncol], wdw[:, cc, 0:1], None, ALU.mult, None, ['uT', 'wdw'], 'yg')
                    else:
                        self.stt(yg[:, cc, 0:ncol], uT[:, cc, k:k + ncol], wdw[:, cc, k:k + 1], yg[:, cc, 0:ncol], ALU.mult, ALU.add, ['uT', 'wdw', 'yg'], 'yg')
            if tiles[-1] == NPT - 1:
                cl = 30 + (len(tiles) - 1) * 128 + 98
                for hf in range(2):
                    for q in range(4):
                        cc = hf * 4 + q
                        self.tr(ps[2 + hf][0:30, q * 128:(q + 1) * 128], uT[:, cc, cl:cl + 30], ident, ['uT', 'consts'], PK[2 + hf])
                    self.cp(utok2[0:30, hf * 512:(hf + 1) * 512], ps[2 + hf][0:30, :], [PK[2 + hf]], 'utok', eng='act')
                self.st(conv_p[:, :], utok2[0:30, :], 'utok', 'conv_p')
            else:
                self.cp(uT[:, :, 0:30], uT[:, :, ncol:ncol + 30], ['uT'], 'uT')
            if cfg.stop == 'c_conv':
                self.finish(dbg, xres, XK)
                return
            for ti, tau in enumerate(tiles):
                if tau >= 1:
                    conv_tail(tau, ti * 128, gP, 'gP')
            if cfg.stop == 'c_tail':
                self.finish(dbg, xres, XK)
                return
        tile_to_hT(xres[:, tS, :], XK[tS], hTc, 0, sample=True)
        glu_cols(SPC, lambda cc: (uS[:, cc, :], 'uS'))
        st4 = stT[:].rearrange("p (c s) k -> p c s k", c=8)
        pflat = prodS[:].rearrange("p a k -> p (a k)")[:, 0:8 * SPC * 30]
        self.ld(pflat, stfm_d, 'prodS')
        self.cp(st4[:, :, :, 0:30], pflat.rearrange("p (c s k) -> p c s k", c=8, s=SPC), ['prodS'], 'stT')
        self.cp(st4[:, :, :, 30], uS[:], ['uS', 'stT'], 'stT')
        self.tt(prodS[:].rearrange("p (c s) k -> p c s k", c=8), st4, wdw[:].unsqueeze(2).broadcast_to([128, 8, SPC, CW]), ALU.mult, ['stT', 'wdw'], 'prodS')
        self.S.op('dve', lambda e: e.tensor_reduce(out=yS[:].rearrange("p c s -> p (c s)"), in_=prodS[:], axis=AX.X, op=ALU.add), reads=['prodS'], writes=['yS'])
        self.memset(yg[:, :, 0:128], 0.0, 'yg')
        self.cp(yg[:, :, 0:SPC], yS[:], ['yS', 'yg'], 'yg')
        conv_tail(tS, 0, gS, 'gS')
        for s_ in range(SPC):
            self.ld(ytok2[s_ * 29:(s_ + 1) * 29, :], sttok_d[s_, 1:30, :], 'ytok')
        for s_ in range(SPC):
            self.st(conv_s[s_, 0:29, :], ytok2[s_ * 29:(s_ + 1) * 29, :], 'ytok', 'conv_s')
        for hf in range(2):
            for q in range(4):
                cc = hf * 4 + q
                self.tr(ps[2 + hf][0:SPC, q * 128:(q + 1) * 128], uS[:, cc, :], ident, ['uS', 'consts'], PK[2 + hf])
            self.cp(utok2[0:SPC, hf * 512:(hf + 1) * 512], ps[2 + hf][0:SPC, :], [PK[2 + hf]], 'utok', eng='act')
        self.st(conv_s[:, 29, :], utok2[0:SPC, :], 'utok', 'conv_s')
        if cfg.stop == 'conv':
            self.finish(dbg, xres, XK)
            return

        moe(1)
        for tau in range(1, NPT):
            self.st(y_p[(tau - 1) * 128:tau * 128, :], xres[:, tau, :], XK[tau], 'y_p')
        self.st(y_s[:, :], xres[0:SPC, tS, :], XK[tS], 'y_s')
        self.finish(dbg, xres, XK)

    def finish(self, dbg, xres, XK):
        cfg = self.cfg
        if dbg is not None:
            for t in range(cfg.NT):
                self.st(dbg[t * 128:(t + 1) * 128, :], xres[:, t, :], XK[t], 'dbg')
        self.S.wait_keys('sp', list(set(self.outkeys)))
        self.es.close()


def prep_inputs(cfg, I):
    f32 = np.float32
    NC, NCB, LS, NSLOT, NPT, NGT, SPC, NBLK = cfg.NC, cfg.NCB, cfg.LS, cfg.NSLOT, cfg.NPT, cfg.NGT, cfg.SPC, cfg.NBLK
    half = 8
    inv_freq = (f32(ROPE_THETA) ** (-np.arange(half, dtype=f32) / f32(half))).astype(f32) if False else None
    half = 16
    inv_freq = np.power(f32(ROPE_THETA), -(np.arange(half, dtype=f32) / f32(half))).astype(f32)
    ident = np.eye(128, dtype=f32)
    tri = (np.arange(128)[:, None] <= np.arange(128)[None, :]).astype(f32)
    consts = np.concatenate([ident, tri], axis=1)
    c2 = np.zeros((128, 512), f32)
    cm = np.zeros((8, 32), f32)
    for s_ in range(4):
        for kv in range(2):
            for h in range(kv * 4, kv * 4 + 4):
                cm[s_ * 2 + kv, h * 4 + s_] = 1.0
    c2[:, 0:256] = cm.reshape(1, 256)
    c2[0:32, 256] = (np.arange(32) // 4) // 4
    for s_ in range(4):
        for h in range(8):
            c2[s_, (257 if h < 4 else 289) + h * 4 + s_] = 1.0
    c2[0:32, 321:353] = np.eye(32, dtype=f32)
    c2[:, 353:361] = np.arange(8, dtype=f32)[None, :]
    shared = {
        "consts": consts, "consts2": c2,
        "w_ada": np.ascontiguousarray(I["w_ada"]), "b_ada": np.ascontiguousarray(I["b_ada"]),
        "b_adaT": np.ascontiguousarray(I["b_ada"].reshape(2, 48, 128).transpose(2, 0, 1).reshape(128, 96)),
        "ln_g": np.ascontiguousarray(I["ln_g"].reshape(4, D)), "ln_b": np.ascontiguousarray(I["ln_b"].reshape(4, D)),
        "w_qkv": np.ascontiguousarray(I["w_qkv"][0]), "w_o": np.ascontiguousarray(I["w_o"][0]),
        "conv_w_in": np.ascontiguousarray(I["conv_w_in"][0]),
        "wdwT": np.ascontiguousarray(I["conv_w_dw"][0].T.reshape(8, 128, CW).transpose(1, 0, 2).reshape(128, 8 * CW)),
        "conv_ln_g": np.ascontiguousarray(I["conv_ln_g"].reshape(1, D)), "conv_ln_b": np.ascontiguousarray(I["conv_ln_b"].reshape(1, D)),
        "conv_w_out": np.ascontiguousarray(I["conv_w_out"][0]),
        "w_router": np.ascontiguousarray(I["w_router"]), "b_router": np.ascontiguousarray(I["b_router"].reshape(1, -1)),
        "w_gate": np.ascontiguousarray(I["w_gate"]), "w_up": np.ascontiguousarray(I["w_up"]), "w_down": np.ascontiguousarray(I["w_down"]),
        "cache_k": np.ascontiguousarray(I["cache_k"].reshape(cfg.NPHYS, -1)),
        "cache_v": np.ascontiguousarray(I["cache_v"].reshape(cfg.NPHYS, -1)),
    }
    maps = []
    pt = np.asarray(I["page_table"]).astype(np.int32)
    for c in range(NC):
        b, j = c // NCB, c % NCB
        rot = LS * j - 1
        m = dict(shared)
        m["xg"] = np.ascontiguousarray(np.roll(I["x_prompt"][b], -rot * 256, axis=0))
        gslot = (rot + np.arange(NSLOT)) % NSLOT
        pos = (gslot[:, None] * 256 + np.arange(256)[None, :]).reshape(-1)
        pos = np.concatenate([pos, np.full(128, cfg.PAST_LEN)]).astype(f32)
        ang = pos[:, None] * inv_freq[None, :]
        tab = np.concatenate([np.cos(ang), np.sin(ang)], axis=1).astype(f32)
        m["rope"] = np.ascontiguousarray(tab.reshape(NGT + 1, 128, 32).transpose(1, 0, 2).reshape(128, (NGT + 1) * 32))
        sid = np.arange(c * SPC, (c + 1) * SPC)
        xs = np.zeros((128, D), f32)
        xs[:SPC] = I["x_sample"][sid, 0]
        m["xs"] = xs
        cv = np.zeros((8, D), f32)
        cv[0] = I["c_prompt"][b]
        cv[1:1 + SPC] = I["c_sample"][sid]
        m["cvec"] = cv
        valid = np.zeros((NPT, NSLOT), bool)
        for tau in range(NPT):
            own = rot + (tau + 1) // 2
            valid[tau] = gslot < own
        m["nmask"] = np.ascontiguousarray(np.broadcast_to(np.where(valid, 0.0, -BIG).astype(f32).reshape(1, -1), (128, NPT * NSLOT)))
        m["v01"] = np.ascontiguousarray(np.broadcast_to(valid.astype(f32).reshape(1, -1), (128, NPT * NSLOT)))
        m["flag"] = np.full((128, 1), 1.0 if j > 0 else 0.0, f32)
        ptA = np.zeros((128, 4), np.int32)
        for r in range(2):
            for eo in range(2):
                for s2 in range(2):
                    ptA[s2 * NBLK:(s2 + 1) * NBLK, r * 2 + eo] = pt[sid[2 * r + s2], eo::2]
        m["ptA"] = ptA
        ptB = np.zeros((32, 2 * NBLK), np.int32)
        for h in range(8):
            for s in range(SPC):
                ptB[h * 4 + s, :NBLK] = pt[sid[s], 0::2]
                ptB[h * 4 + s, NBLK:] = pt[sid[s], 1::2]
        m["ptB"] = ptB
        st = I["state_conv"][0][sid]
        m["st_tok"] = np.ascontiguousarray(st)
        m["st_fm"] = np.ascontiguousarray(st.reshape(SPC, 30, 8, 128).transpose(3, 2, 0, 1).reshape(128, 8 * SPC * 30))
        maps.append(m)
    return maps


_PROG_CACHE = {}


def run_cfg(cfg, inputs):
    key = (cfg.B, cfg.SEQ, cfg.NCB, cfg.DEC_BATCH, cfg.PAST_LEN, cfg.NE, cfg.NG, cfg.MG, cfg.stop)
    if key not in _PROG_CACHE:
        p = Prog(cfg)
        p.build()
        _PROG_CACHE[key] = p
    p = _PROG_CACHE[key]
    maps = prep_inputs(cfg, inputs)
    maps = [{k: v for k, v in m.items() if k in p.din} for m in maps]
    res = run_bass_kernel_spmd(p.nc, maps, core_ids=list(range(cfg.NC)))
    return res.results


def assemble(cfg, R):
    f32 = np.float32
    B, NCB, LS, SPC = cfg.B, cfg.NCB, cfg.LS, cfg.SPC
    y_p = np.zeros((B, cfg.SEQ, D), f32)
    k_p = np.zeros((B, cfg.SEQ // 128, 1, 2, 128, 128), f32)
    v_p = np.zeros_like(k_p)
    conv_p = np.zeros((1, B, 30, D), f32)
    y_s = np.zeros((cfg.DEC_BATCH, 1, D), f32)
    k_s = np.zeros((cfg.DEC_BATCH, 1, 2, 1, 128), f32)
    v_s = np.zeros_like(k_s)
    conv_s = np.zeros((1, cfg.DEC_BATCH, 30, D), f32)
    for c, r in enumerate(R):
        b, j = c // NCB, c % NCB
        t0 = j * LS * 256
        y_p[b, t0:t0 + LS * 256] = r["y_p"]
        k_p[b, j * 2 * LS:(j + 1) * 2 * LS, 0] = r["k_p"]
        v_p[b, j * 2 * LS:(j + 1) * 2 * LS, 0] = r["v_p"]
        if j == NCB - 1:
            conv_p[0, b] = r["conv_p"]
        sl = slice(c * SPC, (c + 1) * SPC)
        y_s[sl, 0] = r["y_s"]
        k_s[sl, 0, :, 0, :] = r["k_s"].reshape(SPC, 2, 128)
        v_s[sl, 0, :, 0, :] = r["v_s"].reshape(SPC, 2, 128)
        conv_s[0, sl] = r["conv_s"]
    return (y_p, y_s, k_p, v_p, conv_p, k_s, v_s, conv_s)


def kernel(**inputs):
    cfg = Cfg()
    inputs = {k: np.asarray(v) for k, v in inputs.items()}
    R = run_cfg(cfg, inputs)
    return assemble(cfg, R)
```

```python
import numpy as np
from contextlib import ExitStack
import concourse.bass as bass
import concourse.mybir as mybir
from concourse.bass_utils import run_bass_kernel_spmd

F32 = mybir.dt.float32
BF16 = mybir.dt.bfloat16
I32 = mybir.dt.int32
U32 = mybir.dt.uint32
AF = mybir.ActivationFunctionType
ALU = mybir.AluOpType
AX = mybir.AxisListType

EPOCH = 24000
D = 1024
H = 8
KVH = 2
HD = 128
ROPE_THETA = 500000.0
ATTN_SCALE = HD ** -0.5
CW = 31
DFF = 512
LN_EPS = 1e-5
BIG = 1.0e30


class Cfg:
    def __init__(self, B=2, SEQ=8192, NCB=4, DEC_BATCH=32, PAST_LEN=16384, NE=32, NG=4, DEPTH=2, MG=3,
                 stop=None):
        self.B, self.SEQ, self.NCB, self.DEC_BATCH, self.PAST_LEN = B, SEQ, NCB, DEC_BATCH, PAST_LEN
        self.NE, self.NG, self.EPG = NE, NG, NE // NG
        self.NC = B * NCB
        self.NSLOT = SEQ // 256
        self.LS = self.NSLOT // NCB
        self.NPT = 2 * self.LS + 1
        self.NT = self.NPT + 1
        self.NGT = 2 * self.NSLOT
        self.SPC = DEC_BATCH // self.NC
        self.NPG = PAST_LEN // 128
        self.NBLK = self.NPG // 2
        self.NPHYS = (DEC_BATCH * self.NPG * 5) // 4
        self.DEPTH = DEPTH
        self.MG = MG
        self.ALPHA = (2 * DEPTH) ** 0.25
        self.stop = stop
        assert self.SPC == 4 and self.EPG == 8 and self.NT % MG == 0


class Sync:
    def __init__(self, nc, es, same_engine_wait=True):
        self.nc = nc
        self.es = es
        self.engs = {'pe': nc.tensor, 'act': nc.scalar, 'dve': nc.vector,
                     'pool': nc.gpsimd, 'sp': nc.sync}
        self.sems = {}
        self.cnt = {k: 0 for k in self.engs}
        self.waited = {k: {} for k in self.engs}
        self.res = {}
        self.dsem = {}
        self.same = same_engine_wait
        self.n_ins = 0

    def _sem(self, key):
        if key not in self.sems:
            nm = "s%d" % len(self.sems)
            self.sems[key] = self.es.enter_context(self.nc.semaphore(nm))
        return self.sems[key]

    def _wait(self, eng, deps):
        best = {}
        for (sk, v) in deps:
            if best.get(sk, 0) < v:
                best[sk] = v
        for sk, v in best.items():
            if sk[0] == 'E' and sk[1] == eng:
                if not self.same or eng == 'pe':
                    continue
            if self.waited[eng].get(sk, 0) >= v:
                continue
            self.engs[eng].wait_ge(self._sem(sk), v)
            self.n_ins += 1
            self.waited[eng][sk] = v

    def _deps(self, reads, writes):
        deps = []
        for k in reads:
            r = self.res.get(k)
            if r and r['w']:
                deps.append(r['w'])
        for k in writes:
            r = self.res.get(k)
            if r:
                if r['w']:
                    deps.append(r['w'])
                deps += r['r']
        return deps

    def _record(self, ev, reads, writes):
        for k in reads:
            self.res.setdefault(k, {'w': None, 'r': []})['r'].append(ev)
        for k in writes:
            self.res[k] = {'w': ev, 'r': []}

    def op(self, eng, fn, reads=(), writes=()):
        self._wait(eng, self._deps(reads, writes))
        ins = fn(self.engs[eng])
        self.cnt[eng] += 1
        ep, v = divmod(self.cnt[eng] - 1, EPOCH)
        sk = ('E', eng, ep)
        ins.then_inc(self._sem(sk), 1)
        self.n_ins += 1
        self._record((sk, v + 1), reads, writes)
        return ins

    def dma(self, eng, out, in_, reads=(), writes=(), key=None, fn=None):
        self._wait(eng, self._deps(reads, writes))
        if key is None:
            key = ('D', writes[0] if writes else reads[0])
        if fn is None:
            ins = self.engs[eng].dma_start(out=out, in_=in_)
        else:
            ins = fn(self.engs[eng])
        c = self.dsem.get(key, 0) + 16
        self.dsem[key] = c
        ins.then_inc(self._sem(key), 16)
        self.n_ins += 1
        self._record((key, c), reads, writes)
        return ins

    def wait_keys(self, eng, keys):
        deps = []
        for k in keys:
            r = self.res.get(k)
            if r:
                if r['w']:
                    deps.append(r['w'])
                deps += r['r']
        self._wait(eng, deps)

    def barrier(self):
        deps = []
        for r in self.res.values():
            if r['w']:
                deps.append(r['w'])
            deps += r['r']
        for eng in self.engs:
            self._wait(eng, deps)
        self.res = {}


class Prog:
    def __init__(self, cfg):
        self.cfg = cfg
        self.nc = bass.Bass("TRN2", target_bir_lowering=False)
        self.es = ExitStack()
        self.din = {}
        self.dout = {}

    def inp(self, name, shape, dt=F32):
        self.din[name] = self.nc.dram_tensor(name, list(shape), dt, kind="ExternalInput").ap()
        return self.din[name]

    def outp(self, name, shape, dt=F32):
        self.dout[name] = self.nc.dram_tensor(name, list(shape), dt, kind="ExternalOutput").ap()
        return self.dout[name]

    def sb(self, name, shape, dt=F32):
        return self.es.enter_context(self.nc.sbuf_tensor("sb_" + name, list(shape), dt))

    def mm(self, out, lhsT, rhs, start, stop, reads, w):
        self.S.op('pe', lambda e: e.matmul(out, lhsT=lhsT, rhs=rhs, start=start, stop=stop), reads=reads, writes=[w])

    def tr(self, out, in_, ident, reads, w):
        self.S.op('pe', lambda e: e.transpose(out=out, in_=in_, identity=ident), reads=reads, writes=[w])

    def act(self, out, in_, func, reads, w, bias=None, scale=None, eng='act'):
        kw = {}
        if bias is not None:
            kw['bias'] = bias
        if scale is not None:
            kw['scale'] = scale
        self.S.op('act', lambda e: e.activation(out=out, in_=in_, func=func, **kw), reads=reads, writes=[w])

    def tt(self, out, a, b, op, reads, w, eng='dve'):
        self.S.op(eng, lambda e: e.tensor_tensor(out=out, in0=a, in1=b, op=op), reads=reads, writes=[w])

    def ts(self, out, a, s1, s2, op0, op1, reads, w, eng='dve'):
        if op1 is None:
            self.S.op(eng, lambda e: e.tensor_scalar(out=out, in0=a, scalar1=s1, scalar2=None, op0=op0), reads=reads, writes=[w])
        else:
            self.S.op(eng, lambda e: e.tensor_scalar(out=out, in0=a, scalar1=s1, scalar2=s2, op0=op0, op1=op1), reads=reads, writes=[w])

    def stt(self, out, a, s, b, op0, op1, reads, w):
        self.S.op('dve', lambda e: e.scalar_tensor_tensor(out=out, in0=a, scalar=s, in1=b, op0=op0, op1=op1), reads=reads, writes=[w])

    def cp(self, out, in_, reads, w, eng='dve'):
        if eng == 'act':
            self.S.op('act', lambda e: e.copy(out=out, in_=in_), reads=reads, writes=[w])
        else:
            self.S.op(eng, lambda e: e.tensor_copy(out=out, in_=in_), reads=reads, writes=[w])

    def memset(self, ap, val, w, eng='dve'):
        self.S.op(eng, lambda e: e.memset(ap, val), writes=[w])

    def ld(self, out, in_, w, reads=(), eng='sp'):
        self.S.dma(eng, out, in_, reads=list(reads), writes=[w])

    def st(self, out, in_, r, okey):
        self.S.dma('sp', out, in_, reads=[r], writes=[okey])
        self.outkeys.append(okey)

    def bcast_row(self, dst, row_ap, n, dkey, tmpname):
        rt = self.rowtmp
        for c0 in range(0, n, 512):
            cw = min(512, n - c0)
            self.ld(rt[0:1, 0:cw], row_ap[:, c0:c0 + cw], 'rowtmp')
            self.mm(self.ps[0][:, 0:cw], self.ones32[0:1, :], rt[0:1, 0:cw], True, True, ['rowtmp', 'ones32'], 'ps0')
            self.cp(dst[:, c0:c0 + cw], self.ps[0][:, 0:cw], ['ps0'], dkey, eng='act')

    def layernorm(self, z, out, gB, bB, zkey, okey, gkey):
        st_, mv, rstd = self.ln_st, self.ln_mv, self.ln_rstd
        for i in range(2):
            self.S.op('dve', lambda e: e.bn_stats(out=st_[:, i, :], in_=z[:, i * 512:(i + 1) * 512]), reads=[zkey], writes=['ln_st'])
        self.S.op('dve', lambda e: e.bn_aggr(out=mv[:], in_=st_[:].rearrange("p a b -> p (a b)")), reads=['ln_st'], writes=['ln_mv'])
        self.act(rstd[:], mv[:, 1:2], AF.Ln, ['ln_mv', 'epsT'], 'ln_rstd', bias=self.epsT[:], scale=1.0)
        self.act(rstd[:], rstd[:], AF.Exp, ['ln_rstd'], 'ln_rstd', scale=-0.5)
        self.ts(z, z, mv[:, 0:1], rstd[:, 0:1], ALU.subtract, ALU.mult, [zkey, 'ln_mv', 'ln_rstd'], zkey)
        self.tt(z, z, gB, ALU.mult, [zkey, gkey], zkey)
        self.tt(out, z, bB, ALU.add, [zkey, gkey], okey)

    def build(self):
        cfg = self.cfg
        nc, es = self.nc, self.es
        NT, NPT, NGT, NSLOT, LS, SPC, NE, NG, EPG = cfg.NT, cfg.NPT, cfg.NGT, cfg.NSLOT, cfg.LS, cfg.SPC, cfg.NE, cfg.NG, cfg.EPG
        NBLK, NPG, NPHYS = cfg.NBLK, cfg.NPG, cfg.NPHYS
        self.outkeys = []
        xg = self.inp("xg", [NGT * 128, D])
        rope_d = self.inp("rope", [128, (NGT + 1) * 32])
        xs_d = self.inp("xs", [128, D])
        cvec_d = self.inp("cvec", [8, D])
        nmask_d = self.inp("nmask", [128, NPT * NSLOT])
        v01_d = self.inp("v01", [128, NPT * NSLOT])
        flag_d = self.inp("flag", [128, 1])
        consts_d = self.inp("consts", [128, 256])
        consts2_d = self.inp("consts2", [128, 512])
        ptA_d = self.inp("ptA", [128, 4], I32)
        ptB_d = self.inp("ptB", [32, 2 * NBLK], I32)
        sttok_d = self.inp("st_tok", [SPC, 30, D])
        stfm_d = self.inp("st_fm", [128, 8 * SPC * 30])
        wada_d = self.inp("w_ada", [2, D, 6 * D])
        bada_d = self.inp("b_ada", [2, 6 * D])
        badaT_d = self.inp("b_adaT", [128, 96])
        lng_d = self.inp("ln_g", [4, D])
        lnb_d = self.inp("ln_b", [4, D])
        wqkv_d = self.inp("w_qkv", [D, 1536])
        wo_d = self.inp("w_o", [D, D])
        win_d = self.inp("conv_w_in", [D, 2 * D])
        wdwT_d = self.inp("wdwT", [128, 8 * CW])
        clng_d = self.inp("conv_ln_g", [1, D])
        clnb_d = self.inp("conv_ln_b", [1, D])
        wout_d = self.inp("conv_w_out", [D, D])
        wr_d = self.inp("w_router", [D, NE])
        br_d = self.inp("b_router", [1, NE])
        wg_d = self.inp("w_gate", [2, NE, D, DFF])
        wu_d = self.inp("w_up", [2, NE, D, DFF])
        wd_d = self.inp("w_down", [2, NE, DFF, D])
        ck_d = self.inp("cache_k", [NPHYS, 2 * 128 * 128])
        cv_d = self.inp("cache_v", [NPHYS, 2 * 128 * 128])

        y_p = self.outp("y_p", [2 * LS * 128, D])
        y_s = self.outp("y_s", [SPC, D])
        k_p = self.outp("k_p", [2 * LS, 2, 128, 128])
        v_p = self.outp("v_p", [2 * LS, 2, 128, 128])
        conv_p = self.outp("conv_p", [30, D])
        k_s = self.outp("k_s", [SPC, 256])
        v_s = self.outp("v_s", [SPC, 256])
        conv_s = self.outp("conv_s", [SPC, 30, D])
        dbg = self.outp("dbg", [NT * 128, D]) if cfg.stop else None

        self.S = S = Sync(nc, es)
        sb = self.sb
        self.ps = [es.enter_context(nc.psum_tensor("ps%d" % i, [128, 512], F32)) for i in range(8)]
        ps = self.ps
        PK = ['ps%d' % i for i in range(8)]
        xres = sb("xres", [128, NT, D])
        XK = ['xres%d' % t for t in range(NT)]
        consts = sb("consts", [128, 256])
        ident = consts[:, 0:128]
        identb = sb("identb", [128, 128], BF16)
        trib = sb("trib", [128, 128], BF16)
        self.ones32 = sb("ones32", [128, 128])
        onesb = sb("onesb", [128, 1], BF16)
        self.rowtmp = sb("rowtmp", [1, 512])
        self.ln_st = sb("ln_st", [128, 2, 6])
        self.ln_mv = sb("ln_mv", [128, 2])
        self.ln_rstd = sb("ln_rstd", [128, 1])
        flag = sb("flag", [128, 1])
        scT = sb("scT", [128, 8, 8])
        modT = sb("modT", [128, 2, 8, 8])
        badaT = sb("badaT", [128, 96])
        gP = sb("gP", [128, D])
        gS = sb("gS", [128, D])
        lnG = sb("lnG", [128, D])
        lnB = sb("lnB", [128, D])
        ks32 = sb("ks32", [128, 2 * NGT])
        kvS = sb("kvS", [128, 512])
        SCR_BYTES = 108 * 1024
        scr = sb("scr", [128, SCR_BYTES // 4])

        def carve(off_bytes, shape, dt):
            n = int(np.prod(shape))
            esz = 4 if dt in (F32, I32, U32) else 2
            assert off_bytes % 4 == 0
            a = scr[:, off_bytes // 4: off_bytes // 4 + (n * esz + 3) // 4]
            if dt != F32:
                a = a.bitcast(dt)
            a = a[:, 0:n]
            if len(shape) == 2:
                pat = "p (a b) -> p a b"
                return a.rearrange(pat, a=shape[0]), off_bytes + n * esz
            if len(shape) == 3:
                return a.rearrange("p (a b c) -> p a b c", a=shape[0], b=shape[1]), off_bytes + n * esz
            if len(shape) == 1:
                return a, off_bytes + n * esz
            raise ValueError

        self.ld(consts[:], consts_d, 'consts')
        self.ld(flag[:], flag_d, 'flag')
        self.ld(badaT[:], badaT_d, 'badaT')
        self.cp(identb[:], consts[:, 0:128], ['consts'], 'identb')
        self.cp(trib[:], consts[:, 128:256], ['consts'], 'trib')
        self.memset(self.ones32[:], 1.0, 'ones32')
        self.memset(onesb[:], 1.0, 'onesb')
        self.epsT = sb("epsT", [128, 1])
        self.memset(self.epsT[:], LN_EPS, 'epsT')

        ctmp_, o_ = carve(0, [1, D], F32)
        ctmp2_, o_ = carve(o_, [1, D], F32)
        ctmp = ctmp_[0:8, 0, :]
        ctmp2 = ctmp2_[0:8, 0, :]
        self.ld(ctmp, cvec_d, 'ctmp')
        self.act(ctmp2, ctmp, AF.Silu, ['ctmp'], 'ctmp2')
        for kc in range(8):
            self.tr(ps[0][:, kc * 8:(kc + 1) * 8], ctmp2[:, kc * 128:(kc + 1) * 128], ident[0:8, 0:8], ['ctmp2', 'consts'], 'ps0')
        self.cp(scT[:].rearrange("p a b -> p (a b)"), ps[0][:, 0:64], ['ps0'], 'scT')
        S.barrier()

        def ada(hl):
            layer, part = hl // 2, hl % 2
            base = part * 3 * D
            S.barrier()
            off = 0
            wa = []
            for i in range(2):
                a, off = carve(off, [8, 512], F32)
                wa.append(a)
            bPl, off = carve(off, [8, 128], F32)
            bSl, off = carve(off, [8, 128], F32)
            for kc in range(8):
                self.ts(bPl[:, kc, :], self.ones32[:], scT[:, kc, 0:1], None, ALU.mult, None, ['ones32', 'scT'], 'bPl')
            self.memset(bSl[:].rearrange("p a b -> p (a b)"), 0.0, 'bSl')
            for kc in range(8):
                self.cp(bSl[:, kc, 0:SPC], scT[:, kc, 1:1 + SPC], ['scT', 'bSl'], 'bSl')
            wsrc = wada_d[layer].rearrange("(kc p) n -> p kc n", p=128)
            for cgi in range(6):
                c0 = base + cgi * 512
                wt = wa[cgi % 2]
                wk = 'wa%d' % (cgi % 2)
                self.ld(wt[:], wsrc[:, :, c0:c0 + 512], wk)
                v = cgi // 2
                if v < 2:
                    for mi in range(4):
                        mc = (cgi % 2) * 4 + mi
                        for kc in range(8):
                            self.mm(ps[1][:, (v * 8 + mc) * 8:(v * 8 + mc) * 8 + 8], wt[:, kc, mi * 128:(mi + 1) * 128], scT[:, kc, :],
                                    kc == 0, kc == 7, [wk, 'scT'], 'ps1')
                else:
                    half = cgi % 2
                    for (lt, lk, pb) in ((bPl, 'bPl', 2), (bSl, 'bSl', 3)):
                        for kc in range(8):
                            self.mm(ps[pb][:, :], lt[:, kc, :], wt[:, kc, :], kc == 0, False, [wk, lk], PK[pb])
                        self.ld(self.rowtmp[0:1, 0:512], bada_d[layer:layer + 1, c0:c0 + 512], 'rowtmp')
                        self.mm(ps[pb][:, :], self.ones32[0:1, :], self.rowtmp[0:1, 0:512], False, True, ['rowtmp', 'ones32'], PK[pb])
                        dst = gP if pb == 2 else gS
                        self.cp(dst[:, half * 512:(half + 1) * 512], ps[pb][:, :], [PK[pb]], 'gP' if pb == 2 else 'gS', eng='act')
            bb = badaT[:, layer * 48 + part * 24: layer * 48 + part * 24 + 16]
            self.tt(modT[:].rearrange("p v c r -> p (v c) r"), ps[1][:, 0:128].rearrange("p (a r) -> p a r", r=8),
                    bb.unsqueeze(2).broadcast_to([128, 16, 8]), ALU.add, ['ps1', 'badaT'], 'modT')
            self.ts(modT[:, 1, :, :], modT[:, 1, :, :], 1.0, None, ALU.add, None, ['modT'], 'modT')
            self.bcast_row(lnG, lng_d[hl:hl + 1, :], D, 'lnG', 'lnG')
            self.bcast_row(lnB, lnb_d[hl:hl + 1, :], D, 'lnB', 'lnB')
            S.barrier()

        def tile_to_hT(src, skey, hdst, col0, sample=False, xT32=None):
            for hf in range(2):
                pb = 4 + hf
                for q in range(4):
                    kc = hf * 4 + q
                    self.tr(ps[pb][:, q * 128:(q + 1) * 128], src[:, kc * 128:(kc + 1) * 128], ident, [skey, 'consts'], PK[pb])
                for q in range(4):
                    kc = hf * 4 + q
                    if not sample:
                        self.act(hdst[:, kc, col0:col0 + 128], ps[pb][:, q * 128:(q + 1) * 128], AF.Identity,
                                 [PK[pb], 'modT'], 'hTg', bias=modT[:, 0, kc, 0:1], scale=modT[:, 1, kc, 0:1])
                        if xT32 is not None:
                            self.cp(xT32[:, kc, :], ps[pb][:, q * 128:(q + 1) * 128], [PK[pb]], 'xT32', eng='act')
                    else:
                        tmp = self.smod
                        self.tt(tmp[:, 0:SPC], ps[pb][:, q * 128:q * 128 + SPC], modT[:, 1, kc, 1:1 + SPC], ALU.mult, [PK[pb], 'modT'], 'smod')
                        self.tt(tmp[:, 0:SPC], tmp[:, 0:SPC], modT[:, 0, kc, 1:1 + SPC], ALU.add, ['smod', 'modT'], 'smod')
                        self.cp(hdst[:, kc, col0:col0 + 128], tmp[:], ['smod'], 'hTg')
                        if xT32 is not None:
                            self.cp(xT32[:, kc, :], tmp[:], ['smod'], 'xT32')

        self.smod = sb("smod", [128, 128])
        self.memset(self.smod[:], 0.0, 'smod')

        rt_ = sb("rot_tmp", [128, 4, 8, 16])

        def rotary(xv, nh, cs, xkey, rkey='rope'):
            cosb = cs[:, 0:16].unsqueeze(1).broadcast_to([128, nh, 16])
            sinb = cs[:, 16:32].unsqueeze(1).broadcast_to([128, nh, 16])
            x1 = xv[:, :, 0:16]
            x2 = xv[:, :, 16:32]
            self.tt(rt_[:, 0, 0:nh, :], x1, cosb, ALU.mult, [xkey, rkey], 'rt0')
            self.tt(rt_[:, 1, 0:nh, :], x2, sinb, ALU.mult, [xkey, rkey], 'rt1')
            self.tt(rt_[:, 2, 0:nh, :], x2, cosb, ALU.mult, [xkey, rkey], 'rt2')
            self.tt(rt_[:, 3, 0:nh, :], x1, sinb, ALU.mult, [xkey, rkey], 'rt3')
            self.tt(x1, rt_[:, 0, 0:nh, :], rt_[:, 1, 0:nh, :], ALU.subtract, ['rt0', 'rt1'], xkey)
            self.tt(x2, rt_[:, 2, 0:nh, :], rt_[:, 3, 0:nh, :], ALU.add, ['rt2', 'rt3'], xkey)

        ada(0)
        off = 0
        KT, off = carve(off, [2, NGT * 128], BF16)
        VS, off = carve(off, [NGT, 2, 130], BF16)
        kmT, off = carve(off, [2, NSLOT], BF16)
        off = (off + 3) // 4 * 4
        offC = off
        wkv, off = carve(off, [8, 512], BF16)
        ropes, off = carve(off, [NGT + 1, 32], F32)
        xg0, off = carve(off, [1, D], F32)
        xg1, off = carve(off, [1, D], F32)
        hT1, off = carve(off, [8, 128], BF16)
        kvf, off = carve(off, [1, 512], F32)
        kb16, off = carve(off, [1, 256], BF16)
        kvf1, off = carve(off, [1, 512], F32)
        kb161, off = carve(off, [1, 256], BF16)
        assert off <= SCR_BYTES, off
        xgt = [xg0[:, 0, :], xg1[:, 0, :]]
        kvf = kvf[:, 0, :]
        kb16 = kb16[:, 0, :]
        kvf1 = kvf1[:, 0, :]
        kb161 = kb161[:, 0, :]
        self.hT1 = hT1

        self.ld(ropes[:].rearrange("p t c -> p (t c)"), rope_d, 'rope')
        self.S.dma('pool', wkv[:], wqkv_d.rearrange("(kc p) n -> p kc n", p=128)[:, :, 1024:1536], writes=['wkv'])
        self.memset(VS[:, :, :, 128:130], 1.0, 'VS')

        kvf_b = [kvf, kvf1]
        kb16_b = [kb16, kb161]

        def kv_part1(src, skey, rope_idx, g):
            par = 0 if g is None else g % 2
            pso = ps[0] if par == 0 else ps[2]
            pk = 'ps0' if par == 0 else 'ps2'
            tile_to_hT(src, skey, hT1, 0, sample=(g is None))
            for kc in range(8):
                self.mm(pso[:, :], hT1[:, kc, 0:128], wkv[:, kc, :], kc == 0, kc == 7, ['hTg', 'wkv'], pk)
            dstf = kvf_b[par] if g is not None else kvS
            dkey = ('kvf%d' % par) if g is not None else 'kvS'
            self.cp(dstf[:], pso[:, :], [pk], dkey)
            rotary(dstf[:, 0:256].rearrange("p (h d) -> p h d", h=2), 2, ropes[:, rope_idx, :], dkey)
            if g is not None:
                self.cp(VS[:, g, :, 0:128], dstf[:, 256:512].rearrange("p (h d) -> p h d", h=2), [dkey], 'VS', eng='act')
                self.cp(kb16_b[par][:], dstf[:, 0:256], [dkey], 'kb16%d' % par, eng='act')
            else:
                self.st(k_s[:, :], kvS[0:SPC, 0:256], 'kvS', 'k_s')
                self.st(v_s[:, :], kvS[0:SPC, 256:512], 'kvS', 'v_s')

        def kv_part2(g):
            par = g % 2
            kf_, kb_ = kvf_b[par], kb16_b[par]
            fk, bk = 'kvf%d' % par, 'kb16%d' % par
            pt = ps[1][:].bitcast(BF16)
            for kv in range(2):
                self.tr(pt[:, kv * 128:(kv + 1) * 128], kb_[:, kv * 128:(kv + 1) * 128], identb[:], [bk, 'identb'], 'ps1')
            self.cp(KT[:, :, g * 128:(g + 1) * 128], pt[:, 0:256].rearrange("p (h t) -> p h t", h=2), ['ps1'], 'KT')
            for kv in range(2):
                self.mm(ps[7][:, kv * NGT + g: kv * NGT + g + 1], kf_[:, kv * 128:(kv + 1) * 128], self.ones32[:, 0:1],
                        True, True, [fk, 'ones32'], 'ps7')
            if 2 <= g < 2 + 2 * LS:
                pg = g - 2
                self.st(k_p[pg].rearrange("h r d -> r h d"), kf_[:, 0:256].rearrange("p (h d) -> p h d", h=2), fk, 'k_p')
                self.st(v_p[pg].rearrange("h r d -> r h d"), kf_[:, 256:512].rearrange("p (h d) -> p h d", h=2), fk, 'v_p')

        for g in range(NGT + 1):
            if g < NGT:
                src, skey = xgt[g % 2], 'xgt%d' % (g % 2)
                self.ld(src, xg[g * 128:(g + 1) * 128, :], skey)
                kv_part1(src, skey, g, g)
            if g >= 1:
                kv_part2(g - 1)
        tS = NT - 1
        self.ld(xres[:, tS, :], xs_d, XK[tS])
        kv_part1(xres[:, tS, :], XK[tS], NGT, None)

        self.cp(ks32[:], ps[7][:, 0:2 * NGT], ['ps7'], 'ks32')
        ksv = ks32[:].rearrange("p (k s two) -> p k s two", k=2, two=2)
        self.tt(ksv[:, :, :, 0], ksv[:, :, :, 0], ksv[:, :, :, 1], ALU.add, ['ks32'], 'ks32')
        self.ts(kmT[:], ksv[:, :, :, 0], 1.0 / 256.0, None, ALU.mult, None, ['ks32'], 'kmT')
        if cfg.stop == 'proj':
            self.finish(dbg, xres, XK)
            return

        S.barrier()
        off = offC
        wqb = []
        for i in range(2):
            a_, off = carve(off, [8, 512], BF16)
            wqb.append(a_)
        ropel, off = carve(off, [NT, 32], F32)
        v01, off = carve(off, [NPT, NSLOT], F32)
        QT1, off = carve(off, [8, 128], BF16)
        hT1, off = carve(off, [8, 128], BF16)
        acc, off = carve(off, [8, 130], F32)
        PT = []
        for i in range(4):
            a_, off = carve(off, [4, 128], BF16)
            PT.append(a_)
        aT = hT1
        qb16, off = carve(off, [8, 128], BF16)
        zq, off = carve(off, [8, 128], F32)
        gsb, off = carve(off, [8, NSLOT], F32)
        sel, off = carve(off, [8, NSLOT], F32)
        t8, off = carve(off, [8, 8], F32)
        nm, off = carve(off, [1, NSLOT], F32)
        rden, off = carve(off, [1, 8], F32)
        assert off <= SCR_BYTES, off
        zq2 = zq[:].rearrange("p h d -> p (h d)")
        qb2 = qb16[:].rearrange("p h d -> p (h d)")

        self.ld(ropel[:, 0:NPT, :].rearrange("p t c -> p (t c)"), rope_d[:, 32:(NPT + 1) * 32], 'ropel')
        self.ld(ropel[:, NPT, :], rope_d[:, NGT * 32:(NGT + 1) * 32], 'ropel')
        self.ld(v01[:].rearrange("p a b -> p (a b)"), v01_d, 'v01')
        wq_src = wqkv_d.rearrange("(kc p) n -> p kc n", p=128)
        wo_src = wo_d.rearrange("(kc p) n -> p kc n", p=128)

        def q_proj(tau, sample):
            tile_to_hT(xres[:, tau, :], XK[tau], hT1, 0, sample=sample)
            for hf in range(2):
                self.S.dma('pool', wqb[hf][:], wq_src[:, :, hf * 512:(hf + 1) * 512], writes=['wqb%d' % hf])
                for kc in range(8):
                    self.mm(ps[2 + hf][:, :], hT1[:, kc, :], wqb[hf][:, kc, :], kc == 0, kc == 7, ['hTg', 'wqb%d' % hf], PK[2 + hf])
                self.cp(zq2[:, hf * 512:(hf + 1) * 512], ps[2 + hf][:, :], [PK[2 + hf]], 'zq', eng='act')
            rotary(zq[:], 8, ropel[:, tau, :], 'zq', 'ropel')

        def load_wo():
            for hf in range(2):
                self.S.dma('pool', wqb[hf][:], wo_src[:, :, hf * 512:(hf + 1) * 512], writes=['wqb%d' % hf])

        def out_proj(tau, gate_tile, gkey, load_w=False):
            pt = ps[6][:].bitcast(BF16)
            for h in range(8):
                self.tr(pt[:, h * 128:(h + 1) * 128], qb16[:, h, :], identb[:], ['qb16', 'identb'], 'ps6')
            self.cp(aT[:].rearrange("p h t -> p (h t)"), pt[:, :], ['ps6'], 'hTg')
            if load_w:
                load_wo()
            for hf in range(2):
                for h in range(8):
                    self.mm(ps[6 + hf][:, :], aT[:, h, :], wqb[hf][:, h, :], h == 0, h == 7, ['hTg', 'wqb%d' % hf], PK[6 + hf])
                self.tt(zq2[:, hf * 512:(hf + 1) * 512], ps[6 + hf][:, :], gate_tile[:, hf * 512:(hf + 1) * 512], ALU.mult, [PK[6 + hf], gkey], 'zq')
            self.stt(zq2, xres[:, tau, :], cfg.ALPHA, zq2, ALU.mult, ALU.add, [XK[tau], 'zq'], 'zq')
            self.layernorm(zq2, xres[:, tau, :], lnG[:], lnB[:], 'zq', XK[tau], 'lnG')

        for tau in range(NPT):
            self.ld(xres[:, tau, :], xg[(tau + 1) * 128:(tau + 2) * 128, :], XK[tau])
            q_proj(tau, False)
            load_wo()
            self.cp(qb2, zq2, ['zq'], 'qb16', eng='act')
            pt = ps[6][:].bitcast(BF16)
            for h in range(8):
                self.tr(pt[:, h * 128:(h + 1) * 128], qb16[:, h, :], identb[:], ['qb16', 'identb'], 'ps6')
            self.cp(QT1[:].rearrange("p h t -> p (h t)"), pt[:, :], ['ps6'], 'QT1')
            for h in range(8):
                self.mm(ps[4][:, h * NSLOT:(h + 1) * NSLOT], QT1[:, h, :], kmT[:, h // 4, :], True, True, ['QT1', 'kmT'], 'ps4')
            self.ts(nm[:, 0, :], v01[:, tau, :], 1.0, BIG, ALU.subtract, ALU.mult, ['v01'], 'nm')
            v01b = v01[:, tau, :].unsqueeze(1).broadcast_to([128, 8, NSLOT])
            self.tt(gsb[:], ps[4][:, 0:8 * NSLOT].rearrange("p (h s) -> p h s", h=8), v01b, ALU.mult, ['ps4', 'v01'], 'gsb')
            self.tt(gsb[:], gsb[:], nm[:, 0, :].unsqueeze(1).broadcast_to([128, 8, NSLOT]), ALU.add, ['gsb', 'nm'], 'gsb')
            for h in range(8):
                self.S.op('dve', lambda e: e.max(out=t8[:, h, :], in_=gsb[:, h, :]), reads=['gsb'], writes=['t8'])
            self.tt(sel[:], gsb[:], t8[:, :, 2:3].broadcast_to([128, 8, NSLOT]), ALU.is_ge, ['gsb', 't8'], 'sel')
            self.tt(sel[:], sel[:], v01b, ALU.mult, ['sel', 'v01'], 'sel')
            for h in range(8):
                self.memset(acc[:, h, :], 0.0, 'acc%d' % h)
            m_own = (tau + 1) // 2
            second = (tau + 1) % 2
            its = [(kv, s_) for kv in range(2) for s_ in range(NSLOT)]

            def kts_of(s_):
                if s_ == m_own and not second:
                    return [0]
                return [0, 1]

            def qk_exp(n):
                kv, s_ = its[n]
                own = (s_ == m_own)
                qrhs = QT1[:, kv * 4:(kv + 1) * 4, :].rearrange("p h q -> p (h q)")
                for kt in kts_of(s_):
                    g = 2 * s_ + kt
                    pi = (n % 2) * 2 + kt
                    self.mm(ps[kt][:, :], KT[:, kv, g * 128:(g + 1) * 128], qrhs, True, True, ['KT', 'QT1'], PK[kt])
                    self.act(PT[pi][:].rearrange("p h q -> p (h q)"), ps[kt][:, :], AF.Exp, [PK[kt]], 'PT%d' % pi, scale=ATTN_SCALE)
                    if own and kt == (1 if second else 0):
                        self.tt(PT[pi][:], PT[pi][:], trib[:].unsqueeze(1).broadcast_to([128, 4, 128]), ALU.mult, ['PT%d' % pi, 'trib'], 'PT%d' % pi)

            def pv_acc(n):
                kv, s_ = its[n]
                own = (s_ == m_own)
                kts = kts_of(s_)
                ob = 2 + 2 * (n % 2)
                for hh in range(4):
                    bank = ps[ob + hh // 2]
                    col = (hh % 2) * 256
                    for i, kt in enumerate(kts):
                        pi = (n % 2) * 2 + kt
                        self.mm(bank[:, col:col + 129], PT[pi][:, hh, :], VS[:, 2 * s_ + kt, kv, 0:129], i == 0, i == len(kts) - 1,
                                ['PT%d' % pi, 'VS'], PK[ob + hh // 2])
                for hh in range(4):
                    h = kv * 4 + hh
                    bank = ps[ob + hh // 2]
                    col = (hh % 2) * 256
                    if own:
                        self.tt(acc[:, h, 0:129], acc[:, h, 0:129], bank[:, col:col + 129], ALU.add, ['acc%d' % h, PK[ob + hh // 2]], 'acc%d' % h)
                    else:
                        self.stt(acc[:, h, 0:129], bank[:, col:col + 129], sel[:, h, s_:s_ + 1], acc[:, h, 0:129], ALU.mult, ALU.add,
                                 ['acc%d' % h, 'sel', PK[ob + hh // 2]], 'acc%d' % h)

            for n in range(len(its) + 1):
                if n < len(its):
                    qk_exp(n)
                if n >= 1:
                    pv_acc(n - 1)
            AK = ['acc%d' % h for h in range(8)]
            self.S.op('dve', lambda e: e.reciprocal(out=rden[:, 0, :], in_=acc[:, :, 128]), reads=AK, writes=['rden'])
            self.tt(qb16[:], acc[:, :, 0:128], rden[:, 0, :].unsqueeze(2).broadcast_to([128, 8, 128]), ALU.mult, AK + ['rden'], 'qb16')
            out_proj(tau, gP, 'gP')

        tS = NT - 1
        q_proj(tS, True)
        for h in range(8):
            self.tr(ps[0][:, h * 4:(h + 1) * 4], zq[0:SPC, h, :], ident[0:SPC, 0:SPC], ['zq', 'consts'], 'ps0')
        S.barrier()
        off = 0
        c2, off = carve(off, [1, 512], F32)
        QTa, off = carve(off, [1, 32], F32)
        QTm, off = carve(off, [8, 32], F32)
        qsel, off = carve(off, [1, 128], F32)
        ptAi, off = carve(off, [1, 4], I32)
        ptBi, off = carve(off, [1, 2 * NBLK], I32)
        ptAf, off = carve(off, [1, 4], F32)
        idxAf, off = carve(off, [4, 8], F32)
        idxA, off = carve(off, [4, 8], I32)
        idxSf, off = carve(off, [6, 4], F32)
        idxS, off = carve(off, [6, 4], I32)
        ptBf, off = carve(off, [1, 2 * NBLK], F32)
        ksum0, off = carve(off, [2, 128], F32)
        ksum1, off = carve(off, [2, 128], F32)
        ksr, off = carve(off, [1, 128], F32)
        kmTs, off = carve(off, [4, 128], F32)
        gate_s, off = carve(off, [1, NBLK], F32)
        t8s, off = carve(off, [1, 8], F32)
        oh, off = carve(off, [1, NBLK], F32)
        ohp, off = carve(off, [1, NBLK], F32)
        phys, off = carve(off, [1, 8], F32)
        idxf, off = carve(off, [1, 8], F32)
        idxi, off = carve(off, [1, 8], I32)
        ksel, off = carve(off, [1, 128], F32)
        vsel, off = carve(off, [1, 128], F32)
        sc, off = carve(off, [1, 6 * 128 + 8], F32)
        Pm, off = carve(off, [1, 6 * 128 + 8], F32)
        den, off = carve(off, [1, 1], F32)
        pv, off = carve(off, [24, 128], F32)
        osum, off = carve(off, [1, 128], F32)
        qb16, off = carve(off, [8, 128], BF16)
        aT, off = carve(off, [8, 128], BF16)
        zq, off = carve(off, [8, 128], F32)
        wqb = []
        for i in range(2):
            a_, off = carve(off, [8, 512], BF16)
            wqb.append(a_)
        G0, off = carve(off, [1, 32 * 128], F32)
        G1, off = carve(off, [1, 32 * 128], F32)
        assert off <= SCR_BYTES, off
        GB = [G0[:, 0, :], G1[:, 0, :]]
        zq2 = zq[:].rearrange("p h d -> p (h d)")
        c2 = c2[:, 0, :]
        ksum = [ksum0, ksum1]
        self.cp(QTa[:, 0, :], ps[0][:, 0:32], ['ps0'], 'QTa')
        self.ld(c2, consts2_d, 'c2')
        self.ld(ptAi[:, 0, :], ptA_d, 'ptAi')
        self.ld(ptBi[0:32, 0, :], ptB_d, 'ptBi')
        self.cp(ptBf[0:32, 0, :], ptBi[0:32, 0, :], ['ptBi'], 'ptBf')
        self.cp(ptAf[:, 0, :], ptAi[:, 0, :], ['ptAi'], 'ptAf')
        self.ts(idxAf[:], ptAf[:, 0, :].unsqueeze(2).broadcast_to([128, 4, 8]), 8.0, None, ALU.mult, None, ['ptAf'], 'idxAf')
        self.tt(idxAf[:], idxAf[:], c2[:, 353:361].unsqueeze(1).broadcast_to([128, 4, 8]), ALU.add, ['idxAf', 'c2'], 'idxAf')
        self.cp(idxA[:], idxAf[:], ['idxAf'], 'idxA')
        ck8 = ck_d.rearrange("n (c x) -> (n c) x", x=4096)
        cv8 = cv_d.rearrange("n (c x) -> (n c) x", x=4096)
        self.tt(QTm[:], QTa[:, 0, :].unsqueeze(1).broadcast_to([128, 8, 32]), c2[:, 0:256].rearrange("p (a b) -> p a b", a=8), ALU.mult, ['QTa', 'c2'], 'QTm')
        self.tr(ps[1][0:32, 0:128], QTa[:, 0, :], ident, ['QTa', 'consts'], 'ps1')
        self.cp(qsel[0:32, 0, :], ps[1][0:32, 0:128], ['ps1'], 'qsel')
        RC = 32
        gi = 0
        for r in range(2):
            for kv in range(2):
                first = True
                for eo in range(2):
                    for rc in range(128 // RC):
                        gb = GB[gi % 2][:, 0:RC * 128]
                        gk = 'G%d' % (gi % 2)
                        self.S.dma('pool', None, None, reads=['idxA'], writes=[gk],
                                   fn=lambda e: e.indirect_dma_start(out=gb, out_offset=None, in_=ck8,
                                                                     in_offset=bass.IndirectOffsetOnAxis(ap=idxA[:, r * 2 + eo, kv * 4 + rc:kv * 4 + rc + 1], axis=0)))
                        dst = ksum[r][:, kv, :] if first else ksr[:, 0, :]
                        dk = 'ksum%d' % r if first else 'ksr'
                        self.S.op('dve', lambda e: e.tensor_reduce(out=dst, in_=gb.rearrange("p (r d) -> p d r", d=128), axis=AX.X, op=ALU.add),
                                  reads=[gk], writes=[dk])
                        if not first:
                            self.tt(ksum[r][:, kv, :], ksum[r][:, kv, :], ksr[:, 0, :], ALU.add, ['ksum%d' % r, 'ksr'], 'ksum%d' % r)
                        first = False
                        gi += 1
        for r in range(2):
            for kv in range(2):
                self.tr(ps[2][:, (kv * 2 + r) * 128:(kv * 2 + r + 1) * 128], ksum[r][:, kv, :], ident, ['ksum%d' % r, 'consts'], 'ps2')
        self.ts(kmTs[:].rearrange("p a b -> p (a b)"), ps[2][:, :], 1.0 / 256.0, None, ALU.mult, None, ['ps2'], 'kmTs')
        n = 0
        for s_ in range(SPC):
            for kv in range(2):
                self.mm(ps[3][0:32, 0:NBLK], QTm[:, s_ * 2 + kv, :], kmTs[:, kv * 2 + s_ // 2, (s_ % 2) * NBLK:(s_ % 2 + 1) * NBLK],
                        n == 0, n == 2 * SPC - 1, ['QTm', 'kmTs'], 'ps3')
                n += 1
        self.cp(gate_s[0:32, 0, :], ps[3][0:32, 0:NBLK], ['ps3'], 'gate_s')
        self.S.op('dve', lambda e: e.max(out=t8s[0:32, 0, :], in_=gate_s[0:32, 0, :]), reads=['gate_s'], writes=['t8s'])
        for j in range(3):
            self.ts(oh[0:32, 0, :], gate_s[0:32, 0, :], t8s[0:32, 0, j:j + 1], None, ALU.is_equal, None, ['gate_s', 't8s'], 'oh')
            for eo in range(2):
                self.tt(ohp[0:32, 0, :], oh[0:32, 0, :], ptBf[0:32, 0, eo * NBLK:(eo + 1) * NBLK], ALU.mult, ['oh', 'ptBf'], 'ohp')
                self.S.op('dve', lambda e: e.tensor_reduce(out=phys[0:32, 0, j * 2 + eo:j * 2 + eo + 1], in_=ohp[0:32, 0, :], axis=AX.X, op=ALU.add),
                          reads=['ohp'], writes=['phys'])
        self.ts(idxf[0:32, 0, 0:6], phys[0:32, 0, 0:6], 2.0, c2[0:32, 256:257], ALU.mult, ALU.add, ['phys', 'c2'], 'idxf')
        self.ts(idxSf[0:32], idxf[0:32, 0, 0:6].unsqueeze(2).broadcast_to([32, 6, 4]), 4.0, None, ALU.mult, None, ['idxf'], 'idxSf')
        self.tt(idxSf[0:32], idxSf[0:32], c2[0:32, 353:357].unsqueeze(1).broadcast_to([32, 6, 4]), ALU.add, ['idxSf', 'c2'], 'idxSf')
        self.cp(idxS[0:32], idxSf[0:32], ['idxSf'], 'idxi')
        for (dst, dk, c0) in ((ksel, 'ksel', 0), (vsel, 'vsel', 256)):
            self.mm(ps[4][0:32, 0:128], c2[0:SPC, 257:289], kvS[0:SPC, c0:c0 + 128], True, False, ['c2', 'kvS'], 'ps4')
            self.mm(ps[4][0:32, 0:128], c2[0:SPC, 289:321], kvS[0:SPC, c0 + 128:c0 + 256], False, True, ['c2', 'kvS'], 'ps4')
            self.cp(dst[0:32, 0, :], ps[4][0:32, 0:128], ['ps4'], dk)
        ck2 = ck_d.rearrange("n (h x) -> (n h) x", h=2)
        cv2 = cv_d.rearrange("n (h x) -> (n h) x", h=2)
        qb_ = qsel[0:32, 0, :].unsqueeze(1).broadcast_to([32, 32, 128])
        for c in range(6):
            for rh in range(4):
                gb = GB[gi % 2][0:32, :]
                gk = 'G%d' % (gi % 2)
                self.S.dma('pool', None, None, reads=['idxi'], writes=[gk],
                           fn=lambda e: e.indirect_dma_start(out=gb, out_offset=None, in_=ck8,
                                                             in_offset=bass.IndirectOffsetOnAxis(ap=idxS[0:32, c, rh:rh + 1], axis=0)))
                g3 = gb.rearrange("p (r d) -> p r d", d=128)
                self.tt(g3, g3, qb_, ALU.mult, [gk, 'qsel'], gk)
                self.S.op('dve', lambda e: e.tensor_reduce(out=sc[0:32, 0, c * 128 + rh * 32:c * 128 + rh * 32 + 32], in_=g3, axis=AX.X, op=ALU.add),
                          reads=[gk], writes=['sc'])
                gi += 1
        self.tt(osum[0:32, 0, :], qsel[0:32, 0, :], ksel[0:32, 0, :], ALU.mult, ['qsel', 'ksel'], 'osum')
        self.S.op('dve', lambda e: e.tensor_reduce(out=sc[0:32, 0, 768:769], in_=osum[0:32, 0, :], axis=AX.X, op=ALU.add), reads=['osum'], writes=['sc'])
        self.S.op('act', lambda e: e.activation(out=Pm[0:32, 0, 0:769], in_=sc[0:32, 0, 0:769], func=AF.Exp, scale=ATTN_SCALE),
                  reads=['sc'], writes=['Pm'])
        self.S.op('dve', lambda e: e.tensor_reduce(out=den[0:32, 0, :], in_=Pm[0:32, 0, 0:769], axis=AX.X, op=ALU.add), reads=['Pm'], writes=['den'])
        for c in range(6):
            for rh in range(4):
                gb = GB[gi % 2][0:32, :]
                gk = 'G%d' % (gi % 2)
                self.S.dma('pool', None, None, reads=['idxi'], writes=[gk],
                           fn=lambda e: e.indirect_dma_start(out=gb, out_offset=None, in_=cv8,
                                                             in_offset=bass.IndirectOffsetOnAxis(ap=idxS[0:32, c, rh:rh + 1], axis=0)))
                g3 = gb.rearrange("p (r d) -> p r d", d=128)
                pb_ = Pm[0:32, 0, c * 128 + rh * 32:c * 128 + rh * 32 + 32].unsqueeze(2).broadcast_to([32, 32, 128])
                self.tt(g3, g3, pb_, ALU.mult, [gk, 'Pm'], gk)
                self.S.op('dve', lambda e: e.tensor_reduce(out=pv[0:32, c * 4 + rh, :], in_=gb.rearrange("p (r d) -> p d r", d=128), axis=AX.X, op=ALU.add),
                          reads=[gk], writes=['pv'])
                gi += 1
        self.S.op('dve', lambda e: e.tensor_reduce(out=osum[0:32, 0, :], in_=pv[0:32, :, :].rearrange("p i d -> p d i"), axis=AX.X, op=ALU.add),
                  reads=['pv'], writes=['osum'])
        self.stt(osum[0:32, 0, :], vsel[0:32, 0, :], Pm[0:32, 0, 768:769], osum[0:32, 0, :], ALU.mult, ALU.add, ['vsel', 'Pm', 'osum'], 'osum')
        self.S.op('dve', lambda e: e.reciprocal(out=den[0:32, 0, :], in_=den[0:32, 0, :]), reads=['den'], writes=['den'])
        self.ts(osum[0:32, 0, :], osum[0:32, 0, :], den[0:32, 0, 0:1], None, ALU.mult, None, ['osum', 'den'], 'osum')
        for h in range(8):
            self.mm(ps[2 + h // 4][0:SPC, (h % 4) * 128:(h % 4 + 1) * 128], c2[0:32, 321 + h * 4:321 + h * 4 + 4], osum[0:32, 0, :], True, True,
                    ['c2', 'osum'], PK[2 + h // 4])
        self.memset(qb16[:].rearrange("p h d -> p (h d)"), 0.0, 'qb16')
        for hf in range(2):
            self.cp(qb16[0:SPC, hf * 4:(hf + 1) * 4, :].rearrange("p h d -> p (h d)"), ps[2 + hf][0:SPC, :], [PK[2 + hf], 'qb16'], 'qb16')
        out_proj(tS, gS, 'gS', load_w=True)

        if cfg.stop == 'attn':
            self.finish(dbg, xres, XK)
            return

        TG = NT // cfg.MG

        def moe(layer):
            ada(2 * layer + 1)
            if cfg.stop == 'ada1':
                return
            off = 0
            wr32, off = carve(off, [8, NE], F32)
            wrm, off = carve(off, [8, NE], F32)
            rb, off = carve(off, [1, NE], F32)
            brB, off = carve(off, [1, NE], F32)
            Wt, off = carve(off, [NT, NE], F32)
            hTm, off = carve(off, [8, TG * 128], BF16)
            xT32, off = carve(off, [8, 128], F32)
            accm, off = carve(off, [TG, D], F32)
            wg2, wu2, wd2, AT, sg, Ab = [], [], [], [], [], []
            for i in range(2):
                a_, off = carve(off, [8, 512], BF16); wg2.append(a_)
                a_, off = carve(off, [8, 512], BF16); wu2.append(a_)
                a_, off = carve(off, [4, D], BF16); wd2.append(a_)
                a_, off = carve(off, [4, 128], BF16); AT.append(a_)
                a_, off = carve(off, [1, 512], F32); sg.append(a_)
                a_, off = carve(off, [1, 512], BF16); Ab.append(a_)
            sc_, off = carve(off, [1, NE], F32)
            bi, off = carve(off, [NG, 8], F32)
            msk, off = carve(off, [NG, 8], F32)
            t8g, off = carve(off, [NG, 8], F32)
            gs, off = carve(off, [1, NG], F32)
            gmax, off = carve(off, [1, 1], F32)
            ohg, off = carve(off, [1, NG], F32)
            t8e, off = carve(off, [1, 8], F32)
            sel2, off = carve(off, [1, NE], F32)
            wun, off = carve(off, [1, NE], F32)
            dsum, off = carve(off, [1, 1], F32)
            zq, off = carve(off, [1, D], F32)
            assert off <= SCR_BYTES, off
            zq2 = zq[:, 0, :]
            bi2 = bi[:].rearrange("p g e -> p (g e)")
            msk2 = msk[:].rearrange("p g e -> p (g e)")

            self.ld(wr32[:], wr_d.rearrange("(kc p) e -> p kc e", p=128), 'wr32')
            for kc in range(8):
                self.ts(wrm[:, kc, :], wr32[:, kc, :], modT[:, 1, kc, 0:1], None, ALU.mult, None, ['wr32', 'modT'], 'wrm')
                self.mm(ps[0][0:1, 0:NE], modT[:, 0, kc, 0:1], wr32[:, kc, :], kc == 0, kc == 7, ['modT', 'wr32'], 'ps0')
            self.cp(rb[0:1, 0, :], ps[0][0:1, 0:NE], ['ps0'], 'rb')
            self.bcast_row(brB[:, 0, :], br_d, NE, 'brB', 'brB')
            if cfg.stop == 'rsetup':
                return

            for G in range(cfg.MG):
                tiles = list(range(G * TG, (G + 1) * TG))
                if layer == 1 and G == 0:
                    tiles = tiles[1:]
                for ti, tau in enumerate(tiles):
                    smp = (tau == NT - 1)
                    tile_to_hT(xres[:, tau, :], XK[tau], hTm, ti * 128, sample=smp, xT32=xT32)
                    if cfg.stop == 'r_0':
                        return
                    wsel, wk = (wr32, 'wr32') if smp else (wrm, 'wrm')
                    for kc in range(8):
                        self.mm(ps[6][:, 0:NE], xT32[:, kc, :], wsel[:, kc, :], kc == 0, smp and kc == 7, ['xT32', wk], 'ps6')
                    if not smp:
                        self.mm(ps[6][:, 0:NE], self.ones32[0:1, :], rb[0:1, 0, :], False, True, ['ones32', 'rb'], 'ps6')
                    if cfg.stop == 'r_a':
                        return
                    self.act(sc_[:, 0, :], ps[6][:, 0:NE], AF.Sigmoid, ['ps6'], 'sc_')
                    self.tt(bi2, sc_[:, 0, :], brB[:, 0, :], ALU.add, ['sc_', 'brB'], 'bi')
                    for g in range(NG):
                        self.S.op('dve', lambda e: e.max(out=t8g[:, g, :], in_=bi[:, g, :]), reads=['bi'], writes=['t8g'])
                    if cfg.stop == 'r_b':
                        return
                    self.tt(gs[:, 0, :], t8g[:, :, 0], t8g[:, :, 1], ALU.add, ['t8g'], 'gs')
                    self.tt(gmax[:, 0, :], gs[:, 0, 0:1], gs[:, 0, 1:2], ALU.max, ['gs'], 'gmax')
                    for g in range(2, NG):
                        self.tt(gmax[:, 0, :], gmax[:, 0, :], gs[:, 0, g:g + 1], ALU.max, ['gs', 'gmax'], 'gmax')
                    self.ts(ohg[:, 0, :], gs[:, 0, :], gmax[:, 0, 0:1], None, ALU.is_equal, None, ['gs', 'gmax'], 'ohg')
                    self.ts(ohg[:, 0, :], ohg[:, 0, :], BIG, -BIG, ALU.mult, ALU.add, ['ohg'], 'ohg')
                    self.tt(msk[:], bi[:], ohg[:, 0, :].unsqueeze(2).broadcast_to([128, NG, 8]), ALU.add, ['bi', 'ohg'], 'msk')
                    self.S.op('dve', lambda e: e.max(out=t8e[:, 0, :], in_=msk2), reads=['msk'], writes=['t8e'])
                    self.ts(sel2[:, 0, :], msk2, t8e[:, 0, 1:2], None, ALU.is_ge, None, ['msk', 't8e'], 'sel2')
                    self.tt(wun[:, 0, :], sc_[:, 0, :], sel2[:, 0, :], ALU.mult, ['sc_', 'sel2'], 'wun')
                    self.S.op('dve', lambda e: e.tensor_reduce(out=dsum[:, 0, :], in_=wun[:, 0, :], axis=AX.X, op=ALU.add), reads=['wun'], writes=['dsum'])
                    self.S.op('dve', lambda e: e.reciprocal(out=dsum[:, 0, :], in_=dsum[:, 0, :]), reads=['dsum'], writes=['dsum'])
                    self.ts(Wt[:, tau, :], wun[:, 0, :], dsum[:, 0, 0:1], None, ALU.mult, None, ['wun', 'dsum'], 'Wt')
                if cfg.stop == 'route':
                    return
                items = [(e_, ti) for e_ in range(NE) for ti in range(len(tiles))]

                def stage_a(n):
                    e_, ti = items[n]
                    sl = e_ % 2
                    ab = n % 2
                    if ti == 0:
                        self.S.dma('pool', wg2[sl][:], wg_d[layer, e_].rearrange("(kc p) n -> p kc n", p=128), writes=['wg%d' % sl])
                        self.S.dma('pool', wu2[sl][:], wu_d[layer, e_].rearrange("(kc p) n -> p kc n", p=128), writes=['wu%d' % sl])
                        self.S.dma('pool', wd2[sl][:], wd_d[layer, e_].rearrange("(kc p) n -> p kc n", p=128), writes=['wd%d' % sl])
                    for kc in range(8):
                        self.mm(ps[ab][:, :], hTm[:, kc, ti * 128:(ti + 1) * 128], wg2[sl][:, kc, :], kc == 0, kc == 7, ['hTg', 'wg%d' % sl], PK[ab])
                    for kc in range(8):
                        self.mm(ps[2 + ab][:, :], hTm[:, kc, ti * 128:(ti + 1) * 128], wu2[sl][:, kc, :], kc == 0, kc == 7, ['hTg', 'wu%d' % sl], PK[2 + ab])
                    self.act(sg[ab][:, 0, :], ps[ab][:, :], AF.Silu, [PK[ab]], 'sg%d' % ab)
                    self.tt(Ab[ab][:, 0, :], sg[ab][:, 0, :], ps[2 + ab][:, :], ALU.mult, ['sg%d' % ab, PK[2 + ab]], 'Ab%d' % ab)

                def stage_b(n):
                    ab = n % 2
                    pt = ps[4 + ab][:].bitcast(BF16)
                    for fc in range(4):
                        self.tr(pt[:, fc * 128:(fc + 1) * 128], Ab[ab][:, 0, fc * 128:(fc + 1) * 128], identb[:], ['Ab%d' % ab, 'identb'], PK[4 + ab])
                    self.cp(AT[ab][:].rearrange("p f t -> p (f t)"), pt[:, 0:512], [PK[4 + ab]], 'AT%d' % ab, eng='act')

                def stage_c(n):
                    e_, ti = items[n]
                    sl = e_ % 2
                    ab = n % 2
                    tau = tiles[ti]
                    for hf in range(2):
                        pb = 6 + hf
                        for fc in range(4):
                            self.mm(ps[pb][:, :], AT[ab][:, fc, :], wd2[sl][:, fc, hf * 512:(hf + 1) * 512], fc == 0, fc == 3, ['AT%d' % ab, 'wd%d' % sl], PK[pb])
                        dst = accm[:, ti, hf * 512:(hf + 1) * 512]
                        if e_ == 0:
                            self.ts(dst, ps[pb][:, :], Wt[:, tau, e_:e_ + 1], None, ALU.mult, None, [PK[pb], 'Wt'], 'accm%d' % ti)
                        else:
                            self.stt(dst, ps[pb][:, :], Wt[:, tau, e_:e_ + 1], dst, ALU.mult, ALU.add, [PK[pb], 'Wt', 'accm%d' % ti], 'accm%d' % ti)

                nit = len(items)
                for step in range(nit + 2):
                    if step < nit:
                        stage_a(step)
                    if 1 <= step <= nit:
                        stage_b(step - 1)
                    if step >= 2:
                        stage_c(step - 2)
                for ti, tau in enumerate(tiles):
                    smp = (tau == NT - 1)
                    gt, gk = (gS, 'gS') if smp else (gP, 'gP')
                    self.tt(zq2, accm[:, ti, :], gt[:], ALU.mult, ['accm%d' % ti, gk], 'zq')
                    self.stt(zq2, xres[:, tau, :], cfg.ALPHA, zq2, ALU.mult, ALU.add, [XK[tau], 'zq'], 'zq')
                    self.layernorm(zq2, xres[:, tau, :], lnG[:], lnB[:], 'zq', XK[tau], 'lnG')

        moe(0)
        if cfg.stop in ('moe0', 'route', 'ada1', 'rsetup', 'r_a', 'r_b', 'r_0'):
            self.finish(dbg, xres, XK)
            return

        ada(2)
        off = 0
        uT, off = carve(off, [8, 30 + 512], F32)
        hTc, off = carve(off, [8, 512], BF16)
        winc = []
        for i in range(2):
            a_, off = carve(off, [8, 256], BF16); winc.append(a_)
        yg, off = carve(off, [8, 512], F32)
        ytok, off = carve(off, [1, D], F32)
        zb, off = carve(off, [8, 128], BF16)
        zT, off = carve(off, [8, 128], BF16)
        wout, off = carve(off, [8, D], BF16)
        wdw, off = carve(off, [8, CW], F32)
        cg_, off = carve(off, [1, D], F32)
        cb_, off = carve(off, [1, D], F32)
        sgm, off = carve(off, [1, 512], F32)
        zq, off = carve(off, [1, D], F32)
        uS, off = carve(off, [8, SPC], F32)
        stT, off = carve(off, [8 * SPC, CW], F32)
        prodS, off = carve(off, [8 * SPC, CW], F32)
        yS, off = carve(off, [8, SPC], F32)
        utok, off = carve(off, [1, D], F32)
        assert off <= SCR_BYTES, off
        zq2 = zq[:, 0, :]
        ytok2 = ytok[:, 0, :]
        utok2 = utok[:, 0, :]
        self.memset(uT[:].rearrange("p a b -> p (a b)"), 0.0, 'uT')
        self.ld(wdw[:].rearrange("p a b -> p (a b)"), wdwT_d, 'wdw')
        self.S.dma('pool', wout[:], wout_d.rearrange("(kc p) n -> p kc n", p=128), writes=['wout'])
        self.bcast_row(cg_[:, 0, :], clng_d, D, 'cgb', 'cg')
        self.bcast_row(cb_[:, 0, :], clnb_d, D, 'cgb', 'cb')
        win_src = win_d.rearrange("(kc p) n -> p kc n", p=128)
        wi = [0]

        def glu_cols(ncol, dst_fn):
            for cc in range(8):
                wb = winc[wi[0] % 2]
                wk = 'winc%d' % (wi[0] % 2)
                wi[0] += 1
                self.S.dma('pool', wb[:, :, 0:128], win_src[:, :, cc * 128:(cc + 1) * 128], writes=[wk])
                self.S.dma('pool', wb[:, :, 128:256], win_src[:, :, D + cc * 128:D + (cc + 1) * 128], writes=[wk])
                for kc in range(8):
                    self.mm(ps[0][:, 0:ncol], wb[:, kc, 0:128], hTc[:, kc, 0:ncol], kc == 0, kc == 7, [wk, 'hTg'], 'ps0')
                for kc in range(8):
                    self.mm(ps[1][:, 0:ncol], wb[:, kc, 128:256], hTc[:, kc, 0:ncol], kc == 0, kc == 7, [wk, 'hTg'], 'ps1')
                self.act(sgm[:, 0, 0:ncol], ps[1][:, 0:ncol], AF.Sigmoid, ['ps1'], 'sgm')
                dst, dk = dst_fn(cc)
                self.tt(dst, ps[0][:, 0:ncol], sgm[:, 0, 0:ncol], ALU.mult, ['ps0', 'sgm'], dk)

        def conv_tail(tau, col0, gate_tile, gkey):
            for hf in range(2):
                for q in range(4):
                    cc = hf * 4 + q
                    self.tr(ps[2 + hf][:, q * 128:(q + 1) * 128], yg[:, cc, col0:col0 + 128], ident, ['yg', 'consts'], PK[2 + hf])
                self.cp(ytok2[:, hf * 512:(hf + 1) * 512], ps[2 + hf][:, :], [PK[2 + hf]], 'ytok', eng='act')
            self.layernorm(ytok2, ytok2, cg_[:, 0, :], cb_[:, 0, :], 'ytok', 'ytok', 'cgb')
            self.act(zb[:].rearrange("p a b -> p (a b)"), ytok2, AF.Silu, ['ytok'], 'zb')
            pt = ps[4][:].bitcast(BF16)
            for cc in range(8):
                self.tr(pt[:, cc * 128:(cc + 1) * 128], zb[:, cc, :], identb[:], ['zb', 'identb'], 'ps4')
            self.cp(zT[:].rearrange("p a b -> p (a b)"), pt[:, :], ['ps4'], 'zT')
            for hf in range(2):
                for cc in range(8):
                    self.mm(ps[6 + hf][:, :], zT[:, cc, :], wout[:, cc, hf * 512:(hf + 1) * 512], cc == 0, cc == 7, ['zT', 'wout'], PK[6 + hf])
                self.tt(zq2[:, hf * 512:(hf + 1) * 512], ps[6 + hf][:, :], gate_tile[:, hf * 512:(hf + 1) * 512], ALU.mult, [PK[6 + hf], gkey], 'zq')
            self.stt(zq2, xres[:, tau, :], cfg.ALPHA, zq2, ALU.mult, ALU.add, [XK[tau], 'zq'], 'zq')
            self.layernorm(zq2, xres[:, tau, :], lnG[:], lnB[:], 'zq', XK[tau], 'lnG')

        for g0 in range(0, NPT, 4):
            tiles = list(range(g0, min(g0 + 4, NPT)))
            ncol = len(tiles) * 128
            for ti, tau in enumerate(tiles):
                tile_to_hT(xres[:, tau, :], XK[tau], hTc, ti * 128)
            glu_cols(ncol, lambda cc: (uT[:, cc, 30:30 + ncol], 'uT'))
            if g0 == 0:
                self.ts(uT[:, :, 30:158], uT[:, :, 30:158], flag[:, 0:1], None, ALU.mult, None, ['uT', 'flag'], 'uT')
            for cc in range(8):
                for k in range(CW):
                    if k == 0:
                        self.ts(yg[:, cc, 0:ncol], uT[:, cc, 0:ncol], wdw[:, cc, 0:1], None, ALU.mult, None, ['uT', 'wdw'], 'yg')
                    else:
                        self.stt(yg[:, cc, 0:ncol], uT[:, cc, k:k + ncol], wdw[:, cc, k:k + 1], yg[:, cc, 0:ncol], ALU.mult, ALU.add, ['uT', 'wdw', 'yg'], 'yg')
            if tiles[-1] == NPT - 1:
                cl = 30 + (len(tiles) - 1) * 128 + 98
                for hf in range(2):
                    for q in range(4):
                        cc = hf * 4 + q
                        self.tr(ps[2 + hf][0:30, q * 128:(q + 1) * 128], uT[:, cc, cl:cl + 30], ident, ['uT', 'consts'], PK[2 + hf])
                    self.cp(utok2[0:30, hf * 512:(hf + 1) * 512], ps[2 + hf][0:30, :], [PK[2 + hf]], 'utok', eng='act')
                self.st(conv_p[:, :], utok2[0:30, :], 'utok', 'conv_p')
            else:
                self.cp(uT[:, :, 0:30], uT[:, :, ncol:ncol + 30], ['uT'], 'uT')
            if cfg.stop == 'c_conv':
                self.finish(dbg, xres, XK)
                return
            for ti, tau in enumerate(tiles):
                if tau >= 1:
                    conv_tail(tau, ti * 128, gP, 'gP')
            if cfg.stop == 'c_tail':
                self.finish(dbg, xres, XK)
                return
        tile_to_hT(xres[:, tS, :], XK[tS], hTc, 0, sample=True)
        glu_cols(SPC, lambda cc: (uS[:, cc, :], 'uS'))
        st4 = stT[:].rearrange("p (c s) k -> p c s k", c=8)
        pflat = prodS[:].rearrange("p a k -> p (a k)")[:, 0:8 * SPC * 30]
        self.ld(pflat, stfm_d, 'prodS')
        self.cp(st4[:, :, :, 0:30], pflat.rearrange("p (c s k) -> p c s k", c=8, s=SPC), ['prodS'], 'stT')
        self.cp(st4[:, :, :, 30], uS[:], ['uS', 'stT'], 'stT')
        self.tt(prodS[:].rearrange("p (c s) k -> p c s k", c=8), st4, wdw[:].unsqueeze(2).broadcast_to([128, 8, SPC, CW]), ALU.mult, ['stT', 'wdw'], 'prodS')
        self.S.op('dve', lambda e: e.tensor_reduce(out=yS[:].rearrange("p c s -> p (c s)"), in_=prodS[:], axis=AX.X, op=ALU.add), reads=['prodS'], writes=['yS'])
        self.memset(yg[:, :, 0:128], 0.0, 'yg')
        self.cp(yg[:, :, 0:SPC], yS[:], ['yS', 'yg'], 'yg')
        conv_tail(tS, 0, gS, 'gS')
        for s_ in range(SPC):
            self.ld(ytok2[s_ * 29:(s_ + 1) * 29, :], sttok_d[s_, 1:30, :], 'ytok')
        for s_ in range(SPC):
            self.st(conv_s[s_, 0:29, :], ytok2[s_ * 29:(s_ + 1) * 29, :], 'ytok', 'conv_s')
        for hf in range(2):
            for q in range(4):
                cc = hf * 4 + q
                self.tr(ps[2 + hf][0:SPC, q * 128:(q + 1) * 128], uS[:, cc, :], ident, ['uS', 'consts'], PK[2 + hf])
            self.cp(utok2[0:SPC, hf * 512:(hf + 1) * 512], ps[2 + hf][0:SPC, :], [PK[2 + hf]], 'utok', eng='act')
        self.st(conv_s[:, 29, :], utok2[0:SPC, :], 'utok', 'conv_s')
        if cfg.stop == 'conv':
            self.finish(dbg, xres, XK)
            return

        moe(1)
        for tau in range(1, NPT):
            self.st(y_p[(tau - 1) * 128:tau * 128, :], xres[:, tau, :], XK[tau], 'y_p')
        self.st(y_s[:, :], xres[0:SPC, tS, :], XK[tS], 'y_s')
        self.finish(dbg, xres, XK)

    def finish(self, dbg, xres, XK):
        cfg = self.cfg
        if dbg is not None:
            for t in range(cfg.NT):
                self.st(dbg[t * 128:(t + 1) * 128, :], xres[:, t, :], XK[t], 'dbg')
        self.S.wait_keys('sp', list(set(self.outkeys)))
        self.es.close()


def prep_inputs(cfg, I):
    f32 = np.float32
    NC, NCB, LS, NSLOT, NPT, NGT, SPC, NBLK = cfg.NC, cfg.NCB, cfg.LS, cfg.NSLOT, cfg.NPT, cfg.NGT, cfg.SPC, cfg.NBLK
    half = 8
    inv_freq = (f32(ROPE_THETA) ** (-np.arange(half, dtype=f32) / f32(half))).astype(f32) if False else None
    half = 16
    inv_freq = np.power(f32(ROPE_THETA), -(np.arange(half, dtype=f32) / f32(half))).astype(f32)
    ident = np.eye(128, dtype=f32)
    tri = (np.arange(128)[:, None] <= np.arange(128)[None, :]).astype(f32)
    consts = np.concatenate([ident, tri], axis=1)
    c2 = np.zeros((128, 512), f32)
    cm = np.zeros((8, 32), f32)
    for s_ in range(4):
        for kv in range(2):
            for h in range(kv * 4, kv * 4 + 4):
                cm[s_ * 2 + kv, h * 4 + s_] = 1.0
    c2[:, 0:256] = cm.reshape(1, 256)
    c2[0:32, 256] = (np.arange(32) // 4) // 4
    for s_ in range(4):
        for h in range(8):
            c2[s_, (257 if h < 4 else 289) + h * 4 + s_] = 1.0
    c2[0:32, 321:353] = np.eye(32, dtype=f32)
    c2[:, 353:361] = np.arange(8, dtype=f32)[None, :]
    shared = {
        "consts": consts, "consts2": c2,
        "w_ada": np.ascontiguousarray(I["w_ada"]), "b_ada": np.ascontiguousarray(I["b_ada"]),
        "b_adaT": np.ascontiguousarray(I["b_ada"].reshape(2, 48, 128).transpose(2, 0, 1).reshape(128, 96)),
        "ln_g": np.ascontiguousarray(I["ln_g"].reshape(4, D)), "ln_b": np.ascontiguousarray(I["ln_b"].reshape(4, D)),
        "w_qkv": np.ascontiguousarray(I["w_qkv"][0]), "w_o": np.ascontiguousarray(I["w_o"][0]),
        "conv_w_in": np.ascontiguousarray(I["conv_w_in"][0]),
        "wdwT": np.ascontiguousarray(I["conv_w_dw"][0].T.reshape(8, 128, CW).transpose(1, 0, 2).reshape(128, 8 * CW)),
        "conv_ln_g": np.ascontiguousarray(I["conv_ln_g"].reshape(1, D)), "conv_ln_b": np.ascontiguousarray(I["conv_ln_b"].reshape(1, D)),
        "conv_w_out": np.ascontiguousarray(I["conv_w_out"][0]),
        "w_router": np.ascontiguousarray(I["w_router"]), "b_router": np.ascontiguousarray(I["b_router"].reshape(1, -1)),
        "w_gate": np.ascontiguousarray(I["w_gate"]), "w_up": np.ascontiguousarray(I["w_up"]), "w_down": np.ascontiguousarray(I["w_down"]),
        "cache_k": np.ascontiguousarray(I["cache_k"].reshape(cfg.NPHYS, -1)),
        "cache_v": np.ascontiguousarray(I["cache_v"].reshape(cfg.NPHYS, -1)),
    }
    maps = []
    pt = np.asarray(I["page_table"]).astype(np.int32)
    for c in range(NC):
        b, j = c // NCB, c % NCB
        rot = LS * j - 1
        m = dict(shared)
        m["xg"] = np.ascontiguousarray(np.roll(I["x_prompt"][b], -rot * 256, axis=0))
        gslot = (rot + np.arange(NSLOT)) % NSLOT
        pos = (gslot[:, None] * 256 + np.arange(256)[None, :]).reshape(-1)
        pos = np.concatenate([pos, np.full(128, cfg.PAST_LEN)]).astype(f32)
        ang = pos[:, None] * inv_freq[None, :]
        tab = np.concatenate([np.cos(ang), np.sin(ang)], axis=1).astype(f32)
        m["rope"] = np.ascontiguousarray(tab.reshape(NGT + 1, 128, 32).transpose(1, 0, 2).reshape(128, (NGT + 1) * 32))
        sid = np.arange(c * SPC, (c + 1) * SPC)
        xs = np.zeros((128, D), f32)
        xs[:SPC] = I["x_sample"][sid, 0]
        m["xs"] = xs
        cv = np.zeros((8, D), f32)
        cv[0] = I["c_prompt"][b]
        cv[1:1 + SPC] = I["c_sample"][sid]
        m["cvec"] = cv
        valid = np.zeros((NPT, NSLOT), bool)
        for tau in range(NPT):
            own = rot + (tau + 1) // 2
            valid[tau] = gslot < own
        m["nmask"] = np.ascontiguousarray(np.broadcast_to(np.where(valid, 0.0, -BIG).astype(f32).reshape(1, -1), (128, NPT * NSLOT)))
        m["v01"] = np.ascontiguousarray(np.broadcast_to(valid.astype(f32).reshape(1, -1), (128, NPT * NSLOT)))
        m["flag"] = np.full((128, 1), 1.0 if j > 0 else 0.0, f32)
        ptA = np.zeros((128, 4), np.int32)
        for r in range(2):
            for eo in range(2):
                for s2 in range(2):
                    ptA[s2 * NBLK:(s2 + 1) * NBLK, r * 2 + eo] = pt[sid[2 * r + s2], eo::2]
        m["ptA"] = ptA
        ptB = np.zeros((32, 2 * NBLK), np.int32)
        for h in range(8):
            for s in range(SPC):
                ptB[h * 4 + s, :NBLK] = pt[sid[s], 0::2]
                ptB[h * 4 + s, NBLK:] = pt[sid[s], 1::2]
        m["ptB"] = ptB
        st = I["state_conv"][0][sid]
        m["st_tok"] = np.ascontiguousarray(st)
        m["st_fm"] = np.ascontiguousarray(st.reshape(SPC, 30, 8, 128).transpose(3, 2, 0, 1).reshape(128, 8 * SPC * 30))
        maps.append(m)
    return maps


_PROG_CACHE = {}


def run_cfg(cfg, inputs):
    key = (cfg.B, cfg.SEQ, cfg.NCB, cfg.DEC_BATCH, cfg.PAST_LEN, cfg.NE, cfg.NG, cfg.MG, cfg.stop)
    if key not in _PROG_CACHE:
        p = Prog(cfg)
        p.build()
        _PROG_CACHE[key] = p
    p = _PROG_CACHE[key]
    maps = prep_inputs(cfg, inputs)
    maps = [{k: v for k, v in m.items() if k in p.din} for m in maps]
    res = run_bass_kernel_spmd(p.nc, maps, core_ids=list(range(cfg.NC)))
    return res.results


def assemble(cfg, R):
    f32 = np.float32
    B, NCB, LS, SPC = cfg.B, cfg.NCB, cfg.LS, cfg.SPC
    y_p = np.zeros((B, cfg.SEQ, D), f32)
    k_p = np.zeros((B, cfg.SEQ // 128, 1, 2, 128, 128), f32)
    v_p = np.zeros_like(k_p)
    conv_p = np.zeros((1, B, 30, D), f32)
    y_s = np.zeros((cfg.DEC_BATCH, 1, D), f32)
    k_s = np.zeros((cfg.DEC_BATCH, 1, 2, 1, 128), f32)
    v_s = np.zeros_like(k_s)
    conv_s = np.zeros((1, cfg.DEC_BATCH, 30, D), f32)
    for c, r in enumerate(R):
        b, j = c // NCB, c % NCB
        t0 = j * LS * 256
        y_p[b, t0:t0 + LS * 256] = r["y_p"]
        k_p[b, j * 2 * LS:(j + 1) * 2 * LS, 0] = r["k_p"]
        v_p[b, j * 2 * LS:(j + 1) * 2 * LS, 0] = r["v_p"]
        if j == NCB - 1:
            conv_p[0, b] = r["conv_p"]
        sl = slice(c * SPC, (c + 1) * SPC)
        y_s[sl, 0] = r["y_s"]
        k_s[sl, 0, :, 0, :] = r["k_s"].reshape(SPC, 2, 128)
        v_s[sl, 0, :, 0, :] = r["v_s"].reshape(SPC, 2, 128)
        conv_s[0, sl] = r["conv_s"]
    return (y_p, y_s, k_p, v_p, conv_p, k_s, v_s, conv_s)


def kernel(**inputs):
    cfg = Cfg()
    inputs = {k: np.asarray(v) for k, v in inputs.items()}
    R = run_cfg(cfg, inputs)
    return assemble(cfg, R)
```
